# Optimizing a Trainium2 kernel written in Bass

```python
import math
import jax
import jax.numpy as jnp
from jax import lax
import numpy as np

D_MODEL = 1024
BATCH = 4
SEQ = 4096
DEPTH = 4

HEAD_DIM = 64
R_HEADS = 4
R_WIDTH = R_HEADS * HEAD_DIM
R_LORA_W = 32
R_LORA_A = 32
R_LORA_G = 64
R_COLS = 3 * R_WIDTH + R_LORA_W + R_LORA_A + R_LORA_G
R_GN_EPS = 64e-5
DF_HEADS = 4
DF_QK = HEAD_DIM
DF_V = 2 * HEAD_DIM
DF_WIDTH = DF_HEADS * DF_V
DF_QCOLS = DF_HEADS * 2 * DF_QK
DF_COLS = 2 * DF_QCOLS + DF_WIDTH
MB_HEADS = 4
MB_WIDTH = MB_HEADS * HEAD_DIM
MB_COLS = 3 * MB_WIDTH
MB_BLOCK = 256
MB_TOPK = 3
MB_Q_CHUNK = 64
ATT_Q_CHUNK = 128
IN_COLS = R_COLS + DF_COLS + MB_COLS
MIX_WIDTH = R_WIDTH + DF_WIDTH + MB_WIDTH
NUM_BUCKETS = 32
MAX_DISTANCE = 1024
N_BIAS_HEADS = DF_HEADS + MB_HEADS
N_GROUPS = 4
EXPERTS_PER_GROUP = 8
N_EXPERTS = N_GROUPS * EXPERTS_PER_GROUP
TOP_K_INNER = 2
EXPERT_HIDDEN = 512
MOE_BLOCK = 256
RMS_EPS = 1e-6

kernel_name = 'hybrid_rwkv7_diffattn_moba_hmoe'

F32 = jnp.float32


def rmsnorm(x, g, eps=RMS_EPS):
    xf = x.astype(F32)
    y = xf * lax.rsqrt(jnp.mean(xf * xf, -1, keepdims=True) + eps)
    return (y * g.astype(F32)).astype(x.dtype)


def t5_bucket(dist):
    n = jnp.maximum(dist, 0)
    max_exact = NUM_BUCKETS // 2
    nf = jnp.maximum(n, 1).astype(F32)
    large = max_exact + (jnp.log(nf / max_exact) / math.log(MAX_DISTANCE / max_exact)
                         * (NUM_BUCKETS - max_exact)).astype(jnp.int32)
    large = jnp.minimum(large, NUM_BUCKETS - 1)
    return jnp.where(n < max_exact, n, large)


def rwkv7_mix(p, mu, w0, w2, a0, a2, g2, k_k, k_a, r_k, ln_g, ln_b):
    B, S, _ = p.shape
    p_prev = jnp.pad(p, ((0, 0), (1, 0), (0, 0)))[:, :-1]
    p = p + (p_prev - p) * mu
    c1 = R_WIDTH
    c2 = 2 * R_WIDTH
    c3 = 3 * R_WIDTH
    c4 = c3 + R_LORA_W
    c5 = c4 + R_LORA_A
    r, k, v, wd, ad, gd = jnp.split(p, [c1, c2, c3, c4, c5], axis=-1)
    w = -jax.nn.softplus(-(w0 + jnp.tanh(wd) @ w2)) - 0.5
    decay = jnp.exp(-jnp.exp(w.astype(F32)))
    a = jax.nn.sigmoid(a0 + ad @ a2)
    g = jax.nn.sigmoid(gd) @ g2
    heads = lambda t: t.astype(F32).reshape(B, S, R_HEADS, HEAD_DIM)
    kk = heads(k * k_k)
    kk = kk / jnp.maximum(jnp.sqrt(jnp.sum(kk * kk, -1, keepdims=True)), 1e-12)
    k = k * (1 + (a - 1) * k_a)
    r_h, k_h, v_h, a_h, w_h = heads(r), heads(k), heads(v), heads(a), heads(decay)

    def step(state, inp):
        r_t, w_t, k_t, v_t, kk_t, a_t = inp
        sa = jnp.einsum('bhvk,bhk->bhv', state, -kk_t)
        state = (state * w_t[:, :, None, :]
                 + jnp.einsum('bhv,bhk->bhvk', sa, kk_t * a_t)
                 + jnp.einsum('bhv,bhk->bhvk', v_t, k_t))
        return state, jnp.einsum('bhvk,bhk->bhv', state, r_t)

    xs = tuple(jnp.moveaxis(t, 1, 0) for t in (r_h, w_h, k_h, v_h, kk, a_h))
    s0 = jnp.zeros((B, R_HEADS, HEAD_DIM, HEAD_DIM), F32)
    _, y = lax.scan(step, s0, xs)
    y = jnp.moveaxis(y, 0, 1)
    mean = jnp.mean(y, -1, keepdims=True)
    var = jnp.mean(jnp.square(y - mean), -1, keepdims=True)
    y = ((y - mean) * lax.rsqrt(var + R_GN_EPS)).reshape(B, S, R_WIDTH)
    y = y * ln_g.astype(F32) + ln_b.astype(F32)
    bonus = jnp.sum(r_h * k_h * r_k.astype(F32), -1, keepdims=True) * v_h
    y = y + bonus.reshape(B, S, R_WIDTH)
    return (y * g.astype(F32)).astype(p.dtype)


def diff_attention(p, q_gain, k_gain, lam, subln_g, bias_tbl, lambda_init):
    B, S, _ = p.shape
    q, k, v = jnp.split(p, [DF_QCOLS, 2 * DF_QCOLS], axis=-1)
    q = rmsnorm(q.reshape(B, S, DF_HEADS, 2, DF_QK), q_gain)
    k = rmsnorm(k.reshape(B, S, DF_HEADS, 2, DF_QK), k_gain)
    v = v.reshape(B, S, DF_HEADS, DF_V).astype(F32)
    lamf = lam.astype(F32)
    lam_full = (jnp.exp(jnp.sum(lamf[0] * lamf[1])) - jnp.exp(jnp.sum(lamf[2] * lamf[3]))
                + lambda_init)
    scale = DF_QK ** -0.5
    nqb = S // ATT_Q_CHUNK
    qb = q.reshape(B, nqb, ATT_Q_CHUNK, DF_HEADS, 2, DF_QK).transpose(1, 0, 3, 4, 2, 5)
    kpos = jnp.arange(S)
    tbl = bias_tbl.astype(F32)

    def block(args):
        qc, i = args
        qpos = i * ATT_Q_CHUNK + jnp.arange(ATT_Q_CHUNK)
        dist = qpos[:, None] - kpos[None, :]
        bias = tbl[t5_bucket(dist)][..., :DF_HEADS].transpose(2, 0, 1)
        logits = jnp.einsum('bhcqd,bkhcd->bhcqk', qc, k).astype(F32) * scale + bias[None, :, None]
        logits = jnp.where(dist >= 0, logits, -jnp.inf)
        probs = jax.nn.softmax(logits, axis=-1)
        attn = probs[:, :, 0] - lam_full * probs[:, :, 1]
        return jnp.einsum('bhqk,bkhe->bqhe', attn, v)

    out = lax.map(block, (qb, jnp.arange(nqb)))
    out = out.transpose(1, 0, 2, 3, 4).reshape(B, S, DF_HEADS, DF_V)
    out = rmsnorm(out, subln_g) * (1.0 - lambda_init)
    return out.reshape(B, S, DF_WIDTH).astype(p.dtype)


def moba_attention(p, q_gain, k_gain, bias_tbl):
    B, S, _ = p.shape
    q, k, v = jnp.split(p, 3, axis=-1)
    q = rmsnorm(q.reshape(B, S, MB_HEADS, HEAD_DIM), q_gain).transpose(0, 2, 1, 3)
    k = rmsnorm(k.reshape(B, S, MB_HEADS, HEAD_DIM), k_gain).transpose(0, 2, 1, 3)
    v = v.reshape(B, S, MB_HEADS, HEAD_DIM).transpose(0, 2, 1, 3)
    nb = -(-S // MB_BLOCK)
    pad = nb * MB_BLOCK - S
    k = jnp.pad(k, ((0, 0), (0, 0), (0, pad), (0, 0)))
    v = jnp.pad(v, ((0, 0), (0, 0), (0, pad), (0, 0)))
    kb = k.reshape(B, MB_HEADS, nb, MB_BLOCK, HEAD_DIM)
    vb = v.reshape(B, MB_HEADS, nb, MB_BLOCK, HEAD_DIM)
    kmean = jnp.mean(kb.astype(F32), axis=3)
    topk = min(MB_TOPK, nb)
    scale = HEAD_DIM ** -0.5
    tbl = bias_tbl[:, DF_HEADS:].T.astype(F32)
    nqc = S // MB_Q_CHUNK
    qc_all = q.reshape(B, MB_HEADS, nqc, MB_Q_CHUNK, HEAD_DIM).transpose(2, 0, 1, 3, 4)
    bi = jnp.arange(B)[:, None, None, None]
    hi = jnp.arange(MB_HEADS)[None, :, None, None]
    blk_ids = jnp.arange(nb)
    offs = jnp.arange(MB_BLOCK)

    def chunk(args):
        qc, i = args
        qpos = i * MB_Q_CHUNK + jnp.arange(MB_Q_CHUNK)
        own = (i * MB_Q_CHUNK) // MB_BLOCK
        gate = jnp.einsum('bhqd,bhnd->bhqn', qc.astype(F32), kmean)
        gate = jnp.where(blk_ids < own, gate, -jnp.inf)
        _, idx = lax.top_k(gate, topk)
        valid = jnp.arange(topk) < own
        kg = kb[bi, hi, idx]
        vg = vb[bi, hi, idx]
        dist_sel = qpos[:, None, None] - (idx[..., None] * MB_BLOCK + offs)
        logit_sel = (jnp.einsum('bhqd,bhqnkd->bhqnk', qc, kg).astype(F32) * scale
                     + tbl[hi[..., None], t5_bucket(dist_sel)])
        logit_sel = jnp.where(valid[:, None], logit_sel, -jnp.inf)
        ko = lax.dynamic_index_in_dim(kb, own, axis=2, keepdims=False)
        vo = lax.dynamic_index_in_dim(vb, own, axis=2, keepdims=False)
        dist_own = qpos[:, None] - (own * MB_BLOCK + offs)[None, :]
        logit_own = (jnp.einsum('bhqd,bhkd->bhqk', qc, ko).astype(F32) * scale
                     + tbl[:, t5_bucket(dist_own)][None])
        logit_own = jnp.where(dist_own >= 0, logit_own, -jnp.inf)
        nq = qc.shape[2]
        logits = jnp.concatenate([logit_sel.reshape(B, MB_HEADS, nq, topk * MB_BLOCK), logit_own], -1)
        probs = jax.nn.softmax(logits, axis=-1)
        p_sel = probs[..., :topk * MB_BLOCK].reshape(B, MB_HEADS, nq, topk, MB_BLOCK)
        p_own = probs[..., topk * MB_BLOCK:]
        return (jnp.einsum('bhqnk,bhqnkd->bhqd', p_sel, vg.astype(F32))
                + jnp.einsum('bhqk,bhkd->bhqd', p_own, vo.astype(F32)))

    out = lax.map(chunk, (qc_all, jnp.arange(nqc)))
    out = out.transpose(1, 0, 3, 2, 4).reshape(B, S, MB_WIDTH)
    return out.astype(p.dtype)


def hier_moe(h, wg, bg, we, be, w1, w3, w2):
    B, S, Dm = h.shape
    T = B * S
    hf = h.reshape(T, Dm)
    g_logits = (hf @ wg + bg).astype(F32)
    pg = jax.nn.softmax(g_logits, axis=-1)
    g_idx = jnp.argmax(g_logits, axis=-1)
    pg_top = jnp.take_along_axis(pg, g_idx[:, None], axis=-1)
    e_logits = (hf @ we + be).astype(F32).reshape(T, N_GROUPS, EXPERTS_PER_GROUP)
    e_logits = jnp.take_along_axis(e_logits, g_idx[:, None, None], axis=1)[:, 0]
    pe = jax.nn.softmax(e_logits, axis=-1)
    pe_top, e_local = lax.top_k(pe, TOP_K_INNER)
    gates = pg_top * pe_top / jnp.sum(pe_top, -1, keepdims=True)
    experts = g_idx[:, None] * EXPERTS_PER_GROUP + e_local
    A = T * TOP_K_INNER
    flat_e = experts.reshape(A)
    order = jnp.argsort(flat_e)
    se = flat_e[order]
    tok = order // TOP_K_INNER
    counts = jnp.zeros((N_EXPERTS,), jnp.int32).at[flat_e].add(1)
    padded = (counts + MOE_BLOCK - 1) // MOE_BLOCK * MOE_BLOCK
    pad_end = jnp.cumsum(padded)
    pad_start = pad_end - padded
    start = jnp.cumsum(counts) - counts
    dest = pad_start[se] + jnp.arange(A) - start[se]
    n_blocks = -(-(A + N_EXPERTS * (MOE_BLOCK - 1)) // MOE_BLOCK)
    buf = jnp.zeros((n_blocks * MOE_BLOCK, Dm), h.dtype).at[dest].set(hf[tok])
    blk_e = jnp.minimum(jnp.searchsorted(pad_end, jnp.arange(n_blocks) * MOE_BLOCK, side='right'),
                        N_EXPERTS - 1)

    def expert_block(args):
        xb, e = args
        return (jax.nn.silu(xb @ w1[e]) * (xb @ w3[e])) @ w2[e]

    yb = lax.map(expert_block, (buf.reshape(n_blocks, MOE_BLOCK, Dm), blk_e))
    y_assign = yb.reshape(-1, Dm)[dest] * gates.reshape(A)[order][:, None].astype(h.dtype)
    y = jnp.zeros((T, Dm), h.dtype).at[tok].add(y_assign)
    return y.reshape(B, S, Dm)


def setup_inputs(seed: int = 0) -> dict:
    key = jax.random.key(seed)
    ks = iter(jax.random.split(key, 40))
    nrm = lambda shape, s: jax.random.normal(next(ks), shape, F32) * s
    uni = lambda shape, lo, hi: jax.random.uniform(next(ks), shape, F32, lo, hi)
    L = DEPTH
    return {
        'x': nrm((BATCH, SEQ, D_MODEL), 1.0),
        'c': nrm((BATCH, D_MODEL), 1.0),
        'ada_w': nrm((L, D_MODEL, 6 * D_MODEL), 0.5 * D_MODEL ** -0.5),
        'ada_b': nrm((L, 6 * D_MODEL), 0.01),
        'norm1_g': 1.0 + nrm((L, D_MODEL), 0.02),
        'norm2_g': 1.0 + nrm((L, D_MODEL), 0.02),
        'w_in': nrm((L, D_MODEL, IN_COLS), D_MODEL ** -0.5),
        'w_out': nrm((L, MIX_WIDTH, D_MODEL), MIX_WIDTH ** -0.5),
        'rwkv_mu': uni((L, R_COLS), 0.0, 1.0),
        'rwkv_w0': uni((L, R_WIDTH), -6.5, -1.5),
        'rwkv_w2': nrm((L, R_LORA_W, R_WIDTH), 0.1 * R_LORA_W ** -0.5),
        'rwkv_a0': nrm((L, R_WIDTH), 0.1),
        'rwkv_a2': nrm((L, R_LORA_A, R_WIDTH), 0.1 * R_LORA_A ** -0.5),
        'rwkv_g2': nrm((L, R_LORA_G, R_WIDTH), R_LORA_G ** -0.5),
        'rwkv_kk': 0.85 + nrm((L, R_WIDTH), 0.02),
        'rwkv_ka': 1.0 + nrm((L, R_WIDTH), 0.02),
        'rwkv_rk': nrm((L, R_HEADS, HEAD_DIM), 0.1),
        'rwkv_ln_g': 1.0 + nrm((L, R_WIDTH), 0.02),
        'rwkv_ln_b': nrm((L, R_WIDTH), 0.01),
        'diff_q_gain': 1.0 + nrm((L, DF_QK), 0.02),
        'diff_k_gain': 1.0 + nrm((L, DF_QK), 0.02),
        'diff_lambda': nrm((L, 4, DF_QK), 0.1),
        'diff_subln_g': 1.0 + nrm((L, DF_V), 0.02),
        'moba_q_gain': 1.0 + nrm((L, HEAD_DIM), 0.02),
        'moba_k_gain': 1.0 + nrm((L, HEAD_DIM), 0.02),
        'rel_bias': nrm((NUM_BUCKETS, N_BIAS_HEADS), 0.5),
        'router_g_w': nrm((L, D_MODEL, N_GROUPS), D_MODEL ** -0.5),
        'router_g_b': nrm((L, N_GROUPS), 0.01),
        'router_e_w': nrm((L, D_MODEL, N_EXPERTS), D_MODEL ** -0.5),
        'router_e_b': nrm((L, N_EXPERTS), 0.01),
        'moe_w1': nrm((L, N_EXPERTS, D_MODEL, EXPERT_HIDDEN), D_MODEL ** -0.5),
        'moe_w3': nrm((L, N_EXPERTS, D_MODEL, EXPERT_HIDDEN), D_MODEL ** -0.5),
        'moe_w2': nrm((L, N_EXPERTS, EXPERT_HIDDEN, D_MODEL), EXPERT_HIDDEN ** -0.5),
    }


def reference(x, c, ada_w, ada_b, norm1_g, norm2_g, w_in, w_out, rwkv_mu, rwkv_w0, rwkv_w2,
              rwkv_a0, rwkv_a2, rwkv_g2, rwkv_kk, rwkv_ka, rwkv_rk, rwkv_ln_g, rwkv_ln_b,
              diff_q_gain, diff_k_gain, diff_lambda, diff_subln_g, moba_q_gain, moba_k_gain,
              rel_bias, router_g_w, router_g_b, router_e_w, router_e_b, moe_w1, moe_w3, moe_w2):
    cs = jax.nn.silu(c)
    for l in range(DEPTH):
        mod = cs @ ada_w[l] + ada_b[l]
        sh1, sc1, g1, sh2, sc2, g2 = jnp.split(mod[:, None, :], 6, axis=-1)
        h = rmsnorm(x, norm1_g[l]) * (1 + sc1) + sh1
        proj = h @ w_in[l]
        p_r, p_d, p_m = jnp.split(proj, [R_COLS, R_COLS + DF_COLS], axis=-1)
        y_r = rwkv7_mix(p_r, rwkv_mu[l], rwkv_w0[l], rwkv_w2[l], rwkv_a0[l], rwkv_a2[l],
                        rwkv_g2[l], rwkv_kk[l], rwkv_ka[l], rwkv_rk[l], rwkv_ln_g[l], rwkv_ln_b[l])
        lambda_init = 0.8 - 0.6 * math.exp(-0.3 * l)
        y_d = diff_attention(p_d, diff_q_gain[l], diff_k_gain[l], diff_lambda[l],
                             diff_subln_g[l], rel_bias, lambda_init)
        y_m = moba_attention(p_m, moba_q_gain[l], moba_k_gain[l], rel_bias)
        mix = jnp.concatenate([y_r, y_d, y_m], axis=-1) @ w_out[l]
        x = x + g1 * mix
        h2 = rmsnorm(x, norm2_g[l]) * (1 + sc2) + sh2
        x = x + g2 * hier_moe(h2, router_g_w[l], router_g_b[l], router_e_w[l], router_e_b[l],
                              moe_w1[l], moe_w3[l], moe_w2[l])
    return x
```

```python
import math
from contextlib import ExitStack
import numpy as np
import ml_dtypes
import concourse.bass as bass
import concourse.mybir as mybir
from concourse.bass_utils import run_bass_kernel_spmd

F32 = mybir.dt.float32
BF16 = mybir.dt.bfloat16
I32 = mybir.dt.int32
AF = mybir.ActivationFunctionType
ALU = mybir.AluOpType
AX = mybir.AxisListType

S = 4096
D = 1024
NTT = S // 128
NTB = S // 512
CAPS = [768, 896, 1280, 1408]
CAPMAX = max(CAPS)
NPP = 26
NEG = -30000.0
DMA_RING = 8
DEBUG_IND = False


class Region:
    __slots__ = ("name", "w", "r")

    def __init__(self, name=""):
        self.name = name
        self.w = None
        self.r = []


class FW:
    def __init__(self, nc, stack):
        self.nc = nc
        self.stack = stack
        self.eng = {"pe": nc.tensor, "act": nc.scalar, "dve": nc.vector,
                    "pool": nc.gpsimd, "sp": nc.sync}
        self.sems = {}
        self.cnt = {}
        self.waited = {e: {} for e in self.eng}
        for e in self.eng:
            self.sems["c_" + e] = stack.enter_context(nc.semaphore("c_" + e))
            self.cnt["c_" + e] = 0
        self.ring = {}
        for q in ("sp", "act", "pool"):
            for i in range(DMA_RING):
                k = f"d_{q}{i}"
                self.sems[k] = stack.enter_context(nc.semaphore(k))
                self.cnt[k] = 0
            self.ring[q] = 0
        self.n_inst = 0
        self.halt = False

    def _wait(self, e, ev):
        if ev is None or self.halt:
            return
        k, v = ev
        if self.waited[e].get(k, 0) >= v:
            return
        self.waited[e][k] = v
        self.eng[e].wait_ge(self.sems[k], v)

    def _deps(self, e, reads, writes, skip_same):
        own = "c_" + e
        for r in reads:
            if r.w is not None and not (skip_same and r.w[0] == own):
                self._wait(e, r.w)
        for r in writes:
            if r.w is not None and not (skip_same and r.w[0] == own):
                self._wait(e, r.w)
            for ev in r.r:
                if not (skip_same and ev[0] == own):
                    self._wait(e, ev)

    def _record(self, ev, reads, writes):
        for r in writes:
            r.w = ev
            r.r = []
        for r in reads:
            if r in writes:
                continue
            r.r = [x for x in r.r if x[0] != ev[0]] + [ev]

    def op(self, e, fn, reads=(), writes=(), skip_same=False):
        if self.halt:
            return
        self._deps(e, reads, writes, skip_same)
        k = "c_" + e
        self.cnt[k] += 1
        fn(self.eng[e]).then_inc(self.sems[k], 1)
        self._record((k, self.cnt[k]), reads, writes)
        self.n_inst += 1

    def dma(self, q, fn, reads=(), writes=()):
        if self.halt:
            return
        i = self.ring[q]
        self.ring[q] = (i + 1) % DMA_RING
        k = f"d_{q}{i}"
        if self.cnt[k] > 0:
            self._wait(q, (k, self.cnt[k]))
        self._deps(q, reads, writes, False)
        self.cnt[k] += 16
        fn(self.eng[q]).then_inc(self.sems[k], 16)
        self._record((k, self.cnt[k]), reads, writes)
        self.n_inst += 1

    def barrier(self):
        for e in self.eng:
            for k, v in self.cnt.items():
                if v > 0:
                    self._wait(e, (k, v))


def t5_bucket_np(dist):
    n = np.maximum(dist, 0)
    nf = np.maximum(n, 1).astype(np.float32)
    large = 16 + (np.log(nf / 16) / math.log(1024 / 16) * 16).astype(np.int32)
    large = np.minimum(large, 31)
    return np.where(n < 16, n, large)


def make_consts():
    c = {}
    p = np.arange(128)[:, None]
    j = np.arange(128)[None, :]
    c["ident"] = (p == j).astype(np.float32)
    su = (p < j).astype(np.float32)
    ui = (p <= j).astype(np.float32)
    sl = (p > j).astype(np.float32)
    c["m4"] = np.concatenate([su, ui, su, ui], 1)
    c["msl"] = sl
    c["mui"] = ui
    c["bdones"] = ((p // 64) == (j // 64)).astype(np.float32)
    cc = np.arange(14 * 128)[None, :]
    dist = (cc // 128 - 3) * 128 + (cc % 128) - p
    c["idxE"] = np.where(dist >= 0, t5_bucket_np(dist), -1).astype(np.float32)
    rm = np.ones((128, 512), np.float32)
    rm[:, ::128] = 0
    c["rmask"] = rm
    qt = np.arange(32)[:, None]
    n = np.arange(16)[None, :]
    past = (n < qt // 2).astype(np.float32)
    c["negpast"] = np.broadcast_to(((1 - past) * -1e30).reshape(1, 512), (128, 512)).copy().astype(np.float32)
    c["past30"] = np.broadcast_to((past * 30000.0).reshape(1, 512), (128, 512)).copy().astype(np.float32)
    lm = np.zeros((128, 7, 2, 2, 128), np.float32)
    tt_ = np.arange(128)[:, None]; ii_ = np.arange(128)[None, :]
    for k in range(7):
        b = 2 ** k
        M = (((tt_ // (2 * b)) == (ii_ // (2 * b))) & ((tt_ % (2 * b)) >= b) & ((ii_ % (2 * b)) < b)).astype(np.float32)
        lm[:, k, :, 0, :] = M[:, None, :]
        lm[:, k, :, 1, :] = M.T[:, None, :]
    c["lvlmask"] = lm.reshape(128, 7 * 512).astype(ml_dtypes.bfloat16)
    oh = (np.arange(S)[None, :] // 256 == np.arange(16)[:, None]).astype(np.float32)
    c["onehotk"] = oh.astype(ml_dtypes.bfloat16)
    return c


CONST_SHAPES = {"ident": ([128, 128], F32), "m4": ([128, 512], F32), "msl": ([128, 128], F32),
                "mui": ([128, 128], F32), "bdones": ([128, 128], F32), "idxE": ([128, 1792], F32),
                "rmask": ([128, 512], F32), "negpast": ([128, 512], F32), "past30": ([128, 512], F32),
                "onehotk": ([16, S], BF16), "lvlmask": ([128, 7 * 512], BF16)}


def prep_inputs(I):
    L = I["ada_w"].shape[0]
    sh = {}
    for k in ("ada_w", "ada_b", "w_in", "w_out", "moe_w1", "moe_w3", "moe_w2"):
        sh[k] = np.ascontiguousarray(I[k], dtype=np.float32)
    sh["n1g"] = np.ascontiguousarray(I["norm1_g"])
    sh["n2g"] = np.ascontiguousarray(I["norm2_g"])
    pp = np.zeros((128, L, NPP), np.float32)
    pidx = np.arange(128)
    for l in range(L):
        pp[:, l, 0:7] = I["rwkv_mu"][l].reshape(7, 128).T
        pp[:, l, 7:9] = I["rwkv_w0"][l].reshape(2, 128).T
        pp[:, l, 9:11] = I["rwkv_a0"][l].reshape(2, 128).T
        pp[:, l, 11:13] = I["rwkv_kk"][l].reshape(2, 128).T
        pp[:, l, 13:15] = I["rwkv_ka"][l].reshape(2, 128).T
        pp[:, l, 15:17] = I["rwkv_rk"][l].reshape(2, 128).T
        pp[:, l, 17:19] = I["rwkv_ln_g"][l].reshape(2, 128).T
        pp[:, l, 19:21] = I["rwkv_ln_b"][l].reshape(2, 128).T
        pp[:, l, 21] = I["diff_q_gain"][l][pidx % 64]
        pp[:, l, 22] = I["diff_k_gain"][l][pidx % 64]
        pp[:, l, 23] = I["moba_q_gain"][l][pidx % 64]
        pp[:, l, 24] = I["moba_k_gain"][l][pidx % 64]
        pp[:, l, 25] = I["diff_subln_g"][l]
    sh["ppar"] = pp.reshape(128, L * NPP)
    sh["lora"] = np.ascontiguousarray(np.concatenate([I["rwkv_w2"], I["rwkv_a2"], I["rwkv_g2"]], axis=1))
    sh["lam"] = np.ascontiguousarray(I["diff_lambda"].reshape(L, 256))
    sh["tbl"] = np.ascontiguousarray(I["rel_bias"].reshape(1, 256))
    sh["tbl2"] = np.ascontiguousarray(I["rel_bias"])
    sh["wr"] = np.ascontiguousarray(np.concatenate([I["router_g_w"], I["router_e_w"]], axis=2))
    sh["br"] = np.ascontiguousarray(np.concatenate([I["router_g_b"], I["router_e_b"]], axis=1))
    sh.update(make_consts())
    per = []
    for b in range(I["x"].shape[0]):
        per.append({"x": np.ascontiguousarray(I["x"][b]),
                    "c8": np.ascontiguousarray(I["c"][b].reshape(8, 128).T)})
    return sh, per


class StopBuild(Exception):
    pass


def build(NL, LTOT=4, dbg=False, stop=None):
    nc = bass.Bass("TRN2", target_bir_lowering=False)

    def din(name, shape, dt=F32):
        return nc.dram_tensor(name, list(shape), dt, kind="ExternalInput").ap()

    x_d = din("x", [S, D]); c8_d = din("c8", [128, 8])
    adaw_d = din("ada_w", [LTOT, D, 6 * D]); adab_d = din("ada_b", [LTOT, 6 * D])
    n1g_d = din("n1g", [LTOT, D]); n2g_d = din("n2g", [LTOT, D])
    win_d = din("w_in", [LTOT, D, 3200]); wout_d = din("w_out", [LTOT, D, D])
    ppar_d = din("ppar", [128, LTOT * NPP]); lora_d = din("lora", [LTOT, 128, 256])
    lam_d = din("lam", [LTOT, 256]); tbl_d = din("tbl", [1, 256]); tbl2_d = din("tbl2", [32, 8])
    wr_d = din("wr", [LTOT, D, 36]); br_d = din("br", [LTOT, 36])
    w1_d = din("moe_w1", [LTOT, 32, D, 512]); w3_d = din("moe_w3", [LTOT, 32, D, 512])
    w2_d = din("moe_w2", [LTOT, 32, 512, D])
    cd = {k: din(k, shp, dt) for k, (shp, dt) in CONST_SHAPES.items()}
    out_d = nc.dram_tensor("out", [S, D], F32, kind="ExternalOutput").ap()
    okind = "ExternalOutput" if dbg else "Internal"
    xr_d = nc.dram_tensor("xr", [S, D], F32, kind=okind).ap()
    hT_d = nc.dram_tensor("hT", [8, 128, S], BF16).ap()
    mixT_d = nc.dram_tensor("mixT", [8, 128, S], BF16, kind=okind).ap()
    Xs_d = nc.dram_tensor("Xs", [32 * CAPMAX + 128, D], BF16).ap()
    Ys_d = nc.dram_tensor("Ys", [32 * CAPMAX + 128, D], F32).ap()

    with ExitStack() as st:
        fw = FW(nc, st)

        uid = [0]

        def sb(stack, name, shape, dt=F32):
            uid[0] += 1
            return stack.enter_context(nc.sbuf_tensor(f"s{uid[0]}_{name}", list(shape), dt))

        pe_cfg = [None]

        def pe_sync(ap):
            cfg = (ap.base_partition(), ap.partition_size())
            if cfg != pe_cfg[0] and fw.cnt["c_pe"] > 0 and not fw.halt:
                fw._wait("pe", ("c_pe", fw.cnt["c_pe"]))
            pe_cfg[0] = cfg

        def MM(out, lhsT, rhs, start, stop, R, W):
            pe_sync(lhsT)
            fw.op("pe", lambda e: e.matmul(out, lhsT=lhsT, rhs=rhs, start=start, stop=stop), R, W, skip_same=True)

        def TR(out, in_, idn, R, W):
            pe_sync(in_)
            fw.op("pe", lambda e: e.transpose(out, in_, idn), R, W, skip_same=True)

        def ACT(out, in_, func, R, W, **kw):
            fw.op("act", lambda e: e.activation(out, in_, func, **kw), R, W)

        def DV(fn, R, W):
            fw.op("dve", fn, R, W)

        def PL(fn, R, W):
            fw.op("pool", fn, R, W)

        def LD(q, out, in_, R, W):
            fw.dma(q, lambda e: e.dma_start(out=out, in_=in_), R, W)

        PF = [st.enter_context(nc.psum_tensor(f"pf{i}", [128, 512], F32)) for i in range(6)]
        RPF = [Region(f"pf{i}") for i in range(6)]
        PB = [st.enter_context(nc.psum_tensor(f"pb{i}", [128, 1024], BF16)) for i in range(2)]
        RPB = [Region(f"pb{i}") for i in range(2)]

        Rc = Region("consts")
        ident = sb(st, "ident", [128, 128]); identb = sb(st, "identb", [128, 128], BF16)
        m4 = sb(st, "m4", [128, 512]); msl = sb(st, "msl", [128, 128]); mui = sb(st, "mui", [128, 128])
        bdones = sb(st, "bdones", [128, 128]); rmask = sb(st, "rmask", [128, 512])
        negpast = sb(st, "negpast", [128, 512]); past30 = sb(st, "past30", [128, 512])
        onesf = sb(st, "onesf", [128, 128]); onesb = sb(st, "onesb", [128, 128], BF16)
        lvlmask = sb(st, "lvlmask", [128, 7, 512], BF16)
        LD("sp", lvlmask[:].rearrange("p k c -> p (k c)"), cd["lvlmask"], [], [Rc])
        ppar = sb(st, "ppar", [128, LTOT * NPP]); tblb = sb(st, "tblb", [128, 256])
        cs8 = sb(st, "cs8", [128, 8]); csrep = sb(st, "csrep", [128, 8, 128])
        modb = sb(st, "modb", [128, 6 * D]); Rmod = Region("modb")
        erel = sb(st, "erel", [128, 8, 1792], BF16); Rerel = Region("erel")
        lamc = sb(st, "lamc", [128, 4]); Rlam = Region("lamc")
        off = sb(st, "off", [128, NTT, 2], I32); Roff = Region()
        gts = sb(st, "gts", [128, NTT, 2]); Rgts = Region()
        RXs = Region(); RYs = Region(); Rout = Region()

        for t, k in ((ident, "ident"), (m4, "m4"), (msl, "msl"), (mui, "mui"), (bdones, "bdones"),
                     (rmask, "rmask"), (negpast, "negpast"), (past30, "past30")):
            LD("sp", t[:], cd[k], [], [Rc])
        LD("sp", ppar[:], ppar_d, [], [Rc])
        LD("sp", tblb[:], tbl_d.partition_broadcast(128), [], [Rc])
        LD("sp", cs8[:], c8_d, [], [Rc])
        PL(lambda e: e.memset(onesf[:], 1.0), [], [Rc])
        PL(lambda e: e.memset(onesb[:], 1.0), [], [Rc])
        DV(lambda e: e.tensor_copy(identb[:], ident[:]), [Rc], [Rc])
        ACT(cs8[:], cs8[:], AF.Silu, [Rc], [Rc])
        for j in range(8):
            DV(lambda e: e.tensor_copy(csrep[:, j, :], cs8[:, j:j + 1].to_broadcast([128, 128])), [Rc], [Rc])

        def pcol(l, i):
            return ppar[:, l * NPP + i: l * NPP + i + 1]

        with ExitStack() as ph:
            idxE = sb(ph, "idxE", [128, 1792]); Ri = Region()
            acc = sb(ph, "eacc", [128, 1792]); Ra = Region()
            tmp = [sb(ph, f"etmp{i}", [128, 1792]) for i in range(2)]; Rt = [Region(), Region()]
            LD("sp", idxE[:], cd["idxE"], [], [Ri])
            it = 0
            for h in range(8):
                PL(lambda e: e.memset(acc[:], 0.0), [], [Ra])
                for b in range(32):
                    t = tmp[it % 2]; rt = Rt[it % 2]; it += 1
                    DV(lambda e: e.tensor_scalar(t[:], idxE[:], float(b), tblb[:, b * 8 + h: b * 8 + h + 1],
                                                 ALU.is_equal, ALU.mult), [Ri, Rc], [rt])
                    PL(lambda e: e.tensor_tensor(acc[:], acc[:], t[:], ALU.add), [rt, Ra], [Ra])
                DV(lambda e: e.tensor_scalar(acc[:], acc[:], tblb[:, 31 * 8 + h: 31 * 8 + h + 1], None, ALU.subtract),
                   [Ra, Rc], [Ra])
                ACT(acc[:], acc[:], AF.Exp, [Ra], [Ra])
                t = tmp[0]
                DV(lambda e: e.tensor_scalar(t[:], idxE[:], 0.0, None, ALU.is_ge), [Ri], [Rt[0]])
                DV(lambda e: e.tensor_tensor(erel[:, h, :], acc[:], t[:], ALU.mult), [Ra, Rt[0]], [Rerel])
            fw.barrier()

        try:
          for l in range(NL):
            lam_init = 0.8 - 0.6 * math.exp(-0.3 * l)
            xsrc = x_d if l == 0 else xr_d
            xdst = out_d if l == NL - 1 else xr_d

            with ExitStack() as ph:
                aw = [sb(ph, f"aw{i}", [128, 8, 512]) for i in range(2)]; Raw = [Region(), Region()]
                ab = [sb(ph, f"ab{i}", [128, 512]) for i in range(2)]; Rab = [Region(), Region()]
                gb = sb(ph, "gb", [128, 2, D]); Rgb = Region()
                lamt = sb(ph, "lamt", [128, 256]); Rlt = Region()
                LD("act", gb[:, 0, :], n1g_d[l:l + 1, :].partition_broadcast(128), [], [Rgb])
                LD("act", gb[:, 1, :], n2g_d[l:l + 1, :].partition_broadcast(128), [], [Rgb])
                LD("act", lamt[:], lam_d[l:l + 1, :].partition_broadcast(128), [], [Rlt])
                for nb in range(12):
                    a = aw[nb % 2]; ra = Raw[nb % 2]; b_ = ab[nb % 2]; rb = Rab[nb % 2]
                    LD("sp", a[:], adaw_d[l].rearrange("(k p) n -> p k n", p=128)[:, :, nb * 512:(nb + 1) * 512], [], [ra])
                    LD("act", b_[:], adab_d[l:l + 1, nb * 512:(nb + 1) * 512].partition_broadcast(128), [], [rb])
                    pf = PF[nb % 2]; rp = RPF[nb % 2]
                    for kc in range(8):
                        MM(pf[:], csrep[:, kc, :], a[:, kc, :], kc == 0, kc == 7, [Rc, ra], [rp])
                    DV(lambda e: e.tensor_tensor(modb[:, nb * 512:(nb + 1) * 512], pf[:], b_[:], ALU.add), [rp, rb], [Rmod])
                for (o, gi) in ((1, 0), (4, 1)):
                    DV(lambda e: e.scalar_tensor_tensor(out=modb[:, o * D:(o + 1) * D], in0=modb[:, o * D:(o + 1) * D],
                                                        scalar=1.0, in1=gb[:, gi, :], op0=ALU.add, op1=ALU.mult),
                       [Rmod, Rgb], [Rmod])
                prod = sb(ph, "lprod", [128, 128]); Rpr = Region()
                DV(lambda e: e.tensor_tensor(prod[:].rearrange("p (a d) -> p a d", a=2),
                                             lamt[:].rearrange("p (a b d) -> p a b d", a=2, b=2)[:, :, 0, :],
                                             lamt[:].rearrange("p (a b d) -> p a b d", a=2, b=2)[:, :, 1, :], ALU.mult),
                   [Rlt], [Rpr])
                DV(lambda e: e.tensor_reduce(lamc[:, 1:3], prod[:].rearrange("p (a d) -> p a d", a=2), AX.X, ALU.add),
                   [Rpr], [Rlam])
                ACT(lamc[:, 1:3], lamc[:, 1:3], AF.Exp, [Rlam], [Rlam])
                DV(lambda e: e.tensor_tensor(lamc[:, 0:1], lamc[:, 2:3], lamc[:, 1:2], ALU.subtract), [Rlam], [Rlam])
                DV(lambda e: e.tensor_scalar(lamc[:, 0:1], lamc[:, 0:1], -lam_init, None, ALU.add), [Rlam], [Rlam])
                DV(lambda e: e.tensor_scalar(lamc[:, 3:4], pcol(l, 25), 1.0 - lam_init, None, ALU.mult), [Rc, Rlam], [Rlam])
                fw.barrier()
            if stop == '0':
                fw.halt = True
            SH1, A1, G1 = modb[:, 0:D], modb[:, D:2 * D], modb[:, 2 * D:3 * D]
            SH2, A2, G2 = modb[:, 3 * D:4 * D], modb[:, 4 * D:5 * D], modb[:, 5 * D:6 * D]

            def norm_mod(ph, xt, rx, Ax, Bx, hf, rhf, small, rsm):
                PL(lambda e: e.memset(small[:, 0:1], 0.0), [], [rsm])
                ACT(hf[:], xt[:], AF.Square, [rx, rsm], [rhf, rsm], accum_out=small[:, 0:1])
                DV(lambda e: e.tensor_scalar(small[:, 1:2], small[:, 0:1], 1.0 / D, 1e-6, ALU.mult, ALU.add), [rsm], [rsm])
                ACT(small[:, 1:2], small[:, 1:2], AF.Sqrt, [rsm], [rsm])
                DV(lambda e: e.reciprocal(small[:, 2:3], small[:, 1:2]), [rsm], [rsm])
                DV(lambda e: e.scalar_tensor_tensor(out=hf[:], in0=xt[:], scalar=small[:, 2:3], in1=Ax,
                                                    op0=ALU.mult, op1=ALU.mult), [rx, rsm, Rmod], [rhf])
                PL(lambda e: e.tensor_tensor(hf[:], hf[:], Bx, ALU.add), [rhf, Rmod], [rhf])

            with ExitStack() as ph:
                xt = [sb(ph, f"xt{i}", [128, D]) for i in range(2)]; Rx = [Region(), Region()]
                hf = [sb(ph, f"hf{i}", [128, D]) for i in range(2)]; Rhf = [Region(), Region()]
                hb = [sb(ph, f"hb{i}", [128, D], BF16) for i in range(2)]; Rhb = [Region(), Region()]
                sm = [sb(ph, f"sm{i}", [128, 4]) for i in range(2)]; Rsm = [Region(), Region()]
                hblk = [sb(ph, f"hblk{i}", [128, 8, 512], BF16) for i in range(2)]; Rhk = [Region(), Region()]
                for tt in range(NTT):
                    i = tt % 2
                    LD("sp", xt[i][:], xsrc[tt * 128:(tt + 1) * 128, :], [], [Rx[i]])
                    norm_mod(ph, xt[i], Rx[i], A1, SH1, hf[i], Rhf[i], sm[i], Rsm[i])
                    DV(lambda e: e.tensor_copy(hb[i][:], hf[i][:]), [Rhf[i]], [Rhb[i]])
                    for kc in range(8):
                        TR(PB[i][:, kc * 128:(kc + 1) * 128], hb[i][:, kc * 128:(kc + 1) * 128], identb[:], [Rhb[i], Rc], [RPB[i]])
                    tb = tt // 4; j = tt % 4; bi = tb % 2
                    ACT(hblk[bi][:, :, j * 128:(j + 1) * 128], PB[i][:].rearrange("p (k t) -> p k t", k=8), AF.Copy,
                        [RPB[i]], [Rhk[bi]])
                    if j == 3:
                        LD("sp", hT_d[:, :, tb * 512:(tb + 1) * 512].rearrange("k p t -> p k t"), hblk[bi][:], [Rhk[bi]], [])
                fw.barrier()
            if stop == 'A':
                fw.halt = True

            with ExitStack() as ph:
                wr_ = sb(ph, "w_r", [128, 8, 896], BF16); Rw = Region()
                fw.dma("pool", lambda e: e.dma_start(out=wr_[:], in_=win_d[l].rearrange("(k p) n -> p k n", p=128)[:, :, 0:896]), [], [Rw])
                lora = sb(ph, "lora", [128, 256]); Rlo = Region()
                LD("sp", lora[:], lora_d[l], [], [Rlo])
                hblk = [sb(ph, f"hblk{i}", [128, 8, 512], BF16) for i in range(2)]; Rhk = [Region(), Region()]
                P7 = sb(ph, "P7", [128, 7, 513]); RP7 = Region()
                D7 = sb(ph, "D7", [128, 7, 512]); RD7 = Region()
                PS7 = sb(ph, "PS7", [128, 7, 512]); RPS7 = Region()
                NT = 14
                T = [sb(ph, f"rt{i}", [128, 512]) for i in range(NT)]; RT = [Region() for _ in range(NT)]
                ARt = sb(ph, "ARt", [128, 4, 256], BF16); RAR = Region()
                Bt = sb(ph, "Bt", [128, 512], BF16); RBt = Region()
                Kt = sb(ph, "Kt", [128, 512], BF16); RKt = Region()
                BHf = sb(ph, "BHf", [128, 512], BF16); KHf = sb(ph, "KHf", [128, 512], BF16); Vf = sb(ph, "Vf", [128, 512], BF16)
                RBH = Region(); RKH = Region(); RVf = Region()
                BHtm = sb(ph, "BHtm", [128, 4, 128], BF16); KHtm = sb(ph, "KHtm", [128, 4, 128], BF16)
                Vtm = sb(ph, "Vtm", [128, 4, 128], BF16); Rtm = Region()
                SB1 = sb(ph, "SB1", [128, 2, 512], BF16); RSB1 = Region()
                SBA = sb(ph, "SBA", [128, 2, 128], BF16); RSBA = Region()
                DE = [sb(ph, f"DE{i}", [128, 2, 2, 128], BF16) for i in range(2)]; RDE = [Region(), Region()]
                ZZ = sb(ph, "ZZ", [128, 2, 2, 128], BF16); RZZ = Region()
                Gm = sb(ph, "Gm", [128, 512]); RGm = Region()
                ST = [sb(ph, f"ST{i}", [128, 64]) for i in range(2)]; RST = [Region(), Region()]
                STb = [sb(ph, f"STb{i}", [128, 64], BF16) for i in range(2)]; RSTb = [Region(), Region()]
                RHSb = sb(ph, "RHSb", [128, 128], BF16); RRH = Region()
                Ub = sb(ph, "Ub", [128, 128], BF16); RUb = Region()
                Ytm = sb(ph, "Ytm", [128, 4, 128]); RY = Region()
                gn = sb(ph, "gn", [128, 8, 4]); Rgn = Region()
                GC = sb(ph, "GC", [128, 4]); RGC = Region()
                mixo = sb(ph, "mixo", [128, 512], BF16); Rmx = Region()
                for hp in range(2):
                    PL(lambda e: e.memset(ST[hp][:], 0.0), [], [RST[hp]])
                    PL(lambda e: e.memset(STb[hp][:], 0.0), [], [RSTb[hp]])
                PL(lambda e: e.memset(P7[:, :, 0:1], 0.0), [], [RP7])

                for tb in range(NTB):
                    hb_ = hblk[tb % 2]; rh = Rhk[tb % 2]
                    LD("sp", hb_[:], hT_d[:, :, tb * 512:(tb + 1) * 512].rearrange("k p t -> p k t"), [], [rh])
                    if tb > 0:
                        DV(lambda e: e.tensor_copy(P7[:, :, 0:1], P7[:, :, 512:513]), [RP7], [RP7])
                    for cc in range(7):
                        pf = PF[cc % 2]; rp = RPF[cc % 2]
                        for kc in range(8):
                            MM(pf[:], wr_[:, kc, cc * 128:(cc + 1) * 128], hb_[:, kc, :], kc == 0, kc == 7, [Rw, rh], [rp])
                        ACT(P7[:, cc, 1:513], pf[:], AF.Copy, [rp], [RP7])
                    DV(lambda e: e.tensor_tensor(D7[:], P7[:, :, 0:512], P7[:, :, 1:513], ALU.subtract), [RP7], [RD7])
                    for cc in range(7):
                        DV(lambda e: e.scalar_tensor_tensor(out=PS7[:, cc, :], in0=D7[:, cc, :], scalar=pcol(l, cc),
                                                            in1=P7[:, cc, 1:513], op0=ALU.mult, op1=ALU.add),
                           [RD7, RP7, Rc], [RPS7])
                    ACT(PS7[0:32, 6, :], PS7[0:32, 6, :], AF.Tanh, [RPS7], [RPS7])
                    ACT(PS7[64:128, 6, :], PS7[64:128, 6, :], AF.Sigmoid, [RPS7], [RPS7])
                    if stop == 'B1':
                        fw.halt = True
                    for hp in range(2):
                        rs, ks, vs = PS7[:, hp, :], PS7[:, 2 + hp, :], PS7[:, 4 + hp, :]
                        cs_ = slice(hp * 128, (hp + 1) * 128)
                        sg, av, gv, kk, sq, kkn, k2, lw, cum, e1, e2, e3, e4, bon = T
                        Rsg, Rav, Rgv, Rkk, Rsq, Rkkn, Rk2, Rlw, Rcum, Re1, Re2, Re3, Re4, Rbon = RT
                        MM(PF[2][:], lora[0:32, cs_], PS7[0:32, 6, :], True, True, [Rlo, RPS7], [RPF[2]])
                        ACT(sg[:], PF[2][:], AF.Sigmoid, [RPF[2], Rc], [Rsg], bias=pcol(l, 7 + hp))
                        MM(PF[3][:], lora[32:64, cs_], PS7[32:64, 6, :], True, True, [Rlo, RPS7], [RPF[3]])
                        ACT(av[:], PF[3][:], AF.Sigmoid, [RPF[3], Rc], [Rav], bias=pcol(l, 9 + hp))
                        MM(PF[2][:], lora[64:128, cs_], PS7[64:128, 6, :], True, True, [Rlo, RPS7], [RPF[2]])
                        ACT(gv[:], PF[2][:], AF.Copy, [RPF[2]], [Rgv])
                        DV(lambda e: e.tensor_scalar(kk[:], ks, pcol(l, 11 + hp), None, ALU.mult), [RPS7, Rc], [Rkk])
                        PL(lambda e: e.tensor_tensor(sq[:], kk[:], kk[:], ALU.mult), [Rkk], [Rsq])
                        MM(PF[3][:], bdones[:], sq[:], True, True, [Rc, Rsq], [RPF[3]])
                        ACT(sq[:], PF[3][:], AF.Sqrt, [RPF[3]], [Rsq])
                        DV(lambda e: e.tensor_scalar(sq[:], sq[:], 1e-12, None, ALU.max), [Rsq], [Rsq])
                        DV(lambda e: e.reciprocal(sq[:], sq[:]), [Rsq], [Rsq])
                        DV(lambda e: e.tensor_tensor(kkn[:], kk[:], sq[:], ALU.mult), [Rkk, Rsq], [Rkkn])
                        DV(lambda e: e.tensor_scalar(k2[:], av[:], 1.0, pcol(l, 13 + hp), ALU.subtract, ALU.mult), [Rav, Rc], [Rk2])
                        DV(lambda e: e.scalar_tensor_tensor(out=k2[:], in0=k2[:], scalar=1.0, in1=ks, op0=ALU.add, op1=ALU.mult),
                           [Rk2, RPS7], [Rk2])
                        PL(lambda e: e.tensor_tensor(kk[:], rs, k2[:], ALU.mult), [RPS7, Rk2, Rkkn], [Rkk])
                        DV(lambda e: e.tensor_scalar(kk[:], kk[:], pcol(l, 15 + hp), None, ALU.mult), [Rkk, Rc], [Rkk])
                        MM(PF[2][:], bdones[:], kk[:], True, True, [Rc, Rkk], [RPF[2]])
                        DV(lambda e: e.tensor_tensor(bon[:], PF[2][:], vs, ALU.mult), [RPF[2], RPS7], [Rbon])
                        DV(lambda e: e.tensor_scalar(lw[:], sg[:], -0.6065306597126334, None, ALU.mult), [Rsg], [Rlw])
                        DV(lambda e: e.tensor_tensor_scan(cum[:], rmask[:], lw[:], 0.0, ALU.mult, ALU.add), [Rc, Rlw], [Rcum])
                        cum3 = cum[:].rearrange("p (c t) -> p c t", t=128)
                        ACT(e1[:], cum[:], AF.Exp, [Rcum], [Re1])
                        ACT(e2[:], cum[:], AF.Exp, [Rcum], [Re2], scale=-1.0)
                        DV(lambda e: e.tensor_tensor(e3[:], cum[:], lw[:], ALU.subtract), [Rcum, Rlw], [Re3])
                        ACT(e3[:], e3[:], AF.Exp, [Re3], [Re3])
                        DV(lambda e: e.tensor_tensor(e4[:].rearrange("p (c t) -> p c t", t=128),
                                                     cum3[:, :, 127:128].to_broadcast([128, 4, 128]), cum3, ALU.subtract),
                           [Rcum], [Re4])
                        ACT(e4[:], e4[:], AF.Exp, [Re4], [Re4])
                        ACT(GC[:].rearrange("p (c o) -> p c o", o=1), cum3[:, :, 127:128], AF.Exp, [Rcum], [RGC])
                        AR3 = ARt[:]
                        DV(lambda e: e.scalar_tensor_tensor(out=AR3[:, :, 0:128], in0=kkn[:].rearrange("p (c t) -> p c t", t=128),
                                                            scalar=-1.0, in1=e3[:].rearrange("p (c t) -> p c t", t=128),
                                                            op0=ALU.mult, op1=ALU.mult), [Rkkn, Re3], [RAR])
                        PL(lambda e: e.tensor_tensor(AR3[:, :, 128:256], PS7[:, hp, :].rearrange("p (c t) -> p c t", t=128),
                                                     e1[:].rearrange("p (c t) -> p c t", t=128), ALU.mult), [RPS7, Re1], [RAR])
                        DV(lambda e: e.tensor_tensor(kkn[:], kkn[:], av[:], ALU.mult), [Rkkn, Rav, RAR], [Rkkn])
                        DV(lambda e: e.tensor_tensor(Bt[:], kkn[:], e2[:], ALU.mult), [Rkkn, Re2], [RBt])
                        PL(lambda e: e.tensor_tensor(BHf[:], kkn[:], e4[:], ALU.mult), [Rkkn, Re4], [RBH])
                        DV(lambda e: e.tensor_tensor(Kt[:], k2[:], e2[:], ALU.mult), [Rk2, Re2], [RKt])
                        PL(lambda e: e.tensor_tensor(KHf[:], k2[:], e4[:], ALU.mult), [Rk2, Re4], [RKH])
                        ACT(Vf[:], vs, AF.Copy, [RPS7], [RVf])
                        for c in range(4):
                            TR(PB[0][:, c * 128:(c + 1) * 128], BHf[:, c * 128:(c + 1) * 128], identb[:], [RBH, Rc], [RPB[0]])
                            TR(PB[0][:, 512 + c * 128:512 + (c + 1) * 128], KHf[:, c * 128:(c + 1) * 128], identb[:], [RKH, Rc], [RPB[0]])
                            TR(PB[1][:, c * 128:(c + 1) * 128], Vf[:, c * 128:(c + 1) * 128], identb[:], [RVf, Rc], [RPB[1]])
                        ACT(BHtm[:], PB[0][:, 0:512].rearrange("p (c t) -> p c t", t=128), AF.Copy, [RPB[0]], [Rtm])
                        DV(lambda e: e.tensor_copy(KHtm[:], PB[0][:, 512:1024].rearrange("p (c t) -> p c t", t=128)), [RPB[0]], [Rtm])
                        ACT(Vtm[:], PB[1][:, 0:512].rearrange("p (c t) -> p c t", t=128), AF.Copy, [RPB[1]], [Rtm])
                        if stop == 'B2':
                            fw.halt = True
                        for c in range(4):
                            cl = slice(c * 128, (c + 1) * 128)
                            for j in range(2):
                                R_ = slice(j * 64, (j + 1) * 64)
                                pf = PF[j]
                                MM(pf[:, 0:256], Bt[R_, cl], ARt[R_, c, :], True, True, [RBt, RAR], [RPF[j]])
                                if stop == 'B3a1':
                                    fw.halt = True
                                MM(pf[:, 256:512], Kt[R_, cl], ARt[R_, c, :], True, True, [RKt, RAR], [RPF[j]])
                                if stop == 'B3a2':
                                    fw.halt = True
                                MM(PF[4][:, j * 128:(j + 1) * 128], ARt[R_, c, 0:128], Bt[R_, cl], True, True, [RAR, RBt], [RPF[4]])
                                if stop == 'B3a' and j == 0:
                                    fw.halt = True
                                if stop == 'B3b' and j == 1:
                                    fw.halt = True
                            for j in range(2):
                                DV(lambda e: e.tensor_tensor(SB1[:, j, :], PF[j][:], m4[:], ALU.mult), [RPF[j], Rc], [RSB1])
                            if stop == 'B3c':
                                fw.halt = True
                            for j in range(2):
                                DV(lambda e: e.tensor_tensor(SBA[:, j, :], PF[4][:, j * 128:(j + 1) * 128], msl[:], ALU.mult),
                                   [RPF[4], Rc], [RSBA])
                            if stop == 'B3d':
                                fw.halt = True
                            for j in range(2):
                                for z in range(2):
                                    PL(lambda e: e.tensor_copy(DE[0][:, j, z, :], identb[:]), [Rc], [RDE[0]])
                            wi = 0
                            for k in range(7):
                                pz = PF[1]; pg = PF[4]
                                for j in range(2):
                                    MM(pz[:, j * 256:j * 256 + 128], SB1[:, j, 0:128], DE[wi][:, j, 0, :], True, True, [RSB1, RDE[wi]], [RPF[1]])
                                    MM(pz[:, j * 256 + 128:j * 256 + 256], SBA[:, j, :], DE[wi][:, j, 1, :], True, True, [RSBA, RDE[wi]], [RPF[1]])
                                ACT(ZZ[:].rearrange("p j z t -> p (j z t)"), pz[:], AF.Copy, [RPF[1]], [RZZ])
                                for j in range(2):
                                    MM(pg[:, j * 256:j * 256 + 128], DE[wi][:, j, 1, :], ZZ[:, j, 0, :], True, True, [RDE[wi], RZZ], [RPF[4]])
                                    MM(pg[:, j * 256 + 128:j * 256 + 256], DE[wi][:, j, 0, :], ZZ[:, j, 1, :], True, True, [RDE[wi], RZZ], [RPF[4]])
                                DV(lambda e: e.tensor_tensor(Gm[:], pg[:], lvlmask[:, k, :], ALU.mult), [RPF[4], Rc], [RGm])
                                PL(lambda e: e.tensor_tensor(DE[1 - wi][:].rearrange("p j z t -> p (j z t)"), Gm[:],
                                                             DE[wi][:].rearrange("p j z t -> p (j z t)"), ALU.add), [RGm, RDE[wi]], [RDE[1 - wi]])
                                wi = 1 - wi
                            W_ = DE[wi]; RW_ = RDE[wi]
                            if stop == 'B3':
                                fw.halt = True
                            pr = PF[5]
                            for j in range(2):
                                R_ = slice(j * 64, (j + 1) * 64); vj = slice(j * 64, (j + 1) * 64)
                                MM(pr[:, vj], ARt[R_, c, 0:128], STb[hp][R_, :], True, False, [RAR, RSTb[hp]], [RPF[5]])
                                MM(pr[:, vj], SB1[:, j, 256:384], Vtm[:, c, vj], False, True, [RSB1, Rtm], [RPF[5]])
                            ACT(RHSb[:], pr[:, 0:128], AF.Copy, [RPF[5]], [RRH])
                            pu = PF[4]
                            for j in range(2):
                                vj = slice(j * 64, (j + 1) * 64)
                                MM(pu[:, 256 + j * 64:256 + (j + 1) * 64], W_[:, j, 1, :], RHSb[:, vj], True, True, [RW_, RRH], [RPF[4]])
                            DV(lambda e: e.tensor_copy(Ub[:], pu[:, 256:384]), [RPF[4]], [RUb])
                            py = PF[5]
                            for j in range(2):
                                R_ = slice(j * 64, (j + 1) * 64); vj = slice(j * 64, (j + 1) * 64)
                                yo = py[:, 128 + j * 64:128 + (j + 1) * 64]
                                MM(yo, ARt[R_, c, 128:256], STb[hp][R_, :], True, False, [RAR, RSTb[hp]], [RPF[5]])
                                MM(yo, SB1[:, j, 128:256], Ub[:, vj], False, False, [RSB1, RUb], [RPF[5]])
                                MM(yo, SB1[:, j, 384:512], Vtm[:, c, vj], False, True, [RSB1, Rtm], [RPF[5]])
                            ACT(Ytm[:, c, :], py[:, 128:256], AF.Copy, [RPF[5]], [RY])
                            pss = PF[5]
                            for j in range(2):
                                R_ = slice(j * 64, (j + 1) * 64); vj = slice(j * 64, (j + 1) * 64)
                                so = pss[R_, 256:320]
                                MM(so, BHtm[:, c, R_], Ub[:, vj], True, False, [Rtm, RUb], [RPF[5]])
                                MM(so, KHtm[:, c, R_], Vtm[:, c, vj], False, True, [Rtm], [RPF[5]])
                            DV(lambda e: e.scalar_tensor_tensor(out=ST[hp][:], in0=ST[hp][:], scalar=GC[:, c:c + 1], in1=pss[:, 256:320],
                                                                op0=ALU.mult, op1=ALU.add), [RST[hp], RGC, RPF[5]], [RST[hp]])
                            ACT(STb[hp][:], ST[hp][:], AF.Copy, [RST[hp]], [RSTb[hp]])
                            if stop == 'B4':
                                fw.halt = True
                        Y8 = Ytm[:].rearrange("p c (j v) -> p (c j) v", j=2)
                        DV(lambda e: e.tensor_reduce(gn[:, :, 0], Y8, AX.X, ALU.add), [RY], [Rgn])
                        DV(lambda e: e.tensor_scalar(gn[:, :, 0], gn[:, :, 0], 1.0 / 64, None, ALU.mult), [Rgn], [Rgn])
                        DV(lambda e: e.tensor_tensor(Y8, Y8, gn[:, :, 0:1].to_broadcast([128, 8, 64]), ALU.subtract), [RY, Rgn], [RY])
                        Ysq = sq[:].rearrange("p (a v) -> p a v", v=64)
                        PL(lambda e: e.tensor_tensor(Ysq, Y8, Y8, ALU.mult), [RY, Rsq], [Rsq])
                        DV(lambda e: e.tensor_reduce(gn[:, :, 1], Ysq, AX.X, ALU.add), [Rsq], [Rgn])
                        DV(lambda e: e.tensor_scalar(gn[:, :, 1], gn[:, :, 1], 1.0 / 64, 64e-5, ALU.mult, ALU.add), [Rgn], [Rgn])
                        ACT(gn[:, :, 1], gn[:, :, 1], AF.Sqrt, [Rgn], [Rgn])
                        DV(lambda e: e.reciprocal(gn[:, :, 2], gn[:, :, 1]), [Rgn], [Rgn])
                        DV(lambda e: e.tensor_tensor(Y8, Y8, gn[:, :, 2:3].to_broadcast([128, 8, 64]), ALU.mult), [RY, Rgn], [RY])
                        if stop == 'B5':
                            fw.halt = True
                        for c in range(4):
                            TR(PF[3][:, c * 128:(c + 1) * 128], Ytm[:, c, :], ident[:], [RY, Rc], [RPF[3]])
                        DV(lambda e: e.tensor_scalar(e1[:], PF[3][:], pcol(l, 17 + hp), pcol(l, 19 + hp), ALU.mult, ALU.add),
                           [RPF[3], Rc, RAR], [Re1])
                        DV(lambda e: e.tensor_tensor(e1[:], e1[:], bon[:], ALU.add), [Re1, Rbon], [Re1])
                        DV(lambda e: e.tensor_tensor(mixo[:], e1[:], gv[:], ALU.mult), [Re1, Rgv], [Rmx])
                        LD("sp", mixT_d[hp, :, tb * 512:(tb + 1) * 512], mixo[:], [Rmx], [])
                        if stop == 'B6':
                            fw.halt = True
                        if stop == 'B7' and hp == 1:
                            fw.halt = True
                        if stop == 'B8' and hp == 1 and tb == 1:
                            fw.halt = True
                fw.barrier()
            if stop == 'B':
                fw.halt = True

            with ExitStack() as ph:
                hblk = [sb(ph, f"hblk{i}", [128, 8, 512], BF16) for i in range(2)]; Rhk = [Region(), Region()]
                wa = [sb(ph, f"wa{i}", [128, 8, 384], BF16) for i in range(2)]; Rwa = [Region(), Region()]
                QT = sb(ph, "QT", [128, S], BF16); KT = sb(ph, "KT", [128, S], BF16); RQ = Region(); RK = Region()
                Vt = sb(ph, "Vt", [128, NTT, 128], BF16); RV = Region()
                qf = sb(ph, "qf", [128, 512]); Rqf = Region()
                sqf = sb(ph, "sqf", [128, 512]); Rsqf = Region()
                rsf = sb(ph, "rsf", [128, 512]); Rrsf = Region()
                Pf = [sb(ph, f"Pf{i}", [128, 512]) for i in range(2)]; RPf = [Region(), Region()]
                Pb = [sb(ph, f"Pb{i}", [128, 512], BF16) for i in range(3)]; RPb = [Region() for _ in range(3)]
                rec = sb(ph, "rec", [128, 2, 512]); Rrec = Region()
                bcs = sb(ph, "bcs", [128, 2, 512]); Rbcs = Region()
                Of = sb(ph, "Of", [128, 512]); ROf = Region()
                Ob = sb(ph, "Ob", [128, 512], BF16); ROb = Region()
                kmT = sb(ph, "kmT", [128, 16]); Rkm = Region()
                gm = sb(ph, "gm", [128, 16]); top8 = sb(ph, "top8", [128, 8]); Rgm = Region()
                nmw = sb(ph, "nmw", [128, 4, 80]); Rnm = Region()
                PL(lambda e: e.memset(nmw[:], 0.0), [], [Rnm])
                pbi = 0

                def proj_fm(hb_, rh, w, rw, c0, M, pf, rp):
                    for kc in range(8):
                        MM(pf[0:M, :], w[:, kc, c0:c0 + M], hb_[:, kc, :], kc == 0, kc == 7, [rw, rh], [rp])

                def headnorm(pf, rp, M, gcol, dst, rdst, keepf=None):
                    ACT(sqf[0:M, :], pf[0:M, :], AF.Square, [rp], [Rsqf])
                    MM(PF[2][0:M, :], bdones[0:M, 0:M], sqf[0:M, :], True, True, [Rc, Rsqf], [RPF[2]])
                    DV(lambda e: e.tensor_scalar(rsf[0:M, :], PF[2][0:M, :], 1.0 / 64, 1e-6, ALU.mult, ALU.add), [RPF[2]], [Rrsf])
                    ACT(rsf[0:M, :], rsf[0:M, :], AF.Sqrt, [Rrsf], [Rrsf])
                    DV(lambda e: e.reciprocal(rsf[0:M, :], rsf[0:M, :]), [Rrsf], [Rrsf])
                    DV(lambda e: e.scalar_tensor_tensor(out=qf[0:M, :], in0=pf[0:M, :], scalar=gcol[0:M, :], in1=rsf[0:M, :],
                                                        op0=ALU.mult, op1=ALU.mult), [rp, Rc, Rrsf], [Rqf])
                    ACT(dst, qf[0:M, :], AF.Copy, [Rqf], [rdst])

                for hd in range(8):
                    moba = hd >= 4
                    h = hd % 4
                    w = wa[hd % 2]; rw = Rwa[hd % 2]
                    win3 = win_d[l].rearrange("(k p) n -> p k n", p=128)
                    if not moba:
                        cols = [(896 + h * 128, 128), (896 + 512 + h * 128, 128), (896 + 1024 + h * 128, 128)]
                    else:
                        cols = [(2432 + h * 64, 64), (2432 + 256 + h * 64, 64), (2432 + 512 + h * 64, 64)]
                    for i, (c0, n) in enumerate(cols):
                        fw.dma("pool", lambda e: e.dma_start(out=w[:, :, i * 128:i * 128 + n], in_=win3[:, :, c0:c0 + n]), [], [rw])
                    M = 64 if moba else 128
                    dv = 64 if moba else 128
                    gq = pcol(l, 23 if moba else 21); gk = pcol(l, 24 if moba else 22)
                    if moba:
                        LD("sp", KT[64:80, :], cd["onehotk"], [], [RK])
                        PL(lambda e: e.memset(kmT[:], 0.0), [], [Rkm])
                        PL(lambda e: e.memset(Vt[:, :, 64:65], 1.0), [], [RV])
                    for tb in range(NTB):
                        hb_ = hblk[tb % 2]; rh = Rhk[tb % 2]
                        LD("sp", hb_[:], hT_d[:, :, tb * 512:(tb + 1) * 512].rearrange("k p t -> p k t"), [], [rh])
                        tsl = slice(tb * 512, (tb + 1) * 512)
                        proj_fm(hb_, rh, w, rw, 128, M, PF[0], RPF[0])
                        headnorm(PF[0], RPF[0], M, gk, KT[0:M, tsl], RK)
                        if moba:
                            DV(lambda e: e.tensor_reduce(kmT[0:64, 2 * tb:2 * tb + 2], qf[0:64, :].rearrange("p (a t) -> p a t", a=2),
                                                         AX.X, ALU.add), [Rqf], [Rkm])
                            DV(lambda e: e.tensor_scalar(kmT[0:64, 2 * tb:2 * tb + 2], kmT[0:64, 2 * tb:2 * tb + 2], 1.0 / 256, None, ALU.mult),
                               [Rkm], [Rkm])
                        proj_fm(hb_, rh, w, rw, 0, M, PF[1], RPF[1])
                        headnorm(PF[1], RPF[1], M, gq, QT[0:M, tsl], RQ)
                        for j in range(4):
                            for kc in range(8):
                                MM(PF[3][:, j * 128:j * 128 + dv], hb_[:, kc, j * 128:(j + 1) * 128], w[:, kc, 256:256 + dv],
                                   kc == 0, kc == 7, [rh, rw], [RPF[3]])
                        ACT(Vt[:, tb * 4:(tb + 1) * 4, 0:dv], PF[3][:].rearrange("p (j t) -> p j t", j=4)[:, :, 0:dv], AF.Copy, [RPF[3]], [RV])
                        if moba:
                            for j in range(4):
                                qt = tb * 4 + j
                                MM(PF[4][:, j * 16:(j + 1) * 16], qf[0:64, j * 128:(j + 1) * 128], kmT[0:64, :], True, True, [Rqf, Rkm], [RPF[4]])
                                DV(lambda e: e.tensor_tensor(gm[:], PF[4][:, j * 16:(j + 1) * 16], negpast[:, qt * 16:(qt + 1) * 16], ALU.add),
                                   [RPF[4], Rc], [Rgm])
                                DV(lambda e: e.max(out=top8[:], in_=gm[:]), [Rgm], [Rgm])
                                DV(lambda e: e.tensor_scalar(gm[:], gm[:], top8[:, 2:3], None, ALU.is_ge), [Rgm], [Rgm])
                                DV(lambda e: e.scalar_tensor_tensor(out=nmw[:, j, 64:80], in0=gm[:], scalar=1.0, in1=past30[:, qt * 16:(qt + 1) * 16],
                                                                    op0=ALU.subtract, op1=ALU.mult), [Rgm, Rc], [Rnm])
                                TR(PF[5][0:80, j * 128:(j + 1) * 128], nmw[:, j, :], ident[:], [Rnm, Rc], [RPF[5]])
                            ACT(QT[64:80, tsl], PF[5][64:80, :], AF.Copy, [RPF[5]], [RQ])
                    KK = 80 if moba else 64
                    for qb in range(NTB):
                        qsl = slice(qb * 512, (qb + 1) * 512)
                        nmap = 1 if moba else 2
                        nkt = 4 * qb + 4
                        for m in range(nmap):
                            rows = slice(0, KK) if moba else slice(m * 64, (m + 1) * 64)
                            po = PF[2 + m]; rpo = RPF[2 + m]
                            for kt in range(nkt):
                                ps = PF[kt % 2]; rps = RPF[kt % 2]
                                MM(ps[:], KT[rows, kt * 128:(kt + 1) * 128], QT[rows, qsl], True, True, [RK, RQ], [rps])
                                o0 = 4 * qb - kt
                                pb = Pb[pbi % 3]; rpb = RPb[pbi % 3]; pbi += 1
                                b31 = tblb[:, 31 * 8 + hd: 31 * 8 + hd + 1]
                                if o0 <= 7:
                                    pfx = Pf[kt % 2]; rpf = RPf[kt % 2]
                                    ACT(pfx[:], ps[:], AF.Exp, [rps, Rc], [rpf], bias=b31, scale=0.125)
                                    DV(lambda e: e.tensor_tensor(pb[:], pfx[:], erel[:, hd, (o0 + 3) * 128:(o0 + 3) * 128 + 512], ALU.mult),
                                       [rpf, Rerel], [rpb])
                                else:
                                    ACT(pb[:], ps[:], AF.Exp, [rps, Rc], [rpb], bias=b31, scale=0.125)
                                if moba:
                                    MM(po[0:65, :], Vt[:, kt, 0:65], pb[:], kt == 0, kt == nkt - 1, [RV, rpb], [rpo])
                                else:
                                    MM(po[:], Vt[:, kt, :], pb[:], kt == 0, kt == nkt - 1, [RV, rpb], [rpo])
                                    MM(PF[4 + m][0:1, :], onesb[:, 0:1], pb[:], kt == 0, kt == nkt - 1, [Rc, rpb], [RPF[4 + m]])
                        if moba:
                            DV(lambda e: e.reciprocal(rec[64:65, 0, :], PF[2][64:65, :]), [RPF[2]], [Rrec])
                            MM(PF[0][0:64, :], onesf[64:65, 0:64], rec[64:65, 0, :], True, True, [Rc, Rrec], [RPF[0]])
                            ACT(bcs[0:64, 0, :], PF[0][0:64, :], AF.Copy, [RPF[0]], [Rbcs])
                            DV(lambda e: e.tensor_tensor(Ob[0:64, :], PF[2][0:64, :], bcs[0:64, 0, :], ALU.mult), [RPF[2], Rbcs], [ROb])
                            LD("sp", mixT_d[6 + h // 2, (h % 2) * 64:(h % 2) * 64 + 64, qsl], Ob[0:64, :], [ROb], [])
                        else:
                            DV(lambda e: e.reciprocal(rec[0:1, 0, :], PF[4][0:1, :]), [RPF[4]], [Rrec])
                            DV(lambda e: e.reciprocal(rec[0:1, 1, :], PF[5][0:1, :]), [RPF[5]], [Rrec])
                            DV(lambda e: e.tensor_scalar(rec[0:1, 1, :], rec[0:1, 1, :], lamc[0:1, 0:1], None, ALU.mult), [Rrec, Rlam], [Rrec])
                            for m in range(2):
                                MM(PF[m][:], onesf[0:1, :], rec[0:1, m, :], True, True, [Rc, Rrec], [RPF[m]])
                                ACT(bcs[:, m, :], PF[m][:], AF.Copy, [RPF[m]], [Rbcs])
                            DV(lambda e: e.tensor_tensor(Of[:], PF[2][:], bcs[:, 0, :], ALU.mult), [RPF[2], Rbcs], [ROf])
                            DV(lambda e: e.tensor_tensor(sqf[:], PF[3][:], bcs[:, 1, :], ALU.mult), [RPF[3], Rbcs], [Rsqf])
                            DV(lambda e: e.tensor_tensor(Of[:], Of[:], sqf[:], ALU.add), [ROf, Rsqf], [ROf])
                            ACT(sqf[:], Of[:], AF.Square, [ROf], [Rsqf])
                            MM(PF[0][:], onesf[:], sqf[:], True, True, [Rc, Rsqf], [RPF[0]])
                            DV(lambda e: e.tensor_scalar(rsf[:], PF[0][:], 1.0 / 128, 1e-6, ALU.mult, ALU.add), [RPF[0]], [Rrsf])
                            ACT(rsf[:], rsf[:], AF.Sqrt, [Rrsf], [Rrsf])
                            DV(lambda e: e.reciprocal(rsf[:], rsf[:]), [Rrsf], [Rrsf])
                            DV(lambda e: e.scalar_tensor_tensor(out=Ob[:], in0=Of[:], scalar=lamc[:, 3:4], in1=rsf[:], op0=ALU.mult, op1=ALU.mult),
                               [ROf, Rlam, Rrsf], [ROb])
                            LD("sp", mixT_d[2 + h, :, qsl], Ob[:], [ROb], [])
                fw.barrier()
            if stop == 'C':
                fw.halt = True

            CAPl = CAPS[l]
            NROW = 32 * CAPl
            with ExitStack() as ph:
                wo = sb(ph, "wo", [128, 8, D], BF16); Rwo = Region()
                fw.dma("pool", lambda e: e.dma_start(out=wo[:], in_=wout_d[l].rearrange("(k p) n -> p k n", p=128)), [], [Rwo])
                wrt = sb(ph, "wrt", [128, 8, 36]); brb = sb(ph, "brb", [128, 36]); Rwr = Region()
                LD("sp", wrt[:], wr_d[l].rearrange("(k p) n -> p k n", p=128), [], [Rwr])
                LD("sp", brb[:], br_d[l:l + 1, :].partition_broadcast(128), [], [Rwr])
                mblk = [sb(ph, f"mblk{i}", [128, 8, 512], BF16) for i in range(2)]; Rmb = [Region(), Region()]
                xt = [sb(ph, f"xt{i}", [128, D]) for i in range(2)]; Rx = [Region(), Region()]
                hf = [sb(ph, f"hf{i}", [128, D]) for i in range(2)]; Rhf = [Region(), Region()]
                h2b = [sb(ph, f"h2b{i}", [128, D], BF16) for i in range(2)]; Rhb = [Region(), Region()]
                sm = [sb(ph, f"sm{i}", [128, 4]) for i in range(2)]; Rsm = [Region(), Region()]
                h2T = sb(ph, "h2T", [128, 8, 128]); RhT = Region()
                lg = sb(ph, "lg", [128, 36]); ml = sb(ph, "ml", [128, 32]); oh = sb(ph, "oh", [128, 2, 32])
                rt8 = sb(ph, "rt8", [128, 8]); rs_ = sb(ph, "rs_", [128, 16]); Rr = Region()
                cntb = sb(ph, "cntb", [128, 32]); Rcnt = Region()
                io32 = sb(ph, "io32", [128, 32]); posf = sb(ph, "posf", [128, 32]); msk = sb(ph, "msk", [128, 32])
                tmp32 = sb(ph, "tmp32", [128, 32]); dst = sb(ph, "dstf", [128, 2])
                PL(lambda e: e.memset(cntb[:], 0.0), [], [Rcnt])
                PL(lambda e: e.iota(io32[:], pattern=[[1, 32]], base=0, channel_multiplier=0, allow_small_or_imprecise_dtypes=True), [], [Rr])
                for tt in range(NTT):
                    i = tt % 2; tb = tt // 4; j = tt % 4
                    if j == 0:
                        LD("sp", mblk[tb % 2][:], mixT_d[:, :, tb * 512:(tb + 1) * 512].rearrange("k p t -> p k t"), [], [Rmb[tb % 2]])
                    mb = mblk[tb % 2]; rmb = Rmb[tb % 2]
                    LD("sp", xt[i][:], xsrc[tt * 128:(tt + 1) * 128, :], [], [Rx[i]])
                    for half in range(2):
                        for kc in range(8):
                            MM(PF[half][:], mb[:, kc, j * 128:(j + 1) * 128], wo[:, kc, half * 512:(half + 1) * 512], kc == 0, kc == 7,
                               [rmb, Rwo], [RPF[half]])
                        hs = slice(half * 512, (half + 1) * 512)
                        DV(lambda e: e.tensor_tensor(hf[i][:, hs], PF[half][:], G1[:, hs], ALU.mult), [RPF[half], Rmod], [Rhf[i]])
                    PL(lambda e: e.tensor_tensor(xt[i][:], xt[i][:], hf[i][:], ALU.add), [Rx[i], Rhf[i]], [Rx[i]])
                    LD("sp", xr_d[tt * 128:(tt + 1) * 128, :], xt[i][:], [Rx[i]], [])
                    norm_mod(ph, xt[i], Rx[i], A2, SH2, hf[i], Rhf[i], sm[i], Rsm[i])
                    ACT(h2b[i][:], hf[i][:], AF.Copy, [Rhf[i]], [Rhb[i]])
                    for kc in range(8):
                        TR(PF[2 + kc // 4][:, (kc % 4) * 128:(kc % 4 + 1) * 128], hf[i][:, kc * 128:(kc + 1) * 128], ident[:], [Rhf[i], Rc],
                           [RPF[2 + kc // 4]])
                    ACT(h2T[:, 0:4, :], PF[2][:].rearrange("p (k t) -> p k t", k=4), AF.Copy, [RPF[2]], [RhT])
                    DV(lambda e: e.tensor_copy(h2T[:, 4:8, :], PF[3][:].rearrange("p (k t) -> p k t", k=4)), [RPF[3]], [RhT])
                    for kc in range(8):
                        MM(PF[4][:, 0:36], h2T[:, kc, :], wrt[:, kc, :], kc == 0, kc == 7, [RhT, Rwr], [RPF[4]])
                    DV(lambda e: e.tensor_tensor(lg[:], PF[4][:, 0:36], brb[:], ALU.add), [RPF[4], Rwr], [Rr])
                    DV(lambda e: e.tensor_reduce(rs_[:, 0:1], lg[:, 0:4], AX.X, ALU.max), [Rr], [Rr])
                    DV(lambda e: e.tensor_scalar(rs_[:, 1:2], rs_[:, 0:1], -1.0, None, ALU.mult), [Rr], [Rr])
                    PL(lambda e: e.memset(rs_[:, 2:3], 0.0), [Rr], [Rr])
                    ACT(rs_[:, 4:8], lg[:, 0:4], AF.Exp, [Rr], [Rr], bias=rs_[:, 1:2], accum_out=rs_[:, 2:3])
                    DV(lambda e: e.reciprocal(rs_[:, 3:4], rs_[:, 2:3]), [Rr], [Rr])
                    DV(lambda e: e.tensor_scalar(rs_[:, 8:12], lg[:, 0:4], rs_[:, 0:1], None, ALU.is_ge), [Rr], [Rr])
                    DV(lambda e: e.tensor_scalar(rs_[:, 8:12], rs_[:, 8:12], 1.0, 1e30, ALU.subtract, ALU.mult), [Rr], [Rr])
                    DV(lambda e: e.tensor_tensor(ml[:].rearrange("p (g e) -> p g e", g=4), lg[:, 4:36].rearrange("p (g e) -> p g e", g=4),
                                                 rs_[:, 8:12].rearrange("p (g o) -> p g o", o=1).to_broadcast([128, 4, 8]), ALU.add), [Rr], [Rr])
                    DV(lambda e: e.max(out=rt8[:], in_=ml[:]), [Rr], [Rr])
                    DV(lambda e: e.tensor_scalar(oh[:, 0, :], ml[:], rt8[:, 0:1], None, ALU.is_equal), [Rr], [Rr])
                    DV(lambda e: e.tensor_scalar(oh[:, 1, :], ml[:], rt8[:, 1:2], None, ALU.is_equal), [Rr], [Rr])
                    DV(lambda e: e.tensor_tensor(rs_[:, 12:13], rt8[:, 0:1], rt8[:, 1:2], ALU.subtract), [Rr], [Rr])
                    ACT(rs_[:, 13:14], rs_[:, 12:13], AF.Sigmoid, [Rr], [Rr])
                    DV(lambda e: e.tensor_tensor(gts[:, tt, 0:1], rs_[:, 13:14], rs_[:, 3:4], ALU.mult), [Rr], [Rgts])
                    DV(lambda e: e.tensor_tensor(gts[:, tt, 1:2], rs_[:, 3:4], gts[:, tt, 0:1], ALU.subtract), [Rr, Rgts], [Rgts])
                    DV(lambda e: e.tensor_tensor(msk[:], oh[:, 0, :], oh[:, 1, :], ALU.add), [Rr], [Rr])
                    MM(PF[5][:, 0:32], mui[:], msk[:], True, True, [Rc, Rr], [RPF[5]])
                    MM(PF[5][:, 32:64], onesf[:], msk[:], True, True, [Rc, Rr], [RPF[5]])
                    DV(lambda e: e.tensor_tensor(posf[:], PF[5][:, 0:32], cntb[:], ALU.add), [RPF[5], Rcnt], [Rr])
                    DV(lambda e: e.tensor_tensor(cntb[:], PF[5][:, 32:64], cntb[:], ALU.add), [RPF[5], Rcnt, Rr], [Rcnt])
                    DV(lambda e: e.tensor_scalar(tmp32[:], posf[:], float(CAPl), 4.0e7, ALU.is_gt, ALU.mult), [Rr], [Rr])
                    DV(lambda e: e.tensor_tensor(posf[:], posf[:], tmp32[:], ALU.add), [Rr], [Rr])
                    DV(lambda e: e.scalar_tensor_tensor(out=posf[:], in0=io32[:], scalar=float(CAPl), in1=posf[:], op0=ALU.mult, op1=ALU.add),
                       [Rr], [Rr])
                    for k in range(2):
                        DV(lambda e: e.tensor_tensor(tmp32[:], oh[:, k, :], posf[:], ALU.mult), [Rr], [Rr])
                        DV(lambda e: e.tensor_reduce(dst[:, k:k + 1], tmp32[:], AX.X, ALU.add), [Rr], [Rr])
                    DV(lambda e: e.tensor_scalar(dst[:], dst[:], -1.0, float(NROW), ALU.add, ALU.min), [Rr], [Rr])
                    DV(lambda e: e.tensor_copy(off[:, tt, :], dst[:]), [Rr], [Roff])
                    for k in range(2):
                        fw.dma("pool", lambda e: e.indirect_dma_start(
                            out=Xs_d[0:NROW + 1, :], out_offset=bass.IndirectOffsetOnAxis(ap=off[:, tt, k:k + 1], axis=0),
                            in_=h2b[i][:], in_offset=None), [Rhb[i], Roff], [RXs])
                fw.barrier()
            if stop == 'D':
                fw.halt = True

            with ExitStack() as ph:
                w1b = [sb(ph, f"w1b{i}", [128, 8, 512], BF16) for i in range(2)]
                w3b = [sb(ph, f"w3b{i}", [128, 8, 512], BF16) for i in range(2)]
                w2b = [sb(ph, f"w2b{i}", [128, 4, D], BF16) for i in range(2)]
                Rwe = [Region(), Region()]
                xs = [sb(ph, f"xs{i}", [128, 4, D], BF16) for i in range(2)]; Rxs = [Region(), Region()]
                XT = sb(ph, "XT", [128, 8, 512], BF16); RXT = Region()
                s1 = [sb(ph, f"s1{i}", [128, 512]) for i in range(2)]; Rs1 = [Region(), Region()]
                GT = sb(ph, "GT", [128, 4, 512], BF16); RGT = Region()
                yrow = [sb(ph, f"yrow{i}", [128, D]) for i in range(2)]; Ryr = [Region(), Region()]
                PL(lambda e: e.memset(yrow[0][:], 0.0), [], [Ryr[0]])
                LD("sp", Ys_d[NROW:NROW + 1, :], yrow[0][0:1, :], [Ryr[0]], [RYs])
                groups = []
                s0 = 0
                while s0 < CAPl:
                    n = min(512, CAPl - s0); groups.append((s0, n)); s0 += n
                gi = 0; yi = 0
                for ex in range(32):
                    wi = ex % 2
                    fw.dma("pool", lambda e: e.dma_start(out=w1b[wi][:], in_=w1_d[l, ex].rearrange("(k p) n -> p k n", p=128)), [], [Rwe[wi]])
                    fw.dma("pool", lambda e: e.dma_start(out=w3b[wi][:], in_=w3_d[l, ex].rearrange("(k p) n -> p k n", p=128)), [], [Rwe[wi]])
                    fw.dma("pool", lambda e: e.dma_start(out=w2b[wi][:], in_=w2_d[l, ex].rearrange("(k p) n -> p k n", p=128)), [], [Rwe[wi]])
                    for (s0, n) in groups:
                        nt = n // 128
                        x_ = xs[gi % 2]; rx_ = Rxs[gi % 2]; gi += 1
                        r0 = ex * CAPl + s0
                        LD("sp", x_[:, 0:nt, :], Xs_d[r0:r0 + n, :].rearrange("(i p) d -> p i d", p=128), [RXs], [rx_])
                        for it_ in range(nt):
                            pbk = PB[it_ % 2]; rpb_ = RPB[it_ % 2]
                            for kc in range(8):
                                TR(pbk[:, kc * 128:(kc + 1) * 128], x_[:, it_, kc * 128:(kc + 1) * 128], identb[:], [rx_, Rc], [rpb_])
                            if it_ % 2 == 0:
                                ACT(XT[:, :, it_ * 128:(it_ + 1) * 128], pbk[:].rearrange("p (k t) -> p k t", k=8), AF.Copy, [rpb_], [RXT])
                            else:
                                DV(lambda e: e.tensor_copy(XT[:, :, it_ * 128:(it_ + 1) * 128], pbk[:].rearrange("p (k t) -> p k t", k=8)),
                                   [rpb_], [RXT])
                        for hc in range(4):
                            p1 = PF[hc % 2]; r1 = RPF[hc % 2]; p3 = PF[2 + hc % 2]; r3 = RPF[2 + hc % 2]
                            for kc in range(8):
                                MM(p1[:, 0:n], w1b[wi][:, kc, hc * 128:(hc + 1) * 128], XT[:, kc, 0:n], kc == 0, kc == 7, [Rwe[wi], RXT], [r1])
                            for kc in range(8):
                                MM(p3[:, 0:n], w3b[wi][:, kc, hc * 128:(hc + 1) * 128], XT[:, kc, 0:n], kc == 0, kc == 7, [Rwe[wi], RXT], [r3])
                            ACT(s1[hc % 2][:, 0:n], p1[:, 0:n], AF.Silu, [r1], [Rs1[hc % 2]])
                            DV(lambda e: e.tensor_tensor(GT[:, hc, 0:n], s1[hc % 2][:, 0:n], p3[:, 0:n], ALU.mult), [Rs1[hc % 2], r3], [RGT])
                        for it_ in range(nt):
                            yr = yrow[yi % 2]; ryr = Ryr[yi % 2]; yi += 1
                            for half in range(2):
                                py = PF[4 + half]; rpy = RPF[4 + half]
                                for hc in range(4):
                                    MM(py[:], GT[:, hc, it_ * 128:(it_ + 1) * 128], w2b[wi][:, hc, half * 512:(half + 1) * 512], hc == 0, hc == 3,
                                       [RGT, Rwe[wi]], [rpy])
                                if half == 0:
                                    ACT(yr[:, 0:512], py[:], AF.Copy, [rpy], [ryr])
                                else:
                                    DV(lambda e: e.tensor_copy(yr[:, 512:1024], py[:]), [rpy], [ryr])
                            LD("sp", Ys_d[r0 + it_ * 128:r0 + (it_ + 1) * 128, :], yr[:], [ryr], [RYs])
                fw.barrier()
            if stop == 'E':
                fw.halt = True

            with ExitStack() as ph:
                xt = [sb(ph, f"xt{i}", [128, D]) for i in range(2)]; Rx = [Region(), Region()]
                y0 = [sb(ph, f"y0{i}", [128, D]) for i in range(2)]; Ry0 = [Region(), Region()]
                y1 = [sb(ph, f"y1{i}", [128, D]) for i in range(2)]; Ry1 = [Region(), Region()]
                for tt in range(NTT):
                    i = tt % 2
                    LD("sp", xt[i][:], xr_d[tt * 128:(tt + 1) * 128, :], [], [Rx[i]])
                    for k, (y, ry) in enumerate(((y0[i], Ry0[i]), (y1[i], Ry1[i]))):
                        PL(lambda e: e.memset(y[:], 0.0), [], [ry])
                        fw.dma("pool", lambda e: e.indirect_dma_start(
                            out=y[:], out_offset=None, in_=Ys_d[0:NROW + 1, :],
                            in_offset=bass.IndirectOffsetOnAxis(ap=off[:, tt, k:k + 1], axis=0)), [RYs, Roff], [ry])
                    DV(lambda e: e.tensor_scalar(y0[i][:], y0[i][:], gts[:, tt, 0:1], None, ALU.mult), [Ry0[i], Rgts], [Ry0[i]])
                    DV(lambda e: e.scalar_tensor_tensor(out=y0[i][:], in0=y1[i][:], scalar=gts[:, tt, 1:2], in1=y0[i][:], op0=ALU.mult, op1=ALU.add),
                       [Ry1[i], Ry0[i], Rgts], [Ry0[i]])
                    PL(lambda e: e.tensor_tensor(y0[i][:], y0[i][:], G2, ALU.mult), [Ry0[i], Rmod], [Ry0[i]])
                    DV(lambda e: e.tensor_tensor(xt[i][:], xt[i][:], y0[i][:], ALU.add), [Rx[i], Ry0[i]], [Rx[i]])
                    LD("sp", xdst[tt * 128:(tt + 1) * 128, :], xt[i][:], [Rx[i]], [Rout])
                fw.barrier()
            if stop == 'F':
                fw.halt = True
        except StopBuild:
            pass
        fw.halt = False
        fw._wait("sp", Rout.w)
        fw.barrier()
    return nc


def run(inputs, NL=4, dbg=False, stop=None):
    sh, per = prep_inputs(inputs)
    key = (NL, dbg)
    nc = build(NL, LTOT=inputs["ada_w"].shape[0], dbg=dbg, stop=stop)
    in_maps = []
    for core in range(8):
        m = dict(sh)
        m.update(per[core % len(per)])
        in_maps.append(m)
    res = run_bass_kernel_spmd(nc, in_maps, core_ids=list(range(8)))
    return res


def kernel(**inputs):
    inputs = {k: np.asarray(v) for k, v in inputs.items()}
    res = run(inputs, NL=4)
    B = inputs["x"].shape[0]
    out = np.stack([np.asarray(res.results[b]["out"], dtype=np.float32).reshape(S, D) for b in range(B)], axis=0)
    return out
```

```python
import math
from contextlib import ExitStack
import numpy as np
import ml_dtypes
import concourse.bass as bass
import concourse.mybir as mybir
from concourse.bass_utils import run_bass_kernel_spmd

F32 = mybir.dt.float32
BF16 = mybir.dt.bfloat16
I32 = mybir.dt.int32
AF = mybir.ActivationFunctionType
ALU = mybir.AluOpType
AX = mybir.AxisListType

S = 4096
D = 1024
NTT = S // 128
NTB = S // 512
CAPS = [768, 896, 1280, 1408]
CAPMAX = max(CAPS)
NPP = 26
NEG = -30000.0
DMA_RING = 8
DEBUG_IND = False


class Region:
    __slots__ = ("name", "w", "r")

    def __init__(self, name=""):
        self.name = name
        self.w = None
        self.r = []


class FW:
    def __init__(self, nc, stack):
        self.nc = nc
        self.stack = stack
        self.eng = {"pe": nc.tensor, "act": nc.scalar, "dve": nc.vector,
                    "pool": nc.gpsimd, "sp": nc.sync}
        self.sems = {}
        self.cnt = {}
        self.waited = {e: {} for e in self.eng}
        for e in self.eng:
            self.sems["c_" + e] = stack.enter_context(nc.semaphore("c_" + e))
            self.cnt["c_" + e] = 0
        self.ring = {}
        for q in ("sp", "act", "pool"):
            for i in range(DMA_RING):
                k = f"d_{q}{i}"
                self.sems[k] = stack.enter_context(nc.semaphore(k))
                self.cnt[k] = 0
            self.ring[q] = 0
        self.n_inst = 0
        self.halt = False

    def _wait(self, e, ev):
        if ev is None or self.halt:
            return
        k, v = ev
        if self.waited[e].get(k, 0) >= v:
            return
        self.waited[e][k] = v
        self.eng[e].wait_ge(self.sems[k], v)

    def _deps(self, e, reads, writes, skip_same):
        own = "c_" + e
        for r in reads:
            if r.w is not None and not (skip_same and r.w[0] == own):
                self._wait(e, r.w)
        for r in writes:
            if r.w is not None and not (skip_same and r.w[0] == own):
                self._wait(e, r.w)
            for ev in r.r:
                if not (skip_same and ev[0] == own):
                    self._wait(e, ev)

    def _record(self, ev, reads, writes):
        for r in writes:
            r.w = ev
            r.r = []
        for r in reads:
            if r in writes:
                continue
            r.r = [x for x in r.r if x[0] != ev[0]] + [ev]

    def op(self, e, fn, reads=(), writes=(), skip_same=False):
        if self.halt:
            return
        self._deps(e, reads, writes, skip_same)
        k = "c_" + e
        self.cnt[k] += 1
        fn(self.eng[e]).then_inc(self.sems[k], 1)
        self._record((k, self.cnt[k]), reads, writes)
        self.n_inst += 1

    def dma(self, q, fn, reads=(), writes=()):
        if self.halt:
            return
        i = self.ring[q]
        self.ring[q] = (i + 1) % DMA_RING
        k = f"d_{q}{i}"
        if self.cnt[k] > 0:
            self._wait(q, (k, self.cnt[k]))
        self._deps(q, reads, writes, False)
        self.cnt[k] += 16
        fn(self.eng[q]).then_inc(self.sems[k], 16)
        self._record((k, self.cnt[k]), reads, writes)
        self.n_inst += 1

    def barrier(self):
        for e in self.eng:
            for k, v in self.cnt.items():
                if v > 0:
                    self._wait(e, (k, v))


def t5_bucket_np(dist):
    n = np.maximum(dist, 0)
    nf = np.maximum(n, 1).astype(np.float32)
    large = 16 + (np.log(nf / 16) / math.log(1024 / 16) * 16).astype(np.int32)
    large = np.minimum(large, 31)
    return np.where(n < 16, n, large)


def make_consts():
    c = {}
    p = np.arange(128)[:, None]
    j = np.arange(128)[None, :]
    c["ident"] = (p == j).astype(np.float32)
    su = (p < j).astype(np.float32)
    ui = (p <= j).astype(np.float32)
    sl = (p > j).astype(np.float32)
    c["m4"] = np.concatenate([su, ui, su, ui], 1)
    c["msl"] = sl
    c["mui"] = ui
    c["bdones"] = ((p // 64) == (j // 64)).astype(np.float32)
    cc = np.arange(14 * 128)[None, :]
    dist = (cc // 128 - 3) * 128 + (cc % 128) - p
    c["idxE"] = np.where(dist >= 0, t5_bucket_np(dist), -1).astype(np.float32)
    rm = np.ones((128, 512), np.float32)
    rm[:, ::128] = 0
    c["rmask"] = rm
    qt = np.arange(32)[:, None]
    n = np.arange(16)[None, :]
    past = (n < qt // 2).astype(np.float32)
    c["negpast"] = np.broadcast_to(((1 - past) * -1e30).reshape(1, 512), (128, 512)).copy().astype(np.float32)
    c["past30"] = np.broadcast_to((past * 30000.0).reshape(1, 512), (128, 512)).copy().astype(np.float32)
    lm = np.zeros((128, 7, 2, 2, 128), np.float32)
    tt_ = np.arange(128)[:, None]; ii_ = np.arange(128)[None, :]
    for k in range(7):
        b = 2 ** k
        M = (((tt_ // (2 * b)) == (ii_ // (2 * b))) & ((tt_ % (2 * b)) >= b) & ((ii_ % (2 * b)) < b)).astype(np.float32)
        lm[:, k, :, 0, :] = M[:, None, :]
        lm[:, k, :, 1, :] = M.T[:, None, :]
    c["lvlmask"] = lm.reshape(128, 7 * 512).astype(ml_dtypes.bfloat16)
    oh = (np.arange(S)[None, :] // 256 == np.arange(16)[:, None]).astype(np.float32)
    c["onehotk"] = oh.astype(ml_dtypes.bfloat16)
    return c


CONST_SHAPES = {"ident": ([128, 128], F32), "m4": ([128, 512], F32), "msl": ([128, 128], F32),
                "mui": ([128, 128], F32), "bdones": ([128, 128], F32), "idxE": ([128, 1792], F32),
                "rmask": ([128, 512], F32), "negpast": ([128, 512], F32), "past30": ([128, 512], F32),
                "onehotk": ([16, S], BF16), "lvlmask": ([128, 7 * 512], BF16)}


def prep_inputs(I):
    L = I["ada_w"].shape[0]
    sh = {}
    for k in ("ada_w", "ada_b", "w_in", "w_out", "moe_w1", "moe_w3", "moe_w2"):
        sh[k] = np.ascontiguousarray(I[k], dtype=np.float32)
    sh["n1g"] = np.ascontiguousarray(I["norm1_g"])
    sh["n2g"] = np.ascontiguousarray(I["norm2_g"])
    pp = np.zeros((128, L, NPP), np.float32)
    pidx = np.arange(128)
    for l in range(L):
        pp[:, l, 0:7] = I["rwkv_mu"][l].reshape(7, 128).T
        pp[:, l, 7:9] = I["rwkv_w0"][l].reshape(2, 128).T
        pp[:, l, 9:11] = I["rwkv_a0"][l].reshape(2, 128).T
        pp[:, l, 11:13] = I["rwkv_kk"][l].reshape(2, 128).T
        pp[:, l, 13:15] = I["rwkv_ka"][l].reshape(2, 128).T
        pp[:, l, 15:17] = I["rwkv_rk"][l].reshape(2, 128).T
        pp[:, l, 17:19] = I["rwkv_ln_g"][l].reshape(2, 128).T
        pp[:, l, 19:21] = I["rwkv_ln_b"][l].reshape(2, 128).T
        pp[:, l, 21] = I["diff_q_gain"][l][pidx % 64]
        pp[:, l, 22] = I["diff_k_gain"][l][pidx % 64]
        pp[:, l, 23] = I["moba_q_gain"][l][pidx % 64]
        pp[:, l, 24] = I["moba_k_gain"][l][pidx % 64]
        pp[:, l, 25] = I["diff_subln_g"][l]
    sh["ppar"] = pp.reshape(128, L * NPP)
    sh["lora"] = np.ascontiguousarray(np.concatenate([I["rwkv_w2"], I["rwkv_a2"], I["rwkv_g2"]], axis=1))
    sh["lam"] = np.ascontiguousarray(I["diff_lambda"].reshape(L, 256))
    sh["tbl"] = np.ascontiguousarray(I["rel_bias"].reshape(1, 256))
    sh["tbl2"] = np.ascontiguousarray(I["rel_bias"])
    sh["wr"] = np.ascontiguousarray(np.concatenate([I["router_g_w"], I["router_e_w"]], axis=2))
    sh["br"] = np.ascontiguousarray(np.concatenate([I["router_g_b"], I["router_e_b"]], axis=1))
    sh.update(make_consts())
    per = []
    for b in range(I["x"].shape[0]):
        per.append({"x": np.ascontiguousarray(I["x"][b]),
                    "c8": np.ascontiguousarray(I["c"][b].reshape(8, 128).T)})
    return sh, per


class StopBuild(Exception):
    pass


def build(NL, LTOT=4, dbg=False, stop=None):
    nc = bass.Bass("TRN2", target_bir_lowering=False)

    def din(name, shape, dt=F32):
        return nc.dram_tensor(name, list(shape), dt, kind="ExternalInput").ap()

    x_d = din("x", [S, D]); c8_d = din("c8", [128, 8])
    adaw_d = din("ada_w", [LTOT, D, 6 * D]); adab_d = din("ada_b", [LTOT, 6 * D])
    n1g_d = din("n1g", [LTOT, D]); n2g_d = din("n2g", [LTOT, D])
    win_d = din("w_in", [LTOT, D, 3200]); wout_d = din("w_out", [LTOT, D, D])
    ppar_d = din("ppar", [128, LTOT * NPP]); lora_d = din("lora", [LTOT, 128, 256])
    lam_d = din("lam", [LTOT, 256]); tbl_d = din("tbl", [1, 256]); tbl2_d = din("tbl2", [32, 8])
    wr_d = din("wr", [LTOT, D, 36]); br_d = din("br", [LTOT, 36])
    w1_d = din("moe_w1", [LTOT, 32, D, 512]); w3_d = din("moe_w3", [LTOT, 32, D, 512])
    w2_d = din("moe_w2", [LTOT, 32, 512, D])
    cd = {k: din(k, shp, dt) for k, (shp, dt) in CONST_SHAPES.items()}
    out_d = nc.dram_tensor("out", [S, D], F32, kind="ExternalOutput").ap()
    okind = "ExternalOutput" if dbg else "Internal"
    xr_d = nc.dram_tensor("xr", [S, D], F32, kind=okind).ap()
    hT_d = nc.dram_tensor("hT", [8, 128, S], BF16).ap()
    mixT_d = nc.dram_tensor("mixT", [8, 128, S], BF16, kind=okind).ap()
    Xs_d = nc.dram_tensor("Xs", [32 * CAPMAX + 128, D], BF16).ap()
    Ys_d = nc.dram_tensor("Ys", [32 * CAPMAX + 128, D], F32).ap()

    with ExitStack() as st:
        fw = FW(nc, st)

        uid = [0]

        def sb(stack, name, shape, dt=F32):
            uid[0] += 1
            return stack.enter_context(nc.sbuf_tensor(f"s{uid[0]}_{name}", list(shape), dt))

        pe_cfg = [None]

        def pe_sync(ap):
            cfg = (ap.base_partition(), ap.partition_size())
            if cfg != pe_cfg[0] and fw.cnt["c_pe"] > 0 and not fw.halt:
                fw._wait("pe", ("c_pe", fw.cnt["c_pe"]))
            pe_cfg[0] = cfg

        def MM(out, lhsT, rhs, start, stop, R, W):
            pe_sync(lhsT)
            fw.op("pe", lambda e: e.matmul(out, lhsT=lhsT, rhs=rhs, start=start, stop=stop), R, W, skip_same=True)

        def TR(out, in_, idn, R, W):
            pe_sync(in_)
            fw.op("pe", lambda e: e.transpose(out, in_, idn), R, W, skip_same=True)

        def ACT(out, in_, func, R, W, **kw):
            fw.op("act", lambda e: e.activation(out, in_, func, **kw), R, W)

        def DV(fn, R, W):
            fw.op("dve", fn, R, W)

        def PL(fn, R, W):
            fw.op("pool", fn, R, W)

        def LD(q, out, in_, R, W):
            fw.dma(q, lambda e: e.dma_start(out=out, in_=in_), R, W)

        PF = [st.enter_context(nc.psum_tensor(f"pf{i}", [128, 512], F32)) for i in range(6)]
        RPF = [Region(f"pf{i}") for i in range(6)]
        PB = [st.enter_context(nc.psum_tensor(f"pb{i}", [128, 1024], BF16)) for i in range(2)]
        RPB = [Region(f"pb{i}") for i in range(2)]

        Rc = Region("consts")
        ident = sb(st, "ident", [128, 128]); identb = sb(st, "identb", [128, 128], BF16)
        m4 = sb(st, "m4", [128, 512]); msl = sb(st, "msl", [128, 128]); mui = sb(st, "mui", [128, 128])
        bdones = sb(st, "bdones", [128, 128]); rmask = sb(st, "rmask", [128, 512])
        negpast = sb(st, "negpast", [128, 512]); past30 = sb(st, "past30", [128, 512])
        onesf = sb(st, "onesf", [128, 128]); onesb = sb(st, "onesb", [128, 128], BF16)
        lvlmask = sb(st, "lvlmask", [128, 7, 512], BF16)
        LD("sp", lvlmask[:].rearrange("p k c -> p (k c)"), cd["lvlmask"], [], [Rc])
        ppar = sb(st, "ppar", [128, LTOT * NPP]); tblb = sb(st, "tblb", [128, 256])
        cs8 = sb(st, "cs8", [128, 8]); csrep = sb(st, "csrep", [128, 8, 128])
        modb = sb(st, "modb", [128, 6 * D]); Rmod = Region("modb")
        erel = sb(st, "erel", [128, 8, 1792], BF16); Rerel = Region("erel")
        lamc = sb(st, "lamc", [128, 4]); Rlam = Region("lamc")
        off = sb(st, "off", [128, NTT, 2], I32); Roff = Region()
        gts = sb(st, "gts", [128, NTT, 2]); Rgts = Region()
        RXs = Region(); RYs = Region(); Rout = Region()

        for t, k in ((ident, "ident"), (m4, "m4"), (msl, "msl"), (mui, "mui"), (bdones, "bdones"),
                     (rmask, "rmask"), (negpast, "negpast"), (past30, "past30")):
            LD("sp", t[:], cd[k], [], [Rc])
        LD("sp", ppar[:], ppar_d, [], [Rc])
        LD("sp", tblb[:], tbl_d.partition_broadcast(128), [], [Rc])
        LD("sp", cs8[:], c8_d, [], [Rc])
        PL(lambda e: e.memset(onesf[:], 1.0), [], [Rc])
        PL(lambda e: e.memset(onesb[:], 1.0), [], [Rc])
        DV(lambda e: e.tensor_copy(identb[:], ident[:]), [Rc], [Rc])
        ACT(cs8[:], cs8[:], AF.Silu, [Rc], [Rc])
        for j in range(8):
            DV(lambda e: e.tensor_copy(csrep[:, j, :], cs8[:, j:j + 1].to_broadcast([128, 128])), [Rc], [Rc])

        def pcol(l, i):
            return ppar[:, l * NPP + i: l * NPP + i + 1]

        with ExitStack() as ph:
            idxE = sb(ph, "idxE", [128, 1792]); Ri = Region()
            acc = sb(ph, "eacc", [128, 1792]); Ra = Region()
            tmp = [sb(ph, f"etmp{i}", [128, 1792]) for i in range(2)]; Rt = [Region(), Region()]
            LD("sp", idxE[:], cd["idxE"], [], [Ri])
            it = 0
            for h in range(8):
                PL(lambda e: e.memset(acc[:], 0.0), [], [Ra])
                for b in range(32):
                    t = tmp[it % 2]; rt = Rt[it % 2]; it += 1
                    DV(lambda e: e.tensor_scalar(t[:], idxE[:], float(b), tblb[:, b * 8 + h: b * 8 + h + 1],
                                                 ALU.is_equal, ALU.mult), [Ri, Rc], [rt])
                    PL(lambda e: e.tensor_tensor(acc[:], acc[:], t[:], ALU.add), [rt, Ra], [Ra])
                DV(lambda e: e.tensor_scalar(acc[:], acc[:], tblb[:, 31 * 8 + h: 31 * 8 + h + 1], None, ALU.subtract),
                   [Ra, Rc], [Ra])
                ACT(acc[:], acc[:], AF.Exp, [Ra], [Ra])
                t = tmp[0]
                DV(lambda e: e.tensor_scalar(t[:], idxE[:], 0.0, None, ALU.is_ge), [Ri], [Rt[0]])
                DV(lambda e: e.tensor_tensor(erel[:, h, :], acc[:], t[:], ALU.mult), [Ra, Rt[0]], [Rerel])
            fw.barrier()

        try:
          for l in range(NL):
            lam_init = 0.8 - 0.6 * math.exp(-0.3 * l)
            xsrc = x_d if l == 0 else xr_d
            xdst = out_d if l == NL - 1 else xr_d

            with ExitStack() as ph:
                aw = [sb(ph, f"aw{i}", [128, 8, 512]) for i in range(2)]; Raw = [Region(), Region()]
                ab = [sb(ph, f"ab{i}", [128, 512]) for i in range(2)]; Rab = [Region(), Region()]
                gb = sb(ph, "gb", [128, 2, D]); Rgb = Region()
                lamt = sb(ph, "lamt", [128, 256]); Rlt = Region()
                LD("act", gb[:, 0, :], n1g_d[l:l + 1, :].partition_broadcast(128), [], [Rgb])
                LD("act", gb[:, 1, :], n2g_d[l:l + 1, :].partition_broadcast(128), [], [Rgb])
                LD("act", lamt[:], lam_d[l:l + 1, :].partition_broadcast(128), [], [Rlt])
                for nb in range(12):
                    a = aw[nb % 2]; ra = Raw[nb % 2]; b_ = ab[nb % 2]; rb = Rab[nb % 2]
                    LD("sp", a[:], adaw_d[l].rearrange("(k p) n -> p k n", p=128)[:, :, nb * 512:(nb + 1) * 512], [], [ra])
                    LD("act", b_[:], adab_d[l:l + 1, nb * 512:(nb + 1) * 512].partition_broadcast(128), [], [rb])
                    pf = PF[nb % 2]; rp = RPF[nb % 2]
                    for kc in range(8):
                        MM(pf[:], csrep[:, kc, :], a[:, kc, :], kc == 0, kc == 7, [Rc, ra], [rp])
                    DV(lambda e: e.tensor_tensor(modb[:, nb * 512:(nb + 1) * 512], pf[:], b_[:], ALU.add), [rp, rb], [Rmod])
                for (o, gi) in ((1, 0), (4, 1)):
                    DV(lambda e: e.scalar_tensor_tensor(out=modb[:, o * D:(o + 1) * D], in0=modb[:, o * D:(o + 1) * D],
                                                        scalar=1.0, in1=gb[:, gi, :], op0=ALU.add, op1=ALU.mult),
                       [Rmod, Rgb], [Rmod])
                prod = sb(ph, "lprod", [128, 128]); Rpr = Region()
                DV(lambda e: e.tensor_tensor(prod[:].rearrange("p (a d) -> p a d", a=2),
                                             lamt[:].rearrange("p (a b d) -> p a b d", a=2, b=2)[:, :, 0, :],
                                             lamt[:].rearrange("p (a b d) -> p a b d", a=2, b=2)[:, :, 1, :], ALU.mult),
                   [Rlt], [Rpr])
                DV(lambda e: e.tensor_reduce(lamc[:, 1:3], prod[:].rearrange("p (a d) -> p a d", a=2), AX.X, ALU.add),
                   [Rpr], [Rlam])
                ACT(lamc[:, 1:3], lamc[:, 1:3], AF.Exp, [Rlam], [Rlam])
                DV(lambda e: e.tensor_tensor(lamc[:, 0:1], lamc[:, 2:3], lamc[:, 1:2], ALU.subtract), [Rlam], [Rlam])
                DV(lambda e: e.tensor_scalar(lamc[:, 0:1], lamc[:, 0:1], -lam_init, None, ALU.add), [Rlam], [Rlam])
                DV(lambda e: e.tensor_scalar(lamc[:, 3:4], pcol(l, 25), 1.0 - lam_init, None, ALU.mult), [Rc, Rlam], [Rlam])
                fw.barrier()
            if stop == '0':
                fw.halt = True
            SH1, A1, G1 = modb[:, 0:D], modb[:, D:2 * D], modb[:, 2 * D:3 * D]
            SH2, A2, G2 = modb[:, 3 * D:4 * D], modb[:, 4 * D:5 * D], modb[:, 5 * D:6 * D]

            def norm_mod(ph, xt, rx, Ax, Bx, hf, rhf, small, rsm):
                PL(lambda e: e.memset(small[:, 0:1], 0.0), [], [rsm])
                ACT(hf[:], xt[:], AF.Square, [rx, rsm], [rhf, rsm], accum_out=small[:, 0:1])
                DV(lambda e: e.tensor_scalar(small[:, 1:2], small[:, 0:1], 1.0 / D, 1e-6, ALU.mult, ALU.add), [rsm], [rsm])
                ACT(small[:, 1:2], small[:, 1:2], AF.Sqrt, [rsm], [rsm])
                DV(lambda e: e.reciprocal(small[:, 2:3], small[:, 1:2]), [rsm], [rsm])
                DV(lambda e: e.scalar_tensor_tensor(out=hf[:], in0=xt[:], scalar=small[:, 2:3], in1=Ax,
                                                    op0=ALU.mult, op1=ALU.mult), [rx, rsm, Rmod], [rhf])
                PL(lambda e: e.tensor_tensor(hf[:], hf[:], Bx, ALU.add), [rhf, Rmod], [rhf])

            with ExitStack() as ph:
                xt = [sb(ph, f"xt{i}", [128, D]) for i in range(2)]; Rx = [Region(), Region()]
                hf = [sb(ph, f"hf{i}", [128, D]) for i in range(2)]; Rhf = [Region(), Region()]
                hb = [sb(ph, f"hb{i}", [128, D], BF16) for i in range(2)]; Rhb = [Region(), Region()]
                sm = [sb(ph, f"sm{i}", [128, 4]) for i in range(2)]; Rsm = [Region(), Region()]
                hblk = [sb(ph, f"hblk{i}", [128, 8, 512], BF16) for i in range(2)]; Rhk = [Region(), Region()]
                for tt in range(NTT):
                    i = tt % 2
                    LD("sp", xt[i][:], xsrc[tt * 128:(tt + 1) * 128, :], [], [Rx[i]])
                    norm_mod(ph, xt[i], Rx[i], A1, SH1, hf[i], Rhf[i], sm[i], Rsm[i])
                    DV(lambda e: e.tensor_copy(hb[i][:], hf[i][:]), [Rhf[i]], [Rhb[i]])
                    for kc in range(8):
                        TR(PB[i][:, kc * 128:(kc + 1) * 128], hb[i][:, kc * 128:(kc + 1) * 128], identb[:], [Rhb[i], Rc], [RPB[i]])
                    tb = tt // 4; j = tt % 4; bi = tb % 2
                    ACT(hblk[bi][:, :, j * 128:(j + 1) * 128], PB[i][:].rearrange("p (k t) -> p k t", k=8), AF.Copy,
                        [RPB[i]], [Rhk[bi]])
                    if j == 3:
                        LD("sp", hT_d[:, :, tb * 512:(tb + 1) * 512].rearrange("k p t -> p k t"), hblk[bi][:], [Rhk[bi]], [])
                fw.barrier()
            if stop == 'A':
                fw.halt = True

            with ExitStack() as ph:
                wr_ = sb(ph, "w_r", [128, 8, 896], BF16); Rw = Region()
                fw.dma("pool", lambda e: e.dma_start(out=wr_[:], in_=win_d[l].rearrange("(k p) n -> p k n", p=128)[:, :, 0:896]), [], [Rw])
                lora = sb(ph, "lora", [128, 256]); Rlo = Region()
                LD("sp", lora[:], lora_d[l], [], [Rlo])
                hblk = [sb(ph, "hblk0", [128, 8, 512], BF16)] * 2; Rhk = [Region()] * 2
                P7 = sb(ph, "P7", [128, 7, 513]); RP7 = Region()
                D7 = sb(ph, "D7", [128, 7, 512]); RD7 = Region()
                PS7 = sb(ph, "PS7", [128, 7, 512]); RPS7 = Region()
                NT = 14
                T = [sb(ph, f"rt{i}", [128, 512]) for i in range(NT)]; RT = [Region() for _ in range(NT)]
                ARt = sb(ph, "ARt", [128, 4, 256], BF16); RAR = Region()
                ARm = [sb(ph, f"ARm{i}", [128, 4, 256], BF16) for i in range(2)]; RARm = [Region(), Region()]
                for j in range(2):
                    PL(lambda e: e.memset(ARm[j][:], 0.0), [], [RARm[j]])
                Bt = sb(ph, "Bt", [128, 512], BF16); RBt = Region()
                Kt = sb(ph, "Kt", [128, 512], BF16); RKt = Region()
                BHf = sb(ph, "BHf", [128, 512], BF16); KHf = sb(ph, "KHf", [128, 512], BF16); Vf = sb(ph, "Vf", [128, 512], BF16)
                RBH = Region(); RKH = Region(); RVf = Region()
                BHtm = sb(ph, "BHtm", [128, 4, 128], BF16); KHtm = sb(ph, "KHtm", [128, 4, 128], BF16)
                Vtm = sb(ph, "Vtm", [128, 4, 128], BF16); Rtm = Region()
                SB1s = [sb(ph, f"SB1s{i}", [128, 2, 512], BF16) for i in range(2)]; RSB1s = [Region(), Region()]
                SBAs = [sb(ph, f"SBAs{i}", [128, 2, 128], BF16) for i in range(2)]; RSBAs = [Region(), Region()]
                DEs = [[sb(ph, f"DE{s_}{i}", [128, 2, 2, 128], BF16) for i in range(2)] for s_ in range(2)]
                RDEs = [[Region(), Region()] for s_ in range(2)]
                ZZs = [sb(ph, f"ZZ{i}", [128, 2, 2, 128], BF16) for i in range(2)]; RZZs = [Region(), Region()]
                Gms = [sb(ph, f"Gm{i}", [128, 512]) for i in range(2)]; RGms = [Region(), Region()]
                ST = [sb(ph, f"ST{i}", [128, 64]) for i in range(2)]; RST = [Region(), Region()]
                STb = [sb(ph, f"STb{i}", [128, 64], BF16) for i in range(2)]; RSTb = [Region(), Region()]
                RHSb = sb(ph, "RHSb", [128, 128], BF16); RRH = Region()
                Ub = sb(ph, "Ub", [128, 128], BF16); RUb = Region()
                Ytm = sb(ph, "Ytm", [128, 4, 128]); RY = Region()
                gn = sb(ph, "gn", [128, 8, 4]); Rgn = Region()
                GC = sb(ph, "GC", [128, 4]); RGC = Region()
                mixo = sb(ph, "mixo", [128, 512], BF16); Rmx = Region()
                for hp in range(2):
                    PL(lambda e: e.memset(ST[hp][:], 0.0), [], [RST[hp]])
                    PL(lambda e: e.memset(STb[hp][:], 0.0), [], [RSTb[hp]])
                PL(lambda e: e.memset(P7[:, :, 0:1], 0.0), [], [RP7])

                for tb in range(NTB):
                    hb_ = hblk[tb % 2]; rh = Rhk[tb % 2]
                    LD("sp", hb_[:], hT_d[:, :, tb * 512:(tb + 1) * 512].rearrange("k p t -> p k t"), [], [rh])
                    if tb > 0:
                        DV(lambda e: e.tensor_copy(P7[:, :, 0:1], P7[:, :, 512:513]), [RP7], [RP7])
                    for cc in range(7):
                        pf = PF[cc % 2]; rp = RPF[cc % 2]
                        for kc in range(8):
                            MM(pf[:], wr_[:, kc, cc * 128:(cc + 1) * 128], hb_[:, kc, :], kc == 0, kc == 7, [Rw, rh], [rp])
                        ACT(P7[:, cc, 1:513], pf[:], AF.Copy, [rp], [RP7])
                    DV(lambda e: e.tensor_tensor(D7[:], P7[:, :, 0:512], P7[:, :, 1:513], ALU.subtract), [RP7], [RD7])
                    for cc in range(7):
                        DV(lambda e: e.scalar_tensor_tensor(out=PS7[:, cc, :], in0=D7[:, cc, :], scalar=pcol(l, cc),
                                                            in1=P7[:, cc, 1:513], op0=ALU.mult, op1=ALU.add),
                           [RD7, RP7, Rc], [RPS7])
                    ACT(PS7[0:32, 6, :], PS7[0:32, 6, :], AF.Tanh, [RPS7], [RPS7])
                    ACT(PS7[64:128, 6, :], PS7[64:128, 6, :], AF.Sigmoid, [RPS7], [RPS7])
                    if stop == 'B1':
                        fw.halt = True
                    for hp in range(2):
                        rs, ks, vs = PS7[:, hp, :], PS7[:, 2 + hp, :], PS7[:, 4 + hp, :]
                        cs_ = slice(hp * 128, (hp + 1) * 128)
                        sg, av, gv, kk, sq, kkn, k2, lw, cum, e1, e2, e3, e4, bon = T
                        Rsg, Rav, Rgv, Rkk, Rsq, Rkkn, Rk2, Rlw, Rcum, Re1, Re2, Re3, Re4, Rbon = RT
                        MM(PF[2][:], lora[0:32, cs_], PS7[0:32, 6, :], True, True, [Rlo, RPS7], [RPF[2]])
                        ACT(sg[:], PF[2][:], AF.Sigmoid, [RPF[2], Rc], [Rsg], bias=pcol(l, 7 + hp))
                        MM(PF[3][:], lora[32:64, cs_], PS7[32:64, 6, :], True, True, [Rlo, RPS7], [RPF[3]])
                        ACT(av[:], PF[3][:], AF.Sigmoid, [RPF[3], Rc], [Rav], bias=pcol(l, 9 + hp))
                        MM(PF[2][:], lora[64:128, cs_], PS7[64:128, 6, :], True, True, [Rlo, RPS7], [RPF[2]])
                        ACT(gv[:], PF[2][:], AF.Copy, [RPF[2]], [Rgv])
                        DV(lambda e: e.tensor_scalar(kk[:], ks, pcol(l, 11 + hp), None, ALU.mult), [RPS7, Rc], [Rkk])
                        PL(lambda e: e.tensor_tensor(sq[:], kk[:], kk[:], ALU.mult), [Rkk], [Rsq])
                        MM(PF[3][:], bdones[:], sq[:], True, True, [Rc, Rsq], [RPF[3]])
                        ACT(sq[:], PF[3][:], AF.Sqrt, [RPF[3]], [Rsq])
                        DV(lambda e: e.tensor_scalar(sq[:], sq[:], 1e-12, None, ALU.max), [Rsq], [Rsq])
                        DV(lambda e: e.reciprocal(sq[:], sq[:]), [Rsq], [Rsq])
                        DV(lambda e: e.tensor_tensor(kkn[:], kk[:], sq[:], ALU.mult), [Rkk, Rsq], [Rkkn])
                        DV(lambda e: e.tensor_scalar(k2[:], av[:], 1.0, pcol(l, 13 + hp), ALU.subtract, ALU.mult), [Rav, Rc], [Rk2])
                        DV(lambda e: e.scalar_tensor_tensor(out=k2[:], in0=k2[:], scalar=1.0, in1=ks, op0=ALU.add, op1=ALU.mult),
                           [Rk2, RPS7], [Rk2])
                        PL(lambda e: e.tensor_tensor(kk[:], rs, k2[:], ALU.mult), [RPS7, Rk2, Rkkn], [Rkk])
                        DV(lambda e: e.tensor_scalar(kk[:], kk[:], pcol(l, 15 + hp), None, ALU.mult), [Rkk, Rc], [Rkk])
                        MM(PF[2][:], bdones[:], kk[:], True, True, [Rc, Rkk], [RPF[2]])
                        DV(lambda e: e.tensor_tensor(bon[:], PF[2][:], vs, ALU.mult), [RPF[2], RPS7], [Rbon])
                        DV(lambda e: e.tensor_scalar(lw[:], sg[:], -0.6065306597126334, None, ALU.mult), [Rsg], [Rlw])
                        DV(lambda e: e.tensor_tensor_scan(cum[:], rmask[:], lw[:], 0.0, ALU.mult, ALU.add), [Rc, Rlw], [Rcum])
                        cum3 = cum[:].rearrange("p (c t) -> p c t", t=128)
                        ACT(e1[:], cum[:], AF.Exp, [Rcum], [Re1])
                        ACT(e2[:], cum[:], AF.Exp, [Rcum], [Re2], scale=-1.0)
                        DV(lambda e: e.tensor_tensor(e3[:], cum[:], lw[:], ALU.subtract), [Rcum, Rlw], [Re3])
                        ACT(e3[:], e3[:], AF.Exp, [Re3], [Re3])
                        DV(lambda e: e.tensor_tensor(e4[:].rearrange("p (c t) -> p c t", t=128),
                                                     cum3[:, :, 127:128].to_broadcast([128, 4, 128]), cum3, ALU.subtract),
                           [Rcum], [Re4])
                        ACT(e4[:], e4[:], AF.Exp, [Re4], [Re4])
                        ACT(GC[:].rearrange("p (c o) -> p c o", o=1), cum3[:, :, 127:128], AF.Exp, [Rcum], [RGC])
                        AR3 = ARt[:]
                        DV(lambda e: e.scalar_tensor_tensor(out=AR3[:, :, 0:128], in0=kkn[:].rearrange("p (c t) -> p c t", t=128),
                                                            scalar=-1.0, in1=e3[:].rearrange("p (c t) -> p c t", t=128),
                                                            op0=ALU.mult, op1=ALU.mult), [Rkkn, Re3], [RAR])
                        PL(lambda e: e.tensor_tensor(AR3[:, :, 128:256], PS7[:, hp, :].rearrange("p (c t) -> p c t", t=128),
                                                     e1[:].rearrange("p (c t) -> p c t", t=128), ALU.mult), [RPS7, Re1], [RAR])
                        ACT(ARm[0][0:64, :, :], ARt[0:64, :, :], AF.Copy, [RAR], [RARm[0]])
                        PL(lambda e: e.tensor_copy(ARm[1][64:128, :, :], ARt[64:128, :, :]), [RAR], [RARm[1]])
                        DV(lambda e: e.tensor_tensor(kkn[:], kkn[:], av[:], ALU.mult), [Rkkn, Rav, RAR], [Rkkn])
                        DV(lambda e: e.tensor_tensor(Bt[:], kkn[:], e2[:], ALU.mult), [Rkkn, Re2], [RBt])
                        PL(lambda e: e.tensor_tensor(BHf[:], kkn[:], e4[:], ALU.mult), [Rkkn, Re4], [RBH])
                        DV(lambda e: e.tensor_tensor(Kt[:], k2[:], e2[:], ALU.mult), [Rk2, Re2], [RKt])
                        PL(lambda e: e.tensor_tensor(KHf[:], k2[:], e4[:], ALU.mult), [Rk2, Re4], [RKH])
                        ACT(Vf[:], vs, AF.Copy, [RPS7], [RVf])
                        for c in range(4):
                            TR(PB[0][:, c * 128:(c + 1) * 128], BHf[:, c * 128:(c + 1) * 128], identb[:], [RBH, Rc], [RPB[0]])
                            TR(PB[0][:, 512 + c * 128:512 + (c + 1) * 128], KHf[:, c * 128:(c + 1) * 128], identb[:], [RKH, Rc], [RPB[0]])
                            TR(PB[1][:, c * 128:(c + 1) * 128], Vf[:, c * 128:(c + 1) * 128], identb[:], [RVf, Rc], [RPB[1]])
                        ACT(BHtm[:], PB[0][:, 0:512].rearrange("p (c t) -> p c t", t=128), AF.Copy, [RPB[0]], [Rtm])
                        DV(lambda e: e.tensor_copy(KHtm[:], PB[0][:, 512:1024].rearrange("p (c t) -> p c t", t=128)), [RPB[0]], [Rtm])
                        ACT(Vtm[:], PB[1][:, 0:512].rearrange("p (c t) -> p c t", t=128), AF.Copy, [RPB[1]], [Rtm])
                        if stop == 'B2':
                            fw.halt = True
                        for c0 in (0, 2):
                            for s_ in range(2):
                                c = c0 + s_
                                cl = slice(c * 128, (c + 1) * 128)
                                b0 = 0
                                pa = PF[4]; rpa = RPF[4]
                                for j in range(2):
                                    pf = PF[b0 + j]; rpf_ = RPF[b0 + j]
                                    MM(pf[:, 0:256], Bt[:, cl], ARm[j][:, c, :], True, True, [RBt, RARm[j]], [rpf_])
                                    MM(pf[:, 256:512], Kt[:, cl], ARm[j][:, c, :], True, True, [RKt, RARm[j]], [rpf_])
                                    MM(pa[:, j * 128:(j + 1) * 128], ARm[j][:, c, 0:128], Bt[:, cl], True, True, [RARm[j], RBt], [rpa])
                                for j in range(2):
                                    DV(lambda e: e.tensor_tensor(SB1s[s_][:, j, :], PF[b0 + j][:], m4[:], ALU.mult), [RPF[b0 + j], Rc], [RSB1s[s_]])
                                for j in range(2):
                                    PL(lambda e: e.tensor_copy(DEs[s_][0][:, j, 0, :], identb[:]), [Rc], [RDEs[s_][0]])
                                    PL(lambda e: e.tensor_copy(DEs[s_][0][:, j, 1, :], identb[:]), [Rc], [RDEs[s_][0]])
                                    DV(lambda e: e.tensor_tensor(SBAs[s_][:, j, :], pa[:, j * 128:(j + 1) * 128], msl[:], ALU.mult),
                                       [rpa, Rc], [RSBAs[s_]])
                            wi = 0
                            for k in range(7):
                                for s_ in range(2):
                                    pz = PF[4 * s_]; rpz = RPF[4 * s_]
                                    for j in range(2):
                                        MM(pz[:, j * 256:j * 256 + 128], SB1s[s_][:, j, 0:128], DEs[s_][wi][:, j, 0, :], True, True,
                                           [RSB1s[s_], RDEs[s_][wi]], [rpz])
                                        MM(pz[:, j * 256 + 128:j * 256 + 256], SBAs[s_][:, j, :], DEs[s_][wi][:, j, 1, :], True, True,
                                           [RSBAs[s_], RDEs[s_][wi]], [rpz])
                                for s_ in range(2):
                                    ACT(ZZs[s_][:].rearrange("p j z t -> p (j z t)"), PF[4 * s_][:], AF.Copy, [RPF[4 * s_]], [RZZs[s_]])
                                for s_ in range(2):
                                    pg = PF[4 * s_ + 1]; rpg = RPF[4 * s_ + 1]
                                    for j in range(2):
                                        MM(pg[:, j * 256:j * 256 + 128], DEs[s_][wi][:, j, 1, :], ZZs[s_][:, j, 0, :], True, True,
                                           [RDEs[s_][wi], RZZs[s_]], [rpg])
                                        MM(pg[:, j * 256 + 128:j * 256 + 256], DEs[s_][wi][:, j, 0, :], ZZs[s_][:, j, 1, :], True, True,
                                           [RDEs[s_][wi], RZZs[s_]], [rpg])
                                for s_ in range(2):
                                    DV(lambda e: e.tensor_tensor(Gms[s_][:], PF[4 * s_ + 1][:], lvlmask[:, k, :], ALU.mult),
                                       [RPF[4 * s_ + 1], Rc], [RGms[s_]])
                                for s_ in range(2):
                                    PL(lambda e: e.tensor_tensor(DEs[s_][1 - wi][:].rearrange("p j z t -> p (j z t)"), Gms[s_][:],
                                                                 DEs[s_][wi][:].rearrange("p j z t -> p (j z t)"), ALU.add),
                                       [RGms[s_], RDEs[s_][wi]], [RDEs[s_][1 - wi]])
                                wi = 1 - wi
                            for s_ in range(2):
                                c = c0 + s_
                                SB1 = SB1s[s_]; RSB1 = RSB1s[s_]
                                W_ = DEs[s_][wi]; RW_ = RDEs[s_][wi]
                                pr = PF[5]
                                for j in range(2):
                                    vj = slice(j * 64, (j + 1) * 64)
                                    MM(pr[:, vj], ARm[j][:, c, 0:128], STb[hp][:, :], True, False, [RARm[j], RSTb[hp]], [RPF[5]])
                                    MM(pr[:, vj], SB1[:, j, 256:384], Vtm[:, c, vj], False, True, [RSB1, Rtm], [RPF[5]])
                                ACT(RHSb[:], pr[:, 0:128], AF.Copy, [RPF[5]], [RRH])
                                pu = PF[4]
                                for j in range(2):
                                    vj = slice(j * 64, (j + 1) * 64)
                                    MM(pu[:, 256 + j * 64:256 + (j + 1) * 64], W_[:, j, 1, :], RHSb[:, vj], True, True, [RW_, RRH], [RPF[4]])
                                DV(lambda e: e.tensor_copy(Ub[:], pu[:, 256:384]), [RPF[4]], [RUb])
                                py = PF[5]
                                for j in range(2):
                                    vj = slice(j * 64, (j + 1) * 64)
                                    yo = py[:, 128 + j * 64:128 + (j + 1) * 64]
                                    MM(yo, ARm[j][:, c, 128:256], STb[hp][:, :], True, False, [RARm[j], RSTb[hp]], [RPF[5]])
                                    MM(yo, SB1[:, j, 128:256], Ub[:, vj], False, False, [RSB1, RUb], [RPF[5]])
                                    MM(yo, SB1[:, j, 384:512], Vtm[:, c, vj], False, True, [RSB1, Rtm], [RPF[5]])
                                ACT(Ytm[:, c, :], py[:, 128:256], AF.Copy, [RPF[5]], [RY])
                                pss = PF[5]
                                for j in range(2):
                                    R_ = slice(j * 64, (j + 1) * 64); vj = slice(j * 64, (j + 1) * 64)
                                    so = pss[R_, 256:320]
                                    MM(so, BHtm[:, c, R_], Ub[:, vj], True, False, [Rtm, RUb], [RPF[5]])
                                    MM(so, KHtm[:, c, R_], Vtm[:, c, vj], False, True, [Rtm], [RPF[5]])
                                DV(lambda e: e.scalar_tensor_tensor(out=ST[hp][:], in0=ST[hp][:], scalar=GC[:, c:c + 1], in1=pss[:, 256:320],
                                                                    op0=ALU.mult, op1=ALU.add), [RST[hp], RGC, RPF[5]], [RST[hp]])
                                ACT(STb[hp][:], ST[hp][:], AF.Copy, [RST[hp]], [RSTb[hp]])
                        Y8 = Ytm[:].rearrange("p c (j v) -> p (c j) v", j=2)
                        DV(lambda e: e.tensor_reduce(gn[:, :, 0], Y8, AX.X, ALU.add), [RY], [Rgn])
                        DV(lambda e: e.tensor_scalar(gn[:, :, 0], gn[:, :, 0], 1.0 / 64, None, ALU.mult), [Rgn], [Rgn])
                        DV(lambda e: e.tensor_tensor(Y8, Y8, gn[:, :, 0:1].to_broadcast([128, 8, 64]), ALU.subtract), [RY, Rgn], [RY])
                        Ysq = sq[:].rearrange("p (a v) -> p a v", v=64)
                        PL(lambda e: e.tensor_tensor(Ysq, Y8, Y8, ALU.mult), [RY, Rsq], [Rsq])
                        DV(lambda e: e.tensor_reduce(gn[:, :, 1], Ysq, AX.X, ALU.add), [Rsq], [Rgn])
                        DV(lambda e: e.tensor_scalar(gn[:, :, 1], gn[:, :, 1], 1.0 / 64, 64e-5, ALU.mult, ALU.add), [Rgn], [Rgn])
                        ACT(gn[:, :, 1], gn[:, :, 1], AF.Sqrt, [Rgn], [Rgn])
                        DV(lambda e: e.reciprocal(gn[:, :, 2], gn[:, :, 1]), [Rgn], [Rgn])
                        DV(lambda e: e.tensor_tensor(Y8, Y8, gn[:, :, 2:3].to_broadcast([128, 8, 64]), ALU.mult), [RY, Rgn], [RY])
                        if stop == 'B5':
                            fw.halt = True
                        for c in range(4):
                            TR(PF[3][:, c * 128:(c + 1) * 128], Ytm[:, c, :], ident[:], [RY, Rc], [RPF[3]])
                        DV(lambda e: e.tensor_scalar(e1[:], PF[3][:], pcol(l, 17 + hp), pcol(l, 19 + hp), ALU.mult, ALU.add),
                           [RPF[3], Rc, RAR], [Re1])
                        DV(lambda e: e.tensor_tensor(e1[:], e1[:], bon[:], ALU.add), [Re1, Rbon], [Re1])
                        DV(lambda e: e.tensor_tensor(mixo[:], e1[:], gv[:], ALU.mult), [Re1, Rgv], [Rmx])
                        LD("sp", mixT_d[hp, :, tb * 512:(tb + 1) * 512], mixo[:], [Rmx], [])
                        if stop == 'B6':
                            fw.halt = True
                        if stop == 'B7' and hp == 1:
                            fw.halt = True
                        if stop == 'B8' and hp == 1 and tb == 1:
                            fw.halt = True
                fw.barrier()
            if stop == 'B':
                fw.halt = True

            with ExitStack() as ph:
                hblk = [sb(ph, f"hblk{i}", [128, 8, 512], BF16) for i in range(2)]; Rhk = [Region(), Region()]
                wa = [sb(ph, f"wa{i}", [128, 8, 384], BF16) for i in range(2)]; Rwa = [Region(), Region()]
                QT = sb(ph, "QT", [128, S], BF16); KT = sb(ph, "KT", [128, S], BF16); RQ = Region(); RK = Region()
                QT1 = sb(ph, "QT1", [128, S], BF16)
                PL(lambda e: e.memset(QT[:], 0.0), [], [RQ])
                PL(lambda e: e.memset(QT1[:], 0.0), [], [RQ])
                Vt = sb(ph, "Vt", [128, NTT, 128], BF16); RV = Region()
                qf = sb(ph, "qf", [128, 512]); Rqf = Region()
                sqf = sb(ph, "sqf", [128, 512]); Rsqf = Region()
                rsf = sb(ph, "rsf", [128, 512]); Rrsf = Region()
                Pf = [sb(ph, f"Pf{i}", [128, 512]) for i in range(2)]; RPf = [Region(), Region()]
                Pb = [sb(ph, f"Pb{i}", [128, 512], BF16) for i in range(3)]; RPb = [Region() for _ in range(3)]
                rec = sb(ph, "rec", [128, 2, 512]); Rrec = Region()
                bcs = sb(ph, "bcs", [128, 2, 512]); Rbcs = Region()
                Of = sb(ph, "Of", [128, 512]); ROf = Region()
                Ob = sb(ph, "Ob", [128, 512], BF16); ROb = Region()
                kmT = sb(ph, "kmT", [128, 16]); Rkm = Region()
                gm = sb(ph, "gm", [128, 16]); top8 = sb(ph, "top8", [128, 8]); Rgm = Region()
                nmw = sb(ph, "nmw", [128, 4, 80]); Rnm = Region()
                PL(lambda e: e.memset(nmw[:], 0.0), [], [Rnm])
                pbi = 0

                def proj_fm(hb_, rh, w, rw, c0, M, pf, rp):
                    for kc in range(8):
                        MM(pf[0:M, :], w[:, kc, c0:c0 + M], hb_[:, kc, :], kc == 0, kc == 7, [rw, rh], [rp])

                def headnorm(pf, rp, M, gcol, dst, rdst, dst2=None):
                    ACT(sqf[0:M, :], pf[0:M, :], AF.Square, [rp], [Rsqf])
                    MM(PF[2][0:M, :], bdones[0:M, 0:M], sqf[0:M, :], True, True, [Rc, Rsqf], [RPF[2]])
                    DV(lambda e: e.tensor_scalar(rsf[0:M, :], PF[2][0:M, :], 1.0 / 64, 1e-6, ALU.mult, ALU.add), [RPF[2]], [Rrsf])
                    ACT(rsf[0:M, :], rsf[0:M, :], AF.Sqrt, [Rrsf], [Rrsf])
                    DV(lambda e: e.reciprocal(rsf[0:M, :], rsf[0:M, :]), [Rrsf], [Rrsf])
                    DV(lambda e: e.scalar_tensor_tensor(out=qf[0:M, :], in0=pf[0:M, :], scalar=gcol[0:M, :], in1=rsf[0:M, :],
                                                        op0=ALU.mult, op1=ALU.mult), [rp, Rc, Rrsf], [Rqf])
                    if dst2 is None:
                        ACT(dst, qf[0:M, :], AF.Copy, [Rqf], [rdst])
                    else:
                        ACT(dst, qf[0:64, :], AF.Copy, [Rqf], [rdst])
                        PL(lambda e: e.tensor_copy(dst2, qf[64:128, :]), [Rqf], [rdst])

                for hd in range(8):
                    moba = hd >= 4
                    h = hd % 4
                    w = wa[hd % 2]; rw = Rwa[hd % 2]
                    win3 = win_d[l].rearrange("(k p) n -> p k n", p=128)
                    if not moba:
                        cols = [(896 + h * 128, 128), (896 + 512 + h * 128, 128), (896 + 1024 + h * 128, 128)]
                    else:
                        cols = [(2432 + h * 64, 64), (2432 + 256 + h * 64, 64), (2432 + 512 + h * 64, 64)]
                    for i, (c0, n) in enumerate(cols):
                        fw.dma("pool", lambda e: e.dma_start(out=w[:, :, i * 128:i * 128 + n], in_=win3[:, :, c0:c0 + n]), [], [rw])
                    M = 64 if moba else 128
                    dv = 64 if moba else 128
                    gq = pcol(l, 23 if moba else 21); gk = pcol(l, 24 if moba else 22)
                    if moba:
                        if hd == 4:
                            PL(lambda e: e.memset(KT[64:128, :], 0.0), [], [RK])
                        LD("sp", KT[64:80, :], cd["onehotk"], [], [RK])
                        PL(lambda e: e.memset(kmT[:], 0.0), [], [Rkm])
                        PL(lambda e: e.memset(Vt[:, :, 64:65], 1.0), [], [RV])
                    for tb in range(NTB):
                        hb_ = hblk[tb % 2]; rh = Rhk[tb % 2]
                        LD("sp", hb_[:], hT_d[:, :, tb * 512:(tb + 1) * 512].rearrange("k p t -> p k t"), [], [rh])
                        tsl = slice(tb * 512, (tb + 1) * 512)
                        proj_fm(hb_, rh, w, rw, 128, M, PF[0], RPF[0])
                        headnorm(PF[0], RPF[0], M, gk, KT[0:M, tsl], RK)
                        if moba:
                            DV(lambda e: e.tensor_reduce(kmT[0:64, 2 * tb:2 * tb + 2], qf[0:64, :].rearrange("p (a t) -> p a t", a=2),
                                                         AX.X, ALU.add), [Rqf], [Rkm])
                            DV(lambda e: e.tensor_scalar(kmT[0:64, 2 * tb:2 * tb + 2], kmT[0:64, 2 * tb:2 * tb + 2], 1.0 / 256, None, ALU.mult),
                               [Rkm], [Rkm])
                        proj_fm(hb_, rh, w, rw, 0, M, PF[1], RPF[1])
                        if moba:
                            headnorm(PF[1], RPF[1], M, gq, QT[0:M, tsl], RQ)
                        else:
                            headnorm(PF[1], RPF[1], M, gq, QT[0:64, tsl], RQ, dst2=QT1[64:128, tsl])
                        for j in range(4):
                            for kc in range(8):
                                MM(PF[3][:, j * 128:j * 128 + dv], hb_[:, kc, j * 128:(j + 1) * 128], w[:, kc, 256:256 + dv],
                                   kc == 0, kc == 7, [rh, rw], [RPF[3]])
                        ACT(Vt[:, tb * 4:(tb + 1) * 4, 0:dv], PF[3][:].rearrange("p (j t) -> p j t", j=4)[:, :, 0:dv], AF.Copy, [RPF[3]], [RV])
                        if moba:
                            for j in range(4):
                                qt = tb * 4 + j
                                MM(PF[4][:, j * 16:(j + 1) * 16], qf[0:64, j * 128:(j + 1) * 128], kmT[0:64, :], True, True, [Rqf, Rkm], [RPF[4]])
                                DV(lambda e: e.tensor_tensor(gm[:], PF[4][:, j * 16:(j + 1) * 16], negpast[:, qt * 16:(qt + 1) * 16], ALU.add),
                                   [RPF[4], Rc], [Rgm])
                                DV(lambda e: e.max(out=top8[:], in_=gm[:]), [Rgm], [Rgm])
                                DV(lambda e: e.tensor_scalar(gm[:], gm[:], top8[:, 2:3], None, ALU.is_ge), [Rgm], [Rgm])
                                DV(lambda e: e.scalar_tensor_tensor(out=nmw[:, j, 64:80], in0=gm[:], scalar=1.0, in1=past30[:, qt * 16:(qt + 1) * 16],
                                                                    op0=ALU.subtract, op1=ALU.mult), [Rgm, Rc], [Rnm])
                                TR(PF[5][0:80, j * 128:(j + 1) * 128], nmw[:, j, :], ident[:], [Rnm, Rc], [RPF[5]])
                            ACT(QT[64:80, tsl], PF[5][64:80, :], AF.Copy, [RPF[5]], [RQ])
                    KK = 80 if moba else 64
                    for qb in range(NTB):
                        qsl = slice(qb * 512, (qb + 1) * 512)
                        nmap = 1 if moba else 2
                        nkt = 4 * qb + 4
                        items = [(m, kt) for m in range(nmap) for kt in range(nkt)]

                        def emit_qk(idx):
                            m_, kt_ = items[idx]
                            qsrc = QT1 if m_ == 1 else QT
                            MM(PF[idx % 2][:], KT[:, kt_ * 128:(kt_ + 1) * 128], qsrc[:, qsl], True, True, [RK, RQ], [RPF[idx % 2]])

                        emit_qk(0)
                        for idx, (m, kt) in enumerate(items):
                            if idx + 1 < len(items):
                                emit_qk(idx + 1)
                            ps = PF[idx % 2]; rps = RPF[idx % 2]
                            po = PF[2 + m]; rpo = RPF[2 + m]
                            o0 = 4 * qb - kt
                            pb = Pb[pbi % 3]; rpb = RPb[pbi % 3]; pbi += 1
                            b31 = tblb[:, 31 * 8 + hd: 31 * 8 + hd + 1]
                            if o0 <= 7:
                                pfx = Pf[idx % 2]; rpf = RPf[idx % 2]
                                ACT(pfx[:], ps[:], AF.Exp, [rps, Rc], [rpf], bias=b31, scale=0.125)
                                DV(lambda e: e.tensor_tensor(pb[:], pfx[:], erel[:, hd, (o0 + 3) * 128:(o0 + 3) * 128 + 512], ALU.mult),
                                   [rpf, Rerel], [rpb])
                            else:
                                ACT(pb[:], ps[:], AF.Exp, [rps, Rc], [rpb], bias=b31, scale=0.125)
                            if moba:
                                MM(po[0:65, :], Vt[:, kt, 0:65], pb[:], kt == 0, kt == nkt - 1, [RV, rpb], [rpo])
                            else:
                                MM(po[:], Vt[:, kt, :], pb[:], kt == 0, kt == nkt - 1, [RV, rpb], [rpo])
                                MM(PF[4 + m][0:1, :], onesb[:, 0:1], pb[:], kt == 0, kt == nkt - 1, [Rc, rpb], [RPF[4 + m]])
                        if moba:
                            DV(lambda e: e.reciprocal(rec[64:65, 0, :], PF[2][64:65, :]), [RPF[2]], [Rrec])
                            MM(PF[0][0:64, :], onesf[64:65, 0:64], rec[64:65, 0, :], True, True, [Rc, Rrec], [RPF[0]])
                            ACT(bcs[0:64, 0, :], PF[0][0:64, :], AF.Copy, [RPF[0]], [Rbcs])
                            DV(lambda e: e.tensor_tensor(Ob[0:64, :], PF[2][0:64, :], bcs[0:64, 0, :], ALU.mult), [RPF[2], Rbcs], [ROb])
                            LD("sp", mixT_d[6 + h // 2, (h % 2) * 64:(h % 2) * 64 + 64, qsl], Ob[0:64, :], [ROb], [])
                        else:
                            DV(lambda e: e.reciprocal(rec[0:1, 0, :], PF[4][0:1, :]), [RPF[4]], [Rrec])
                            DV(lambda e: e.reciprocal(rec[0:1, 1, :], PF[5][0:1, :]), [RPF[5]], [Rrec])
                            DV(lambda e: e.tensor_scalar(rec[0:1, 1, :], rec[0:1, 1, :], lamc[0:1, 0:1], None, ALU.mult), [Rrec, Rlam], [Rrec])
                            for m in range(2):
                                MM(PF[m][:], onesf[0:1, :], rec[0:1, m, :], True, True, [Rc, Rrec], [RPF[m]])
                                ACT(bcs[:, m, :], PF[m][:], AF.Copy, [RPF[m]], [Rbcs])
                            DV(lambda e: e.tensor_tensor(Of[:], PF[2][:], bcs[:, 0, :], ALU.mult), [RPF[2], Rbcs], [ROf])
                            DV(lambda e: e.tensor_tensor(sqf[:], PF[3][:], bcs[:, 1, :], ALU.mult), [RPF[3], Rbcs], [Rsqf])
                            DV(lambda e: e.tensor_tensor(Of[:], Of[:], sqf[:], ALU.add), [ROf, Rsqf], [ROf])
                            ACT(sqf[:], Of[:], AF.Square, [ROf], [Rsqf])
                            MM(PF[0][:], onesf[:], sqf[:], True, True, [Rc, Rsqf], [RPF[0]])
                            DV(lambda e: e.tensor_scalar(rsf[:], PF[0][:], 1.0 / 128, 1e-6, ALU.mult, ALU.add), [RPF[0]], [Rrsf])
                            ACT(rsf[:], rsf[:], AF.Sqrt, [Rrsf], [Rrsf])
                            DV(lambda e: e.reciprocal(rsf[:], rsf[:]), [Rrsf], [Rrsf])
                            DV(lambda e: e.scalar_tensor_tensor(out=Ob[:], in0=Of[:], scalar=lamc[:, 3:4], in1=rsf[:], op0=ALU.mult, op1=ALU.mult),
                               [ROf, Rlam, Rrsf], [ROb])
                            LD("sp", mixT_d[2 + h, :, qsl], Ob[:], [ROb], [])
                fw.barrier()
            if stop == 'C':
                fw.halt = True

            CAPl = CAPS[l]
            NROW = 32 * CAPl
            with ExitStack() as ph:
                wo = sb(ph, "wo", [128, 8, D], BF16); Rwo = Region()
                fw.dma("pool", lambda e: e.dma_start(out=wo[:], in_=wout_d[l].rearrange("(k p) n -> p k n", p=128)), [], [Rwo])
                wrt = sb(ph, "wrt", [128, 8, 36]); brb = sb(ph, "brb", [128, 36]); Rwr = Region()
                LD("sp", wrt[:], wr_d[l].rearrange("(k p) n -> p k n", p=128), [], [Rwr])
                LD("sp", brb[:], br_d[l:l + 1, :].partition_broadcast(128), [], [Rwr])
                mblk = [sb(ph, f"mblk{i}", [128, 8, 512], BF16) for i in range(2)]; Rmb = [Region(), Region()]
                xt = [sb(ph, f"xt{i}", [128, D]) for i in range(2)]; Rx = [Region(), Region()]
                hf = [sb(ph, f"hf{i}", [128, D]) for i in range(2)]; Rhf = [Region(), Region()]
                h2b = [sb(ph, f"h2b{i}", [128, D], BF16) for i in range(2)]; Rhb = [Region(), Region()]
                sm = [sb(ph, f"sm{i}", [128, 4]) for i in range(2)]; Rsm = [Region(), Region()]
                h2T = sb(ph, "h2T", [128, 8, 128]); RhT = Region()
                lg = sb(ph, "lg", [128, 36]); ml = sb(ph, "ml", [128, 32]); oh = sb(ph, "oh", [128, 2, 32])
                rt8 = sb(ph, "rt8", [128, 8]); rs_ = sb(ph, "rs_", [128, 16]); Rr = Region()
                cntb = sb(ph, "cntb", [128, 32]); Rcnt = Region()
                io32 = sb(ph, "io32", [128, 32]); posf = sb(ph, "posf", [128, 32]); msk = sb(ph, "msk", [128, 32])
                tmp32 = sb(ph, "tmp32", [128, 32]); dst = sb(ph, "dstf", [128, 2])
                PL(lambda e: e.memset(cntb[:], 0.0), [], [Rcnt])
                PL(lambda e: e.iota(io32[:], pattern=[[1, 32]], base=0, channel_multiplier=0, allow_small_or_imprecise_dtypes=True), [], [Rr])
                for tt in range(NTT):
                    i = tt % 2; tb = tt // 4; j = tt % 4
                    if j == 0:
                        LD("sp", mblk[tb % 2][:], mixT_d[:, :, tb * 512:(tb + 1) * 512].rearrange("k p t -> p k t"), [], [Rmb[tb % 2]])
                    mb = mblk[tb % 2]; rmb = Rmb[tb % 2]
                    LD("sp", xt[i][:], xsrc[tt * 128:(tt + 1) * 128, :], [], [Rx[i]])
                    for half in range(2):
                        for kc in range(8):
                            MM(PF[half][:], mb[:, kc, j * 128:(j + 1) * 128], wo[:, kc, half * 512:(half + 1) * 512], kc == 0, kc == 7,
                               [rmb, Rwo], [RPF[half]])
                        hs = slice(half * 512, (half + 1) * 512)
                        DV(lambda e: e.tensor_tensor(hf[i][:, hs], PF[half][:], G1[:, hs], ALU.mult), [RPF[half], Rmod], [Rhf[i]])
                    PL(lambda e: e.tensor_tensor(xt[i][:], xt[i][:], hf[i][:], ALU.add), [Rx[i], Rhf[i]], [Rx[i]])
                    LD("sp", xr_d[tt * 128:(tt + 1) * 128, :], xt[i][:], [Rx[i]], [])
                    norm_mod(ph, xt[i], Rx[i], A2, SH2, hf[i], Rhf[i], sm[i], Rsm[i])
                    ACT(h2b[i][:], hf[i][:], AF.Copy, [Rhf[i]], [Rhb[i]])
                    for kc in range(8):
                        TR(PF[2 + kc // 4][:, (kc % 4) * 128:(kc % 4 + 1) * 128], hf[i][:, kc * 128:(kc + 1) * 128], ident[:], [Rhf[i], Rc],
                           [RPF[2 + kc // 4]])
                    ACT(h2T[:, 0:4, :], PF[2][:].rearrange("p (k t) -> p k t", k=4), AF.Copy, [RPF[2]], [RhT])
                    DV(lambda e: e.tensor_copy(h2T[:, 4:8, :], PF[3][:].rearrange("p (k t) -> p k t", k=4)), [RPF[3]], [RhT])
                    for kc in range(8):
                        MM(PF[4][:, 0:36], h2T[:, kc, :], wrt[:, kc, :], kc == 0, kc == 7, [RhT, Rwr], [RPF[4]])
                    DV(lambda e: e.tensor_tensor(lg[:], PF[4][:, 0:36], brb[:], ALU.add), [RPF[4], Rwr], [Rr])
                    DV(lambda e: e.tensor_reduce(rs_[:, 0:1], lg[:, 0:4], AX.X, ALU.max), [Rr], [Rr])
                    DV(lambda e: e.tensor_scalar(rs_[:, 1:2], rs_[:, 0:1], -1.0, None, ALU.mult), [Rr], [Rr])
                    PL(lambda e: e.memset(rs_[:, 2:3], 0.0), [Rr], [Rr])
                    ACT(rs_[:, 4:8], lg[:, 0:4], AF.Exp, [Rr], [Rr], bias=rs_[:, 1:2], accum_out=rs_[:, 2:3])
                    DV(lambda e: e.reciprocal(rs_[:, 3:4], rs_[:, 2:3]), [Rr], [Rr])
                    DV(lambda e: e.tensor_scalar(rs_[:, 8:12], lg[:, 0:4], rs_[:, 0:1], None, ALU.is_ge), [Rr], [Rr])
                    DV(lambda e: e.tensor_scalar(rs_[:, 8:12], rs_[:, 8:12], 1.0, 1e30, ALU.subtract, ALU.mult), [Rr], [Rr])
                    DV(lambda e: e.tensor_tensor(ml[:].rearrange("p (g e) -> p g e", g=4), lg[:, 4:36].rearrange("p (g e) -> p g e", g=4),
                                                 rs_[:, 8:12].rearrange("p (g o) -> p g o", o=1).to_broadcast([128, 4, 8]), ALU.add), [Rr], [Rr])
                    DV(lambda e: e.max(out=rt8[:], in_=ml[:]), [Rr], [Rr])
                    DV(lambda e: e.tensor_scalar(oh[:, 0, :], ml[:], rt8[:, 0:1], None, ALU.is_equal), [Rr], [Rr])
                    DV(lambda e: e.tensor_scalar(oh[:, 1, :], ml[:], rt8[:, 1:2], None, ALU.is_equal), [Rr], [Rr])
                    DV(lambda e: e.tensor_tensor(rs_[:, 12:13], rt8[:, 0:1], rt8[:, 1:2], ALU.subtract), [Rr], [Rr])
                    ACT(rs_[:, 13:14], rs_[:, 12:13], AF.Sigmoid, [Rr], [Rr])
                    DV(lambda e: e.tensor_tensor(gts[:, tt, 0:1], rs_[:, 13:14], rs_[:, 3:4], ALU.mult), [Rr], [Rgts])
                    DV(lambda e: e.tensor_tensor(gts[:, tt, 1:2], rs_[:, 3:4], gts[:, tt, 0:1], ALU.subtract), [Rr, Rgts], [Rgts])
                    DV(lambda e: e.tensor_tensor(msk[:], oh[:, 0, :], oh[:, 1, :], ALU.add), [Rr], [Rr])
                    MM(PF[5][:, 0:32], mui[:], msk[:], True, True, [Rc, Rr], [RPF[5]])
                    MM(PF[5][:, 32:64], onesf[:], msk[:], True, True, [Rc, Rr], [RPF[5]])
                    DV(lambda e: e.tensor_tensor(posf[:], PF[5][:, 0:32], cntb[:], ALU.add), [RPF[5], Rcnt], [Rr])
                    DV(lambda e: e.tensor_tensor(cntb[:], PF[5][:, 32:64], cntb[:], ALU.add), [RPF[5], Rcnt, Rr], [Rcnt])
                    DV(lambda e: e.tensor_scalar(tmp32[:], posf[:], float(CAPl), 4.0e7, ALU.is_gt, ALU.mult), [Rr], [Rr])
                    DV(lambda e: e.tensor_tensor(posf[:], posf[:], tmp32[:], ALU.add), [Rr], [Rr])
                    DV(lambda e: e.scalar_tensor_tensor(out=posf[:], in0=io32[:], scalar=float(CAPl), in1=posf[:], op0=ALU.mult, op1=ALU.add),
                       [Rr], [Rr])
                    for k in range(2):
                        DV(lambda e: e.tensor_tensor(tmp32[:], oh[:, k, :], posf[:], ALU.mult), [Rr], [Rr])
                        DV(lambda e: e.tensor_reduce(dst[:, k:k + 1], tmp32[:], AX.X, ALU.add), [Rr], [Rr])
                    DV(lambda e: e.tensor_scalar(dst[:], dst[:], -1.0, float(NROW), ALU.add, ALU.min), [Rr], [Rr])
                    DV(lambda e: e.tensor_copy(off[:, tt, :], dst[:]), [Rr], [Roff])
                    for k in range(2):
                        fw.dma("pool", lambda e: e.indirect_dma_start(
                            out=Xs_d[0:NROW + 1, :], out_offset=bass.IndirectOffsetOnAxis(ap=off[:, tt, k:k + 1], axis=0),
                            in_=h2b[i][:], in_offset=None), [Rhb[i], Roff], [RXs])
                fw.barrier()
            if stop == 'D':
                fw.halt = True

            with ExitStack() as ph:
                w1b = [sb(ph, f"w1b{i}", [128, 8, 512], BF16) for i in range(2)]
                w3b = [sb(ph, f"w3b{i}", [128, 8, 512], BF16) for i in range(2)]
                w2b = [sb(ph, f"w2b{i}", [128, 4, D], BF16) for i in range(2)]
                Rwe = [Region(), Region()]
                xs = [sb(ph, f"xs{i}", [128, 4, D], BF16) for i in range(2)]; Rxs = [Region(), Region()]
                XT = sb(ph, "XT", [128, 8, 512], BF16); RXT = Region()
                s1 = [sb(ph, f"s1{i}", [128, 512]) for i in range(2)]; Rs1 = [Region(), Region()]
                GT = sb(ph, "GT", [128, 4, 512], BF16); RGT = Region()
                yrow = [sb(ph, f"yrow{i}", [128, D]) for i in range(2)]; Ryr = [Region(), Region()]
                PL(lambda e: e.memset(yrow[0][:], 0.0), [], [Ryr[0]])
                LD("sp", Ys_d[NROW:NROW + 1, :], yrow[0][0:1, :], [Ryr[0]], [RYs])
                groups = []
                s0 = 0
                while s0 < CAPl:
                    n = min(512, CAPl - s0); groups.append((s0, n)); s0 += n
                gi = 0; yi = 0
                for ex in range(32):
                    wi = ex % 2
                    fw.dma("pool", lambda e: e.dma_start(out=w1b[wi][:], in_=w1_d[l, ex].rearrange("(k p) n -> p k n", p=128)), [], [Rwe[wi]])
                    fw.dma("pool", lambda e: e.dma_start(out=w3b[wi][:], in_=w3_d[l, ex].rearrange("(k p) n -> p k n", p=128)), [], [Rwe[wi]])
                    fw.dma("pool", lambda e: e.dma_start(out=w2b[wi][:], in_=w2_d[l, ex].rearrange("(k p) n -> p k n", p=128)), [], [Rwe[wi]])
                    for (s0, n) in groups:
                        nt = n // 128
                        x_ = xs[gi % 2]; rx_ = Rxs[gi % 2]; gi += 1
                        r0 = ex * CAPl + s0
                        LD("sp", x_[:, 0:nt, :], Xs_d[r0:r0 + n, :].rearrange("(i p) d -> p i d", p=128), [RXs], [rx_])
                        for it_ in range(nt):
                            pbk = PB[it_ % 2]; rpb_ = RPB[it_ % 2]
                            for kc in range(8):
                                TR(pbk[:, kc * 128:(kc + 1) * 128], x_[:, it_, kc * 128:(kc + 1) * 128], identb[:], [rx_, Rc], [rpb_])
                            if it_ % 2 == 0:
                                ACT(XT[:, :, it_ * 128:(it_ + 1) * 128], pbk[:].rearrange("p (k t) -> p k t", k=8), AF.Copy, [rpb_], [RXT])
                            else:
                                DV(lambda e: e.tensor_copy(XT[:, :, it_ * 128:(it_ + 1) * 128], pbk[:].rearrange("p (k t) -> p k t", k=8)),
                                   [rpb_], [RXT])
                        for hc in range(4):
                            p1 = PF[hc % 2]; r1 = RPF[hc % 2]; p3 = PF[2 + hc % 2]; r3 = RPF[2 + hc % 2]
                            for kc in range(8):
                                MM(p1[:, 0:n], w1b[wi][:, kc, hc * 128:(hc + 1) * 128], XT[:, kc, 0:n], kc == 0, kc == 7, [Rwe[wi], RXT], [r1])
                            for kc in range(8):
                                MM(p3[:, 0:n], w3b[wi][:, kc, hc * 128:(hc + 1) * 128], XT[:, kc, 0:n], kc == 0, kc == 7, [Rwe[wi], RXT], [r3])
                            ACT(s1[hc % 2][:, 0:n], p1[:, 0:n], AF.Silu, [r1], [Rs1[hc % 2]])
                            DV(lambda e: e.tensor_tensor(GT[:, hc, 0:n], s1[hc % 2][:, 0:n], p3[:, 0:n], ALU.mult), [Rs1[hc % 2], r3], [RGT])
                        for it_ in range(nt):
                            yr = yrow[yi % 2]; ryr = Ryr[yi % 2]; yi += 1
                            for half in range(2):
                                py = PF[4 + half]; rpy = RPF[4 + half]
                                for hc in range(4):
                                    MM(py[:], GT[:, hc, it_ * 128:(it_ + 1) * 128], w2b[wi][:, hc, half * 512:(half + 1) * 512], hc == 0, hc == 3,
                                       [RGT, Rwe[wi]], [rpy])
                                if half == 0:
                                    ACT(yr[:, 0:512], py[:], AF.Copy, [rpy], [ryr])
                                else:
                                    DV(lambda e: e.tensor_copy(yr[:, 512:1024], py[:]), [rpy], [ryr])
                            LD("sp", Ys_d[r0 + it_ * 128:r0 + (it_ + 1) * 128, :], yr[:], [ryr], [RYs])
                fw.barrier()
            if stop == 'E':
                fw.halt = True

            with ExitStack() as ph:
                xt = [sb(ph, f"xt{i}", [128, D]) for i in range(2)]; Rx = [Region(), Region()]
                y0 = [sb(ph, f"y0{i}", [128, D]) for i in range(2)]; Ry0 = [Region(), Region()]
                y1 = [sb(ph, f"y1{i}", [128, D]) for i in range(2)]; Ry1 = [Region(), Region()]
                for tt in range(NTT):
                    i = tt % 2
                    LD("sp", xt[i][:], xr_d[tt * 128:(tt + 1) * 128, :], [], [Rx[i]])
                    for k, (y, ry) in enumerate(((y0[i], Ry0[i]), (y1[i], Ry1[i]))):
                        PL(lambda e: e.memset(y[:], 0.0), [], [ry])
                        fw.dma("pool", lambda e: e.indirect_dma_start(
                            out=y[:], out_offset=None, in_=Ys_d[0:NROW + 1, :],
                            in_offset=bass.IndirectOffsetOnAxis(ap=off[:, tt, k:k + 1], axis=0)), [RYs, Roff], [ry])
                    DV(lambda e: e.tensor_scalar(y0[i][:], y0[i][:], gts[:, tt, 0:1], None, ALU.mult), [Ry0[i], Rgts], [Ry0[i]])
                    DV(lambda e: e.scalar_tensor_tensor(out=y0[i][:], in0=y1[i][:], scalar=gts[:, tt, 1:2], in1=y0[i][:], op0=ALU.mult, op1=ALU.add),
                       [Ry1[i], Ry0[i], Rgts], [Ry0[i]])
                    PL(lambda e: e.tensor_tensor(y0[i][:], y0[i][:], G2, ALU.mult), [Ry0[i], Rmod], [Ry0[i]])
                    DV(lambda e: e.tensor_tensor(xt[i][:], xt[i][:], y0[i][:], ALU.add), [Rx[i], Ry0[i]], [Rx[i]])
                    LD("sp", xdst[tt * 128:(tt + 1) * 128, :], xt[i][:], [Rx[i]], [Rout])
                fw.barrier()
            if stop == 'F':
                fw.halt = True
        except StopBuild:
            pass
        fw.halt = False
        fw._wait("sp", Rout.w)
        fw.barrier()
    return nc


def run(inputs, NL=4, dbg=False, stop=None):
    sh, per = prep_inputs(inputs)
    key = (NL, dbg)
    nc = build(NL, LTOT=inputs["ada_w"].shape[0], dbg=dbg, stop=stop)
    in_maps = []
    for core in range(8):
        m = dict(sh)
        m.update(per[core % len(per)])
        in_maps.append(m)
    res = run_bass_kernel_spmd(nc, in_maps, core_ids=list(range(8)))
    return res


def kernel(**inputs):
    inputs = {k: np.asarray(v) for k, v in inputs.items()}
    res = run(inputs, NL=4)
    B = inputs["x"].shape[0]
    out = np.stack([np.asarray(res.results[b]["out"], dtype=np.float32).reshape(S, D) for b in range(B)], axis=0)
    return out
```

```python
import math
from contextlib import ExitStack
import numpy as np
import ml_dtypes
import concourse.bass as bass
import concourse.mybir as mybir
from concourse.bass_utils import run_bass_kernel_spmd

F32 = mybir.dt.float32
BF16 = mybir.dt.bfloat16
I32 = mybir.dt.int32
AF = mybir.ActivationFunctionType
ALU = mybir.AluOpType
AX = mybir.AxisListType

S = 4096
D = 1024
NTT = S // 128
NTB = S // 512
CAPS = [768, 896, 1280, 1408]
CAPMAX = max(CAPS)
NPP = 26
NEG = -30000.0
DMA_RING = 8
DEBUG_IND = False


class Region:
    __slots__ = ("name", "w", "r")

    def __init__(self, name=""):
        self.name = name
        self.w = None
        self.r = []


class FW:
    def __init__(self, nc, stack):
        self.nc = nc
        self.stack = stack
        self.eng = {"pe": nc.tensor, "act": nc.scalar, "dve": nc.vector,
                    "pool": nc.gpsimd, "sp": nc.sync}
        self.sems = {}
        self.cnt = {}
        self.waited = {e: {} for e in self.eng}
        for e in self.eng:
            self.sems["c_" + e] = stack.enter_context(nc.semaphore("c_" + e))
            self.cnt["c_" + e] = 0
        self.ring = {}
        for q in ("sp", "act", "pool"):
            for i in range(DMA_RING):
                k = f"d_{q}{i}"
                self.sems[k] = stack.enter_context(nc.semaphore(k))
                self.cnt[k] = 0
            self.ring[q] = 0
        self.n_inst = 0
        self.halt = False

    def _wait(self, e, ev):
        if ev is None or self.halt:
            return
        k, v = ev
        if self.waited[e].get(k, 0) >= v:
            return
        self.waited[e][k] = v
        self.eng[e].wait_ge(self.sems[k], v)

    def _deps(self, e, reads, writes, skip_same):
        own = "c_" + e
        for r in reads:
            if r.w is not None and not (skip_same and r.w[0] == own):
                self._wait(e, r.w)
        for r in writes:
            if r.w is not None and not (skip_same and r.w[0] == own):
                self._wait(e, r.w)
            for ev in r.r:
                if not (skip_same and ev[0] == own):
                    self._wait(e, ev)

    def _record(self, ev, reads, writes):
        for r in writes:
            r.w = ev
            r.r = []
        for r in reads:
            if r in writes:
                continue
            r.r = [x for x in r.r if x[0] != ev[0]] + [ev]

    def op(self, e, fn, reads=(), writes=(), skip_same=False):
        if self.halt:
            return
        self._deps(e, reads, writes, skip_same)
        k = "c_" + e
        self.cnt[k] += 1
        fn(self.eng[e]).then_inc(self.sems[k], 1)
        self._record((k, self.cnt[k]), reads, writes)
        self.n_inst += 1

    def dma(self, q, fn, reads=(), writes=()):
        if self.halt:
            return
        i = self.ring[q]
        self.ring[q] = (i + 1) % DMA_RING
        k = f"d_{q}{i}"
        if self.cnt[k] > 0:
            self._wait(q, (k, self.cnt[k]))
        self._deps(q, reads, writes, False)
        self.cnt[k] += 16
        fn(self.eng[q]).then_inc(self.sems[k], 16)
        self._record((k, self.cnt[k]), reads, writes)
        self.n_inst += 1

    def barrier(self):
        for e in self.eng:
            for k, v in self.cnt.items():
                if v > 0:
                    self._wait(e, (k, v))


def t5_bucket_np(dist):
    n = np.maximum(dist, 0)
    nf = np.maximum(n, 1).astype(np.float32)
    large = 16 + (np.log(nf / 16) / math.log(1024 / 16) * 16).astype(np.int32)
    large = np.minimum(large, 31)
    return np.where(n < 16, n, large)


def make_consts():
    c = {}
    p = np.arange(128)[:, None]
    j = np.arange(128)[None, :]
    c["ident"] = (p == j).astype(np.float32)
    su = (p < j).astype(np.float32)
    ui = (p <= j).astype(np.float32)
    sl = (p > j).astype(np.float32)
    c["m4"] = np.concatenate([su, ui, su, ui], 1)
    c["msl"] = sl
    c["mui"] = ui
    c["bdones"] = ((p // 64) == (j // 64)).astype(np.float32)
    cc = np.arange(14 * 128)[None, :]
    dist = (cc // 128 - 3) * 128 + (cc % 128) - p
    c["idxE"] = np.where(dist >= 0, t5_bucket_np(dist), -1).astype(np.float32)
    rm = np.ones((128, 512), np.float32)
    rm[:, ::128] = 0
    c["rmask"] = rm
    qt = np.arange(32)[:, None]
    n = np.arange(16)[None, :]
    past = (n < qt // 2).astype(np.float32)
    c["negpast"] = np.broadcast_to(((1 - past) * -1e30).reshape(1, 512), (128, 512)).copy().astype(np.float32)
    c["past30"] = np.broadcast_to((past * 30000.0).reshape(1, 512), (128, 512)).copy().astype(np.float32)
    lm = np.zeros((128, 7, 2, 2, 128), np.float32)
    tt_ = np.arange(128)[:, None]; ii_ = np.arange(128)[None, :]
    for k in range(7):
        b = 2 ** k
        M = (((tt_ // (2 * b)) == (ii_ // (2 * b))) & ((tt_ % (2 * b)) >= b) & ((ii_ % (2 * b)) < b)).astype(np.float32)
        lm[:, k, :, 0, :] = M[:, None, :]
        lm[:, k, :, 1, :] = M.T[:, None, :]
    c["lvlmask"] = lm.reshape(128, 7 * 512).astype(ml_dtypes.bfloat16)
    oh = (np.arange(S)[None, :] // 256 == np.arange(16)[:, None]).astype(np.float32)
    c["onehotk"] = oh.astype(ml_dtypes.bfloat16)
    return c


CONST_SHAPES = {"ident": ([128, 128], F32), "m4": ([128, 512], F32), "msl": ([128, 128], F32),
                "mui": ([128, 128], F32), "bdones": ([128, 128], F32), "idxE": ([128, 1792], F32),
                "rmask": ([128, 512], F32), "negpast": ([128, 512], F32), "past30": ([128, 512], F32),
                "onehotk": ([16, S], BF16), "lvlmask": ([128, 7 * 512], BF16)}


def prep_inputs(I):
    L = I["ada_w"].shape[0]
    sh = {}
    for k in ("ada_w", "ada_b", "w_in", "w_out", "moe_w1", "moe_w3", "moe_w2"):
        sh[k] = np.ascontiguousarray(I[k], dtype=np.float32)
    sh["n1g"] = np.ascontiguousarray(I["norm1_g"])
    sh["n2g"] = np.ascontiguousarray(I["norm2_g"])
    pp = np.zeros((128, L, NPP), np.float32)
    pidx = np.arange(128)
    for l in range(L):
        pp[:, l, 0:7] = I["rwkv_mu"][l].reshape(7, 128).T
        pp[:, l, 7:9] = I["rwkv_w0"][l].reshape(2, 128).T
        pp[:, l, 9:11] = I["rwkv_a0"][l].reshape(2, 128).T
        pp[:, l, 11:13] = I["rwkv_kk"][l].reshape(2, 128).T
        pp[:, l, 13:15] = I["rwkv_ka"][l].reshape(2, 128).T
        pp[:, l, 15:17] = I["rwkv_rk"][l].reshape(2, 128).T
        pp[:, l, 17:19] = I["rwkv_ln_g"][l].reshape(2, 128).T
        pp[:, l, 19:21] = I["rwkv_ln_b"][l].reshape(2, 128).T
        pp[:, l, 21] = I["diff_q_gain"][l][pidx % 64]
        pp[:, l, 22] = I["diff_k_gain"][l][pidx % 64]
        pp[:, l, 23] = I["moba_q_gain"][l][pidx % 64]
        pp[:, l, 24] = I["moba_k_gain"][l][pidx % 64]
        pp[:, l, 25] = I["diff_subln_g"][l]
    sh["ppar"] = pp.reshape(128, L * NPP)
    sh["lora"] = np.ascontiguousarray(np.concatenate([I["rwkv_w2"], I["rwkv_a2"], I["rwkv_g2"]], axis=1))
    sh["lam"] = np.ascontiguousarray(I["diff_lambda"].reshape(L, 256))
    sh["tbl2"] = np.ascontiguousarray(I["rel_bias"])
    sh["wr"] = np.ascontiguousarray(np.concatenate([I["router_g_w"], I["router_e_w"]], axis=2))
    sh["br"] = np.ascontiguousarray(np.concatenate([I["router_g_b"], I["router_e_b"]], axis=1))
    sh.update(make_consts())
    par = []
    for hh in range(2):
        wa = np.zeros((L, D, 1536), np.float32)
        tb_ = np.zeros((1, 128), np.float32)
        for lh in range(4):
            if lh < 2:
                h = 2 * hh + lh
                srcs = [(896 + h * 128, 128), (896 + 512 + h * 128, 128), (896 + 1024 + h * 128, 128)]
                gh = h
            else:
                h = 2 * hh + (lh - 2)
                srcs = [(2432 + h * 64, 64), (2432 + 256 + h * 64, 64), (2432 + 512 + h * 64, 64)]
                gh = 4 + h
            for i, (c0, n) in enumerate(srcs):
                wa[:, :, lh * 384 + i * 128: lh * 384 + i * 128 + n] = I["w_in"][:, :, c0:c0 + n]
            tb_[0, np.arange(32) * 4 + lh] = I["rel_bias"][:, gh]
        par.append({"wa_in": wa, "tbl": tb_})
    per = []
    for b in range(I["x"].shape[0]):
        per.append({"x": np.ascontiguousarray(I["x"][b]),
                    "c8": np.ascontiguousarray(I["c"][b].reshape(8, 128).T)})
    return sh, per, par


class StopBuild(Exception):
    pass


def build(NL, LTOT=4, dbg=False, stop=None, ncores=8):
    nc = bass.Bass("TRN2", target_bir_lowering=False)

    def din(name, shape, dt=F32):
        return nc.dram_tensor(name, list(shape), dt, kind="ExternalInput").ap()

    x_d = din("x", [S, D]); c8_d = din("c8", [128, 8])
    adaw_d = din("ada_w", [LTOT, D, 6 * D]); adab_d = din("ada_b", [LTOT, 6 * D])
    n1g_d = din("n1g", [LTOT, D]); n2g_d = din("n2g", [LTOT, D])
    win_d = din("w_in", [LTOT, D, 3200]); wout_d = din("w_out", [LTOT, D, D])
    ppar_d = din("ppar", [128, LTOT * NPP]); lora_d = din("lora", [LTOT, 128, 256])
    lam_d = din("lam", [LTOT, 256]); tbl_d = din("tbl", [1, 128]); wa_d = din("wa_in", [LTOT, D, 1536]); tbl2_d = din("tbl2", [32, 8])
    wr_d = din("wr", [LTOT, D, 36]); br_d = din("br", [LTOT, 36])
    w1_d = din("moe_w1", [LTOT, 32, D, 512]); w3_d = din("moe_w3", [LTOT, 32, D, 512])
    w2_d = din("moe_w2", [LTOT, 32, 512, D])
    cd = {k: din(k, shp, dt) for k, (shp, dt) in CONST_SHAPES.items()}
    out_d = nc.dram_tensor("out", [S, D], F32, kind="ExternalOutput").ap()
    okind = "ExternalOutput" if dbg else "Internal"
    xr_d = nc.dram_tensor("xr", [S, D], F32, kind=okind).ap()
    hT_d = nc.dram_tensor("hT", [8, 128, S], BF16).ap()
    mixT_d = nc.dram_tensor("mixT", [8, 128, S], BF16, kind=okind).ap()
    Mine_f = [nc.dram_tensor(f"Mine{i}", [64, S // 2], F32).ap() for i in range(6)]
    G_f = [nc.dram_tensor(f"Gth{i}", [128, S // 2], F32).ap() for i in range(6)]
    Mine_b = [t.bitcast(BF16) for t in Mine_f]
    G_b = [t.bitcast(BF16) for t in G_f]
    Xs_d = nc.dram_tensor("Xs", [32 * CAPMAX + 128, D], BF16).ap()
    Ys_d = nc.dram_tensor("Ys", [32 * CAPMAX + 128, D], F32).ap()

    with ExitStack() as st:
        fw = FW(nc, st)
        ccsem = st.enter_context(nc.semaphore("ccsem"))

        uid = [0]

        def sb(stack, name, shape, dt=F32):
            uid[0] += 1
            return stack.enter_context(nc.sbuf_tensor(f"s{uid[0]}_{name}", list(shape), dt))

        pe_cfg = [None]

        def pe_sync(ap):
            cfg = (ap.base_partition(), ap.partition_size())
            if cfg != pe_cfg[0] and fw.cnt["c_pe"] > 0 and not fw.halt:
                fw._wait("pe", ("c_pe", fw.cnt["c_pe"]))
            pe_cfg[0] = cfg

        def MM(out, lhsT, rhs, start, stop, R, W):
            pe_sync(lhsT)
            fw.op("pe", lambda e: e.matmul(out, lhsT=lhsT, rhs=rhs, start=start, stop=stop), R, W, skip_same=True)

        def TR(out, in_, idn, R, W):
            pe_sync(in_)
            fw.op("pe", lambda e: e.transpose(out, in_, idn), R, W, skip_same=True)

        def ACT(out, in_, func, R, W, **kw):
            fw.op("act", lambda e: e.activation(out, in_, func, **kw), R, W)

        def DV(fn, R, W):
            fw.op("dve", fn, R, W)

        def PL(fn, R, W):
            fw.op("pool", fn, R, W)

        def LD(q, out, in_, R, W):
            fw.dma(q, lambda e: e.dma_start(out=out, in_=in_), R, W)

        PF = [st.enter_context(nc.psum_tensor(f"pf{i}", [128, 512], F32)) for i in range(6)]
        RPF = [Region(f"pf{i}") for i in range(6)]
        PB = [st.enter_context(nc.psum_tensor(f"pb{i}", [128, 1024], BF16)) for i in range(2)]
        RPB = [Region(f"pb{i}") for i in range(2)]

        Rc = Region("consts")
        ident = sb(st, "ident", [128, 128]); identb = sb(st, "identb", [128, 128], BF16)
        m4 = sb(st, "m4", [128, 512]); msl = sb(st, "msl", [128, 128]); mui = sb(st, "mui", [128, 128])
        bdones = sb(st, "bdones", [128, 128]); rmask = sb(st, "rmask", [128, 512])
        negpast = sb(st, "negpast", [128, 512]); past30 = sb(st, "past30", [128, 512])
        onesf = sb(st, "onesf", [128, 128]); onesb = sb(st, "onesb", [128, 128], BF16)
        lvlmask = sb(st, "lvlmask", [128, 7, 512], BF16)
        LD("sp", lvlmask[:].rearrange("p k c -> p (k c)"), cd["lvlmask"], [], [Rc])
        ppar = sb(st, "ppar", [128, LTOT * NPP]); tblb = sb(st, "tblb", [128, 128])
        cs8 = sb(st, "cs8", [128, 8]); csrep = sb(st, "csrep", [128, 8, 128])
        modb = sb(st, "modb", [128, 6 * D]); Rmod = Region("modb")
        erel = sb(st, "erel", [128, 4, 1792], BF16); Rerel = Region("erel")
        lamc = sb(st, "lamc", [128, 4]); Rlam = Region("lamc")
        off = sb(st, "off", [128, NTT, 2], I32); Roff = Region()
        gts = sb(st, "gts", [128, NTT, 2]); Rgts = Region()
        RXs = Region(); RYs = Region(); Rout = Region()
        ccdummy = sb(st, "ccdummy", [128, 2]); Rccd = Region()

        for t, k in ((ident, "ident"), (m4, "m4"), (msl, "msl"), (mui, "mui"), (bdones, "bdones"),
                     (rmask, "rmask"), (negpast, "negpast"), (past30, "past30")):
            LD("sp", t[:], cd[k], [], [Rc])
        LD("sp", ppar[:], ppar_d, [], [Rc])
        LD("sp", tblb[:], tbl_d.partition_broadcast(128), [], [Rc])
        LD("sp", cs8[:], c8_d, [], [Rc])
        PL(lambda e: e.memset(onesf[:], 1.0), [], [Rc])
        PL(lambda e: e.memset(onesb[:], 1.0), [], [Rc])
        DV(lambda e: e.tensor_copy(identb[:], ident[:]), [Rc], [Rc])
        ACT(cs8[:], cs8[:], AF.Silu, [Rc], [Rc])
        for j in range(8):
            DV(lambda e: e.tensor_copy(csrep[:, j, :], cs8[:, j:j + 1].to_broadcast([128, 128])), [Rc], [Rc])

        def pcol(l, i):
            return ppar[:, l * NPP + i: l * NPP + i + 1]

        with ExitStack() as ph:
            idxE = sb(ph, "idxE", [128, 1792]); Ri = Region()
            acc = sb(ph, "eacc", [128, 1792]); Ra = Region()
            tmp = [sb(ph, f"etmp{i}", [128, 1792]) for i in range(2)]; Rt = [Region(), Region()]
            LD("sp", idxE[:], cd["idxE"], [], [Ri])
            it = 0
            for h in range(4):
                PL(lambda e: e.memset(acc[:], 0.0), [], [Ra])
                for b in range(32):
                    t = tmp[it % 2]; rt = Rt[it % 2]; it += 1
                    DV(lambda e: e.tensor_scalar(t[:], idxE[:], float(b), tblb[:, b * 4 + h: b * 4 + h + 1],
                                                 ALU.is_equal, ALU.mult), [Ri, Rc], [rt])
                    PL(lambda e: e.tensor_tensor(acc[:], acc[:], t[:], ALU.add), [rt, Ra], [Ra])
                DV(lambda e: e.tensor_scalar(acc[:], acc[:], tblb[:, 31 * 4 + h: 31 * 4 + h + 1], None, ALU.subtract),
                   [Ra, Rc], [Ra])
                ACT(acc[:], acc[:], AF.Exp, [Ra], [Ra])
                t = tmp[0]
                DV(lambda e: e.tensor_scalar(t[:], idxE[:], 0.0, None, ALU.is_ge), [Ri], [Rt[0]])
                DV(lambda e: e.tensor_tensor(erel[:, h, :], acc[:], t[:], ALU.mult), [Ra, Rt[0]], [Rerel])
            fw.barrier()

        try:
          for l in range(NL):
            lam_init = 0.8 - 0.6 * math.exp(-0.3 * l)
            xsrc = x_d if l == 0 else xr_d
            xdst = out_d if l == NL - 1 else xr_d

            with ExitStack() as ph:
                aw = [sb(ph, f"aw{i}", [128, 8, 512]) for i in range(2)]; Raw = [Region(), Region()]
                ab = [sb(ph, f"ab{i}", [128, 512]) for i in range(2)]; Rab = [Region(), Region()]
                gb = sb(ph, "gb", [128, 2, D]); Rgb = Region()
                lamt = sb(ph, "lamt", [128, 256]); Rlt = Region()
                LD("act", gb[:, 0, :], n1g_d[l:l + 1, :].partition_broadcast(128), [], [Rgb])
                LD("act", gb[:, 1, :], n2g_d[l:l + 1, :].partition_broadcast(128), [], [Rgb])
                LD("act", lamt[:], lam_d[l:l + 1, :].partition_broadcast(128), [], [Rlt])
                for nb in range(12):
                    a = aw[nb % 2]; ra = Raw[nb % 2]; b_ = ab[nb % 2]; rb = Rab[nb % 2]
                    LD("sp", a[:], adaw_d[l].rearrange("(k p) n -> p k n", p=128)[:, :, nb * 512:(nb + 1) * 512], [], [ra])
                    LD("act", b_[:], adab_d[l:l + 1, nb * 512:(nb + 1) * 512].partition_broadcast(128), [], [rb])
                    pf = PF[nb % 2]; rp = RPF[nb % 2]
                    for kc in range(8):
                        MM(pf[:], csrep[:, kc, :], a[:, kc, :], kc == 0, kc == 7, [Rc, ra], [rp])
                    DV(lambda e: e.tensor_tensor(modb[:, nb * 512:(nb + 1) * 512], pf[:], b_[:], ALU.add), [rp, rb], [Rmod])
                for (o, gi) in ((1, 0), (4, 1)):
                    DV(lambda e: e.scalar_tensor_tensor(out=modb[:, o * D:(o + 1) * D], in0=modb[:, o * D:(o + 1) * D],
                                                        scalar=1.0, in1=gb[:, gi, :], op0=ALU.add, op1=ALU.mult),
                       [Rmod, Rgb], [Rmod])
                prod = sb(ph, "lprod", [128, 128]); Rpr = Region()
                DV(lambda e: e.tensor_tensor(prod[:].rearrange("p (a d) -> p a d", a=2),
                                             lamt[:].rearrange("p (a b d) -> p a b d", a=2, b=2)[:, :, 0, :],
                                             lamt[:].rearrange("p (a b d) -> p a b d", a=2, b=2)[:, :, 1, :], ALU.mult),
                   [Rlt], [Rpr])
                DV(lambda e: e.tensor_reduce(lamc[:, 1:3], prod[:].rearrange("p (a d) -> p a d", a=2), AX.X, ALU.add),
                   [Rpr], [Rlam])
                ACT(lamc[:, 1:3], lamc[:, 1:3], AF.Exp, [Rlam], [Rlam])
                DV(lambda e: e.tensor_tensor(lamc[:, 0:1], lamc[:, 2:3], lamc[:, 1:2], ALU.subtract), [Rlam], [Rlam])
                DV(lambda e: e.tensor_scalar(lamc[:, 0:1], lamc[:, 0:1], -lam_init, None, ALU.add), [Rlam], [Rlam])
                DV(lambda e: e.tensor_scalar(lamc[:, 3:4], pcol(l, 25), 1.0 - lam_init, None, ALU.mult), [Rc, Rlam], [Rlam])
                fw.barrier()
            if stop == '0':
                fw.halt = True
            SH1, A1, G1 = modb[:, 0:D], modb[:, D:2 * D], modb[:, 2 * D:3 * D]
            SH2, A2, G2 = modb[:, 3 * D:4 * D], modb[:, 4 * D:5 * D], modb[:, 5 * D:6 * D]

            def norm_mod(ph, xt, rx, Ax, Bx, hf, rhf, small, rsm):
                PL(lambda e: e.memset(small[:, 0:1], 0.0), [], [rsm])
                ACT(hf[:], xt[:], AF.Square, [rx, rsm], [rhf, rsm], accum_out=small[:, 0:1])
                DV(lambda e: e.tensor_scalar(small[:, 1:2], small[:, 0:1], 1.0 / D, 1e-6, ALU.mult, ALU.add), [rsm], [rsm])
                ACT(small[:, 1:2], small[:, 1:2], AF.Sqrt, [rsm], [rsm])
                DV(lambda e: e.reciprocal(small[:, 2:3], small[:, 1:2]), [rsm], [rsm])
                DV(lambda e: e.scalar_tensor_tensor(out=hf[:], in0=xt[:], scalar=small[:, 2:3], in1=Ax,
                                                    op0=ALU.mult, op1=ALU.mult), [rx, rsm, Rmod], [rhf])
                PL(lambda e: e.tensor_tensor(hf[:], hf[:], Bx, ALU.add), [rhf, Rmod], [rhf])

            with ExitStack() as ph:
                xt = [sb(ph, f"xt{i}", [128, D]) for i in range(2)]; Rx = [Region(), Region()]
                hf = [sb(ph, f"hf{i}", [128, D]) for i in range(2)]; Rhf = [Region(), Region()]
                hb = [sb(ph, f"hb{i}", [128, D], BF16) for i in range(2)]; Rhb = [Region(), Region()]
                sm = [sb(ph, f"sm{i}", [128, 4]) for i in range(2)]; Rsm = [Region(), Region()]
                hblk = [sb(ph, f"hblk{i}", [128, 8, 512], BF16) for i in range(2)]; Rhk = [Region(), Region()]
                for tt in range(NTT):
                    i = tt % 2
                    LD("sp", xt[i][:], xsrc[tt * 128:(tt + 1) * 128, :], [], [Rx[i]])
                    norm_mod(ph, xt[i], Rx[i], A1, SH1, hf[i], Rhf[i], sm[i], Rsm[i])
                    DV(lambda e: e.tensor_copy(hb[i][:], hf[i][:]), [Rhf[i]], [Rhb[i]])
                    for kc in range(8):
                        TR(PB[i][:, kc * 128:(kc + 1) * 128], hb[i][:, kc * 128:(kc + 1) * 128], identb[:], [Rhb[i], Rc], [RPB[i]])
                    tb = tt // 4; j = tt % 4; bi = tb % 2
                    ACT(hblk[bi][:, :, j * 128:(j + 1) * 128], PB[i][:].rearrange("p (k t) -> p k t", k=8), AF.Copy,
                        [RPB[i]], [Rhk[bi]])
                    if j == 3:
                        LD("sp", hT_d[:, :, tb * 512:(tb + 1) * 512].rearrange("k p t -> p k t"), hblk[bi][:], [Rhk[bi]], [])
                fw.barrier()
            if stop == 'A':
                fw.halt = True

            with ExitStack() as ph:
                wr_ = sb(ph, "w_r", [128, 8, 896], BF16); Rw = Region()
                fw.dma("pool", lambda e: e.dma_start(out=wr_[:], in_=win_d[l].rearrange("(k p) n -> p k n", p=128)[:, :, 0:896]), [], [Rw])
                lora = sb(ph, "lora", [128, 256]); Rlo = Region()
                LD("sp", lora[:], lora_d[l], [], [Rlo])
                hblk = [sb(ph, "hblk0", [128, 8, 512], BF16)] * 2; Rhk = [Region()] * 2
                P7 = sb(ph, "P7", [128, 7, 513]); RP7 = Region()
                D7 = sb(ph, "D7", [128, 7, 512]); RD7 = Region()
                PS7 = sb(ph, "PS7", [128, 7, 512]); RPS7 = Region()
                NT = 14
                T = [sb(ph, f"rt{i}", [128, 512]) for i in range(NT)]; RT = [Region() for _ in range(NT)]
                ARt = sb(ph, "ARt", [128, 4, 256], BF16); RAR = Region()
                ARm = [sb(ph, f"ARm{i}", [128, 4, 256], BF16) for i in range(2)]; RARm = [Region(), Region()]
                for j in range(2):
                    PL(lambda e: e.memset(ARm[j][:], 0.0), [], [RARm[j]])
                Bt = sb(ph, "Bt", [128, 512], BF16); RBt = Region()
                Kt = sb(ph, "Kt", [128, 512], BF16); RKt = Region()
                BHf = sb(ph, "BHf", [128, 512], BF16); KHf = sb(ph, "KHf", [128, 512], BF16); Vf = sb(ph, "Vf", [128, 512], BF16)
                RBH = Region(); RKH = Region(); RVf = Region()
                BHtm = sb(ph, "BHtm", [128, 4, 128], BF16); KHtm = sb(ph, "KHtm", [128, 4, 128], BF16)
                Vtm = sb(ph, "Vtm", [128, 4, 128], BF16); Rtm = Region()
                SB1s = [sb(ph, f"SB1s{i}", [128, 2, 512], BF16) for i in range(2)]; RSB1s = [Region(), Region()]
                SBAs = [sb(ph, f"SBAs{i}", [128, 2, 128], BF16) for i in range(2)]; RSBAs = [Region(), Region()]
                DEs = [[sb(ph, f"DE{s_}{i}", [128, 2, 2, 128], BF16) for i in range(2)] for s_ in range(2)]
                RDEs = [[Region(), Region()] for s_ in range(2)]
                ZZs = [sb(ph, f"ZZ{i}", [128, 2, 2, 128], BF16) for i in range(2)]; RZZs = [Region(), Region()]
                Gms = [sb(ph, f"Gm{i}", [128, 512]) for i in range(2)]; RGms = [Region(), Region()]
                ST = [sb(ph, f"ST{i}", [128, 64]) for i in range(2)]; RST = [Region(), Region()]
                STb = [sb(ph, f"STb{i}", [128, 64], BF16) for i in range(2)]; RSTb = [Region(), Region()]
                RHSb = sb(ph, "RHSb", [128, 128], BF16); RRH = Region()
                Ub = sb(ph, "Ub", [128, 128], BF16); RUb = Region()
                Ytm = sb(ph, "Ytm", [128, 4, 128]); RY = Region()
                gn = sb(ph, "gn", [128, 8, 4]); Rgn = Region()
                GC = sb(ph, "GC", [128, 4]); RGC = Region()
                mixo = sb(ph, "mixo", [128, 512], BF16); Rmx = Region()
                for hp in range(2):
                    PL(lambda e: e.memset(ST[hp][:], 0.0), [], [RST[hp]])
                    PL(lambda e: e.memset(STb[hp][:], 0.0), [], [RSTb[hp]])
                PL(lambda e: e.memset(P7[:, :, 0:1], 0.0), [], [RP7])

                for tb in range(NTB):
                    hb_ = hblk[tb % 2]; rh = Rhk[tb % 2]
                    LD("sp", hb_[:], hT_d[:, :, tb * 512:(tb + 1) * 512].rearrange("k p t -> p k t"), [], [rh])
                    if tb > 0:
                        DV(lambda e: e.tensor_copy(P7[:, :, 0:1], P7[:, :, 512:513]), [RP7], [RP7])
                    for cc in range(7):
                        pf = PF[cc % 2]; rp = RPF[cc % 2]
                        for kc in range(8):
                            MM(pf[:], wr_[:, kc, cc * 128:(cc + 1) * 128], hb_[:, kc, :], kc == 0, kc == 7, [Rw, rh], [rp])
                        ACT(P7[:, cc, 1:513], pf[:], AF.Copy, [rp], [RP7])
                    DV(lambda e: e.tensor_tensor(D7[:], P7[:, :, 0:512], P7[:, :, 1:513], ALU.subtract), [RP7], [RD7])
                    for cc in range(7):
                        DV(lambda e: e.scalar_tensor_tensor(out=PS7[:, cc, :], in0=D7[:, cc, :], scalar=pcol(l, cc),
                                                            in1=P7[:, cc, 1:513], op0=ALU.mult, op1=ALU.add),
                           [RD7, RP7, Rc], [RPS7])
                    ACT(PS7[0:32, 6, :], PS7[0:32, 6, :], AF.Tanh, [RPS7], [RPS7])
                    ACT(PS7[64:128, 6, :], PS7[64:128, 6, :], AF.Sigmoid, [RPS7], [RPS7])
                    if stop == 'B1':
                        fw.halt = True
                    for hp in range(2):
                        rs, ks, vs = PS7[:, hp, :], PS7[:, 2 + hp, :], PS7[:, 4 + hp, :]
                        cs_ = slice(hp * 128, (hp + 1) * 128)
                        sg, av, gv, kk, sq, kkn, k2, lw, cum, e1, e2, e3, e4, bon = T
                        Rsg, Rav, Rgv, Rkk, Rsq, Rkkn, Rk2, Rlw, Rcum, Re1, Re2, Re3, Re4, Rbon = RT
                        MM(PF[2][:], lora[0:32, cs_], PS7[0:32, 6, :], True, True, [Rlo, RPS7], [RPF[2]])
                        ACT(sg[:], PF[2][:], AF.Sigmoid, [RPF[2], Rc], [Rsg], bias=pcol(l, 7 + hp))
                        MM(PF[3][:], lora[32:64, cs_], PS7[32:64, 6, :], True, True, [Rlo, RPS7], [RPF[3]])
                        ACT(av[:], PF[3][:], AF.Sigmoid, [RPF[3], Rc], [Rav], bias=pcol(l, 9 + hp))
                        MM(PF[2][:], lora[64:128, cs_], PS7[64:128, 6, :], True, True, [Rlo, RPS7], [RPF[2]])
                        ACT(gv[:], PF[2][:], AF.Copy, [RPF[2]], [Rgv])
                        DV(lambda e: e.tensor_scalar(kk[:], ks, pcol(l, 11 + hp), None, ALU.mult), [RPS7, Rc], [Rkk])
                        PL(lambda e: e.tensor_tensor(sq[:], kk[:], kk[:], ALU.mult), [Rkk], [Rsq])
                        MM(PF[3][:], bdones[:], sq[:], True, True, [Rc, Rsq], [RPF[3]])
                        ACT(sq[:], PF[3][:], AF.Sqrt, [RPF[3]], [Rsq])
                        DV(lambda e: e.tensor_scalar(sq[:], sq[:], 1e-12, None, ALU.max), [Rsq], [Rsq])
                        DV(lambda e: e.reciprocal(sq[:], sq[:]), [Rsq], [Rsq])
                        DV(lambda e: e.tensor_tensor(kkn[:], kk[:], sq[:], ALU.mult), [Rkk, Rsq], [Rkkn])
                        DV(lambda e: e.tensor_scalar(k2[:], av[:], 1.0, pcol(l, 13 + hp), ALU.subtract, ALU.mult), [Rav, Rc], [Rk2])
                        DV(lambda e: e.scalar_tensor_tensor(out=k2[:], in0=k2[:], scalar=1.0, in1=ks, op0=ALU.add, op1=ALU.mult),
                           [Rk2, RPS7], [Rk2])
                        PL(lambda e: e.tensor_tensor(kk[:], rs, k2[:], ALU.mult), [RPS7, Rk2, Rkkn], [Rkk])
                        DV(lambda e: e.tensor_scalar(kk[:], kk[:], pcol(l, 15 + hp), None, ALU.mult), [Rkk, Rc], [Rkk])
                        MM(PF[2][:], bdones[:], kk[:], True, True, [Rc, Rkk], [RPF[2]])
                        DV(lambda e: e.tensor_tensor(bon[:], PF[2][:], vs, ALU.mult), [RPF[2], RPS7], [Rbon])
                        DV(lambda e: e.tensor_scalar(lw[:], sg[:], -0.6065306597126334, None, ALU.mult), [Rsg], [Rlw])
                        DV(lambda e: e.tensor_tensor_scan(cum[:], rmask[:], lw[:], 0.0, ALU.mult, ALU.add), [Rc, Rlw], [Rcum])
                        cum3 = cum[:].rearrange("p (c t) -> p c t", t=128)
                        ACT(e1[:], cum[:], AF.Exp, [Rcum], [Re1])
                        ACT(e2[:], cum[:], AF.Exp, [Rcum], [Re2], scale=-1.0)
                        DV(lambda e: e.tensor_tensor(e3[:], cum[:], lw[:], ALU.subtract), [Rcum, Rlw], [Re3])
                        ACT(e3[:], e3[:], AF.Exp, [Re3], [Re3])
                        DV(lambda e: e.tensor_tensor(e4[:].rearrange("p (c t) -> p c t", t=128),
                                                     cum3[:, :, 127:128].to_broadcast([128, 4, 128]), cum3, ALU.subtract),
                           [Rcum], [Re4])
                        ACT(e4[:], e4[:], AF.Exp, [Re4], [Re4])
                        ACT(GC[:].rearrange("p (c o) -> p c o", o=1), cum3[:, :, 127:128], AF.Exp, [Rcum], [RGC])
                        AR3 = ARt[:]
                        DV(lambda e: e.scalar_tensor_tensor(out=AR3[:, :, 0:128], in0=kkn[:].rearrange("p (c t) -> p c t", t=128),
                                                            scalar=-1.0, in1=e3[:].rearrange("p (c t) -> p c t", t=128),
                                                            op0=ALU.mult, op1=ALU.mult), [Rkkn, Re3], [RAR])
                        PL(lambda e: e.tensor_tensor(AR3[:, :, 128:256], PS7[:, hp, :].rearrange("p (c t) -> p c t", t=128),
                                                     e1[:].rearrange("p (c t) -> p c t", t=128), ALU.mult), [RPS7, Re1], [RAR])
                        ACT(ARm[0][0:64, :, :], ARt[0:64, :, :], AF.Copy, [RAR], [RARm[0]])
                        PL(lambda e: e.tensor_copy(ARm[1][64:128, :, :], ARt[64:128, :, :]), [RAR], [RARm[1]])
                        DV(lambda e: e.tensor_tensor(kkn[:], kkn[:], av[:], ALU.mult), [Rkkn, Rav, RAR], [Rkkn])
                        DV(lambda e: e.tensor_tensor(Bt[:], kkn[:], e2[:], ALU.mult), [Rkkn, Re2], [RBt])
                        PL(lambda e: e.tensor_tensor(BHf[:], kkn[:], e4[:], ALU.mult), [Rkkn, Re4], [RBH])
                        DV(lambda e: e.tensor_tensor(Kt[:], k2[:], e2[:], ALU.mult), [Rk2, Re2], [RKt])
                        PL(lambda e: e.tensor_tensor(KHf[:], k2[:], e4[:], ALU.mult), [Rk2, Re4], [RKH])
                        ACT(Vf[:], vs, AF.Copy, [RPS7], [RVf])
                        for c in range(4):
                            TR(PB[0][:, c * 128:(c + 1) * 128], BHf[:, c * 128:(c + 1) * 128], identb[:], [RBH, Rc], [RPB[0]])
                            TR(PB[0][:, 512 + c * 128:512 + (c + 1) * 128], KHf[:, c * 128:(c + 1) * 128], identb[:], [RKH, Rc], [RPB[0]])
                            TR(PB[1][:, c * 128:(c + 1) * 128], Vf[:, c * 128:(c + 1) * 128], identb[:], [RVf, Rc], [RPB[1]])
                        ACT(BHtm[:], PB[0][:, 0:512].rearrange("p (c t) -> p c t", t=128), AF.Copy, [RPB[0]], [Rtm])
                        DV(lambda e: e.tensor_copy(KHtm[:], PB[0][:, 512:1024].rearrange("p (c t) -> p c t", t=128)), [RPB[0]], [Rtm])
                        ACT(Vtm[:], PB[1][:, 0:512].rearrange("p (c t) -> p c t", t=128), AF.Copy, [RPB[1]], [Rtm])
                        if stop == 'B2':
                            fw.halt = True
                        for c0 in (0, 2):
                            for s_ in range(2):
                                c = c0 + s_
                                cl = slice(c * 128, (c + 1) * 128)
                                b0 = 0
                                pa = PF[4]; rpa = RPF[4]
                                for j in range(2):
                                    pf = PF[b0 + j]; rpf_ = RPF[b0 + j]
                                    MM(pf[:, 0:256], Bt[:, cl], ARm[j][:, c, :], True, True, [RBt, RARm[j]], [rpf_])
                                    MM(pf[:, 256:512], Kt[:, cl], ARm[j][:, c, :], True, True, [RKt, RARm[j]], [rpf_])
                                    MM(pa[:, j * 128:(j + 1) * 128], ARm[j][:, c, 0:128], Bt[:, cl], True, True, [RARm[j], RBt], [rpa])
                                for j in range(2):
                                    DV(lambda e: e.tensor_tensor(SB1s[s_][:, j, :], PF[b0 + j][:], m4[:], ALU.mult), [RPF[b0 + j], Rc], [RSB1s[s_]])
                                for j in range(2):
                                    PL(lambda e: e.tensor_copy(DEs[s_][0][:, j, 0, :], identb[:]), [Rc], [RDEs[s_][0]])
                                    PL(lambda e: e.tensor_copy(DEs[s_][0][:, j, 1, :], identb[:]), [Rc], [RDEs[s_][0]])
                                    DV(lambda e: e.tensor_tensor(SBAs[s_][:, j, :], pa[:, j * 128:(j + 1) * 128], msl[:], ALU.mult),
                                       [rpa, Rc], [RSBAs[s_]])
                            wi = 0
                            for k in range(7):
                                for s_ in range(2):
                                    pz = PF[4 * s_]; rpz = RPF[4 * s_]
                                    for j in range(2):
                                        MM(pz[:, j * 256:j * 256 + 128], SB1s[s_][:, j, 0:128], DEs[s_][wi][:, j, 0, :], True, True,
                                           [RSB1s[s_], RDEs[s_][wi]], [rpz])
                                        MM(pz[:, j * 256 + 128:j * 256 + 256], SBAs[s_][:, j, :], DEs[s_][wi][:, j, 1, :], True, True,
                                           [RSBAs[s_], RDEs[s_][wi]], [rpz])
                                for s_ in range(2):
                                    ACT(ZZs[s_][:].rearrange("p j z t -> p (j z t)"), PF[4 * s_][:], AF.Copy, [RPF[4 * s_]], [RZZs[s_]])
                                for s_ in range(2):
                                    pg = PF[4 * s_ + 1]; rpg = RPF[4 * s_ + 1]
                                    for j in range(2):
                                        MM(pg[:, j * 256:j * 256 + 128], DEs[s_][wi][:, j, 1, :], ZZs[s_][:, j, 0, :], True, True,
                                           [RDEs[s_][wi], RZZs[s_]], [rpg])
                                        MM(pg[:, j * 256 + 128:j * 256 + 256], DEs[s_][wi][:, j, 0, :], ZZs[s_][:, j, 1, :], True, True,
                                           [RDEs[s_][wi], RZZs[s_]], [rpg])
                                for s_ in range(2):
                                    DV(lambda e: e.tensor_tensor(Gms[s_][:], PF[4 * s_ + 1][:], lvlmask[:, k, :], ALU.mult),
                                       [RPF[4 * s_ + 1], Rc], [RGms[s_]])
                                for s_ in range(2):
                                    PL(lambda e: e.tensor_tensor(DEs[s_][1 - wi][:].rearrange("p j z t -> p (j z t)"), Gms[s_][:],
                                                                 DEs[s_][wi][:].rearrange("p j z t -> p (j z t)"), ALU.add),
                                       [RGms[s_], RDEs[s_][wi]], [RDEs[s_][1 - wi]])
                                wi = 1 - wi
                            for s_ in range(2):
                                c = c0 + s_
                                SB1 = SB1s[s_]; RSB1 = RSB1s[s_]
                                W_ = DEs[s_][wi]; RW_ = RDEs[s_][wi]
                                pr = PF[5]
                                for j in range(2):
                                    vj = slice(j * 64, (j + 1) * 64)
                                    MM(pr[:, vj], ARm[j][:, c, 0:128], STb[hp][:, :], True, False, [RARm[j], RSTb[hp]], [RPF[5]])
                                    MM(pr[:, vj], SB1[:, j, 256:384], Vtm[:, c, vj], False, True, [RSB1, Rtm], [RPF[5]])
                                ACT(RHSb[:], pr[:, 0:128], AF.Copy, [RPF[5]], [RRH])
                                pu = PF[4]
                                for j in range(2):
                                    vj = slice(j * 64, (j + 1) * 64)
                                    MM(pu[:, 256 + j * 64:256 + (j + 1) * 64], W_[:, j, 1, :], RHSb[:, vj], True, True, [RW_, RRH], [RPF[4]])
                                DV(lambda e: e.tensor_copy(Ub[:], pu[:, 256:384]), [RPF[4]], [RUb])
                                py = PF[5]
                                for j in range(2):
                                    vj = slice(j * 64, (j + 1) * 64)
                                    yo = py[:, 128 + j * 64:128 + (j + 1) * 64]
                                    MM(yo, ARm[j][:, c, 128:256], STb[hp][:, :], True, False, [RARm[j], RSTb[hp]], [RPF[5]])
                                    MM(yo, SB1[:, j, 128:256], Ub[:, vj], False, False, [RSB1, RUb], [RPF[5]])
                                    MM(yo, SB1[:, j, 384:512], Vtm[:, c, vj], False, True, [RSB1, Rtm], [RPF[5]])
                                ACT(Ytm[:, c, :], py[:, 128:256], AF.Copy, [RPF[5]], [RY])
                                pss = PF[5]
                                for j in range(2):
                                    R_ = slice(j * 64, (j + 1) * 64); vj = slice(j * 64, (j + 1) * 64)
                                    so = pss[R_, 256:320]
                                    MM(so, BHtm[:, c, R_], Ub[:, vj], True, False, [Rtm, RUb], [RPF[5]])
                                    MM(so, KHtm[:, c, R_], Vtm[:, c, vj], False, True, [Rtm], [RPF[5]])
                                DV(lambda e: e.scalar_tensor_tensor(out=ST[hp][:], in0=ST[hp][:], scalar=GC[:, c:c + 1], in1=pss[:, 256:320],
                                                                    op0=ALU.mult, op1=ALU.add), [RST[hp], RGC, RPF[5]], [RST[hp]])
                                ACT(STb[hp][:], ST[hp][:], AF.Copy, [RST[hp]], [RSTb[hp]])
                        Y8 = Ytm[:].rearrange("p c (j v) -> p (c j) v", j=2)
                        DV(lambda e: e.tensor_reduce(gn[:, :, 0], Y8, AX.X, ALU.add), [RY], [Rgn])
                        DV(lambda e: e.tensor_scalar(gn[:, :, 0], gn[:, :, 0], 1.0 / 64, None, ALU.mult), [Rgn], [Rgn])
                        DV(lambda e: e.tensor_tensor(Y8, Y8, gn[:, :, 0:1].to_broadcast([128, 8, 64]), ALU.subtract), [RY, Rgn], [RY])
                        Ysq = sq[:].rearrange("p (a v) -> p a v", v=64)
                        PL(lambda e: e.tensor_tensor(Ysq, Y8, Y8, ALU.mult), [RY, Rsq], [Rsq])
                        DV(lambda e: e.tensor_reduce(gn[:, :, 1], Ysq, AX.X, ALU.add), [Rsq], [Rgn])
                        DV(lambda e: e.tensor_scalar(gn[:, :, 1], gn[:, :, 1], 1.0 / 64, 64e-5, ALU.mult, ALU.add), [Rgn], [Rgn])
                        ACT(gn[:, :, 1], gn[:, :, 1], AF.Sqrt, [Rgn], [Rgn])
                        DV(lambda e: e.reciprocal(gn[:, :, 2], gn[:, :, 1]), [Rgn], [Rgn])
                        DV(lambda e: e.tensor_tensor(Y8, Y8, gn[:, :, 2:3].to_broadcast([128, 8, 64]), ALU.mult), [RY, Rgn], [RY])
                        if stop == 'B5':
                            fw.halt = True
                        for c in range(4):
                            TR(PF[3][:, c * 128:(c + 1) * 128], Ytm[:, c, :], ident[:], [RY, Rc], [RPF[3]])
                        DV(lambda e: e.tensor_scalar(e1[:], PF[3][:], pcol(l, 17 + hp), pcol(l, 19 + hp), ALU.mult, ALU.add),
                           [RPF[3], Rc, RAR], [Re1])
                        DV(lambda e: e.tensor_tensor(e1[:], e1[:], bon[:], ALU.add), [Re1, Rbon], [Re1])
                        DV(lambda e: e.tensor_tensor(mixo[:], e1[:], gv[:], ALU.mult), [Re1, Rgv], [Rmx])
                        LD("sp", mixT_d[hp, :, tb * 512:(tb + 1) * 512], mixo[:], [Rmx], [])
                        if stop == 'B6':
                            fw.halt = True
                        if stop == 'B7' and hp == 1:
                            fw.halt = True
                        if stop == 'B8' and hp == 1 and tb == 1:
                            fw.halt = True
                fw.barrier()
            if stop == 'B':
                fw.halt = True

            with ExitStack() as ph:
                hblk = [sb(ph, f"hblk{i}", [128, 8, 512], BF16) for i in range(2)]; Rhk = [Region(), Region()]
                wa = [sb(ph, f"wa{i}", [128, 8, 384], BF16) for i in range(2)]; Rwa = [Region(), Region()]
                QT = sb(ph, "QT", [128, S], BF16); KT = sb(ph, "KT", [128, S], BF16); RQ = Region(); RK = Region()
                QT1 = sb(ph, "QT1", [128, S], BF16)
                PL(lambda e: e.memset(QT[:], 0.0), [], [RQ])
                PL(lambda e: e.memset(QT1[:], 0.0), [], [RQ])
                Vt = sb(ph, "Vt", [128, NTT, 128], BF16); RV = Region()
                qf = sb(ph, "qf", [128, 512]); Rqf = Region()
                sqf = sb(ph, "sqf", [128, 512]); Rsqf = Region()
                rsf = sb(ph, "rsf", [128, 512]); Rrsf = Region()
                Pf = [sb(ph, f"Pf{i}", [128, 512]) for i in range(2)]; RPf = [Region(), Region()]
                Pb = [sb(ph, f"Pb{i}", [128, 512], BF16) for i in range(3)]; RPb = [Region() for _ in range(3)]
                rec = sb(ph, "rec", [128, 2, 512]); Rrec = Region()
                bcs = sb(ph, "bcs", [128, 2, 512]); Rbcs = Region()
                Of = sb(ph, "Of", [128, 512]); ROf = Region()
                Ob = sb(ph, "Ob", [128, 512], BF16); ROb = Region()
                kmT = sb(ph, "kmT", [128, 16]); Rkm = Region()
                gm = sb(ph, "gm", [128, 16]); top8 = sb(ph, "top8", [128, 8]); Rgm = Region()
                nmw = sb(ph, "nmw", [128, 4, 80]); Rnm = Region()
                PL(lambda e: e.memset(nmw[:], 0.0), [], [Rnm])
                pbi = 0

                def proj_fm(hb_, rh, w, rw, c0, M, pf, rp):
                    for kc in range(8):
                        MM(pf[0:M, :], w[:, kc, c0:c0 + M], hb_[:, kc, :], kc == 0, kc == 7, [rw, rh], [rp])

                def headnorm(pf, rp, M, gcol, dst, rdst, dst2=None):
                    ACT(sqf[0:M, :], pf[0:M, :], AF.Square, [rp], [Rsqf])
                    MM(PF[2][0:M, :], bdones[0:M, 0:M], sqf[0:M, :], True, True, [Rc, Rsqf], [RPF[2]])
                    DV(lambda e: e.tensor_scalar(rsf[0:M, :], PF[2][0:M, :], 1.0 / 64, 1e-6, ALU.mult, ALU.add), [RPF[2]], [Rrsf])
                    ACT(rsf[0:M, :], rsf[0:M, :], AF.Sqrt, [Rrsf], [Rrsf])
                    DV(lambda e: e.reciprocal(rsf[0:M, :], rsf[0:M, :]), [Rrsf], [Rrsf])
                    DV(lambda e: e.scalar_tensor_tensor(out=qf[0:M, :], in0=pf[0:M, :], scalar=gcol[0:M, :], in1=rsf[0:M, :],
                                                        op0=ALU.mult, op1=ALU.mult), [rp, Rc, Rrsf], [Rqf])
                    if dst2 is None:
                        ACT(dst, qf[0:M, :], AF.Copy, [Rqf], [rdst])
                    else:
                        ACT(dst, qf[0:64, :], AF.Copy, [Rqf], [rdst])
                        PL(lambda e: e.tensor_copy(dst2, qf[64:128, :]), [Rqf], [rdst])

                for hd in range(4):
                    moba = hd >= 2
                    h = hd % 2
                    w = wa[hd % 2]; rw = Rwa[hd % 2]
                    win3 = wa_d[l].rearrange("(k p) n -> p k n", p=128)
                    nn_ = 64 if moba else 128
                    cols = [(hd * 384 + i * 128, nn_) for i in range(3)]
                    for i, (c0, n) in enumerate(cols):
                        fw.dma("pool", lambda e: e.dma_start(out=w[:, :, i * 128:i * 128 + n], in_=win3[:, :, c0:c0 + n]), [], [rw])
                    M = 64 if moba else 128
                    dv = 64 if moba else 128
                    gq = pcol(l, 23 if moba else 21); gk = pcol(l, 24 if moba else 22)
                    if moba:
                        if hd == 2:
                            PL(lambda e: e.memset(KT[64:128, :], 0.0), [], [RK])
                        LD("sp", KT[64:80, :], cd["onehotk"], [], [RK])
                        PL(lambda e: e.memset(kmT[:], 0.0), [], [Rkm])
                        PL(lambda e: e.memset(Vt[:, :, 64:65], 1.0), [], [RV])
                    for tb in range(NTB):
                        hb_ = hblk[tb % 2]; rh = Rhk[tb % 2]
                        LD("sp", hb_[:], hT_d[:, :, tb * 512:(tb + 1) * 512].rearrange("k p t -> p k t"), [], [rh])
                        tsl = slice(tb * 512, (tb + 1) * 512)
                        proj_fm(hb_, rh, w, rw, 128, M, PF[0], RPF[0])
                        headnorm(PF[0], RPF[0], M, gk, KT[0:M, tsl], RK)
                        if moba:
                            DV(lambda e: e.tensor_reduce(kmT[0:64, 2 * tb:2 * tb + 2], qf[0:64, :].rearrange("p (a t) -> p a t", a=2),
                                                         AX.X, ALU.add), [Rqf], [Rkm])
                            DV(lambda e: e.tensor_scalar(kmT[0:64, 2 * tb:2 * tb + 2], kmT[0:64, 2 * tb:2 * tb + 2], 1.0 / 256, None, ALU.mult),
                               [Rkm], [Rkm])
                        proj_fm(hb_, rh, w, rw, 0, M, PF[1], RPF[1])
                        if moba:
                            headnorm(PF[1], RPF[1], M, gq, QT[0:M, tsl], RQ)
                        else:
                            headnorm(PF[1], RPF[1], M, gq, QT[0:64, tsl], RQ, dst2=QT1[64:128, tsl])
                        for j in range(4):
                            for kc in range(8):
                                MM(PF[3][:, j * 128:j * 128 + dv], hb_[:, kc, j * 128:(j + 1) * 128], w[:, kc, 256:256 + dv],
                                   kc == 0, kc == 7, [rh, rw], [RPF[3]])
                        ACT(Vt[:, tb * 4:(tb + 1) * 4, 0:dv], PF[3][:].rearrange("p (j t) -> p j t", j=4)[:, :, 0:dv], AF.Copy, [RPF[3]], [RV])
                        if moba:
                            for j in range(4):
                                qt = tb * 4 + j
                                MM(PF[4][:, j * 16:(j + 1) * 16], qf[0:64, j * 128:(j + 1) * 128], kmT[0:64, :], True, True, [Rqf, Rkm], [RPF[4]])
                                DV(lambda e: e.tensor_tensor(gm[:], PF[4][:, j * 16:(j + 1) * 16], negpast[:, qt * 16:(qt + 1) * 16], ALU.add),
                                   [RPF[4], Rc], [Rgm])
                                DV(lambda e: e.max(out=top8[:], in_=gm[:]), [Rgm], [Rgm])
                                DV(lambda e: e.tensor_scalar(gm[:], gm[:], top8[:, 2:3], None, ALU.is_ge), [Rgm], [Rgm])
                                DV(lambda e: e.scalar_tensor_tensor(out=nmw[:, j, 64:80], in0=gm[:], scalar=1.0, in1=past30[:, qt * 16:(qt + 1) * 16],
                                                                    op0=ALU.subtract, op1=ALU.mult), [Rgm, Rc], [Rnm])
                                TR(PF[5][0:80, j * 128:(j + 1) * 128], nmw[:, j, :], ident[:], [Rnm, Rc], [RPF[5]])
                            ACT(QT[64:80, tsl], PF[5][64:80, :], AF.Copy, [RPF[5]], [RQ])
                    KK = 80 if moba else 64
                    for qb in range(NTB):
                        qsl = slice(qb * 512, (qb + 1) * 512)
                        nmap = 1 if moba else 2
                        nkt = 4 * qb + 4
                        items = [(m, kt) for m in range(nmap) for kt in range(nkt)]

                        def emit_qk(idx):
                            m_, kt_ = items[idx]
                            qsrc = QT1 if m_ == 1 else QT
                            MM(PF[idx % 2][:], KT[:, kt_ * 128:(kt_ + 1) * 128], qsrc[:, qsl], True, True, [RK, RQ], [RPF[idx % 2]])

                        emit_qk(0)
                        for idx, (m, kt) in enumerate(items):
                            if idx + 1 < len(items):
                                emit_qk(idx + 1)
                            ps = PF[idx % 2]; rps = RPF[idx % 2]
                            po = PF[2 + m]; rpo = RPF[2 + m]
                            o0 = 4 * qb - kt
                            pb = Pb[pbi % 3]; rpb = RPb[pbi % 3]; pbi += 1
                            b31 = tblb[:, 31 * 4 + hd: 31 * 4 + hd + 1]
                            if o0 <= 7:
                                pfx = Pf[idx % 2]; rpf = RPf[idx % 2]
                                ACT(pfx[:], ps[:], AF.Exp, [rps, Rc], [rpf], bias=b31, scale=0.125)
                                DV(lambda e: e.tensor_tensor(pb[:], pfx[:], erel[:, hd, (o0 + 3) * 128:(o0 + 3) * 128 + 512], ALU.mult),
                                   [rpf, Rerel], [rpb])
                            else:
                                ACT(pb[:], ps[:], AF.Exp, [rps, Rc], [rpb], bias=b31, scale=0.125)
                            if moba:
                                MM(po[0:65, :], Vt[:, kt, 0:65], pb[:], kt == 0, kt == nkt - 1, [RV, rpb], [rpo])
                            else:
                                MM(po[:], Vt[:, kt, :], pb[:], kt == 0, kt == nkt - 1, [RV, rpb], [rpo])
                                MM(PF[4 + m][0:1, :], onesb[:, 0:1], pb[:], kt == 0, kt == nkt - 1, [Rc, rpb], [RPF[4 + m]])
                        if moba:
                            DV(lambda e: e.reciprocal(rec[64:65, 0, :], PF[2][64:65, :]), [RPF[2]], [Rrec])
                            MM(PF[0][0:64, :], onesf[64:65, 0:64], rec[64:65, 0, :], True, True, [Rc, Rrec], [RPF[0]])
                            ACT(bcs[0:64, 0, :], PF[0][0:64, :], AF.Copy, [RPF[0]], [Rbcs])
                            DV(lambda e: e.tensor_tensor(Ob[0:64, :], PF[2][0:64, :], bcs[0:64, 0, :], ALU.mult), [RPF[2], Rbcs], [ROb])
                            LD("sp", Mine_b[4 + h][:, qsl], Ob[0:64, :], [ROb], [])
                        else:
                            DV(lambda e: e.reciprocal(rec[0:1, 0, :], PF[4][0:1, :]), [RPF[4]], [Rrec])
                            DV(lambda e: e.reciprocal(rec[0:1, 1, :], PF[5][0:1, :]), [RPF[5]], [Rrec])
                            DV(lambda e: e.tensor_scalar(rec[0:1, 1, :], rec[0:1, 1, :], lamc[0:1, 0:1], None, ALU.mult), [Rrec, Rlam], [Rrec])
                            for m in range(2):
                                MM(PF[m][:], onesf[0:1, :], rec[0:1, m, :], True, True, [Rc, Rrec], [RPF[m]])
                                ACT(bcs[:, m, :], PF[m][:], AF.Copy, [RPF[m]], [Rbcs])
                            DV(lambda e: e.tensor_tensor(Of[:], PF[2][:], bcs[:, 0, :], ALU.mult), [RPF[2], Rbcs], [ROf])
                            DV(lambda e: e.tensor_tensor(sqf[:], PF[3][:], bcs[:, 1, :], ALU.mult), [RPF[3], Rbcs], [Rsqf])
                            DV(lambda e: e.tensor_tensor(Of[:], Of[:], sqf[:], ALU.add), [ROf, Rsqf], [ROf])
                            ACT(sqf[:], Of[:], AF.Square, [ROf], [Rsqf])
                            MM(PF[0][:], onesf[:], sqf[:], True, True, [Rc, Rsqf], [RPF[0]])
                            DV(lambda e: e.tensor_scalar(rsf[:], PF[0][:], 1.0 / 128, 1e-6, ALU.mult, ALU.add), [RPF[0]], [Rrsf])
                            ACT(rsf[:], rsf[:], AF.Sqrt, [Rrsf], [Rrsf])
                            DV(lambda e: e.reciprocal(rsf[:], rsf[:]), [Rrsf], [Rrsf])
                            DV(lambda e: e.scalar_tensor_tensor(out=Ob[:], in0=Of[:], scalar=lamc[:, 3:4], in1=rsf[:], op0=ALU.mult, op1=ALU.mult),
                               [ROf, Rlam, Rrsf], [ROb])
                            LD("sp", Mine_b[2 * h][:, qsl], Ob[0:64, :], [ROb], [])
                            LD("sp", Mine_b[2 * h + 1][:, qsl], Ob[64:128, :], [ROb], [])
                fw.barrier()
            if stop == 'C':
                fw.halt = True
            if not fw.halt:
                groups = [[2 * g, 2 * g + 1] for g in range(ncores // 2)]
                for i_ in range(6):
                    nc.gpsimd.collective_compute("AllGather", ALU.bypass, replica_groups=groups, ins=[Mine_f[i_]], outs=[G_f[i_]]).then_inc(ccsem, 1)
                nc.gpsimd.wait_ge(ccsem, 6 * (l + 1))
                PL(lambda e: e.memset(ccdummy[:], 0.0), [], [Rccd])
                fw.barrier()

            CAPl = CAPS[l]
            NROW = 32 * CAPl
            with ExitStack() as ph:
                wo = sb(ph, "wo", [128, 8, D], BF16); Rwo = Region()
                fw.dma("pool", lambda e: e.dma_start(out=wo[:], in_=wout_d[l].rearrange("(k p) n -> p k n", p=128)), [], [Rwo])
                wrt = sb(ph, "wrt", [128, 8, 36]); brb = sb(ph, "brb", [128, 36]); Rwr = Region()
                LD("sp", wrt[:], wr_d[l].rearrange("(k p) n -> p k n", p=128), [], [Rwr])
                LD("sp", brb[:], br_d[l:l + 1, :].partition_broadcast(128), [], [Rwr])
                mblk = [sb(ph, f"mblk{i}", [128, 8, 512], BF16) for i in range(2)]; Rmb = [Region(), Region()]
                xt = [sb(ph, f"xt{i}", [128, D]) for i in range(2)]; Rx = [Region(), Region()]
                hf = [sb(ph, f"hf{i}", [128, D]) for i in range(2)]; Rhf = [Region(), Region()]
                h2b = [sb(ph, f"h2b{i}", [128, D], BF16) for i in range(2)]; Rhb = [Region(), Region()]
                sm = [sb(ph, f"sm{i}", [128, 4]) for i in range(2)]; Rsm = [Region(), Region()]
                h2T = sb(ph, "h2T", [128, 8, 128]); RhT = Region()
                lg = sb(ph, "lg", [128, 36]); ml = sb(ph, "ml", [128, 32]); oh = sb(ph, "oh", [128, 2, 32])
                rt8 = sb(ph, "rt8", [128, 8]); rs_ = sb(ph, "rs_", [128, 16]); Rr = Region()
                cntb = sb(ph, "cntb", [128, 32]); Rcnt = Region()
                io32 = sb(ph, "io32", [128, 32]); posf = sb(ph, "posf", [128, 32]); msk = sb(ph, "msk", [128, 32])
                tmp32 = sb(ph, "tmp32", [128, 32]); dst = sb(ph, "dstf", [128, 2])
                PL(lambda e: e.memset(cntb[:], 0.0), [], [Rcnt])
                PL(lambda e: e.iota(io32[:], pattern=[[1, 32]], base=0, channel_multiplier=0, allow_small_or_imprecise_dtypes=True), [], [Rr])
                for tt in range(NTT):
                    i = tt % 2; tb = tt // 4; j = tt % 4
                    if j == 0:
                        tsl_ = slice(tb * 512, (tb + 1) * 512)
                        LD("sp", mblk[tb % 2][:, 0:2, :], mixT_d[0:2, :, tsl_].rearrange("k p t -> p k t"), [], [Rmb[tb % 2]])
                        for r_ in range(2):
                            rr = slice(r_ * 64, (r_ + 1) * 64)
                            for lh_ in range(2):
                                LD("sp", mblk[tb % 2][0:64, 2 + 2 * r_ + lh_, :], G_b[2 * lh_][rr, tsl_], [], [Rmb[tb % 2]])
                                LD("act", mblk[tb % 2][64:128, 2 + 2 * r_ + lh_, :], G_b[2 * lh_ + 1][rr, tsl_], [], [Rmb[tb % 2]])
                            LD("sp", mblk[tb % 2][0:64, 6 + r_, :], G_b[4][rr, tsl_], [], [Rmb[tb % 2]])
                            LD("act", mblk[tb % 2][64:128, 6 + r_, :], G_b[5][rr, tsl_], [], [Rmb[tb % 2]])
                    mb = mblk[tb % 2]; rmb = Rmb[tb % 2]
                    LD("sp", xt[i][:], xsrc[tt * 128:(tt + 1) * 128, :], [], [Rx[i]])
                    for half in range(2):
                        for kc in range(8):
                            MM(PF[half][:], mb[:, kc, j * 128:(j + 1) * 128], wo[:, kc, half * 512:(half + 1) * 512], kc == 0, kc == 7,
                               [rmb, Rwo], [RPF[half]])
                        hs = slice(half * 512, (half + 1) * 512)
                        DV(lambda e: e.tensor_tensor(hf[i][:, hs], PF[half][:], G1[:, hs], ALU.mult), [RPF[half], Rmod], [Rhf[i]])
                    PL(lambda e: e.tensor_tensor(xt[i][:], xt[i][:], hf[i][:], ALU.add), [Rx[i], Rhf[i]], [Rx[i]])
                    LD("sp", xr_d[tt * 128:(tt + 1) * 128, :], xt[i][:], [Rx[i]], [])
                    norm_mod(ph, xt[i], Rx[i], A2, SH2, hf[i], Rhf[i], sm[i], Rsm[i])
                    ACT(h2b[i][:], hf[i][:], AF.Copy, [Rhf[i]], [Rhb[i]])
                    for kc in range(8):
                        TR(PF[2 + kc // 4][:, (kc % 4) * 128:(kc % 4 + 1) * 128], hf[i][:, kc * 128:(kc + 1) * 128], ident[:], [Rhf[i], Rc],
                           [RPF[2 + kc // 4]])
                    ACT(h2T[:, 0:4, :], PF[2][:].rearrange("p (k t) -> p k t", k=4), AF.Copy, [RPF[2]], [RhT])
                    DV(lambda e: e.tensor_copy(h2T[:, 4:8, :], PF[3][:].rearrange("p (k t) -> p k t", k=4)), [RPF[3]], [RhT])
                    for kc in range(8):
                        MM(PF[4][:, 0:36], h2T[:, kc, :], wrt[:, kc, :], kc == 0, kc == 7, [RhT, Rwr], [RPF[4]])
                    DV(lambda e: e.tensor_tensor(lg[:], PF[4][:, 0:36], brb[:], ALU.add), [RPF[4], Rwr], [Rr])
                    DV(lambda e: e.tensor_reduce(rs_[:, 0:1], lg[:, 0:4], AX.X, ALU.max), [Rr], [Rr])
                    DV(lambda e: e.tensor_scalar(rs_[:, 1:2], rs_[:, 0:1], -1.0, None, ALU.mult), [Rr], [Rr])
                    PL(lambda e: e.memset(rs_[:, 2:3], 0.0), [Rr], [Rr])
                    ACT(rs_[:, 4:8], lg[:, 0:4], AF.Exp, [Rr], [Rr], bias=rs_[:, 1:2], accum_out=rs_[:, 2:3])
                    DV(lambda e: e.reciprocal(rs_[:, 3:4], rs_[:, 2:3]), [Rr], [Rr])
                    DV(lambda e: e.tensor_scalar(rs_[:, 8:12], lg[:, 0:4], rs_[:, 0:1], None, ALU.is_ge), [Rr], [Rr])
                    DV(lambda e: e.tensor_scalar(rs_[:, 8:12], rs_[:, 8:12], 1.0, 1e30, ALU.subtract, ALU.mult), [Rr], [Rr])
                    DV(lambda e: e.tensor_tensor(ml[:].rearrange("p (g e) -> p g e", g=4), lg[:, 4:36].rearrange("p (g e) -> p g e", g=4),
                                                 rs_[:, 8:12].rearrange("p (g o) -> p g o", o=1).to_broadcast([128, 4, 8]), ALU.add), [Rr], [Rr])
                    DV(lambda e: e.max(out=rt8[:], in_=ml[:]), [Rr], [Rr])
                    DV(lambda e: e.tensor_scalar(oh[:, 0, :], ml[:], rt8[:, 0:1], None, ALU.is_equal), [Rr], [Rr])
                    DV(lambda e: e.tensor_scalar(oh[:, 1, :], ml[:], rt8[:, 1:2], None, ALU.is_equal), [Rr], [Rr])
                    DV(lambda e: e.tensor_tensor(rs_[:, 12:13], rt8[:, 0:1], rt8[:, 1:2], ALU.subtract), [Rr], [Rr])
                    ACT(rs_[:, 13:14], rs_[:, 12:13], AF.Sigmoid, [Rr], [Rr])
                    DV(lambda e: e.tensor_tensor(gts[:, tt, 0:1], rs_[:, 13:14], rs_[:, 3:4], ALU.mult), [Rr], [Rgts])
                    DV(lambda e: e.tensor_tensor(gts[:, tt, 1:2], rs_[:, 3:4], gts[:, tt, 0:1], ALU.subtract), [Rr, Rgts], [Rgts])
                    DV(lambda e: e.tensor_tensor(msk[:], oh[:, 0, :], oh[:, 1, :], ALU.add), [Rr], [Rr])
                    MM(PF[5][:, 0:32], mui[:], msk[:], True, True, [Rc, Rr], [RPF[5]])
                    MM(PF[5][:, 32:64], onesf[:], msk[:], True, True, [Rc, Rr], [RPF[5]])
                    DV(lambda e: e.tensor_tensor(posf[:], PF[5][:, 0:32], cntb[:], ALU.add), [RPF[5], Rcnt], [Rr])
                    DV(lambda e: e.tensor_tensor(cntb[:], PF[5][:, 32:64], cntb[:], ALU.add), [RPF[5], Rcnt, Rr], [Rcnt])
                    DV(lambda e: e.tensor_scalar(tmp32[:], posf[:], float(CAPl), 4.0e7, ALU.is_gt, ALU.mult), [Rr], [Rr])
                    DV(lambda e: e.tensor_tensor(posf[:], posf[:], tmp32[:], ALU.add), [Rr], [Rr])
                    DV(lambda e: e.scalar_tensor_tensor(out=posf[:], in0=io32[:], scalar=float(CAPl), in1=posf[:], op0=ALU.mult, op1=ALU.add),
                       [Rr], [Rr])
                    for k in range(2):
                        DV(lambda e: e.tensor_tensor(tmp32[:], oh[:, k, :], posf[:], ALU.mult), [Rr], [Rr])
                        DV(lambda e: e.tensor_reduce(dst[:, k:k + 1], tmp32[:], AX.X, ALU.add), [Rr], [Rr])
                    DV(lambda e: e.tensor_scalar(dst[:], dst[:], -1.0, float(NROW), ALU.add, ALU.min), [Rr], [Rr])
                    DV(lambda e: e.tensor_copy(off[:, tt, :], dst[:]), [Rr], [Roff])
                    for k in range(2):
                        fw.dma("pool", lambda e: e.indirect_dma_start(
                            out=Xs_d[0:NROW + 1, :], out_offset=bass.IndirectOffsetOnAxis(ap=off[:, tt, k:k + 1], axis=0),
                            in_=h2b[i][:], in_offset=None), [Rhb[i], Roff], [RXs])
                fw.barrier()
            if stop == 'D':
                fw.halt = True

            with ExitStack() as ph:
                w1b = [sb(ph, f"w1b{i}", [128, 8, 512], BF16) for i in range(2)]
                w3b = [sb(ph, f"w3b{i}", [128, 8, 512], BF16) for i in range(2)]
                w2b = [sb(ph, f"w2b{i}", [128, 4, D], BF16) for i in range(2)]
                Rwe = [Region(), Region()]
                xs = [sb(ph, f"xs{i}", [128, 4, D], BF16) for i in range(2)]; Rxs = [Region(), Region()]
                XT = sb(ph, "XT", [128, 8, 512], BF16); RXT = Region()
                s1 = [sb(ph, f"s1{i}", [128, 512]) for i in range(2)]; Rs1 = [Region(), Region()]
                GT = sb(ph, "GT", [128, 4, 512], BF16); RGT = Region()
                yrow = [sb(ph, f"yrow{i}", [128, D]) for i in range(2)]; Ryr = [Region(), Region()]
                PL(lambda e: e.memset(yrow[0][:], 0.0), [], [Ryr[0]])
                LD("sp", Ys_d[NROW:NROW + 1, :], yrow[0][0:1, :], [Ryr[0]], [RYs])
                groups = []
                s0 = 0
                while s0 < CAPl:
                    n = min(512, CAPl - s0); groups.append((s0, n)); s0 += n
                gi = 0; yi = 0
                for ex in range(32):
                    wi = ex % 2
                    fw.dma("pool", lambda e: e.dma_start(out=w1b[wi][:], in_=w1_d[l, ex].rearrange("(k p) n -> p k n", p=128)), [], [Rwe[wi]])
                    fw.dma("pool", lambda e: e.dma_start(out=w3b[wi][:], in_=w3_d[l, ex].rearrange("(k p) n -> p k n", p=128)), [], [Rwe[wi]])
                    fw.dma("pool", lambda e: e.dma_start(out=w2b[wi][:], in_=w2_d[l, ex].rearrange("(k p) n -> p k n", p=128)), [], [Rwe[wi]])
                    for (s0, n) in groups:
                        nt = n // 128
                        x_ = xs[gi % 2]; rx_ = Rxs[gi % 2]; gi += 1
                        r0 = ex * CAPl + s0
                        LD("sp", x_[:, 0:nt, :], Xs_d[r0:r0 + n, :].rearrange("(i p) d -> p i d", p=128), [RXs], [rx_])
                        for it_ in range(nt):
                            pbk = PB[it_ % 2]; rpb_ = RPB[it_ % 2]
                            for kc in range(8):
                                TR(pbk[:, kc * 128:(kc + 1) * 128], x_[:, it_, kc * 128:(kc + 1) * 128], identb[:], [rx_, Rc], [rpb_])
                            if it_ % 2 == 0:
                                ACT(XT[:, :, it_ * 128:(it_ + 1) * 128], pbk[:].rearrange("p (k t) -> p k t", k=8), AF.Copy, [rpb_], [RXT])
                            else:
                                DV(lambda e: e.tensor_copy(XT[:, :, it_ * 128:(it_ + 1) * 128], pbk[:].rearrange("p (k t) -> p k t", k=8)),
                                   [rpb_], [RXT])
                        for hc in range(4):
                            p1 = PF[hc % 2]; r1 = RPF[hc % 2]; p3 = PF[2 + hc % 2]; r3 = RPF[2 + hc % 2]
                            for kc in range(8):
                                MM(p1[:, 0:n], w1b[wi][:, kc, hc * 128:(hc + 1) * 128], XT[:, kc, 0:n], kc == 0, kc == 7, [Rwe[wi], RXT], [r1])
                            for kc in range(8):
                                MM(p3[:, 0:n], w3b[wi][:, kc, hc * 128:(hc + 1) * 128], XT[:, kc, 0:n], kc == 0, kc == 7, [Rwe[wi], RXT], [r3])
                            ACT(s1[hc % 2][:, 0:n], p1[:, 0:n], AF.Silu, [r1], [Rs1[hc % 2]])
                            DV(lambda e: e.tensor_tensor(GT[:, hc, 0:n], s1[hc % 2][:, 0:n], p3[:, 0:n], ALU.mult), [Rs1[hc % 2], r3], [RGT])
                        for it_ in range(nt):
                            yr = yrow[yi % 2]; ryr = Ryr[yi % 2]; yi += 1
                            for half in range(2):
                                py = PF[4 + half]; rpy = RPF[4 + half]
                                for hc in range(4):
                                    MM(py[:], GT[:, hc, it_ * 128:(it_ + 1) * 128], w2b[wi][:, hc, half * 512:(half + 1) * 512], hc == 0, hc == 3,
                                       [RGT, Rwe[wi]], [rpy])
                                if half == 0:
                                    ACT(yr[:, 0:512], py[:], AF.Copy, [rpy], [ryr])
                                else:
                                    DV(lambda e: e.tensor_copy(yr[:, 512:1024], py[:]), [rpy], [ryr])
                            LD("sp", Ys_d[r0 + it_ * 128:r0 + (it_ + 1) * 128, :], yr[:], [ryr], [RYs])
                fw.barrier()
            if stop == 'E':
                fw.halt = True

            with ExitStack() as ph:
                xt = [sb(ph, f"xt{i}", [128, D]) for i in range(2)]; Rx = [Region(), Region()]
                y0 = [sb(ph, f"y0{i}", [128, D]) for i in range(2)]; Ry0 = [Region(), Region()]
                y1 = [sb(ph, f"y1{i}", [128, D]) for i in range(2)]; Ry1 = [Region(), Region()]
                for tt in range(NTT):
                    i = tt % 2
                    LD("sp", xt[i][:], xr_d[tt * 128:(tt + 1) * 128, :], [], [Rx[i]])
                    for k, (y, ry) in enumerate(((y0[i], Ry0[i]), (y1[i], Ry1[i]))):
                        PL(lambda e: e.memset(y[:], 0.0), [], [ry])
                        fw.dma("pool", lambda e: e.indirect_dma_start(
                            out=y[:], out_offset=None, in_=Ys_d[0:NROW + 1, :],
                            in_offset=bass.IndirectOffsetOnAxis(ap=off[:, tt, k:k + 1], axis=0)), [RYs, Roff], [ry])
                    DV(lambda e: e.tensor_scalar(y0[i][:], y0[i][:], gts[:, tt, 0:1], None, ALU.mult), [Ry0[i], Rgts], [Ry0[i]])
                    DV(lambda e: e.scalar_tensor_tensor(out=y0[i][:], in0=y1[i][:], scalar=gts[:, tt, 1:2], in1=y0[i][:], op0=ALU.mult, op1=ALU.add),
                       [Ry1[i], Ry0[i], Rgts], [Ry0[i]])
                    PL(lambda e: e.tensor_tensor(y0[i][:], y0[i][:], G2, ALU.mult), [Ry0[i], Rmod], [Ry0[i]])
                    DV(lambda e: e.tensor_tensor(xt[i][:], xt[i][:], y0[i][:], ALU.add), [Rx[i], Ry0[i]], [Rx[i]])
                    LD("sp", xdst[tt * 128:(tt + 1) * 128, :], xt[i][:], [Rx[i]], [Rout])
                fw.barrier()
            if stop == 'F':
                fw.halt = True
        except StopBuild:
            pass
        fw.halt = False
        fw._wait("sp", Rout.w)
        fw.barrier()
    return nc


def run(inputs, NL=4, dbg=False, stop=None):
    sh, per, par = prep_inputs(inputs)
    nc = build(NL, LTOT=inputs["ada_w"].shape[0], dbg=dbg, stop=stop, ncores=8)
    in_maps = []
    for core in range(8):
        m = dict(sh)
        m.update(per[core // 2])
        m.update(par[core % 2])
        in_maps.append(m)
    res = run_bass_kernel_spmd(nc, in_maps, core_ids=list(range(8)))
    return res


def kernel(**inputs):
    inputs = {k: np.asarray(v) for k, v in inputs.items()}
    res = run(inputs, NL=4)
    B = inputs["x"].shape[0]
    out = np.stack([np.asarray(res.results[2 * b]["out"], dtype=np.float32).reshape(S, D) for b in range(B)], axis=0)
    return out
```

```python
import math
from contextlib import ExitStack
import numpy as np
import ml_dtypes
import concourse.bass as bass
import concourse.mybir as mybir
from concourse.bass_utils import run_bass_kernel_spmd

F32 = mybir.dt.float32
BF16 = mybir.dt.bfloat16
I32 = mybir.dt.int32
AF = mybir.ActivationFunctionType
ALU = mybir.AluOpType
AX = mybir.AxisListType

S = 4096
D = 1024
NTT = S // 128
NTB = S // 512
CAPS = [768, 896, 1280, 1408]
CAPMAX = max(CAPS)
NPP = 26
NEG = -30000.0
DMA_RING = 8
DEBUG_IND = False


class Region:
    __slots__ = ("name", "w", "r")

    def __init__(self, name=""):
        self.name = name
        self.w = None
        self.r = []


class FW:
    def __init__(self, nc, stack):
        self.nc = nc
        self.stack = stack
        self.eng = {"pe": nc.tensor, "act": nc.scalar, "dve": nc.vector,
                    "pool": nc.gpsimd, "sp": nc.sync}
        self.sems = {}
        self.cnt = {}
        self.waited = {e: {} for e in self.eng}
        for e in self.eng:
            self.sems["c_" + e] = stack.enter_context(nc.semaphore("c_" + e))
            self.cnt["c_" + e] = 0
        self.ring = {}
        for q in ("sp", "act", "pool"):
            for i in range(DMA_RING):
                k = f"d_{q}{i}"
                self.sems[k] = stack.enter_context(nc.semaphore(k))
                self.cnt[k] = 0
            self.ring[q] = 0
        self.n_inst = 0
        self.halt = False

    def _wait(self, e, ev):
        if ev is None or self.halt:
            return
        k, v = ev
        if self.waited[e].get(k, 0) >= v:
            return
        self.waited[e][k] = v
        self.eng[e].wait_ge(self.sems[k], v)

    def _deps(self, e, reads, writes, skip_same):
        own = "c_" + e
        for r in reads:
            if r.w is not None and not (skip_same and r.w[0] == own):
                self._wait(e, r.w)
        for r in writes:
            if r.w is not None and not (skip_same and r.w[0] == own):
                self._wait(e, r.w)
            for ev in r.r:
                if not (skip_same and ev[0] == own):
                    self._wait(e, ev)

    def _record(self, ev, reads, writes):
        for r in writes:
            r.w = ev
            r.r = []
        for r in reads:
            if r in writes:
                continue
            r.r = [x for x in r.r if x[0] != ev[0]] + [ev]

    def op(self, e, fn, reads=(), writes=(), skip_same=False):
        if self.halt:
            return
        self._deps(e, reads, writes, skip_same)
        k = "c_" + e
        self.cnt[k] += 1
        fn(self.eng[e]).then_inc(self.sems[k], 1)
        self._record((k, self.cnt[k]), reads, writes)
        self.n_inst += 1

    def dma(self, q, fn, reads=(), writes=()):
        if self.halt:
            return
        i = self.ring[q]
        self.ring[q] = (i + 1) % DMA_RING
        k = f"d_{q}{i}"
        if self.cnt[k] > 0:
            self._wait(q, (k, self.cnt[k]))
        self._deps(q, reads, writes, False)
        self.cnt[k] += 16
        fn(self.eng[q]).then_inc(self.sems[k], 16)
        self._record((k, self.cnt[k]), reads, writes)
        self.n_inst += 1

    def barrier(self):
        for e in self.eng:
            for k, v in self.cnt.items():
                if v > 0:
                    self._wait(e, (k, v))


def t5_bucket_np(dist):
    n = np.maximum(dist, 0)
    nf = np.maximum(n, 1).astype(np.float32)
    large = 16 + (np.log(nf / 16) / math.log(1024 / 16) * 16).astype(np.int32)
    large = np.minimum(large, 31)
    return np.where(n < 16, n, large)


def make_consts():
    c = {}
    p = np.arange(128)[:, None]
    j = np.arange(128)[None, :]
    c["ident"] = (p == j).astype(np.float32)
    su = (p < j).astype(np.float32)
    ui = (p <= j).astype(np.float32)
    sl = (p > j).astype(np.float32)
    c["m4"] = np.concatenate([su, ui, su, ui], 1)
    c["msl"] = sl
    c["mui"] = ui
    c["bdones"] = ((p // 64) == (j // 64)).astype(np.float32)
    cc = np.arange(14 * 128)[None, :]
    dist = (cc // 128 - 3) * 128 + (cc % 128) - p
    c["idxE"] = np.where(dist >= 0, t5_bucket_np(dist), -1).astype(np.float32)
    rm = np.ones((128, 512), np.float32)
    rm[:, ::128] = 0
    c["rmask"] = rm
    qt = np.arange(32)[:, None]
    n = np.arange(16)[None, :]
    past = (n < qt // 2).astype(np.float32)
    c["negpast"] = np.broadcast_to(((1 - past) * -1e30).reshape(1, 512), (128, 512)).copy().astype(np.float32)
    c["past30"] = np.broadcast_to((past * 30000.0).reshape(1, 512), (128, 512)).copy().astype(np.float32)
    lm = np.zeros((128, 7, 2, 2, 128), np.float32)
    tt_ = np.arange(128)[:, None]; ii_ = np.arange(128)[None, :]
    for k in range(7):
        b = 2 ** k
        M = (((tt_ // (2 * b)) == (ii_ // (2 * b))) & ((tt_ % (2 * b)) >= b) & ((ii_ % (2 * b)) < b)).astype(np.float32)
        lm[:, k, :, 0, :] = M[:, None, :]
        lm[:, k, :, 1, :] = M.T[:, None, :]
    c["lvlmask"] = lm.reshape(128, 7 * 512).astype(ml_dtypes.bfloat16)
    oh = (np.arange(S)[None, :] // 256 == np.arange(16)[:, None]).astype(np.float32)
    c["onehotk"] = oh.astype(ml_dtypes.bfloat16)
    return c


CONST_SHAPES = {"ident": ([128, 128], F32), "m4": ([128, 512], F32), "msl": ([128, 128], F32),
                "mui": ([128, 128], F32), "bdones": ([128, 128], F32), "idxE": ([128, 1792], F32),
                "rmask": ([128, 512], F32), "negpast": ([128, 512], F32), "past30": ([128, 512], F32),
                "onehotk": ([16, S], BF16), "lvlmask": ([128, 7 * 512], BF16)}


def prep_inputs(I):
    L = I["ada_w"].shape[0]
    sh = {}
    for k in ("ada_w", "ada_b", "w_in", "w_out", "moe_w1", "moe_w3", "moe_w2"):
        sh[k] = np.ascontiguousarray(I[k], dtype=np.float32)
    sh["n1g"] = np.ascontiguousarray(I["norm1_g"])
    sh["n2g"] = np.ascontiguousarray(I["norm2_g"])
    pp = np.zeros((128, L, NPP), np.float32)
    pidx = np.arange(128)
    for l in range(L):
        pp[:, l, 0:7] = I["rwkv_mu"][l].reshape(7, 128).T
        pp[:, l, 7:9] = I["rwkv_w0"][l].reshape(2, 128).T
        pp[:, l, 9:11] = I["rwkv_a0"][l].reshape(2, 128).T
        pp[:, l, 11:13] = I["rwkv_kk"][l].reshape(2, 128).T
        pp[:, l, 13:15] = I["rwkv_ka"][l].reshape(2, 128).T
        pp[:, l, 15:17] = I["rwkv_rk"][l].reshape(2, 128).T
        pp[:, l, 17:19] = I["rwkv_ln_g"][l].reshape(2, 128).T
        pp[:, l, 19:21] = I["rwkv_ln_b"][l].reshape(2, 128).T
        pp[:, l, 21] = I["diff_q_gain"][l][pidx % 64]
        pp[:, l, 22] = I["diff_k_gain"][l][pidx % 64]
        pp[:, l, 23] = I["moba_q_gain"][l][pidx % 64]
        pp[:, l, 24] = I["moba_k_gain"][l][pidx % 64]
        pp[:, l, 25] = I["diff_subln_g"][l]
    sh["lora"] = np.ascontiguousarray(np.concatenate([I["rwkv_w2"], I["rwkv_a2"], I["rwkv_g2"]], axis=1))
    sh["lam"] = np.ascontiguousarray(I["diff_lambda"].reshape(L, 256))
    sh["tbl2"] = np.ascontiguousarray(I["rel_bias"])
    sh["wr"] = np.ascontiguousarray(np.concatenate([I["router_g_w"], I["router_e_w"]], axis=2))
    sh["br"] = np.ascontiguousarray(np.concatenate([I["router_g_b"], I["router_e_b"]], axis=1))
    sh.update(make_consts())
    par = []
    for hh in range(2):
        wa = np.zeros((L, D, 1536), np.float32)
        tb_ = np.zeros((1, 128), np.float32)
        for lh in range(4):
            if lh < 2:
                h = 2 * hh + lh
                srcs = [(896 + h * 128, 128), (896 + 512 + h * 128, 128), (896 + 1024 + h * 128, 128)]
                gh = h
            else:
                h = 2 * hh + (lh - 2)
                srcs = [(2432 + h * 64, 64), (2432 + 256 + h * 64, 64), (2432 + 512 + h * 64, 64)]
                gh = 4 + h
            for i, (c0, n) in enumerate(srcs):
                wa[:, :, lh * 384 + i * 128: lh * 384 + i * 128 + n] = I["w_in"][:, :, c0:c0 + n]
            tb_[0, np.arange(32) * 4 + lh] = I["rel_bias"][:, gh]
        wr = np.concatenate([I["w_in"][:, :, hh * 128:(hh + 1) * 128], I["w_in"][:, :, 256 + hh * 128:256 + (hh + 1) * 128],
                             I["w_in"][:, :, 512 + hh * 128:512 + (hh + 1) * 128], I["w_in"][:, :, 768:896]], axis=2)
        pph = pp.copy()
        for l in range(L):
            mu7 = I["rwkv_mu"][l].reshape(7, 128).T
            pph[:, l, 0:4] = mu7[:, [hh, 2 + hh, 4 + hh, 6]]
            for cbase in (7, 9, 11, 13, 15, 17, 19):
                pph[:, l, cbase] = pp[:, l, cbase + hh]
        par.append({"wa_in": wa, "tbl": tb_, "wr_in": np.ascontiguousarray(wr), "ppar": pph.reshape(128, L * NPP),
                    "lora": np.ascontiguousarray(sh["lora"][:, :, hh * 128:(hh + 1) * 128])})
    per = []
    for b in range(I["x"].shape[0]):
        per.append({"x": np.ascontiguousarray(I["x"][b]),
                    "c8": np.ascontiguousarray(I["c"][b].reshape(8, 128).T)})
    return sh, per, par


class StopBuild(Exception):
    pass


def build(NL, LTOT=4, dbg=False, stop=None, ncores=8):
    nc = bass.Bass("TRN2", target_bir_lowering=False)

    def din(name, shape, dt=F32):
        return nc.dram_tensor(name, list(shape), dt, kind="ExternalInput").ap()

    x_d = din("x", [S, D]); c8_d = din("c8", [128, 8])
    adaw_d = din("ada_w", [LTOT, D, 6 * D]); adab_d = din("ada_b", [LTOT, 6 * D])
    n1g_d = din("n1g", [LTOT, D]); n2g_d = din("n2g", [LTOT, D])
    win_d = din("w_in", [LTOT, D, 3200]); wout_d = din("w_out", [LTOT, D, D])
    ppar_d = din("ppar", [128, LTOT * NPP]); lora_d = din("lora", [LTOT, 128, 128]); wrk_d = din("wr_in", [LTOT, D, 512])
    lam_d = din("lam", [LTOT, 256]); tbl_d = din("tbl", [1, 128]); wa_d = din("wa_in", [LTOT, D, 1536]); tbl2_d = din("tbl2", [32, 8])
    wr_d = din("wr", [LTOT, D, 36]); br_d = din("br", [LTOT, 36])
    w1_d = din("moe_w1", [LTOT, 32, D, 512]); w3_d = din("moe_w3", [LTOT, 32, D, 512])
    w2_d = din("moe_w2", [LTOT, 32, 512, D])
    cd = {k: din(k, shp, dt) for k, (shp, dt) in CONST_SHAPES.items()}
    out_d = nc.dram_tensor("out", [S, D], F32, kind="ExternalOutput").ap()
    okind = "ExternalOutput" if dbg else "Internal"
    xr_d = nc.dram_tensor("xr", [S, D], F32, kind=okind).ap()
    hT_d = nc.dram_tensor("hT", [8, 128, S], BF16).ap()
    mixT_d = nc.dram_tensor("mixT", [8, 128, S], BF16, kind=okind).ap()
    Mine_f = [nc.dram_tensor(f"Mine{i}", [64, S // 2], F32).ap() for i in range(8)]
    G_f = [nc.dram_tensor(f"Gth{i}", [128, S // 2], F32).ap() for i in range(8)]
    Mine_b = [t.bitcast(BF16) for t in Mine_f]
    G_b = [t.bitcast(BF16) for t in G_f]
    Xs_d = nc.dram_tensor("Xs", [32 * CAPMAX + 128, D], BF16).ap()
    Ys_d = nc.dram_tensor("Ys", [32 * CAPMAX + 128, D], F32).ap()

    with ExitStack() as st:
        fw = FW(nc, st)
        ccsem = st.enter_context(nc.semaphore("ccsem"))

        uid = [0]

        def sb(stack, name, shape, dt=F32):
            uid[0] += 1
            return stack.enter_context(nc.sbuf_tensor(f"s{uid[0]}_{name}", list(shape), dt))

        pe_cfg = [None]

        def pe_sync(ap):
            cfg = (ap.base_partition(), ap.partition_size())
            if cfg != pe_cfg[0] and fw.cnt["c_pe"] > 0 and not fw.halt:
                fw._wait("pe", ("c_pe", fw.cnt["c_pe"]))
            pe_cfg[0] = cfg

        def MM(out, lhsT, rhs, start, stop, R, W):
            pe_sync(lhsT)
            fw.op("pe", lambda e: e.matmul(out, lhsT=lhsT, rhs=rhs, start=start, stop=stop), R, W, skip_same=True)

        def TR(out, in_, idn, R, W):
            pe_sync(in_)
            fw.op("pe", lambda e: e.transpose(out, in_, idn), R, W, skip_same=True)

        def ACT(out, in_, func, R, W, **kw):
            fw.op("act", lambda e: e.activation(out, in_, func, **kw), R, W)

        def DV(fn, R, W):
            fw.op("dve", fn, R, W)

        def PL(fn, R, W):
            fw.op("pool", fn, R, W)

        def LD(q, out, in_, R, W):
            fw.dma(q, lambda e: e.dma_start(out=out, in_=in_), R, W)

        PF = [st.enter_context(nc.psum_tensor(f"pf{i}", [128, 512], F32)) for i in range(6)]
        RPF = [Region(f"pf{i}") for i in range(6)]
        PB = [st.enter_context(nc.psum_tensor(f"pb{i}", [128, 1024], BF16)) for i in range(2)]
        RPB = [Region(f"pb{i}") for i in range(2)]

        Rc = Region("consts")
        ident = sb(st, "ident", [128, 128]); identb = sb(st, "identb", [128, 128], BF16)
        m4 = sb(st, "m4", [128, 512]); msl = sb(st, "msl", [128, 128]); mui = sb(st, "mui", [128, 128])
        bdones = sb(st, "bdones", [128, 128]); rmask = sb(st, "rmask", [128, 512])
        negpast = sb(st, "negpast", [128, 512]); past30 = sb(st, "past30", [128, 512])
        onesf = sb(st, "onesf", [128, 128]); onesb = sb(st, "onesb", [128, 128], BF16)
        lvlmask = sb(st, "lvlmask", [128, 7, 512], BF16)
        LD("sp", lvlmask[:].rearrange("p k c -> p (k c)"), cd["lvlmask"], [], [Rc])
        ppar = sb(st, "ppar", [128, LTOT * NPP]); tblb = sb(st, "tblb", [128, 128])
        cs8 = sb(st, "cs8", [128, 8]); csrep = sb(st, "csrep", [128, 8, 128])
        modb = sb(st, "modb", [128, 6 * D]); Rmod = Region("modb")
        erel = sb(st, "erel", [128, 4, 1792], BF16); Rerel = Region("erel")
        lamc = sb(st, "lamc", [128, 4]); Rlam = Region("lamc")
        off = sb(st, "off", [128, NTT, 2], I32); Roff = Region()
        gts = sb(st, "gts", [128, NTT, 2]); Rgts = Region()
        RXs = Region(); RYs = Region(); Rout = Region()
        ccdummy = sb(st, "ccdummy", [128, 2]); Rccd = Region()

        for t, k in ((ident, "ident"), (m4, "m4"), (msl, "msl"), (mui, "mui"), (bdones, "bdones"),
                     (rmask, "rmask"), (negpast, "negpast"), (past30, "past30")):
            LD("sp", t[:], cd[k], [], [Rc])
        LD("sp", ppar[:], ppar_d, [], [Rc])
        LD("sp", tblb[:], tbl_d.partition_broadcast(128), [], [Rc])
        LD("sp", cs8[:], c8_d, [], [Rc])
        PL(lambda e: e.memset(onesf[:], 1.0), [], [Rc])
        PL(lambda e: e.memset(onesb[:], 1.0), [], [Rc])
        DV(lambda e: e.tensor_copy(identb[:], ident[:]), [Rc], [Rc])
        ACT(cs8[:], cs8[:], AF.Silu, [Rc], [Rc])
        for j in range(8):
            DV(lambda e: e.tensor_copy(csrep[:, j, :], cs8[:, j:j + 1].to_broadcast([128, 128])), [Rc], [Rc])

        def pcol(l, i):
            return ppar[:, l * NPP + i: l * NPP + i + 1]

        with ExitStack() as ph:
            idxE = sb(ph, "idxE", [128, 1792]); Ri = Region()
            acc = sb(ph, "eacc", [128, 1792]); Ra = Region()
            tmp = [sb(ph, f"etmp{i}", [128, 1792]) for i in range(2)]; Rt = [Region(), Region()]
            LD("sp", idxE[:], cd["idxE"], [], [Ri])
            it = 0
            for h in range(4):
                PL(lambda e: e.memset(acc[:], 0.0), [], [Ra])
                for b in range(32):
                    t = tmp[it % 2]; rt = Rt[it % 2]; it += 1
                    DV(lambda e: e.tensor_scalar(t[:], idxE[:], float(b), tblb[:, b * 4 + h: b * 4 + h + 1],
                                                 ALU.is_equal, ALU.mult), [Ri, Rc], [rt])
                    PL(lambda e: e.tensor_tensor(acc[:], acc[:], t[:], ALU.add), [rt, Ra], [Ra])
                DV(lambda e: e.tensor_scalar(acc[:], acc[:], tblb[:, 31 * 4 + h: 31 * 4 + h + 1], None, ALU.subtract),
                   [Ra, Rc], [Ra])
                ACT(acc[:], acc[:], AF.Exp, [Ra], [Ra])
                t = tmp[0]
                DV(lambda e: e.tensor_scalar(t[:], idxE[:], 0.0, None, ALU.is_ge), [Ri], [Rt[0]])
                DV(lambda e: e.tensor_tensor(erel[:, h, :], acc[:], t[:], ALU.mult), [Ra, Rt[0]], [Rerel])
            fw.barrier()

        try:
          for l in range(NL):
            lam_init = 0.8 - 0.6 * math.exp(-0.3 * l)
            xsrc = x_d if l == 0 else xr_d
            xdst = out_d if l == NL - 1 else xr_d

            with ExitStack() as ph:
                aw = [sb(ph, f"aw{i}", [128, 8, 512]) for i in range(2)]; Raw = [Region(), Region()]
                ab = [sb(ph, f"ab{i}", [128, 512]) for i in range(2)]; Rab = [Region(), Region()]
                gb = sb(ph, "gb", [128, 2, D]); Rgb = Region()
                lamt = sb(ph, "lamt", [128, 256]); Rlt = Region()
                LD("act", gb[:, 0, :], n1g_d[l:l + 1, :].partition_broadcast(128), [], [Rgb])
                LD("act", gb[:, 1, :], n2g_d[l:l + 1, :].partition_broadcast(128), [], [Rgb])
                LD("act", lamt[:], lam_d[l:l + 1, :].partition_broadcast(128), [], [Rlt])
                for nb in range(12):
                    a = aw[nb % 2]; ra = Raw[nb % 2]; b_ = ab[nb % 2]; rb = Rab[nb % 2]
                    LD("sp", a[:], adaw_d[l].rearrange("(k p) n -> p k n", p=128)[:, :, nb * 512:(nb + 1) * 512], [], [ra])
                    LD("act", b_[:], adab_d[l:l + 1, nb * 512:(nb + 1) * 512].partition_broadcast(128), [], [rb])
                    pf = PF[nb % 2]; rp = RPF[nb % 2]
                    for kc in range(8):
                        MM(pf[:], csrep[:, kc, :], a[:, kc, :], kc == 0, kc == 7, [Rc, ra], [rp])
                    DV(lambda e: e.tensor_tensor(modb[:, nb * 512:(nb + 1) * 512], pf[:], b_[:], ALU.add), [rp, rb], [Rmod])
                for (o, gi) in ((1, 0), (4, 1)):
                    DV(lambda e: e.scalar_tensor_tensor(out=modb[:, o * D:(o + 1) * D], in0=modb[:, o * D:(o + 1) * D],
                                                        scalar=1.0, in1=gb[:, gi, :], op0=ALU.add, op1=ALU.mult),
                       [Rmod, Rgb], [Rmod])
                prod = sb(ph, "lprod", [128, 128]); Rpr = Region()
                DV(lambda e: e.tensor_tensor(prod[:].rearrange("p (a d) -> p a d", a=2),
                                             lamt[:].rearrange("p (a b d) -> p a b d", a=2, b=2)[:, :, 0, :],
                                             lamt[:].rearrange("p (a b d) -> p a b d", a=2, b=2)[:, :, 1, :], ALU.mult),
                   [Rlt], [Rpr])
                DV(lambda e: e.tensor_reduce(lamc[:, 1:3], prod[:].rearrange("p (a d) -> p a d", a=2), AX.X, ALU.add),
                   [Rpr], [Rlam])
                ACT(lamc[:, 1:3], lamc[:, 1:3], AF.Exp, [Rlam], [Rlam])
                DV(lambda e: e.tensor_tensor(lamc[:, 0:1], lamc[:, 2:3], lamc[:, 1:2], ALU.subtract), [Rlam], [Rlam])
                DV(lambda e: e.tensor_scalar(lamc[:, 0:1], lamc[:, 0:1], -lam_init, None, ALU.add), [Rlam], [Rlam])
                DV(lambda e: e.tensor_scalar(lamc[:, 3:4], pcol(l, 25), 1.0 - lam_init, None, ALU.mult), [Rc, Rlam], [Rlam])
                fw.barrier()
            if stop == '0':
                fw.halt = True
            SH1, A1, G1 = modb[:, 0:D], modb[:, D:2 * D], modb[:, 2 * D:3 * D]
            SH2, A2, G2 = modb[:, 3 * D:4 * D], modb[:, 4 * D:5 * D], modb[:, 5 * D:6 * D]

            def norm_mod(ph, xt, rx, Ax, Bx, hf, rhf, small, rsm):
                PL(lambda e: e.memset(small[:, 0:1], 0.0), [], [rsm])
                ACT(hf[:], xt[:], AF.Square, [rx, rsm], [rhf, rsm], accum_out=small[:, 0:1])
                DV(lambda e: e.tensor_scalar(small[:, 1:2], small[:, 0:1], 1.0 / D, 1e-6, ALU.mult, ALU.add), [rsm], [rsm])
                ACT(small[:, 1:2], small[:, 1:2], AF.Sqrt, [rsm], [rsm])
                DV(lambda e: e.reciprocal(small[:, 2:3], small[:, 1:2]), [rsm], [rsm])
                DV(lambda e: e.scalar_tensor_tensor(out=hf[:], in0=xt[:], scalar=small[:, 2:3], in1=Ax,
                                                    op0=ALU.mult, op1=ALU.mult), [rx, rsm, Rmod], [rhf])
                PL(lambda e: e.tensor_tensor(hf[:], hf[:], Bx, ALU.add), [rhf, Rmod], [rhf])

            with ExitStack() as ph:
                xt = [sb(ph, f"xt{i}", [128, D]) for i in range(2)]; Rx = [Region(), Region()]
                hf = [sb(ph, f"hf{i}", [128, D]) for i in range(2)]; Rhf = [Region(), Region()]
                hb = [sb(ph, f"hb{i}", [128, D], BF16) for i in range(2)]; Rhb = [Region(), Region()]
                sm = [sb(ph, f"sm{i}", [128, 4]) for i in range(2)]; Rsm = [Region(), Region()]
                hblk = [sb(ph, f"hblk{i}", [128, 8, 512], BF16) for i in range(2)]; Rhk = [Region(), Region()]
                for tt in range(NTT):
                    i = tt % 2
                    LD("sp", xt[i][:], xsrc[tt * 128:(tt + 1) * 128, :], [], [Rx[i]])
                    norm_mod(ph, xt[i], Rx[i], A1, SH1, hf[i], Rhf[i], sm[i], Rsm[i])
                    DV(lambda e: e.tensor_copy(hb[i][:], hf[i][:]), [Rhf[i]], [Rhb[i]])
                    for kc in range(8):
                        TR(PB[i][:, kc * 128:(kc + 1) * 128], hb[i][:, kc * 128:(kc + 1) * 128], identb[:], [Rhb[i], Rc], [RPB[i]])
                    tb = tt // 4; j = tt % 4; bi = tb % 2
                    ACT(hblk[bi][:, :, j * 128:(j + 1) * 128], PB[i][:].rearrange("p (k t) -> p k t", k=8), AF.Copy,
                        [RPB[i]], [Rhk[bi]])
                    if j == 3:
                        LD("sp", hT_d[:, :, tb * 512:(tb + 1) * 512].rearrange("k p t -> p k t"), hblk[bi][:], [Rhk[bi]], [])
                fw.barrier()
            if stop == 'A':
                fw.halt = True

            with ExitStack() as ph:
                wr_ = sb(ph, "w_r", [128, 8, 512], BF16); Rw = Region()
                fw.dma("pool", lambda e: e.dma_start(out=wr_[:], in_=wrk_d[l].rearrange("(k p) n -> p k n", p=128)), [], [Rw])
                lora = sb(ph, "lora", [128, 256]); Rlo = Region()
                LD("sp", lora[:, 0:128], lora_d[l], [], [Rlo])
                hblk = [sb(ph, "hblk0", [128, 8, 512], BF16)] * 2; Rhk = [Region()] * 2
                P7 = sb(ph, "P7", [128, 7, 513]); RP7 = Region()
                D7 = sb(ph, "D7", [128, 7, 512]); RD7 = Region()
                PS7 = sb(ph, "PS7", [128, 7, 512]); RPS7 = Region()
                NT = 14
                T = [sb(ph, f"rt{i}", [128, 512]) for i in range(NT)]; RT = [Region() for _ in range(NT)]
                ARt = sb(ph, "ARt", [128, 4, 256], BF16); RAR = Region()
                ARm = [sb(ph, f"ARm{i}", [128, 4, 256], BF16) for i in range(2)]; RARm = [Region(), Region()]
                for j in range(2):
                    PL(lambda e: e.memset(ARm[j][:], 0.0), [], [RARm[j]])
                Bt = sb(ph, "Bt", [128, 512], BF16); RBt = Region()
                Kt = sb(ph, "Kt", [128, 512], BF16); RKt = Region()
                BHf = sb(ph, "BHf", [128, 512], BF16); KHf = sb(ph, "KHf", [128, 512], BF16); Vf = sb(ph, "Vf", [128, 512], BF16)
                RBH = Region(); RKH = Region(); RVf = Region()
                BHtm = sb(ph, "BHtm", [128, 4, 128], BF16); KHtm = sb(ph, "KHtm", [128, 4, 128], BF16)
                Vtm = sb(ph, "Vtm", [128, 4, 128], BF16); Rtm = Region()
                SB1s = [sb(ph, f"SB1s{i}", [128, 2, 512], BF16) for i in range(2)]; RSB1s = [Region(), Region()]
                SBAs = [sb(ph, f"SBAs{i}", [128, 2, 128], BF16) for i in range(2)]; RSBAs = [Region(), Region()]
                DEs = [[sb(ph, f"DE{s_}{i}", [128, 2, 2, 128], BF16) for i in range(2)] for s_ in range(2)]
                RDEs = [[Region(), Region()] for s_ in range(2)]
                ZZs = [sb(ph, f"ZZ{i}", [128, 2, 2, 128], BF16) for i in range(2)]; RZZs = [Region(), Region()]
                Gms = [sb(ph, f"Gm{i}", [128, 512]) for i in range(2)]; RGms = [Region(), Region()]
                ST = [sb(ph, f"ST{i}", [128, 64]) for i in range(2)]; RST = [Region(), Region()]
                STb = [sb(ph, f"STb{i}", [128, 64], BF16) for i in range(2)]; RSTb = [Region(), Region()]
                RHSb = sb(ph, "RHSb", [128, 128], BF16); RRH = Region()
                Ub = sb(ph, "Ub", [128, 128], BF16); RUb = Region()
                Ytm = sb(ph, "Ytm", [128, 4, 128]); RY = Region()
                gn = sb(ph, "gn", [128, 8, 4]); Rgn = Region()
                GC = sb(ph, "GC", [128, 4]); RGC = Region()
                mixo = sb(ph, "mixo", [128, 512], BF16); Rmx = Region()
                for hp in range(1):
                    PL(lambda e: e.memset(ST[hp][:], 0.0), [], [RST[hp]])
                    PL(lambda e: e.memset(STb[hp][:], 0.0), [], [RSTb[hp]])
                PL(lambda e: e.memset(P7[:, :, 0:1], 0.0), [], [RP7])

                for tb in range(NTB):
                    hb_ = hblk[tb % 2]; rh = Rhk[tb % 2]
                    LD("sp", hb_[:], hT_d[:, :, tb * 512:(tb + 1) * 512].rearrange("k p t -> p k t"), [], [rh])
                    if tb > 0:
                        DV(lambda e: e.tensor_copy(P7[:, :, 0:1], P7[:, :, 512:513]), [RP7], [RP7])
                    for cc in range(4):
                        pf = PF[cc % 2]; rp = RPF[cc % 2]
                        for kc in range(8):
                            MM(pf[:], wr_[:, kc, cc * 128:(cc + 1) * 128], hb_[:, kc, :], kc == 0, kc == 7, [Rw, rh], [rp])
                        ACT(P7[:, cc, 1:513], pf[:], AF.Copy, [rp], [RP7])
                    DV(lambda e: e.tensor_tensor(D7[:], P7[:, :, 0:512], P7[:, :, 1:513], ALU.subtract), [RP7], [RD7])
                    for cc in range(4):
                        DV(lambda e: e.scalar_tensor_tensor(out=PS7[:, cc, :], in0=D7[:, cc, :], scalar=pcol(l, cc),
                                                            in1=P7[:, cc, 1:513], op0=ALU.mult, op1=ALU.add),
                           [RD7, RP7, Rc], [RPS7])
                    ACT(PS7[0:32, 3, :], PS7[0:32, 3, :], AF.Tanh, [RPS7], [RPS7])
                    ACT(PS7[64:128, 3, :], PS7[64:128, 3, :], AF.Sigmoid, [RPS7], [RPS7])
                    if stop == 'B1':
                        fw.halt = True
                    for hp in range(1):
                        rs, ks, vs = PS7[:, 0, :], PS7[:, 1, :], PS7[:, 2, :]
                        cs_ = slice(hp * 128, (hp + 1) * 128)
                        sg, av, gv, kk, sq, kkn, k2, lw, cum, e1, e2, e3, e4, bon = T
                        Rsg, Rav, Rgv, Rkk, Rsq, Rkkn, Rk2, Rlw, Rcum, Re1, Re2, Re3, Re4, Rbon = RT
                        MM(PF[2][:], lora[0:32, cs_], PS7[0:32, 3, :], True, True, [Rlo, RPS7], [RPF[2]])
                        ACT(sg[:], PF[2][:], AF.Sigmoid, [RPF[2], Rc], [Rsg], bias=pcol(l, 7 + hp))
                        MM(PF[3][:], lora[32:64, cs_], PS7[32:64, 3, :], True, True, [Rlo, RPS7], [RPF[3]])
                        ACT(av[:], PF[3][:], AF.Sigmoid, [RPF[3], Rc], [Rav], bias=pcol(l, 9 + hp))
                        MM(PF[2][:], lora[64:128, cs_], PS7[64:128, 3, :], True, True, [Rlo, RPS7], [RPF[2]])
                        ACT(gv[:], PF[2][:], AF.Copy, [RPF[2]], [Rgv])
                        DV(lambda e: e.tensor_scalar(kk[:], ks, pcol(l, 11 + hp), None, ALU.mult), [RPS7, Rc], [Rkk])
                        PL(lambda e: e.tensor_tensor(sq[:], kk[:], kk[:], ALU.mult), [Rkk], [Rsq])
                        MM(PF[3][:], bdones[:], sq[:], True, True, [Rc, Rsq], [RPF[3]])
                        ACT(sq[:], PF[3][:], AF.Sqrt, [RPF[3]], [Rsq])
                        DV(lambda e: e.tensor_scalar(sq[:], sq[:], 1e-12, None, ALU.max), [Rsq], [Rsq])
                        DV(lambda e: e.reciprocal(sq[:], sq[:]), [Rsq], [Rsq])
                        DV(lambda e: e.tensor_tensor(kkn[:], kk[:], sq[:], ALU.mult), [Rkk, Rsq], [Rkkn])
                        DV(lambda e: e.tensor_scalar(k2[:], av[:], 1.0, pcol(l, 13 + hp), ALU.subtract, ALU.mult), [Rav, Rc], [Rk2])
                        DV(lambda e: e.scalar_tensor_tensor(out=k2[:], in0=k2[:], scalar=1.0, in1=ks, op0=ALU.add, op1=ALU.mult),
                           [Rk2, RPS7], [Rk2])
                        PL(lambda e: e.tensor_tensor(kk[:], rs, k2[:], ALU.mult), [RPS7, Rk2, Rkkn], [Rkk])
                        DV(lambda e: e.tensor_scalar(kk[:], kk[:], pcol(l, 15 + hp), None, ALU.mult), [Rkk, Rc], [Rkk])
                        MM(PF[2][:], bdones[:], kk[:], True, True, [Rc, Rkk], [RPF[2]])
                        DV(lambda e: e.tensor_tensor(bon[:], PF[2][:], vs, ALU.mult), [RPF[2], RPS7], [Rbon])
                        DV(lambda e: e.tensor_scalar(lw[:], sg[:], -0.6065306597126334, None, ALU.mult), [Rsg], [Rlw])
                        DV(lambda e: e.tensor_tensor_scan(cum[:], rmask[:], lw[:], 0.0, ALU.mult, ALU.add), [Rc, Rlw], [Rcum])
                        cum3 = cum[:].rearrange("p (c t) -> p c t", t=128)
                        ACT(e1[:], cum[:], AF.Exp, [Rcum], [Re1])
                        ACT(e2[:], cum[:], AF.Exp, [Rcum], [Re2], scale=-1.0)
                        DV(lambda e: e.tensor_tensor(e3[:], cum[:], lw[:], ALU.subtract), [Rcum, Rlw], [Re3])
                        ACT(e3[:], e3[:], AF.Exp, [Re3], [Re3])
                        DV(lambda e: e.tensor_tensor(e4[:].rearrange("p (c t) -> p c t", t=128),
                                                     cum3[:, :, 127:128].to_broadcast([128, 4, 128]), cum3, ALU.subtract),
                           [Rcum], [Re4])
                        ACT(e4[:], e4[:], AF.Exp, [Re4], [Re4])
                        ACT(GC[:].rearrange("p (c o) -> p c o", o=1), cum3[:, :, 127:128], AF.Exp, [Rcum], [RGC])
                        AR3 = ARt[:]
                        DV(lambda e: e.scalar_tensor_tensor(out=AR3[:, :, 0:128], in0=kkn[:].rearrange("p (c t) -> p c t", t=128),
                                                            scalar=-1.0, in1=e3[:].rearrange("p (c t) -> p c t", t=128),
                                                            op0=ALU.mult, op1=ALU.mult), [Rkkn, Re3], [RAR])
                        PL(lambda e: e.tensor_tensor(AR3[:, :, 128:256], PS7[:, hp, :].rearrange("p (c t) -> p c t", t=128),
                                                     e1[:].rearrange("p (c t) -> p c t", t=128), ALU.mult), [RPS7, Re1], [RAR])
                        ACT(ARm[0][0:64, :, :], ARt[0:64, :, :], AF.Copy, [RAR], [RARm[0]])
                        PL(lambda e: e.tensor_copy(ARm[1][64:128, :, :], ARt[64:128, :, :]), [RAR], [RARm[1]])
                        DV(lambda e: e.tensor_tensor(kkn[:], kkn[:], av[:], ALU.mult), [Rkkn, Rav, RAR], [Rkkn])
                        DV(lambda e: e.tensor_tensor(Bt[:], kkn[:], e2[:], ALU.mult), [Rkkn, Re2], [RBt])
                        PL(lambda e: e.tensor_tensor(BHf[:], kkn[:], e4[:], ALU.mult), [Rkkn, Re4], [RBH])
                        DV(lambda e: e.tensor_tensor(Kt[:], k2[:], e2[:], ALU.mult), [Rk2, Re2], [RKt])
                        PL(lambda e: e.tensor_tensor(KHf[:], k2[:], e4[:], ALU.mult), [Rk2, Re4], [RKH])
                        ACT(Vf[:], vs, AF.Copy, [RPS7], [RVf])
                        for c in range(4):
                            TR(PB[0][:, c * 128:(c + 1) * 128], BHf[:, c * 128:(c + 1) * 128], identb[:], [RBH, Rc], [RPB[0]])
                            TR(PB[0][:, 512 + c * 128:512 + (c + 1) * 128], KHf[:, c * 128:(c + 1) * 128], identb[:], [RKH, Rc], [RPB[0]])
                            TR(PB[1][:, c * 128:(c + 1) * 128], Vf[:, c * 128:(c + 1) * 128], identb[:], [RVf, Rc], [RPB[1]])
                        ACT(BHtm[:], PB[0][:, 0:512].rearrange("p (c t) -> p c t", t=128), AF.Copy, [RPB[0]], [Rtm])
                        DV(lambda e: e.tensor_copy(KHtm[:], PB[0][:, 512:1024].rearrange("p (c t) -> p c t", t=128)), [RPB[0]], [Rtm])
                        ACT(Vtm[:], PB[1][:, 0:512].rearrange("p (c t) -> p c t", t=128), AF.Copy, [RPB[1]], [Rtm])
                        if stop == 'B2':
                            fw.halt = True
                        for c0 in (0, 2):
                            for s_ in range(2):
                                c = c0 + s_
                                cl = slice(c * 128, (c + 1) * 128)
                                b0 = 0
                                pa = PF[4]; rpa = RPF[4]
                                for j in range(2):
                                    pf = PF[b0 + j]; rpf_ = RPF[b0 + j]
                                    MM(pf[:, 0:256], Bt[:, cl], ARm[j][:, c, :], True, True, [RBt, RARm[j]], [rpf_])
                                    MM(pf[:, 256:512], Kt[:, cl], ARm[j][:, c, :], True, True, [RKt, RARm[j]], [rpf_])
                                    MM(pa[:, j * 128:(j + 1) * 128], ARm[j][:, c, 0:128], Bt[:, cl], True, True, [RARm[j], RBt], [rpa])
                                for j in range(2):
                                    DV(lambda e: e.tensor_tensor(SB1s[s_][:, j, :], PF[b0 + j][:], m4[:], ALU.mult), [RPF[b0 + j], Rc], [RSB1s[s_]])
                                for j in range(2):
                                    PL(lambda e: e.tensor_copy(DEs[s_][0][:, j, 0, :], identb[:]), [Rc], [RDEs[s_][0]])
                                    PL(lambda e: e.tensor_copy(DEs[s_][0][:, j, 1, :], identb[:]), [Rc], [RDEs[s_][0]])
                                    DV(lambda e: e.tensor_tensor(SBAs[s_][:, j, :], pa[:, j * 128:(j + 1) * 128], msl[:], ALU.mult),
                                       [rpa, Rc], [RSBAs[s_]])
                            wi = 0
                            for k in range(7):
                                for s_ in range(2):
                                    pz = PF[4 * s_]; rpz = RPF[4 * s_]
                                    for j in range(2):
                                        MM(pz[:, j * 256:j * 256 + 128], SB1s[s_][:, j, 0:128], DEs[s_][wi][:, j, 0, :], True, True,
                                           [RSB1s[s_], RDEs[s_][wi]], [rpz])
                                        MM(pz[:, j * 256 + 128:j * 256 + 256], SBAs[s_][:, j, :], DEs[s_][wi][:, j, 1, :], True, True,
                                           [RSBAs[s_], RDEs[s_][wi]], [rpz])
                                for s_ in range(2):
                                    ACT(ZZs[s_][:].rearrange("p j z t -> p (j z t)"), PF[4 * s_][:], AF.Copy, [RPF[4 * s_]], [RZZs[s_]])
                                for s_ in range(2):
                                    pg = PF[4 * s_ + 1]; rpg = RPF[4 * s_ + 1]
                                    for j in range(2):
                                        MM(pg[:, j * 256:j * 256 + 128], DEs[s_][wi][:, j, 1, :], ZZs[s_][:, j, 0, :], True, True,
                                           [RDEs[s_][wi], RZZs[s_]], [rpg])
                                        MM(pg[:, j * 256 + 128:j * 256 + 256], DEs[s_][wi][:, j, 0, :], ZZs[s_][:, j, 1, :], True, True,
                                           [RDEs[s_][wi], RZZs[s_]], [rpg])
                                for s_ in range(2):
                                    DV(lambda e: e.tensor_tensor(Gms[s_][:], PF[4 * s_ + 1][:], lvlmask[:, k, :], ALU.mult),
                                       [RPF[4 * s_ + 1], Rc], [RGms[s_]])
                                for s_ in range(2):
                                    PL(lambda e: e.tensor_tensor(DEs[s_][1 - wi][:].rearrange("p j z t -> p (j z t)"), Gms[s_][:],
                                                                 DEs[s_][wi][:].rearrange("p j z t -> p (j z t)"), ALU.add),
                                       [RGms[s_], RDEs[s_][wi]], [RDEs[s_][1 - wi]])
                                wi = 1 - wi
                            for s_ in range(2):
                                c = c0 + s_
                                SB1 = SB1s[s_]; RSB1 = RSB1s[s_]
                                W_ = DEs[s_][wi]; RW_ = RDEs[s_][wi]
                                pr = PF[5]
                                for j in range(2):
                                    vj = slice(j * 64, (j + 1) * 64)
                                    MM(pr[:, vj], ARm[j][:, c, 0:128], STb[hp][:, :], True, False, [RARm[j], RSTb[hp]], [RPF[5]])
                                    MM(pr[:, vj], SB1[:, j, 256:384], Vtm[:, c, vj], False, True, [RSB1, Rtm], [RPF[5]])
                                ACT(RHSb[:], pr[:, 0:128], AF.Copy, [RPF[5]], [RRH])
                                pu = PF[4]
                                for j in range(2):
                                    vj = slice(j * 64, (j + 1) * 64)
                                    MM(pu[:, 256 + j * 64:256 + (j + 1) * 64], W_[:, j, 1, :], RHSb[:, vj], True, True, [RW_, RRH], [RPF[4]])
                                DV(lambda e: e.tensor_copy(Ub[:], pu[:, 256:384]), [RPF[4]], [RUb])
                                py = PF[5]
                                for j in range(2):
                                    vj = slice(j * 64, (j + 1) * 64)
                                    yo = py[:, 128 + j * 64:128 + (j + 1) * 64]
                                    MM(yo, ARm[j][:, c, 128:256], STb[hp][:, :], True, False, [RARm[j], RSTb[hp]], [RPF[5]])
                                    MM(yo, SB1[:, j, 128:256], Ub[:, vj], False, False, [RSB1, RUb], [RPF[5]])
                                    MM(yo, SB1[:, j, 384:512], Vtm[:, c, vj], False, True, [RSB1, Rtm], [RPF[5]])
                                ACT(Ytm[:, c, :], py[:, 128:256], AF.Copy, [RPF[5]], [RY])
                                pss = PF[5]
                                for j in range(2):
                                    R_ = slice(j * 64, (j + 1) * 64); vj = slice(j * 64, (j + 1) * 64)
                                    so = pss[R_, 256:320]
                                    MM(so, BHtm[:, c, R_], Ub[:, vj], True, False, [Rtm, RUb], [RPF[5]])
                                    MM(so, KHtm[:, c, R_], Vtm[:, c, vj], False, True, [Rtm], [RPF[5]])
                                DV(lambda e: e.scalar_tensor_tensor(out=ST[hp][:], in0=ST[hp][:], scalar=GC[:, c:c + 1], in1=pss[:, 256:320],
                                                                    op0=ALU.mult, op1=ALU.add), [RST[hp], RGC, RPF[5]], [RST[hp]])
                                ACT(STb[hp][:], ST[hp][:], AF.Copy, [RST[hp]], [RSTb[hp]])
                        Y8 = Ytm[:].rearrange("p c (j v) -> p (c j) v", j=2)
                        DV(lambda e: e.tensor_reduce(gn[:, :, 0], Y8, AX.X, ALU.add), [RY], [Rgn])
                        DV(lambda e: e.tensor_scalar(gn[:, :, 0], gn[:, :, 0], 1.0 / 64, None, ALU.mult), [Rgn], [Rgn])
                        DV(lambda e: e.tensor_tensor(Y8, Y8, gn[:, :, 0:1].to_broadcast([128, 8, 64]), ALU.subtract), [RY, Rgn], [RY])
                        Ysq = sq[:].rearrange("p (a v) -> p a v", v=64)
                        PL(lambda e: e.tensor_tensor(Ysq, Y8, Y8, ALU.mult), [RY, Rsq], [Rsq])
                        DV(lambda e: e.tensor_reduce(gn[:, :, 1], Ysq, AX.X, ALU.add), [Rsq], [Rgn])
                        DV(lambda e: e.tensor_scalar(gn[:, :, 1], gn[:, :, 1], 1.0 / 64, 64e-5, ALU.mult, ALU.add), [Rgn], [Rgn])
                        ACT(gn[:, :, 1], gn[:, :, 1], AF.Sqrt, [Rgn], [Rgn])
                        DV(lambda e: e.reciprocal(gn[:, :, 2], gn[:, :, 1]), [Rgn], [Rgn])
                        DV(lambda e: e.tensor_tensor(Y8, Y8, gn[:, :, 2:3].to_broadcast([128, 8, 64]), ALU.mult), [RY, Rgn], [RY])
                        if stop == 'B5':
                            fw.halt = True
                        for c in range(4):
                            TR(PF[3][:, c * 128:(c + 1) * 128], Ytm[:, c, :], ident[:], [RY, Rc], [RPF[3]])
                        DV(lambda e: e.tensor_scalar(e1[:], PF[3][:], pcol(l, 17 + hp), pcol(l, 19 + hp), ALU.mult, ALU.add),
                           [RPF[3], Rc, RAR], [Re1])
                        DV(lambda e: e.tensor_tensor(e1[:], e1[:], bon[:], ALU.add), [Re1, Rbon], [Re1])
                        DV(lambda e: e.tensor_tensor(mixo[:], e1[:], gv[:], ALU.mult), [Re1, Rgv], [Rmx])
                        LD("sp", Mine_b[6][:, tb * 512:(tb + 1) * 512], mixo[0:64, :], [Rmx], [])
                        LD("sp", Mine_b[7][:, tb * 512:(tb + 1) * 512], mixo[64:128, :], [Rmx], [])
                        if stop == 'B6':
                            fw.halt = True
                        if stop == 'B7' and hp == 1:
                            fw.halt = True
                        if stop == 'B8' and hp == 1 and tb == 1:
                            fw.halt = True
                fw.barrier()
            if stop == 'B':
                fw.halt = True

            with ExitStack() as ph:
                hblk = [sb(ph, f"hblk{i}", [128, 8, 512], BF16) for i in range(2)]; Rhk = [Region(), Region()]
                wa = [sb(ph, f"wa{i}", [128, 8, 384], BF16) for i in range(2)]; Rwa = [Region(), Region()]
                QT = sb(ph, "QT", [128, S], BF16); KT = sb(ph, "KT", [128, S], BF16); RQ = Region(); RK = Region()
                QT1 = sb(ph, "QT1", [128, S], BF16)
                PL(lambda e: e.memset(QT[:], 0.0), [], [RQ])
                PL(lambda e: e.memset(QT1[:], 0.0), [], [RQ])
                Vt = sb(ph, "Vt", [128, NTT, 128], BF16); RV = Region()
                qf = sb(ph, "qf", [128, 512]); Rqf = Region()
                sqf = sb(ph, "sqf", [128, 512]); Rsqf = Region()
                rsf = sb(ph, "rsf", [128, 512]); Rrsf = Region()
                Pf = [sb(ph, f"Pf{i}", [128, 512]) for i in range(2)]; RPf = [Region(), Region()]
                Pb = [sb(ph, f"Pb{i}", [128, 512], BF16) for i in range(3)]; RPb = [Region() for _ in range(3)]
                rec = sb(ph, "rec", [128, 2, 512]); Rrec = Region()
                bcs = sb(ph, "bcs", [128, 2, 512]); Rbcs = Region()
                Of = sb(ph, "Of", [128, 512]); ROf = Region()
                Ob = sb(ph, "Ob", [128, 512], BF16); ROb = Region()
                kmT = sb(ph, "kmT", [128, 16]); Rkm = Region()
                gm = sb(ph, "gm", [128, 16]); top8 = sb(ph, "top8", [128, 8]); Rgm = Region()
                nmw = sb(ph, "nmw", [128, 4, 80]); Rnm = Region()
                PL(lambda e: e.memset(nmw[:], 0.0), [], [Rnm])
                pbi = 0

                def proj_fm(hb_, rh, w, rw, c0, M, pf, rp):
                    for kc in range(8):
                        MM(pf[0:M, :], w[:, kc, c0:c0 + M], hb_[:, kc, :], kc == 0, kc == 7, [rw, rh], [rp])

                def headnorm(pf, rp, M, gcol, dst, rdst, dst2=None):
                    ACT(sqf[0:M, :], pf[0:M, :], AF.Square, [rp], [Rsqf])
                    MM(PF[2][0:M, :], bdones[0:M, 0:M], sqf[0:M, :], True, True, [Rc, Rsqf], [RPF[2]])
                    DV(lambda e: e.tensor_scalar(rsf[0:M, :], PF[2][0:M, :], 1.0 / 64, 1e-6, ALU.mult, ALU.add), [RPF[2]], [Rrsf])
                    ACT(rsf[0:M, :], rsf[0:M, :], AF.Sqrt, [Rrsf], [Rrsf])
                    DV(lambda e: e.reciprocal(rsf[0:M, :], rsf[0:M, :]), [Rrsf], [Rrsf])
                    DV(lambda e: e.scalar_tensor_tensor(out=qf[0:M, :], in0=pf[0:M, :], scalar=gcol[0:M, :], in1=rsf[0:M, :],
                                                        op0=ALU.mult, op1=ALU.mult), [rp, Rc, Rrsf], [Rqf])
                    if dst2 is None:
                        ACT(dst, qf[0:M, :], AF.Copy, [Rqf], [rdst])
                    else:
                        ACT(dst, qf[0:64, :], AF.Copy, [Rqf], [rdst])
                        PL(lambda e: e.tensor_copy(dst2, qf[64:128, :]), [Rqf], [rdst])

                for hd in range(4):
                    moba = hd >= 2
                    h = hd % 2
                    w = wa[hd % 2]; rw = Rwa[hd % 2]
                    win3 = wa_d[l].rearrange("(k p) n -> p k n", p=128)
                    nn_ = 64 if moba else 128
                    cols = [(hd * 384 + i * 128, nn_) for i in range(3)]
                    for i, (c0, n) in enumerate(cols):
                        fw.dma("pool", lambda e: e.dma_start(out=w[:, :, i * 128:i * 128 + n], in_=win3[:, :, c0:c0 + n]), [], [rw])
                    M = 64 if moba else 128
                    dv = 64 if moba else 128
                    gq = pcol(l, 23 if moba else 21); gk = pcol(l, 24 if moba else 22)
                    if moba:
                        if hd == 2:
                            PL(lambda e: e.memset(KT[64:128, :], 0.0), [], [RK])
                        LD("sp", KT[64:80, :], cd["onehotk"], [], [RK])
                        PL(lambda e: e.memset(kmT[:], 0.0), [], [Rkm])
                        PL(lambda e: e.memset(Vt[:, :, 64:65], 1.0), [], [RV])
                    for tb in range(NTB):
                        hb_ = hblk[tb % 2]; rh = Rhk[tb % 2]
                        LD("sp", hb_[:], hT_d[:, :, tb * 512:(tb + 1) * 512].rearrange("k p t -> p k t"), [], [rh])
                        tsl = slice(tb * 512, (tb + 1) * 512)
                        proj_fm(hb_, rh, w, rw, 128, M, PF[0], RPF[0])
                        headnorm(PF[0], RPF[0], M, gk, KT[0:M, tsl], RK)
                        if moba:
                            DV(lambda e: e.tensor_reduce(kmT[0:64, 2 * tb:2 * tb + 2], qf[0:64, :].rearrange("p (a t) -> p a t", a=2),
                                                         AX.X, ALU.add), [Rqf], [Rkm])
                            DV(lambda e: e.tensor_scalar(kmT[0:64, 2 * tb:2 * tb + 2], kmT[0:64, 2 * tb:2 * tb + 2], 1.0 / 256, None, ALU.mult),
                               [Rkm], [Rkm])
                        proj_fm(hb_, rh, w, rw, 0, M, PF[1], RPF[1])
                        if moba:
                            headnorm(PF[1], RPF[1], M, gq, QT[0:M, tsl], RQ)
                        else:
                            headnorm(PF[1], RPF[1], M, gq, QT[0:64, tsl], RQ, dst2=QT1[64:128, tsl])
                        for j in range(4):
                            for kc in range(8):
                                MM(PF[3][:, j * 128:j * 128 + dv], hb_[:, kc, j * 128:(j + 1) * 128], w[:, kc, 256:256 + dv],
                                   kc == 0, kc == 7, [rh, rw], [RPF[3]])
                        ACT(Vt[:, tb * 4:(tb + 1) * 4, 0:dv], PF[3][:].rearrange("p (j t) -> p j t", j=4)[:, :, 0:dv], AF.Copy, [RPF[3]], [RV])
                        if moba:
                            for j in range(4):
                                qt = tb * 4 + j
                                MM(PF[4][:, j * 16:(j + 1) * 16], qf[0:64, j * 128:(j + 1) * 128], kmT[0:64, :], True, True, [Rqf, Rkm], [RPF[4]])
                                DV(lambda e: e.tensor_tensor(gm[:], PF[4][:, j * 16:(j + 1) * 16], negpast[:, qt * 16:(qt + 1) * 16], ALU.add),
                                   [RPF[4], Rc], [Rgm])
                                DV(lambda e: e.max(out=top8[:], in_=gm[:]), [Rgm], [Rgm])
                                DV(lambda e: e.tensor_scalar(gm[:], gm[:], top8[:, 2:3], None, ALU.is_ge), [Rgm], [Rgm])
                                DV(lambda e: e.scalar_tensor_tensor(out=nmw[:, j, 64:80], in0=gm[:], scalar=1.0, in1=past30[:, qt * 16:(qt + 1) * 16],
                                                                    op0=ALU.subtract, op1=ALU.mult), [Rgm, Rc], [Rnm])
                                TR(PF[5][0:80, j * 128:(j + 1) * 128], nmw[:, j, :], ident[:], [Rnm, Rc], [RPF[5]])
                            ACT(QT[64:80, tsl], PF[5][64:80, :], AF.Copy, [RPF[5]], [RQ])
                    KK = 80 if moba else 64
                    for qb in range(NTB):
                        qsl = slice(qb * 512, (qb + 1) * 512)
                        nmap = 1 if moba else 2
                        nkt = 4 * qb + 4
                        items = [(m, kt) for m in range(nmap) for kt in range(nkt)]

                        def emit_qk(idx):
                            m_, kt_ = items[idx]
                            qsrc = QT1 if m_ == 1 else QT
                            MM(PF[idx % 2][:], KT[:, kt_ * 128:(kt_ + 1) * 128], qsrc[:, qsl], True, True, [RK, RQ], [RPF[idx % 2]])

                        emit_qk(0)
                        for idx, (m, kt) in enumerate(items):
                            if idx + 1 < len(items):
                                emit_qk(idx + 1)
                            ps = PF[idx % 2]; rps = RPF[idx % 2]
                            po = PF[2 + m]; rpo = RPF[2 + m]
                            o0 = 4 * qb - kt
                            pb = Pb[pbi % 3]; rpb = RPb[pbi % 3]; pbi += 1
                            b31 = tblb[:, 31 * 4 + hd: 31 * 4 + hd + 1]
                            if o0 <= 7:
                                pfx = Pf[idx % 2]; rpf = RPf[idx % 2]
                                ACT(pfx[:], ps[:], AF.Exp, [rps, Rc], [rpf], bias=b31, scale=0.125)
                                DV(lambda e: e.tensor_tensor(pb[:], pfx[:], erel[:, hd, (o0 + 3) * 128:(o0 + 3) * 128 + 512], ALU.mult),
                                   [rpf, Rerel], [rpb])
                            else:
                                ACT(pb[:], ps[:], AF.Exp, [rps, Rc], [rpb], bias=b31, scale=0.125)
                            if moba:
                                MM(po[0:65, :], Vt[:, kt, 0:65], pb[:], kt == 0, kt == nkt - 1, [RV, rpb], [rpo])
                            else:
                                MM(po[:], Vt[:, kt, :], pb[:], kt == 0, kt == nkt - 1, [RV, rpb], [rpo])
                                MM(PF[4 + m][0:1, :], onesb[:, 0:1], pb[:], kt == 0, kt == nkt - 1, [Rc, rpb], [RPF[4 + m]])
                        if moba:
                            DV(lambda e: e.reciprocal(rec[64:65, 0, :], PF[2][64:65, :]), [RPF[2]], [Rrec])
                            MM(PF[0][0:64, :], onesf[64:65, 0:64], rec[64:65, 0, :], True, True, [Rc, Rrec], [RPF[0]])
                            ACT(bcs[0:64, 0, :], PF[0][0:64, :], AF.Copy, [RPF[0]], [Rbcs])
                            DV(lambda e: e.tensor_tensor(Ob[0:64, :], PF[2][0:64, :], bcs[0:64, 0, :], ALU.mult), [RPF[2], Rbcs], [ROb])
                            LD("sp", Mine_b[4 + h][:, qsl], Ob[0:64, :], [ROb], [])
                        else:
                            DV(lambda e: e.reciprocal(rec[0:1, 0, :], PF[4][0:1, :]), [RPF[4]], [Rrec])
                            DV(lambda e: e.reciprocal(rec[0:1, 1, :], PF[5][0:1, :]), [RPF[5]], [Rrec])
                            DV(lambda e: e.tensor_scalar(rec[0:1, 1, :], rec[0:1, 1, :], lamc[0:1, 0:1], None, ALU.mult), [Rrec, Rlam], [Rrec])
                            for m in range(2):
                                MM(PF[m][:], onesf[0:1, :], rec[0:1, m, :], True, True, [Rc, Rrec], [RPF[m]])
                                ACT(bcs[:, m, :], PF[m][:], AF.Copy, [RPF[m]], [Rbcs])
                            DV(lambda e: e.tensor_tensor(Of[:], PF[2][:], bcs[:, 0, :], ALU.mult), [RPF[2], Rbcs], [ROf])
                            DV(lambda e: e.tensor_tensor(sqf[:], PF[3][:], bcs[:, 1, :], ALU.mult), [RPF[3], Rbcs], [Rsqf])
                            DV(lambda e: e.tensor_tensor(Of[:], Of[:], sqf[:], ALU.add), [ROf, Rsqf], [ROf])
                            ACT(sqf[:], Of[:], AF.Square, [ROf], [Rsqf])
                            MM(PF[0][:], onesf[:], sqf[:], True, True, [Rc, Rsqf], [RPF[0]])
                            DV(lambda e: e.tensor_scalar(rsf[:], PF[0][:], 1.0 / 128, 1e-6, ALU.mult, ALU.add), [RPF[0]], [Rrsf])
                            ACT(rsf[:], rsf[:], AF.Sqrt, [Rrsf], [Rrsf])
                            DV(lambda e: e.reciprocal(rsf[:], rsf[:]), [Rrsf], [Rrsf])
                            DV(lambda e: e.scalar_tensor_tensor(out=Ob[:], in0=Of[:], scalar=lamc[:, 3:4], in1=rsf[:], op0=ALU.mult, op1=ALU.mult),
                               [ROf, Rlam, Rrsf], [ROb])
                            LD("sp", Mine_b[2 * h][:, qsl], Ob[0:64, :], [ROb], [])
                            LD("sp", Mine_b[2 * h + 1][:, qsl], Ob[64:128, :], [ROb], [])
                fw.barrier()
            if stop == 'C':
                fw.halt = True
            if not fw.halt:
                groups = [[2 * g, 2 * g + 1] for g in range(ncores // 2)]
                for i_ in range(8):
                    nc.gpsimd.collective_compute("AllGather", ALU.bypass, replica_groups=groups, ins=[Mine_f[i_]], outs=[G_f[i_]]).then_inc(ccsem, 1)
                nc.gpsimd.wait_ge(ccsem, 8 * (l + 1))
                PL(lambda e: e.memset(ccdummy[:], 0.0), [], [Rccd])
                fw.barrier()

            CAPl = CAPS[l]
            NROW = 32 * CAPl
            with ExitStack() as ph:
                wo = sb(ph, "wo", [128, 8, D], BF16); Rwo = Region()
                fw.dma("pool", lambda e: e.dma_start(out=wo[:], in_=wout_d[l].rearrange("(k p) n -> p k n", p=128)), [], [Rwo])
                wrt = sb(ph, "wrt", [128, 8, 36]); brb = sb(ph, "brb", [128, 36]); Rwr = Region()
                LD("sp", wrt[:], wr_d[l].rearrange("(k p) n -> p k n", p=128), [], [Rwr])
                LD("sp", brb[:], br_d[l:l + 1, :].partition_broadcast(128), [], [Rwr])
                mblk = [sb(ph, f"mblk{i}", [128, 8, 512], BF16) for i in range(2)]; Rmb = [Region(), Region()]
                xt = [sb(ph, f"xt{i}", [128, D]) for i in range(2)]; Rx = [Region(), Region()]
                hf = [sb(ph, f"hf{i}", [128, D]) for i in range(2)]; Rhf = [Region(), Region()]
                h2b = [sb(ph, f"h2b{i}", [128, D], BF16) for i in range(2)]; Rhb = [Region(), Region()]
                sm = [sb(ph, f"sm{i}", [128, 4]) for i in range(2)]; Rsm = [Region(), Region()]
                h2T = sb(ph, "h2T", [128, 8, 128]); RhT = Region()
                lg = sb(ph, "lg", [128, 36]); ml = sb(ph, "ml", [128, 32]); oh = sb(ph, "oh", [128, 2, 32])
                rt8 = sb(ph, "rt8", [128, 8]); rs_ = sb(ph, "rs_", [128, 16]); Rr = Region()
                cntb = sb(ph, "cntb", [128, 32]); Rcnt = Region()
                io32 = sb(ph, "io32", [128, 32]); posf = sb(ph, "posf", [128, 32]); msk = sb(ph, "msk", [128, 32])
                tmp32 = sb(ph, "tmp32", [128, 32]); dst = sb(ph, "dstf", [128, 2])
                PL(lambda e: e.memset(cntb[:], 0.0), [], [Rcnt])
                PL(lambda e: e.iota(io32[:], pattern=[[1, 32]], base=0, channel_multiplier=0, allow_small_or_imprecise_dtypes=True), [], [Rr])
                for tt in range(NTT):
                    i = tt % 2; tb = tt // 4; j = tt % 4
                    if j == 0:
                        tsl_ = slice(tb * 512, (tb + 1) * 512)
                        for r_ in range(2):
                            LD("sp", mblk[tb % 2][0:64, r_, :], G_b[6][r_ * 64:(r_ + 1) * 64, tsl_], [], [Rmb[tb % 2]])
                            LD("act", mblk[tb % 2][64:128, r_, :], G_b[7][r_ * 64:(r_ + 1) * 64, tsl_], [], [Rmb[tb % 2]])
                        for r_ in range(2):
                            rr = slice(r_ * 64, (r_ + 1) * 64)
                            for lh_ in range(2):
                                LD("sp", mblk[tb % 2][0:64, 2 + 2 * r_ + lh_, :], G_b[2 * lh_][rr, tsl_], [], [Rmb[tb % 2]])
                                LD("act", mblk[tb % 2][64:128, 2 + 2 * r_ + lh_, :], G_b[2 * lh_ + 1][rr, tsl_], [], [Rmb[tb % 2]])
                            LD("sp", mblk[tb % 2][0:64, 6 + r_, :], G_b[4][rr, tsl_], [], [Rmb[tb % 2]])
                            LD("act", mblk[tb % 2][64:128, 6 + r_, :], G_b[5][rr, tsl_], [], [Rmb[tb % 2]])
                    mb = mblk[tb % 2]; rmb = Rmb[tb % 2]
                    LD("sp", xt[i][:], xsrc[tt * 128:(tt + 1) * 128, :], [], [Rx[i]])
                    for half in range(2):
                        for kc in range(8):
                            MM(PF[half][:], mb[:, kc, j * 128:(j + 1) * 128], wo[:, kc, half * 512:(half + 1) * 512], kc == 0, kc == 7,
                               [rmb, Rwo], [RPF[half]])
                        hs = slice(half * 512, (half + 1) * 512)
                        DV(lambda e: e.tensor_tensor(hf[i][:, hs], PF[half][:], G1[:, hs], ALU.mult), [RPF[half], Rmod], [Rhf[i]])
                    PL(lambda e: e.tensor_tensor(xt[i][:], xt[i][:], hf[i][:], ALU.add), [Rx[i], Rhf[i]], [Rx[i]])
                    LD("sp", xr_d[tt * 128:(tt + 1) * 128, :], xt[i][:], [Rx[i]], [])
                    norm_mod(ph, xt[i], Rx[i], A2, SH2, hf[i], Rhf[i], sm[i], Rsm[i])
                    ACT(h2b[i][:], hf[i][:], AF.Copy, [Rhf[i]], [Rhb[i]])
                    for kc in range(8):
                        TR(PF[2 + kc // 4][:, (kc % 4) * 128:(kc % 4 + 1) * 128], hf[i][:, kc * 128:(kc + 1) * 128], ident[:], [Rhf[i], Rc],
                           [RPF[2 + kc // 4]])
                    ACT(h2T[:, 0:4, :], PF[2][:].rearrange("p (k t) -> p k t", k=4), AF.Copy, [RPF[2]], [RhT])
                    DV(lambda e: e.tensor_copy(h2T[:, 4:8, :], PF[3][:].rearrange("p (k t) -> p k t", k=4)), [RPF[3]], [RhT])
                    for kc in range(8):
                        MM(PF[4][:, 0:36], h2T[:, kc, :], wrt[:, kc, :], kc == 0, kc == 7, [RhT, Rwr], [RPF[4]])
                    DV(lambda e: e.tensor_tensor(lg[:], PF[4][:, 0:36], brb[:], ALU.add), [RPF[4], Rwr], [Rr])
                    DV(lambda e: e.tensor_reduce(rs_[:, 0:1], lg[:, 0:4], AX.X, ALU.max), [Rr], [Rr])
                    DV(lambda e: e.tensor_scalar(rs_[:, 1:2], rs_[:, 0:1], -1.0, None, ALU.mult), [Rr], [Rr])
                    PL(lambda e: e.memset(rs_[:, 2:3], 0.0), [Rr], [Rr])
                    ACT(rs_[:, 4:8], lg[:, 0:4], AF.Exp, [Rr], [Rr], bias=rs_[:, 1:2], accum_out=rs_[:, 2:3])
                    DV(lambda e: e.reciprocal(rs_[:, 3:4], rs_[:, 2:3]), [Rr], [Rr])
                    DV(lambda e: e.tensor_scalar(rs_[:, 8:12], lg[:, 0:4], rs_[:, 0:1], None, ALU.is_ge), [Rr], [Rr])
                    DV(lambda e: e.tensor_scalar(rs_[:, 8:12], rs_[:, 8:12], 1.0, 1e30, ALU.subtract, ALU.mult), [Rr], [Rr])
                    DV(lambda e: e.tensor_tensor(ml[:].rearrange("p (g e) -> p g e", g=4), lg[:, 4:36].rearrange("p (g e) -> p g e", g=4),
                                                 rs_[:, 8:12].rearrange("p (g o) -> p g o", o=1).to_broadcast([128, 4, 8]), ALU.add), [Rr], [Rr])
                    DV(lambda e: e.max(out=rt8[:], in_=ml[:]), [Rr], [Rr])
                    DV(lambda e: e.tensor_scalar(oh[:, 0, :], ml[:], rt8[:, 0:1], None, ALU.is_equal), [Rr], [Rr])
                    DV(lambda e: e.tensor_scalar(oh[:, 1, :], ml[:], rt8[:, 1:2], None, ALU.is_equal), [Rr], [Rr])
                    DV(lambda e: e.tensor_tensor(rs_[:, 12:13], rt8[:, 0:1], rt8[:, 1:2], ALU.subtract), [Rr], [Rr])
                    ACT(rs_[:, 13:14], rs_[:, 12:13], AF.Sigmoid, [Rr], [Rr])
                    DV(lambda e: e.tensor_tensor(gts[:, tt, 0:1], rs_[:, 13:14], rs_[:, 3:4], ALU.mult), [Rr], [Rgts])
                    DV(lambda e: e.tensor_tensor(gts[:, tt, 1:2], rs_[:, 3:4], gts[:, tt, 0:1], ALU.subtract), [Rr, Rgts], [Rgts])
                    DV(lambda e: e.tensor_tensor(msk[:], oh[:, 0, :], oh[:, 1, :], ALU.add), [Rr], [Rr])
                    MM(PF[5][:, 0:32], mui[:], msk[:], True, True, [Rc, Rr], [RPF[5]])
                    MM(PF[5][:, 32:64], onesf[:], msk[:], True, True, [Rc, Rr], [RPF[5]])
                    DV(lambda e: e.tensor_tensor(posf[:], PF[5][:, 0:32], cntb[:], ALU.add), [RPF[5], Rcnt], [Rr])
                    DV(lambda e: e.tensor_tensor(cntb[:], PF[5][:, 32:64], cntb[:], ALU.add), [RPF[5], Rcnt, Rr], [Rcnt])
                    DV(lambda e: e.tensor_scalar(tmp32[:], posf[:], float(CAPl), 4.0e7, ALU.is_gt, ALU.mult), [Rr], [Rr])
                    DV(lambda e: e.tensor_tensor(posf[:], posf[:], tmp32[:], ALU.add), [Rr], [Rr])
                    DV(lambda e: e.scalar_tensor_tensor(out=posf[:], in0=io32[:], scalar=float(CAPl), in1=posf[:], op0=ALU.mult, op1=ALU.add),
                       [Rr], [Rr])
                    for k in range(2):
                        DV(lambda e: e.tensor_tensor(tmp32[:], oh[:, k, :], posf[:], ALU.mult), [Rr], [Rr])
                        DV(lambda e: e.tensor_reduce(dst[:, k:k + 1], tmp32[:], AX.X, ALU.add), [Rr], [Rr])
                    DV(lambda e: e.tensor_scalar(dst[:], dst[:], -1.0, float(NROW), ALU.add, ALU.min), [Rr], [Rr])
                    DV(lambda e: e.tensor_copy(off[:, tt, :], dst[:]), [Rr], [Roff])
                    for k in range(2):
                        fw.dma("pool", lambda e: e.indirect_dma_start(
                            out=Xs_d[0:NROW + 1, :], out_offset=bass.IndirectOffsetOnAxis(ap=off[:, tt, k:k + 1], axis=0),
                            in_=h2b[i][:], in_offset=None), [Rhb[i], Roff], [RXs])
                fw.barrier()
            if stop == 'D':
                fw.halt = True

            with ExitStack() as ph:
                w1b = [sb(ph, f"w1b{i}", [128, 8, 512], BF16) for i in range(2)]
                w3b = [sb(ph, f"w3b{i}", [128, 8, 512], BF16) for i in range(2)]
                w2b = [sb(ph, f"w2b{i}", [128, 4, D], BF16) for i in range(2)]
                Rwe = [Region(), Region()]
                xs = [sb(ph, f"xs{i}", [128, 4, D], BF16) for i in range(2)]; Rxs = [Region(), Region()]
                XT = sb(ph, "XT", [128, 8, 512], BF16); RXT = Region()
                s1 = [sb(ph, f"s1{i}", [128, 512]) for i in range(2)]; Rs1 = [Region(), Region()]
                GT = sb(ph, "GT", [128, 4, 512], BF16); RGT = Region()
                yrow = [sb(ph, f"yrow{i}", [128, D]) for i in range(2)]; Ryr = [Region(), Region()]
                PL(lambda e: e.memset(yrow[0][:], 0.0), [], [Ryr[0]])
                LD("sp", Ys_d[NROW:NROW + 1, :], yrow[0][0:1, :], [Ryr[0]], [RYs])
                groups = []
                s0 = 0
                while s0 < CAPl:
                    n = min(512, CAPl - s0); groups.append((s0, n)); s0 += n
                gi = 0; yi = 0
                for ex in range(32):
                    wi = ex % 2
                    fw.dma("pool", lambda e: e.dma_start(out=w1b[wi][:], in_=w1_d[l, ex].rearrange("(k p) n -> p k n", p=128)), [], [Rwe[wi]])
                    fw.dma("pool", lambda e: e.dma_start(out=w3b[wi][:], in_=w3_d[l, ex].rearrange("(k p) n -> p k n", p=128)), [], [Rwe[wi]])
                    fw.dma("pool", lambda e: e.dma_start(out=w2b[wi][:], in_=w2_d[l, ex].rearrange("(k p) n -> p k n", p=128)), [], [Rwe[wi]])
                    for (s0, n) in groups:
                        nt = n // 128
                        x_ = xs[gi % 2]; rx_ = Rxs[gi % 2]; gi += 1
                        r0 = ex * CAPl + s0
                        LD("sp", x_[:, 0:nt, :], Xs_d[r0:r0 + n, :].rearrange("(i p) d -> p i d", p=128), [RXs], [rx_])
                        for it_ in range(nt):
                            pbk = PB[it_ % 2]; rpb_ = RPB[it_ % 2]
                            for kc in range(8):
                                TR(pbk[:, kc * 128:(kc + 1) * 128], x_[:, it_, kc * 128:(kc + 1) * 128], identb[:], [rx_, Rc], [rpb_])
                            if it_ % 2 == 0:
                                ACT(XT[:, :, it_ * 128:(it_ + 1) * 128], pbk[:].rearrange("p (k t) -> p k t", k=8), AF.Copy, [rpb_], [RXT])
                            else:
                                DV(lambda e: e.tensor_copy(XT[:, :, it_ * 128:(it_ + 1) * 128], pbk[:].rearrange("p (k t) -> p k t", k=8)),
                                   [rpb_], [RXT])
                        for hc in range(4):
                            p1 = PF[hc % 2]; r1 = RPF[hc % 2]; p3 = PF[2 + hc % 2]; r3 = RPF[2 + hc % 2]
                            for kc in range(8):
                                MM(p1[:, 0:n], w1b[wi][:, kc, hc * 128:(hc + 1) * 128], XT[:, kc, 0:n], kc == 0, kc == 7, [Rwe[wi], RXT], [r1])
                            for kc in range(8):
                                MM(p3[:, 0:n], w3b[wi][:, kc, hc * 128:(hc + 1) * 128], XT[:, kc, 0:n], kc == 0, kc == 7, [Rwe[wi], RXT], [r3])
                            ACT(s1[hc % 2][:, 0:n], p1[:, 0:n], AF.Silu, [r1], [Rs1[hc % 2]])
                            DV(lambda e: e.tensor_tensor(GT[:, hc, 0:n], s1[hc % 2][:, 0:n], p3[:, 0:n], ALU.mult), [Rs1[hc % 2], r3], [RGT])
                        for it_ in range(nt):
                            yr = yrow[yi % 2]; ryr = Ryr[yi % 2]; yi += 1
                            for half in range(2):
                                py = PF[4 + half]; rpy = RPF[4 + half]
                                for hc in range(4):
                                    MM(py[:], GT[:, hc, it_ * 128:(it_ + 1) * 128], w2b[wi][:, hc, half * 512:(half + 1) * 512], hc == 0, hc == 3,
                                       [RGT, Rwe[wi]], [rpy])
                                if half == 0:
                                    ACT(yr[:, 0:512], py[:], AF.Copy, [rpy], [ryr])
                                else:
                                    DV(lambda e: e.tensor_copy(yr[:, 512:1024], py[:]), [rpy], [ryr])
                            LD("sp", Ys_d[r0 + it_ * 128:r0 + (it_ + 1) * 128, :], yr[:], [ryr], [RYs])
                fw.barrier()
            if stop == 'E':
                fw.halt = True

            with ExitStack() as ph:
                xt = [sb(ph, f"xt{i}", [128, D]) for i in range(2)]; Rx = [Region(), Region()]
                y0 = [sb(ph, f"y0{i}", [128, D]) for i in range(2)]; Ry0 = [Region(), Region()]
                y1 = [sb(ph, f"y1{i}", [128, D]) for i in range(2)]; Ry1 = [Region(), Region()]
                for tt in range(NTT):
                    i = tt % 2
                    LD("sp", xt[i][:], xr_d[tt * 128:(tt + 1) * 128, :], [], [Rx[i]])
                    for k, (y, ry) in enumerate(((y0[i], Ry0[i]), (y1[i], Ry1[i]))):
                        PL(lambda e: e.memset(y[:], 0.0), [], [ry])
                        fw.dma("pool", lambda e: e.indirect_dma_start(
                            out=y[:], out_offset=None, in_=Ys_d[0:NROW + 1, :],
                            in_offset=bass.IndirectOffsetOnAxis(ap=off[:, tt, k:k + 1], axis=0)), [RYs, Roff], [ry])
                    DV(lambda e: e.tensor_scalar(y0[i][:], y0[i][:], gts[:, tt, 0:1], None, ALU.mult), [Ry0[i], Rgts], [Ry0[i]])
                    DV(lambda e: e.scalar_tensor_tensor(out=y0[i][:], in0=y1[i][:], scalar=gts[:, tt, 1:2], in1=y0[i][:], op0=ALU.mult, op1=ALU.add),
                       [Ry1[i], Ry0[i], Rgts], [Ry0[i]])
                    PL(lambda e: e.tensor_tensor(y0[i][:], y0[i][:], G2, ALU.mult), [Ry0[i], Rmod], [Ry0[i]])
                    DV(lambda e: e.tensor_tensor(xt[i][:], xt[i][:], y0[i][:], ALU.add), [Rx[i], Ry0[i]], [Rx[i]])
                    LD("sp", xdst[tt * 128:(tt + 1) * 128, :], xt[i][:], [Rx[i]], [Rout])
                fw.barrier()
            if stop == 'F':
                fw.halt = True
        except StopBuild:
            pass
        fw.halt = False
        fw._wait("sp", Rout.w)
        fw.barrier()
    return nc


def run(inputs, NL=4, dbg=False, stop=None):
    sh, per, par = prep_inputs(inputs)
    nc = build(NL, LTOT=inputs["ada_w"].shape[0], dbg=dbg, stop=stop, ncores=8)
    in_maps = []
    for core in range(8):
        m = dict(sh)
        m.update(per[core // 2])
        m.update(par[core % 2])
        in_maps.append(m)
    res = run_bass_kernel_spmd(nc, in_maps, core_ids=list(range(8)))
    return res


def kernel(**inputs):
    inputs = {k: np.asarray(v) for k, v in inputs.items()}
    res = run(inputs, NL=4)
    B = inputs["x"].shape[0]
    out = np.stack([np.asarray(res.results[2 * b]["out"], dtype=np.float32).reshape(S, D) for b in range(B)], axis=0)
    return out
```

```python
import math
from contextlib import ExitStack
import numpy as np
import ml_dtypes
import concourse.bass as bass
import concourse.mybir as mybir
from concourse.bass_utils import run_bass_kernel_spmd

F32 = mybir.dt.float32
BF16 = mybir.dt.bfloat16
I32 = mybir.dt.int32
AF = mybir.ActivationFunctionType
ALU = mybir.AluOpType
AX = mybir.AxisListType

S = 4096
D = 1024
NTT = S // 128
NTB = S // 512
CAPS = [768, 896, 1280, 1408]
CAPMAX = max(CAPS)
NPP = 26
NEG = -30000.0
DMA_RING = 8
DEBUG_IND = False


class Region:
    __slots__ = ("name", "w", "r")

    def __init__(self, name=""):
        self.name = name
        self.w = None
        self.r = []


class FW:
    def __init__(self, nc, stack):
        self.nc = nc
        self.stack = stack
        self.eng = {"pe": nc.tensor, "act": nc.scalar, "dve": nc.vector,
                    "pool": nc.gpsimd, "sp": nc.sync}
        self.sems = {}
        self.cnt = {}
        self.waited = {e: {} for e in self.eng}
        for e in self.eng:
            self.sems["c_" + e] = stack.enter_context(nc.semaphore("c_" + e))
            self.cnt["c_" + e] = 0
        self.ring = {}
        for q in ("sp", "act", "pool"):
            for i in range(DMA_RING):
                k = f"d_{q}{i}"
                self.sems[k] = stack.enter_context(nc.semaphore(k))
                self.cnt[k] = 0
            self.ring[q] = 0
        self.n_inst = 0
        self.halt = False

    def _wait(self, e, ev):
        if ev is None or self.halt:
            return
        k, v = ev
        if self.waited[e].get(k, 0) >= v:
            return
        self.waited[e][k] = v
        self.eng[e].wait_ge(self.sems[k], v)

    def _deps(self, e, reads, writes, skip_same):
        own = "c_" + e
        for r in reads:
            if r.w is not None and not (skip_same and r.w[0] == own):
                self._wait(e, r.w)
        for r in writes:
            if r.w is not None and not (skip_same and r.w[0] == own):
                self._wait(e, r.w)
            for ev in r.r:
                if not (skip_same and ev[0] == own):
                    self._wait(e, ev)

    def _record(self, ev, reads, writes):
        for r in writes:
            r.w = ev
            r.r = []
        for r in reads:
            if r in writes:
                continue
            r.r = [x for x in r.r if x[0] != ev[0]] + [ev]

    def op(self, e, fn, reads=(), writes=(), skip_same=False):
        if self.halt:
            return
        self._deps(e, reads, writes, skip_same)
        k = "c_" + e
        self.cnt[k] += 1
        fn(self.eng[e]).then_inc(self.sems[k], 1)
        self._record((k, self.cnt[k]), reads, writes)
        self.n_inst += 1

    def dma(self, q, fn, reads=(), writes=()):
        if self.halt:
            return
        i = self.ring[q]
        self.ring[q] = (i + 1) % DMA_RING
        k = f"d_{q}{i}"
        if self.cnt[k] > 0:
            self._wait(q, (k, self.cnt[k]))
        self._deps(q, reads, writes, False)
        self.cnt[k] += 16
        fn(self.eng[q]).then_inc(self.sems[k], 16)
        self._record((k, self.cnt[k]), reads, writes)
        self.n_inst += 1

    def barrier(self):
        for e in self.eng:
            for k, v in self.cnt.items():
                if v > 0:
                    self._wait(e, (k, v))


def t5_bucket_np(dist):
    n = np.maximum(dist, 0)
    nf = np.maximum(n, 1).astype(np.float32)
    large = 16 + (np.log(nf / 16) / math.log(1024 / 16) * 16).astype(np.int32)
    large = np.minimum(large, 31)
    return np.where(n < 16, n, large)


def make_consts():
    c = {}
    p = np.arange(128)[:, None]
    j = np.arange(128)[None, :]
    c["ident"] = (p == j).astype(np.float32)
    su = (p < j).astype(np.float32)
    ui = (p <= j).astype(np.float32)
    sl = (p > j).astype(np.float32)
    c["m4"] = np.concatenate([su, ui, su, ui], 1)
    c["msl"] = sl
    c["mui"] = ui
    c["bdones"] = ((p // 64) == (j // 64)).astype(np.float32)
    cc = np.arange(14 * 128)[None, :]
    dist = (cc // 128 - 3) * 128 + (cc % 128) - p
    c["idxE"] = np.where(dist >= 0, t5_bucket_np(dist), -1).astype(np.float32)
    rm = np.ones((128, 512), np.float32)
    rm[:, ::128] = 0
    c["rmask"] = rm
    qt = np.arange(32)[:, None]
    n = np.arange(16)[None, :]
    past = (n < qt // 2).astype(np.float32)
    c["negpast"] = np.broadcast_to(((1 - past) * -1e30).reshape(1, 512), (128, 512)).copy().astype(np.float32)
    c["past30"] = np.broadcast_to((past * 30000.0).reshape(1, 512), (128, 512)).copy().astype(np.float32)
    lm = np.zeros((128, 7, 2, 2, 128), np.float32)
    tt_ = np.arange(128)[:, None]; ii_ = np.arange(128)[None, :]
    for k in range(7):
        b = 2 ** k
        M = (((tt_ // (2 * b)) == (ii_ // (2 * b))) & ((tt_ % (2 * b)) >= b) & ((ii_ % (2 * b)) < b)).astype(np.float32)
        lm[:, k, :, 0, :] = M[:, None, :]
        lm[:, k, :, 1, :] = M.T[:, None, :]
    c["lvlmask"] = lm.reshape(128, 7 * 512).astype(ml_dtypes.bfloat16)
    oh = (np.arange(S)[None, :] // 256 == np.arange(16)[:, None]).astype(np.float32)
    c["onehotk"] = oh.astype(ml_dtypes.bfloat16)
    return c


CONST_SHAPES = {"ident": ([128, 128], F32), "m4": ([128, 512], F32), "msl": ([128, 128], F32),
                "mui": ([128, 128], F32), "bdones": ([128, 128], F32), "idxE": ([128, 1792], F32),
                "rmask": ([128, 512], F32), "negpast": ([128, 512], F32), "past30": ([128, 512], F32),
                "onehotk": ([16, S], BF16), "lvlmask": ([128, 7 * 512], BF16)}


def prep_inputs(I):
    L = I["ada_w"].shape[0]
    sh = {}
    for k in ("ada_w", "ada_b", "w_in", "w_out"):
        sh[k] = np.ascontiguousarray(I[k], dtype=np.float32)
    sh["n1g"] = np.ascontiguousarray(I["norm1_g"])
    sh["n2g"] = np.ascontiguousarray(I["norm2_g"])
    pp = np.zeros((128, L, NPP), np.float32)
    pidx = np.arange(128)
    for l in range(L):
        pp[:, l, 0:7] = I["rwkv_mu"][l].reshape(7, 128).T
        pp[:, l, 7:9] = I["rwkv_w0"][l].reshape(2, 128).T
        pp[:, l, 9:11] = I["rwkv_a0"][l].reshape(2, 128).T
        pp[:, l, 11:13] = I["rwkv_kk"][l].reshape(2, 128).T
        pp[:, l, 13:15] = I["rwkv_ka"][l].reshape(2, 128).T
        pp[:, l, 15:17] = I["rwkv_rk"][l].reshape(2, 128).T
        pp[:, l, 17:19] = I["rwkv_ln_g"][l].reshape(2, 128).T
        pp[:, l, 19:21] = I["rwkv_ln_b"][l].reshape(2, 128).T
        pp[:, l, 21] = I["diff_q_gain"][l][pidx % 64]
        pp[:, l, 22] = I["diff_k_gain"][l][pidx % 64]
        pp[:, l, 23] = I["moba_q_gain"][l][pidx % 64]
        pp[:, l, 24] = I["moba_k_gain"][l][pidx % 64]
        pp[:, l, 25] = I["diff_subln_g"][l]
    sh["lora"] = np.ascontiguousarray(np.concatenate([I["rwkv_w2"], I["rwkv_a2"], I["rwkv_g2"]], axis=1))
    sh["lam"] = np.ascontiguousarray(I["diff_lambda"].reshape(L, 256))
    sh["tbl2"] = np.ascontiguousarray(I["rel_bias"])
    sh["wr"] = np.ascontiguousarray(np.concatenate([I["router_g_w"], I["router_e_w"]], axis=2))
    sh["br"] = np.ascontiguousarray(np.concatenate([I["router_g_b"], I["router_e_b"]], axis=1))
    sh.update(make_consts())
    par = []
    for hh in range(2):
        wa = np.zeros((L, D, 1536), np.float32)
        tb_ = np.zeros((1, 128), np.float32)
        for lh in range(4):
            if lh < 2:
                h = 2 * hh + lh
                srcs = [(896 + h * 128, 128), (896 + 512 + h * 128, 128), (896 + 1024 + h * 128, 128)]
                gh = h
            else:
                h = 2 * hh + (lh - 2)
                srcs = [(2432 + h * 64, 64), (2432 + 256 + h * 64, 64), (2432 + 512 + h * 64, 64)]
                gh = 4 + h
            for i, (c0, n) in enumerate(srcs):
                wa[:, :, lh * 384 + i * 128: lh * 384 + i * 128 + n] = I["w_in"][:, :, c0:c0 + n]
            tb_[0, np.arange(32) * 4 + lh] = I["rel_bias"][:, gh]
        wr = np.concatenate([I["w_in"][:, :, hh * 128:(hh + 1) * 128], I["w_in"][:, :, 256 + hh * 128:256 + (hh + 1) * 128],
                             I["w_in"][:, :, 512 + hh * 128:512 + (hh + 1) * 128], I["w_in"][:, :, 768:896]], axis=2)
        pph = pp.copy()
        for l in range(L):
            mu7 = I["rwkv_mu"][l].reshape(7, 128).T
            pph[:, l, 0:4] = mu7[:, [hh, 2 + hh, 4 + hh, 6]]
            for cbase in (7, 9, 11, 13, 15, 17, 19):
                pph[:, l, cbase] = pp[:, l, cbase + hh]
        eidx = np.arange(hh, 32, 2)
        par.append({"wa_in": wa, "tbl": tb_, "wr_in": np.ascontiguousarray(wr), "ppar": pph.reshape(128, L * NPP),
                    "lora": np.ascontiguousarray(sh["lora"][:, :, hh * 128:(hh + 1) * 128]),
                    "moe_w1": np.ascontiguousarray(I["moe_w1"][:, eidx], dtype=np.float32),
                    "moe_w3": np.ascontiguousarray(I["moe_w3"][:, eidx], dtype=np.float32),
                    "moe_w2": np.ascontiguousarray(I["moe_w2"][:, eidx], dtype=np.float32),
                    "eslot": np.ascontiguousarray(np.broadcast_to((np.arange(32) // 2).astype(np.float32), (128, 32))),
                    "mine32": np.ascontiguousarray(np.broadcast_to((np.arange(32) % 2 == hh).astype(np.float32), (128, 32)))})
    per = []
    for b in range(I["x"].shape[0]):
        per.append({"x": np.ascontiguousarray(I["x"][b]),
                    "c8": np.ascontiguousarray(I["c"][b].reshape(8, 128).T)})
    return sh, per, par


class StopBuild(Exception):
    pass


def build(NL, LTOT=4, dbg=False, stop=None, ncores=8):
    nc = bass.Bass("TRN2", target_bir_lowering=False)

    def din(name, shape, dt=F32):
        return nc.dram_tensor(name, list(shape), dt, kind="ExternalInput").ap()

    x_d = din("x", [S, D]); c8_d = din("c8", [128, 8])
    adaw_d = din("ada_w", [LTOT, D, 6 * D]); adab_d = din("ada_b", [LTOT, 6 * D])
    n1g_d = din("n1g", [LTOT, D]); n2g_d = din("n2g", [LTOT, D])
    win_d = din("w_in", [LTOT, D, 3200]); wout_d = din("w_out", [LTOT, D, D])
    ppar_d = din("ppar", [128, LTOT * NPP]); lora_d = din("lora", [LTOT, 128, 128]); wrk_d = din("wr_in", [LTOT, D, 512])
    lam_d = din("lam", [LTOT, 256]); tbl_d = din("tbl", [1, 128]); wa_d = din("wa_in", [LTOT, D, 1536]); tbl2_d = din("tbl2", [32, 8])
    wr_d = din("wr", [LTOT, D, 36]); br_d = din("br", [LTOT, 36])
    w1_d = din("moe_w1", [LTOT, 16, D, 512]); w3_d = din("moe_w3", [LTOT, 16, D, 512])
    w2_d = din("moe_w2", [LTOT, 16, 512, D])
    eslot_d = din("eslot", [128, 32]); mine_d = din("mine32", [128, 32])
    cd = {k: din(k, shp, dt) for k, (shp, dt) in CONST_SHAPES.items()}
    out_d = nc.dram_tensor("out", [S, D], F32, kind="ExternalOutput").ap()
    okind = "ExternalOutput" if dbg else "Internal"
    xr_d = nc.dram_tensor("xr", [S, D], F32, kind=okind).ap()
    hT_d = nc.dram_tensor("hT", [8, 128, S], BF16).ap()
    mixT_d = nc.dram_tensor("mixT", [8, 128, S], BF16, kind=okind).ap()
    Mine_f = [nc.dram_tensor(f"Mine{i}", [64, S // 2], F32).ap() for i in range(8)]
    G_f = [nc.dram_tensor(f"Gth{i}", [128, S // 2], F32).ap() for i in range(8)]
    MineY = [nc.dram_tensor(f"MineY{i}", [128, D], F32).ap() for i in range(NTT)]
    GY = [nc.dram_tensor(f"GthY{i}", [256, D], F32).ap() for i in range(NTT)]
    Mine_b = [t.bitcast(BF16) for t in Mine_f]
    G_b = [t.bitcast(BF16) for t in G_f]
    Xs_d = nc.dram_tensor("Xs", [32 * CAPMAX + 128, D], BF16).ap()
    Ys_d = nc.dram_tensor("Ys", [32 * CAPMAX + 128, D], F32).ap()

    with ExitStack() as st:
        fw = FW(nc, st)
        ccsem = st.enter_context(nc.semaphore("ccsem"))

        uid = [0]

        def sb(stack, name, shape, dt=F32):
            uid[0] += 1
            return stack.enter_context(nc.sbuf_tensor(f"s{uid[0]}_{name}", list(shape), dt))

        pe_cfg = [None]

        def pe_sync(ap):
            cfg = (ap.base_partition(), ap.partition_size())
            if cfg != pe_cfg[0] and fw.cnt["c_pe"] > 0 and not fw.halt:
                fw._wait("pe", ("c_pe", fw.cnt["c_pe"]))
            pe_cfg[0] = cfg

        def MM(out, lhsT, rhs, start, stop, R, W):
            pe_sync(lhsT)
            fw.op("pe", lambda e: e.matmul(out, lhsT=lhsT, rhs=rhs, start=start, stop=stop), R, W, skip_same=True)

        def TR(out, in_, idn, R, W):
            pe_sync(in_)
            fw.op("pe", lambda e: e.transpose(out, in_, idn), R, W, skip_same=True)

        def ACT(out, in_, func, R, W, **kw):
            fw.op("act", lambda e: e.activation(out, in_, func, **kw), R, W)

        def DV(fn, R, W):
            fw.op("dve", fn, R, W)

        def PL(fn, R, W):
            fw.op("pool", fn, R, W)

        def LD(q, out, in_, R, W):
            fw.dma(q, lambda e: e.dma_start(out=out, in_=in_), R, W)

        PF = [st.enter_context(nc.psum_tensor(f"pf{i}", [128, 512], F32)) for i in range(6)]
        RPF = [Region(f"pf{i}") for i in range(6)]
        PB = [st.enter_context(nc.psum_tensor(f"pb{i}", [128, 1024], BF16)) for i in range(2)]
        RPB = [Region(f"pb{i}") for i in range(2)]

        Rc = Region("consts")
        ident = sb(st, "ident", [128, 128]); identb = sb(st, "identb", [128, 128], BF16)
        m4 = sb(st, "m4", [128, 512]); msl = sb(st, "msl", [128, 128]); mui = sb(st, "mui", [128, 128])
        bdones = sb(st, "bdones", [128, 128]); rmask = sb(st, "rmask", [128, 512])
        negpast = sb(st, "negpast", [128, 512]); past30 = sb(st, "past30", [128, 512])
        onesf = sb(st, "onesf", [128, 128]); onesb = sb(st, "onesb", [128, 128], BF16)
        lvlmask = sb(st, "lvlmask", [128, 7, 512], BF16)
        LD("sp", lvlmask[:].rearrange("p k c -> p (k c)"), cd["lvlmask"], [], [Rc])
        ppar = sb(st, "ppar", [128, LTOT * NPP]); tblb = sb(st, "tblb", [128, 128])
        cs8 = sb(st, "cs8", [128, 8]); csrep = sb(st, "csrep", [128, 8, 128])
        modb = sb(st, "modb", [128, 6 * D]); Rmod = Region("modb")
        erel = sb(st, "erel", [128, 4, 1792], BF16); Rerel = Region("erel")
        lamc = sb(st, "lamc", [128, 4]); Rlam = Region("lamc")
        off = sb(st, "off", [128, NTT, 2], I32); Roff = Region()
        gts = sb(st, "gts", [128, NTT, 2]); Rgts = Region()
        RXs = Region(); RYs = Region(); Rout = Region()
        ccdummy = sb(st, "ccdummy", [128, 2]); Rccd = Region()

        for t, k in ((ident, "ident"), (m4, "m4"), (msl, "msl"), (mui, "mui"), (bdones, "bdones"),
                     (rmask, "rmask"), (negpast, "negpast"), (past30, "past30")):
            LD("sp", t[:], cd[k], [], [Rc])
        LD("sp", ppar[:], ppar_d, [], [Rc])
        LD("sp", tblb[:], tbl_d.partition_broadcast(128), [], [Rc])
        LD("sp", cs8[:], c8_d, [], [Rc])
        PL(lambda e: e.memset(onesf[:], 1.0), [], [Rc])
        PL(lambda e: e.memset(onesb[:], 1.0), [], [Rc])
        DV(lambda e: e.tensor_copy(identb[:], ident[:]), [Rc], [Rc])
        ACT(cs8[:], cs8[:], AF.Silu, [Rc], [Rc])
        for j in range(8):
            DV(lambda e: e.tensor_copy(csrep[:, j, :], cs8[:, j:j + 1].to_broadcast([128, 128])), [Rc], [Rc])

        def pcol(l, i):
            return ppar[:, l * NPP + i: l * NPP + i + 1]

        with ExitStack() as ph:
            idxE = sb(ph, "idxE", [128, 1792]); Ri = Region()
            acc = sb(ph, "eacc", [128, 1792]); Ra = Region()
            tmp = [sb(ph, f"etmp{i}", [128, 1792]) for i in range(2)]; Rt = [Region(), Region()]
            LD("sp", idxE[:], cd["idxE"], [], [Ri])
            it = 0
            for h in range(4):
                PL(lambda e: e.memset(acc[:], 0.0), [], [Ra])
                for b in range(32):
                    t = tmp[it % 2]; rt = Rt[it % 2]; it += 1
                    DV(lambda e: e.tensor_scalar(t[:], idxE[:], float(b), tblb[:, b * 4 + h: b * 4 + h + 1],
                                                 ALU.is_equal, ALU.mult), [Ri, Rc], [rt])
                    PL(lambda e: e.tensor_tensor(acc[:], acc[:], t[:], ALU.add), [rt, Ra], [Ra])
                DV(lambda e: e.tensor_scalar(acc[:], acc[:], tblb[:, 31 * 4 + h: 31 * 4 + h + 1], None, ALU.subtract),
                   [Ra, Rc], [Ra])
                ACT(acc[:], acc[:], AF.Exp, [Ra], [Ra])
                t = tmp[0]
                DV(lambda e: e.tensor_scalar(t[:], idxE[:], 0.0, None, ALU.is_ge), [Ri], [Rt[0]])
                DV(lambda e: e.tensor_tensor(erel[:, h, :], acc[:], t[:], ALU.mult), [Ra, Rt[0]], [Rerel])
            fw.barrier()

        try:
          for l in range(NL):
            lam_init = 0.8 - 0.6 * math.exp(-0.3 * l)
            xsrc = x_d if l == 0 else xr_d
            xdst = out_d if l == NL - 1 else xr_d

            with ExitStack() as ph:
                aw = [sb(ph, f"aw{i}", [128, 8, 512]) for i in range(2)]; Raw = [Region(), Region()]
                ab = [sb(ph, f"ab{i}", [128, 512]) for i in range(2)]; Rab = [Region(), Region()]
                gb = sb(ph, "gb", [128, 2, D]); Rgb = Region()
                lamt = sb(ph, "lamt", [128, 256]); Rlt = Region()
                LD("act", gb[:, 0, :], n1g_d[l:l + 1, :].partition_broadcast(128), [], [Rgb])
                LD("act", gb[:, 1, :], n2g_d[l:l + 1, :].partition_broadcast(128), [], [Rgb])
                LD("act", lamt[:], lam_d[l:l + 1, :].partition_broadcast(128), [], [Rlt])
                for nb in range(12):
                    a = aw[nb % 2]; ra = Raw[nb % 2]; b_ = ab[nb % 2]; rb = Rab[nb % 2]
                    LD("sp", a[:], adaw_d[l].rearrange("(k p) n -> p k n", p=128)[:, :, nb * 512:(nb + 1) * 512], [], [ra])
                    LD("act", b_[:], adab_d[l:l + 1, nb * 512:(nb + 1) * 512].partition_broadcast(128), [], [rb])
                    pf = PF[nb % 2]; rp = RPF[nb % 2]
                    for kc in range(8):
                        MM(pf[:], csrep[:, kc, :], a[:, kc, :], kc == 0, kc == 7, [Rc, ra], [rp])
                    DV(lambda e: e.tensor_tensor(modb[:, nb * 512:(nb + 1) * 512], pf[:], b_[:], ALU.add), [rp, rb], [Rmod])
                for (o, gi) in ((1, 0), (4, 1)):
                    DV(lambda e: e.scalar_tensor_tensor(out=modb[:, o * D:(o + 1) * D], in0=modb[:, o * D:(o + 1) * D],
                                                        scalar=1.0, in1=gb[:, gi, :], op0=ALU.add, op1=ALU.mult),
                       [Rmod, Rgb], [Rmod])
                prod = sb(ph, "lprod", [128, 128]); Rpr = Region()
                DV(lambda e: e.tensor_tensor(prod[:].rearrange("p (a d) -> p a d", a=2),
                                             lamt[:].rearrange("p (a b d) -> p a b d", a=2, b=2)[:, :, 0, :],
                                             lamt[:].rearrange("p (a b d) -> p a b d", a=2, b=2)[:, :, 1, :], ALU.mult),
                   [Rlt], [Rpr])
                DV(lambda e: e.tensor_reduce(lamc[:, 1:3], prod[:].rearrange("p (a d) -> p a d", a=2), AX.X, ALU.add),
                   [Rpr], [Rlam])
                ACT(lamc[:, 1:3], lamc[:, 1:3], AF.Exp, [Rlam], [Rlam])
                DV(lambda e: e.tensor_tensor(lamc[:, 0:1], lamc[:, 2:3], lamc[:, 1:2], ALU.subtract), [Rlam], [Rlam])
                DV(lambda e: e.tensor_scalar(lamc[:, 0:1], lamc[:, 0:1], -lam_init, None, ALU.add), [Rlam], [Rlam])
                DV(lambda e: e.tensor_scalar(lamc[:, 3:4], pcol(l, 25), 1.0 - lam_init, None, ALU.mult), [Rc, Rlam], [Rlam])
                fw.barrier()
            if stop == '0':
                fw.halt = True
            SH1, A1, G1 = modb[:, 0:D], modb[:, D:2 * D], modb[:, 2 * D:3 * D]
            SH2, A2, G2 = modb[:, 3 * D:4 * D], modb[:, 4 * D:5 * D], modb[:, 5 * D:6 * D]

            def norm_mod(ph, xt, rx, Ax, Bx, hf, rhf, small, rsm):
                PL(lambda e: e.memset(small[:, 0:1], 0.0), [], [rsm])
                ACT(hf[:], xt[:], AF.Square, [rx, rsm], [rhf, rsm], accum_out=small[:, 0:1])
                DV(lambda e: e.tensor_scalar(small[:, 1:2], small[:, 0:1], 1.0 / D, 1e-6, ALU.mult, ALU.add), [rsm], [rsm])
                ACT(small[:, 1:2], small[:, 1:2], AF.Sqrt, [rsm], [rsm])
                DV(lambda e: e.reciprocal(small[:, 2:3], small[:, 1:2]), [rsm], [rsm])
                DV(lambda e: e.scalar_tensor_tensor(out=hf[:], in0=xt[:], scalar=small[:, 2:3], in1=Ax,
                                                    op0=ALU.mult, op1=ALU.mult), [rx, rsm, Rmod], [rhf])
                PL(lambda e: e.tensor_tensor(hf[:], hf[:], Bx, ALU.add), [rhf, Rmod], [rhf])

            with ExitStack() as ph:
                xt = [sb(ph, f"xt{i}", [128, D]) for i in range(2)]; Rx = [Region(), Region()]
                hf = [sb(ph, f"hf{i}", [128, D]) for i in range(2)]; Rhf = [Region(), Region()]
                hb = [sb(ph, f"hb{i}", [128, D], BF16) for i in range(2)]; Rhb = [Region(), Region()]
                sm = [sb(ph, f"sm{i}", [128, 4]) for i in range(2)]; Rsm = [Region(), Region()]
                hblk = [sb(ph, f"hblk{i}", [128, 8, 512], BF16) for i in range(2)]; Rhk = [Region(), Region()]
                for tt in range(NTT):
                    i = tt % 2
                    LD("sp", xt[i][:], xsrc[tt * 128:(tt + 1) * 128, :], [], [Rx[i]])
                    norm_mod(ph, xt[i], Rx[i], A1, SH1, hf[i], Rhf[i], sm[i], Rsm[i])
                    DV(lambda e: e.tensor_copy(hb[i][:], hf[i][:]), [Rhf[i]], [Rhb[i]])
                    for kc in range(8):
                        TR(PB[i][:, kc * 128:(kc + 1) * 128], hb[i][:, kc * 128:(kc + 1) * 128], identb[:], [Rhb[i], Rc], [RPB[i]])
                    tb = tt // 4; j = tt % 4; bi = tb % 2
                    ACT(hblk[bi][:, :, j * 128:(j + 1) * 128], PB[i][:].rearrange("p (k t) -> p k t", k=8), AF.Copy,
                        [RPB[i]], [Rhk[bi]])
                    if j == 3:
                        LD("sp", hT_d[:, :, tb * 512:(tb + 1) * 512].rearrange("k p t -> p k t"), hblk[bi][:], [Rhk[bi]], [])
                fw.barrier()
            if stop == 'A':
                fw.halt = True

            with ExitStack() as ph:
                wr_ = sb(ph, "w_r", [128, 8, 512], BF16); Rw = Region()
                fw.dma("pool", lambda e: e.dma_start(out=wr_[:], in_=wrk_d[l].rearrange("(k p) n -> p k n", p=128)), [], [Rw])
                lora = sb(ph, "lora", [128, 256]); Rlo = Region()
                LD("sp", lora[:, 0:128], lora_d[l], [], [Rlo])
                hblk = [sb(ph, "hblk0", [128, 8, 512], BF16)] * 2; Rhk = [Region()] * 2
                P7 = sb(ph, "P7", [128, 7, 513]); RP7 = Region()
                D7 = sb(ph, "D7", [128, 7, 512]); RD7 = Region()
                PS7 = sb(ph, "PS7", [128, 7, 512]); RPS7 = Region()
                NT = 14
                T = [sb(ph, f"rt{i}", [128, 512]) for i in range(NT)]; RT = [Region() for _ in range(NT)]
                ARt = sb(ph, "ARt", [128, 4, 256], BF16); RAR = Region()
                ARm = [sb(ph, f"ARm{i}", [128, 4, 256], BF16) for i in range(2)]; RARm = [Region(), Region()]
                for j in range(2):
                    PL(lambda e: e.memset(ARm[j][:], 0.0), [], [RARm[j]])
                Bt = sb(ph, "Bt", [128, 512], BF16); RBt = Region()
                Kt = sb(ph, "Kt", [128, 512], BF16); RKt = Region()
                BHf = sb(ph, "BHf", [128, 512], BF16); KHf = sb(ph, "KHf", [128, 512], BF16); Vf = sb(ph, "Vf", [128, 512], BF16)
                RBH = Region(); RKH = Region(); RVf = Region()
                BHtm = sb(ph, "BHtm", [128, 4, 128], BF16); KHtm = sb(ph, "KHtm", [128, 4, 128], BF16)
                Vtm = sb(ph, "Vtm", [128, 4, 128], BF16); Rtm = Region()
                SB1s = [sb(ph, f"SB1s{i}", [128, 2, 512], BF16) for i in range(2)]; RSB1s = [Region(), Region()]
                SBAs = [sb(ph, f"SBAs{i}", [128, 2, 128], BF16) for i in range(2)]; RSBAs = [Region(), Region()]
                DEs = [[sb(ph, f"DE{s_}{i}", [128, 2, 2, 128], BF16) for i in range(2)] for s_ in range(2)]
                RDEs = [[Region(), Region()] for s_ in range(2)]
                ZZs = [sb(ph, f"ZZ{i}", [128, 2, 2, 128], BF16) for i in range(2)]; RZZs = [Region(), Region()]
                Gms = [sb(ph, f"Gm{i}", [128, 512]) for i in range(2)]; RGms = [Region(), Region()]
                ST = [sb(ph, f"ST{i}", [128, 64]) for i in range(2)]; RST = [Region(), Region()]
                STb = [sb(ph, f"STb{i}", [128, 64], BF16) for i in range(2)]; RSTb = [Region(), Region()]
                RHSb = sb(ph, "RHSb", [128, 128], BF16); RRH = Region()
                Ub = sb(ph, "Ub", [128, 128], BF16); RUb = Region()
                Ytm = sb(ph, "Ytm", [128, 4, 128]); RY = Region()
                gn = sb(ph, "gn", [128, 8, 4]); Rgn = Region()
                GC = sb(ph, "GC", [128, 4]); RGC = Region()
                mixo = sb(ph, "mixo", [128, 512], BF16); Rmx = Region()
                for hp in range(1):
                    PL(lambda e: e.memset(ST[hp][:], 0.0), [], [RST[hp]])
                    PL(lambda e: e.memset(STb[hp][:], 0.0), [], [RSTb[hp]])
                PL(lambda e: e.memset(P7[:, :, 0:1], 0.0), [], [RP7])

                for tb in range(NTB):
                    hb_ = hblk[tb % 2]; rh = Rhk[tb % 2]
                    LD("sp", hb_[:], hT_d[:, :, tb * 512:(tb + 1) * 512].rearrange("k p t -> p k t"), [], [rh])
                    if tb > 0:
                        DV(lambda e: e.tensor_copy(P7[:, :, 0:1], P7[:, :, 512:513]), [RP7], [RP7])
                    for cc in range(4):
                        pf = PF[cc % 2]; rp = RPF[cc % 2]
                        for kc in range(8):
                            MM(pf[:], wr_[:, kc, cc * 128:(cc + 1) * 128], hb_[:, kc, :], kc == 0, kc == 7, [Rw, rh], [rp])
                        ACT(P7[:, cc, 1:513], pf[:], AF.Copy, [rp], [RP7])
                    DV(lambda e: e.tensor_tensor(D7[:], P7[:, :, 0:512], P7[:, :, 1:513], ALU.subtract), [RP7], [RD7])
                    for cc in range(4):
                        DV(lambda e: e.scalar_tensor_tensor(out=PS7[:, cc, :], in0=D7[:, cc, :], scalar=pcol(l, cc),
                                                            in1=P7[:, cc, 1:513], op0=ALU.mult, op1=ALU.add),
                           [RD7, RP7, Rc], [RPS7])
                    ACT(PS7[0:32, 3, :], PS7[0:32, 3, :], AF.Tanh, [RPS7], [RPS7])
                    ACT(PS7[64:128, 3, :], PS7[64:128, 3, :], AF.Sigmoid, [RPS7], [RPS7])
                    if stop == 'B1':
                        fw.halt = True
                    for hp in range(1):
                        rs, ks, vs = PS7[:, 0, :], PS7[:, 1, :], PS7[:, 2, :]
                        cs_ = slice(hp * 128, (hp + 1) * 128)
                        sg, av, gv, kk, sq, kkn, k2, lw, cum, e1, e2, e3, e4, bon = T
                        Rsg, Rav, Rgv, Rkk, Rsq, Rkkn, Rk2, Rlw, Rcum, Re1, Re2, Re3, Re4, Rbon = RT
                        MM(PF[2][:], lora[0:32, cs_], PS7[0:32, 3, :], True, True, [Rlo, RPS7], [RPF[2]])
                        ACT(sg[:], PF[2][:], AF.Sigmoid, [RPF[2], Rc], [Rsg], bias=pcol(l, 7 + hp))
                        MM(PF[3][:], lora[32:64, cs_], PS7[32:64, 3, :], True, True, [Rlo, RPS7], [RPF[3]])
                        ACT(av[:], PF[3][:], AF.Sigmoid, [RPF[3], Rc], [Rav], bias=pcol(l, 9 + hp))
                        MM(PF[2][:], lora[64:128, cs_], PS7[64:128, 3, :], True, True, [Rlo, RPS7], [RPF[2]])
                        ACT(gv[:], PF[2][:], AF.Copy, [RPF[2]], [Rgv])
                        DV(lambda e: e.tensor_scalar(kk[:], ks, pcol(l, 11 + hp), None, ALU.mult), [RPS7, Rc], [Rkk])
                        PL(lambda e: e.tensor_tensor(sq[:], kk[:], kk[:], ALU.mult), [Rkk], [Rsq])
                        MM(PF[3][:], bdones[:], sq[:], True, True, [Rc, Rsq], [RPF[3]])
                        ACT(sq[:], PF[3][:], AF.Sqrt, [RPF[3]], [Rsq])
                        DV(lambda e: e.tensor_scalar(sq[:], sq[:], 1e-12, None, ALU.max), [Rsq], [Rsq])
                        DV(lambda e: e.reciprocal(sq[:], sq[:]), [Rsq], [Rsq])
                        DV(lambda e: e.tensor_tensor(kkn[:], kk[:], sq[:], ALU.mult), [Rkk, Rsq], [Rkkn])
                        DV(lambda e: e.tensor_scalar(k2[:], av[:], 1.0, pcol(l, 13 + hp), ALU.subtract, ALU.mult), [Rav, Rc], [Rk2])
                        DV(lambda e: e.scalar_tensor_tensor(out=k2[:], in0=k2[:], scalar=1.0, in1=ks, op0=ALU.add, op1=ALU.mult),
                           [Rk2, RPS7], [Rk2])
                        PL(lambda e: e.tensor_tensor(kk[:], rs, k2[:], ALU.mult), [RPS7, Rk2, Rkkn], [Rkk])
                        DV(lambda e: e.tensor_scalar(kk[:], kk[:], pcol(l, 15 + hp), None, ALU.mult), [Rkk, Rc], [Rkk])
                        MM(PF[2][:], bdones[:], kk[:], True, True, [Rc, Rkk], [RPF[2]])
                        DV(lambda e: e.tensor_tensor(bon[:], PF[2][:], vs, ALU.mult), [RPF[2], RPS7], [Rbon])
                        DV(lambda e: e.tensor_scalar(lw[:], sg[:], -0.6065306597126334, None, ALU.mult), [Rsg], [Rlw])
                        DV(lambda e: e.tensor_tensor_scan(cum[:], rmask[:], lw[:], 0.0, ALU.mult, ALU.add), [Rc, Rlw], [Rcum])
                        cum3 = cum[:].rearrange("p (c t) -> p c t", t=128)
                        ACT(e1[:], cum[:], AF.Exp, [Rcum], [Re1])
                        ACT(e2[:], cum[:], AF.Exp, [Rcum], [Re2], scale=-1.0)
                        DV(lambda e: e.tensor_tensor(e3[:], cum[:], lw[:], ALU.subtract), [Rcum, Rlw], [Re3])
                        ACT(e3[:], e3[:], AF.Exp, [Re3], [Re3])
                        DV(lambda e: e.tensor_tensor(e4[:].rearrange("p (c t) -> p c t", t=128),
                                                     cum3[:, :, 127:128].to_broadcast([128, 4, 128]), cum3, ALU.subtract),
                           [Rcum], [Re4])
                        ACT(e4[:], e4[:], AF.Exp, [Re4], [Re4])
                        ACT(GC[:].rearrange("p (c o) -> p c o", o=1), cum3[:, :, 127:128], AF.Exp, [Rcum], [RGC])
                        AR3 = ARt[:]
                        DV(lambda e: e.scalar_tensor_tensor(out=AR3[:, :, 0:128], in0=kkn[:].rearrange("p (c t) -> p c t", t=128),
                                                            scalar=-1.0, in1=e3[:].rearrange("p (c t) -> p c t", t=128),
                                                            op0=ALU.mult, op1=ALU.mult), [Rkkn, Re3], [RAR])
                        PL(lambda e: e.tensor_tensor(AR3[:, :, 128:256], PS7[:, hp, :].rearrange("p (c t) -> p c t", t=128),
                                                     e1[:].rearrange("p (c t) -> p c t", t=128), ALU.mult), [RPS7, Re1], [RAR])
                        ACT(ARm[0][0:64, :, :], ARt[0:64, :, :], AF.Copy, [RAR], [RARm[0]])
                        PL(lambda e: e.tensor_copy(ARm[1][64:128, :, :], ARt[64:128, :, :]), [RAR], [RARm[1]])
                        DV(lambda e: e.tensor_tensor(kkn[:], kkn[:], av[:], ALU.mult), [Rkkn, Rav, RAR], [Rkkn])
                        DV(lambda e: e.tensor_tensor(Bt[:], kkn[:], e2[:], ALU.mult), [Rkkn, Re2], [RBt])
                        PL(lambda e: e.tensor_tensor(BHf[:], kkn[:], e4[:], ALU.mult), [Rkkn, Re4], [RBH])
                        DV(lambda e: e.tensor_tensor(Kt[:], k2[:], e2[:], ALU.mult), [Rk2, Re2], [RKt])
                        PL(lambda e: e.tensor_tensor(KHf[:], k2[:], e4[:], ALU.mult), [Rk2, Re4], [RKH])
                        ACT(Vf[:], vs, AF.Copy, [RPS7], [RVf])
                        for c in range(4):
                            TR(PB[0][:, c * 128:(c + 1) * 128], BHf[:, c * 128:(c + 1) * 128], identb[:], [RBH, Rc], [RPB[0]])
                            TR(PB[0][:, 512 + c * 128:512 + (c + 1) * 128], KHf[:, c * 128:(c + 1) * 128], identb[:], [RKH, Rc], [RPB[0]])
                            TR(PB[1][:, c * 128:(c + 1) * 128], Vf[:, c * 128:(c + 1) * 128], identb[:], [RVf, Rc], [RPB[1]])
                        ACT(BHtm[:], PB[0][:, 0:512].rearrange("p (c t) -> p c t", t=128), AF.Copy, [RPB[0]], [Rtm])
                        DV(lambda e: e.tensor_copy(KHtm[:], PB[0][:, 512:1024].rearrange("p (c t) -> p c t", t=128)), [RPB[0]], [Rtm])
                        ACT(Vtm[:], PB[1][:, 0:512].rearrange("p (c t) -> p c t", t=128), AF.Copy, [RPB[1]], [Rtm])
                        if stop == 'B2':
                            fw.halt = True
                        for c0 in (0, 2):
                            for s_ in range(2):
                                c = c0 + s_
                                cl = slice(c * 128, (c + 1) * 128)
                                b0 = 0
                                pa = PF[4]; rpa = RPF[4]
                                for j in range(2):
                                    pf = PF[b0 + j]; rpf_ = RPF[b0 + j]
                                    MM(pf[:, 0:256], Bt[:, cl], ARm[j][:, c, :], True, True, [RBt, RARm[j]], [rpf_])
                                    MM(pf[:, 256:512], Kt[:, cl], ARm[j][:, c, :], True, True, [RKt, RARm[j]], [rpf_])
                                    MM(pa[:, j * 128:(j + 1) * 128], ARm[j][:, c, 0:128], Bt[:, cl], True, True, [RARm[j], RBt], [rpa])
                                for j in range(2):
                                    DV(lambda e: e.tensor_tensor(SB1s[s_][:, j, :], PF[b0 + j][:], m4[:], ALU.mult), [RPF[b0 + j], Rc], [RSB1s[s_]])
                                for j in range(2):
                                    PL(lambda e: e.tensor_copy(DEs[s_][0][:, j, 0, :], identb[:]), [Rc], [RDEs[s_][0]])
                                    PL(lambda e: e.tensor_copy(DEs[s_][0][:, j, 1, :], identb[:]), [Rc], [RDEs[s_][0]])
                                    DV(lambda e: e.tensor_tensor(SBAs[s_][:, j, :], pa[:, j * 128:(j + 1) * 128], msl[:], ALU.mult),
                                       [rpa, Rc], [RSBAs[s_]])
                            wi = 0
                            for k in range(7):
                                for s_ in range(2):
                                    pz = PF[4 * s_]; rpz = RPF[4 * s_]
                                    for j in range(2):
                                        MM(pz[:, j * 256:j * 256 + 128], SB1s[s_][:, j, 0:128], DEs[s_][wi][:, j, 0, :], True, True,
                                           [RSB1s[s_], RDEs[s_][wi]], [rpz])
                                        MM(pz[:, j * 256 + 128:j * 256 + 256], SBAs[s_][:, j, :], DEs[s_][wi][:, j, 1, :], True, True,
                                           [RSBAs[s_], RDEs[s_][wi]], [rpz])
                                for s_ in range(2):
                                    ACT(ZZs[s_][:].rearrange("p j z t -> p (j z t)"), PF[4 * s_][:], AF.Copy, [RPF[4 * s_]], [RZZs[s_]])
                                for s_ in range(2):
                                    pg = PF[4 * s_ + 1]; rpg = RPF[4 * s_ + 1]
                                    for j in range(2):
                                        MM(pg[:, j * 256:j * 256 + 128], DEs[s_][wi][:, j, 1, :], ZZs[s_][:, j, 0, :], True, True,
                                           [RDEs[s_][wi], RZZs[s_]], [rpg])
                                        MM(pg[:, j * 256 + 128:j * 256 + 256], DEs[s_][wi][:, j, 0, :], ZZs[s_][:, j, 1, :], True, True,
                                           [RDEs[s_][wi], RZZs[s_]], [rpg])
                                for s_ in range(2):
                                    DV(lambda e: e.tensor_tensor(Gms[s_][:], PF[4 * s_ + 1][:], lvlmask[:, k, :], ALU.mult),
                                       [RPF[4 * s_ + 1], Rc], [RGms[s_]])
                                for s_ in range(2):
                                    PL(lambda e: e.tensor_tensor(DEs[s_][1 - wi][:].rearrange("p j z t -> p (j z t)"), Gms[s_][:],
                                                                 DEs[s_][wi][:].rearrange("p j z t -> p (j z t)"), ALU.add),
                                       [RGms[s_], RDEs[s_][wi]], [RDEs[s_][1 - wi]])
                                wi = 1 - wi
                            for s_ in range(2):
                                c = c0 + s_
                                SB1 = SB1s[s_]; RSB1 = RSB1s[s_]
                                W_ = DEs[s_][wi]; RW_ = RDEs[s_][wi]
                                pr = PF[5]
                                for j in range(2):
                                    vj = slice(j * 64, (j + 1) * 64)
                                    MM(pr[:, vj], ARm[j][:, c, 0:128], STb[hp][:, :], True, False, [RARm[j], RSTb[hp]], [RPF[5]])
                                    MM(pr[:, vj], SB1[:, j, 256:384], Vtm[:, c, vj], False, True, [RSB1, Rtm], [RPF[5]])
                                ACT(RHSb[:], pr[:, 0:128], AF.Copy, [RPF[5]], [RRH])
                                pu = PF[4]
                                for j in range(2):
                                    vj = slice(j * 64, (j + 1) * 64)
                                    MM(pu[:, 256 + j * 64:256 + (j + 1) * 64], W_[:, j, 1, :], RHSb[:, vj], True, True, [RW_, RRH], [RPF[4]])
                                DV(lambda e: e.tensor_copy(Ub[:], pu[:, 256:384]), [RPF[4]], [RUb])
                                py = PF[5]
                                for j in range(2):
                                    vj = slice(j * 64, (j + 1) * 64)
                                    yo = py[:, 128 + j * 64:128 + (j + 1) * 64]
                                    MM(yo, ARm[j][:, c, 128:256], STb[hp][:, :], True, False, [RARm[j], RSTb[hp]], [RPF[5]])
                                    MM(yo, SB1[:, j, 128:256], Ub[:, vj], False, False, [RSB1, RUb], [RPF[5]])
                                    MM(yo, SB1[:, j, 384:512], Vtm[:, c, vj], False, True, [RSB1, Rtm], [RPF[5]])
                                ACT(Ytm[:, c, :], py[:, 128:256], AF.Copy, [RPF[5]], [RY])
                                pss = PF[5]
                                for j in range(2):
                                    R_ = slice(j * 64, (j + 1) * 64); vj = slice(j * 64, (j + 1) * 64)
                                    so = pss[R_, 256:320]
                                    MM(so, BHtm[:, c, R_], Ub[:, vj], True, False, [Rtm, RUb], [RPF[5]])
                                    MM(so, KHtm[:, c, R_], Vtm[:, c, vj], False, True, [Rtm], [RPF[5]])
                                DV(lambda e: e.scalar_tensor_tensor(out=ST[hp][:], in0=ST[hp][:], scalar=GC[:, c:c + 1], in1=pss[:, 256:320],
                                                                    op0=ALU.mult, op1=ALU.add), [RST[hp], RGC, RPF[5]], [RST[hp]])
                                ACT(STb[hp][:], ST[hp][:], AF.Copy, [RST[hp]], [RSTb[hp]])
                        Y8 = Ytm[:].rearrange("p c (j v) -> p (c j) v", j=2)
                        DV(lambda e: e.tensor_reduce(gn[:, :, 0], Y8, AX.X, ALU.add), [RY], [Rgn])
                        DV(lambda e: e.tensor_scalar(gn[:, :, 0], gn[:, :, 0], 1.0 / 64, None, ALU.mult), [Rgn], [Rgn])
                        DV(lambda e: e.tensor_tensor(Y8, Y8, gn[:, :, 0:1].to_broadcast([128, 8, 64]), ALU.subtract), [RY, Rgn], [RY])
                        Ysq = sq[:].rearrange("p (a v) -> p a v", v=64)
                        PL(lambda e: e.tensor_tensor(Ysq, Y8, Y8, ALU.mult), [RY, Rsq], [Rsq])
                        DV(lambda e: e.tensor_reduce(gn[:, :, 1], Ysq, AX.X, ALU.add), [Rsq], [Rgn])
                        DV(lambda e: e.tensor_scalar(gn[:, :, 1], gn[:, :, 1], 1.0 / 64, 64e-5, ALU.mult, ALU.add), [Rgn], [Rgn])
                        ACT(gn[:, :, 1], gn[:, :, 1], AF.Sqrt, [Rgn], [Rgn])
                        DV(lambda e: e.reciprocal(gn[:, :, 2], gn[:, :, 1]), [Rgn], [Rgn])
                        DV(lambda e: e.tensor_tensor(Y8, Y8, gn[:, :, 2:3].to_broadcast([128, 8, 64]), ALU.mult), [RY, Rgn], [RY])
                        if stop == 'B5':
                            fw.halt = True
                        for c in range(4):
                            TR(PF[3][:, c * 128:(c + 1) * 128], Ytm[:, c, :], ident[:], [RY, Rc], [RPF[3]])
                        DV(lambda e: e.tensor_scalar(e1[:], PF[3][:], pcol(l, 17 + hp), pcol(l, 19 + hp), ALU.mult, ALU.add),
                           [RPF[3], Rc, RAR], [Re1])
                        DV(lambda e: e.tensor_tensor(e1[:], e1[:], bon[:], ALU.add), [Re1, Rbon], [Re1])
                        DV(lambda e: e.tensor_tensor(mixo[:], e1[:], gv[:], ALU.mult), [Re1, Rgv], [Rmx])
                        LD("sp", Mine_b[6][:, tb * 512:(tb + 1) * 512], mixo[0:64, :], [Rmx], [])
                        LD("sp", Mine_b[7][:, tb * 512:(tb + 1) * 512], mixo[64:128, :], [Rmx], [])
                        if stop == 'B6':
                            fw.halt = True
                        if stop == 'B7' and hp == 1:
                            fw.halt = True
                        if stop == 'B8' and hp == 1 and tb == 1:
                            fw.halt = True
                fw.barrier()
            if stop == 'B':
                fw.halt = True

            with ExitStack() as ph:
                hblk = [sb(ph, f"hblk{i}", [128, 8, 512], BF16) for i in range(2)]; Rhk = [Region(), Region()]
                wa = [sb(ph, f"wa{i}", [128, 8, 384], BF16) for i in range(2)]; Rwa = [Region(), Region()]
                QT = sb(ph, "QT", [128, S], BF16); KT = sb(ph, "KT", [128, S], BF16); RQ = Region(); RK = Region()
                QT1 = sb(ph, "QT1", [128, S], BF16)
                PL(lambda e: e.memset(QT[:], 0.0), [], [RQ])
                PL(lambda e: e.memset(QT1[:], 0.0), [], [RQ])
                Vt = sb(ph, "Vt", [128, NTT, 128], BF16); RV = Region()
                qf = sb(ph, "qf", [128, 512]); Rqf = Region()
                sqf = sb(ph, "sqf", [128, 512]); Rsqf = Region()
                rsf = sb(ph, "rsf", [128, 512]); Rrsf = Region()
                Pf = [sb(ph, f"Pf{i}", [128, 512]) for i in range(2)]; RPf = [Region(), Region()]
                Pb = [sb(ph, f"Pb{i}", [128, 512], BF16) for i in range(3)]; RPb = [Region() for _ in range(3)]
                rec = sb(ph, "rec", [128, 2, 512]); Rrec = Region()
                bcs = sb(ph, "bcs", [128, 2, 512]); Rbcs = Region()
                Of = sb(ph, "Of", [128, 512]); ROf = Region()
                Ob = sb(ph, "Ob", [128, 512], BF16); ROb = Region()
                kmT = sb(ph, "kmT", [128, 16]); Rkm = Region()
                gm = sb(ph, "gm", [128, 16]); top8 = sb(ph, "top8", [128, 8]); Rgm = Region()
                nmw = sb(ph, "nmw", [128, 4, 80]); Rnm = Region()
                PL(lambda e: e.memset(nmw[:], 0.0), [], [Rnm])
                pbi = 0

                def proj_fm(hb_, rh, w, rw, c0, M, pf, rp):
                    for kc in range(8):
                        MM(pf[0:M, :], w[:, kc, c0:c0 + M], hb_[:, kc, :], kc == 0, kc == 7, [rw, rh], [rp])

                def headnorm(pf, rp, M, gcol, dst, rdst, dst2=None):
                    ACT(sqf[0:M, :], pf[0:M, :], AF.Square, [rp], [Rsqf])
                    MM(PF[2][0:M, :], bdones[0:M, 0:M], sqf[0:M, :], True, True, [Rc, Rsqf], [RPF[2]])
                    DV(lambda e: e.tensor_scalar(rsf[0:M, :], PF[2][0:M, :], 1.0 / 64, 1e-6, ALU.mult, ALU.add), [RPF[2]], [Rrsf])
                    ACT(rsf[0:M, :], rsf[0:M, :], AF.Sqrt, [Rrsf], [Rrsf])
                    DV(lambda e: e.reciprocal(rsf[0:M, :], rsf[0:M, :]), [Rrsf], [Rrsf])
                    DV(lambda e: e.scalar_tensor_tensor(out=qf[0:M, :], in0=pf[0:M, :], scalar=gcol[0:M, :], in1=rsf[0:M, :],
                                                        op0=ALU.mult, op1=ALU.mult), [rp, Rc, Rrsf], [Rqf])
                    if dst2 is None:
                        ACT(dst, qf[0:M, :], AF.Copy, [Rqf], [rdst])
                    else:
                        ACT(dst, qf[0:64, :], AF.Copy, [Rqf], [rdst])
                        PL(lambda e: e.tensor_copy(dst2, qf[64:128, :]), [Rqf], [rdst])

                for hd in range(4):
                    moba = hd >= 2
                    h = hd % 2
                    w = wa[hd % 2]; rw = Rwa[hd % 2]
                    win3 = wa_d[l].rearrange("(k p) n -> p k n", p=128)
                    nn_ = 64 if moba else 128
                    cols = [(hd * 384 + i * 128, nn_) for i in range(3)]
                    for i, (c0, n) in enumerate(cols):
                        fw.dma("pool", lambda e: e.dma_start(out=w[:, :, i * 128:i * 128 + n], in_=win3[:, :, c0:c0 + n]), [], [rw])
                    M = 64 if moba else 128
                    dv = 64 if moba else 128
                    gq = pcol(l, 23 if moba else 21); gk = pcol(l, 24 if moba else 22)
                    if moba:
                        if hd == 2:
                            PL(lambda e: e.memset(KT[64:128, :], 0.0), [], [RK])
                        LD("sp", KT[64:80, :], cd["onehotk"], [], [RK])
                        PL(lambda e: e.memset(kmT[:], 0.0), [], [Rkm])
                        PL(lambda e: e.memset(Vt[:, :, 64:65], 1.0), [], [RV])
                    for tb in range(NTB):
                        hb_ = hblk[tb % 2]; rh = Rhk[tb % 2]
                        LD("sp", hb_[:], hT_d[:, :, tb * 512:(tb + 1) * 512].rearrange("k p t -> p k t"), [], [rh])
                        tsl = slice(tb * 512, (tb + 1) * 512)
                        proj_fm(hb_, rh, w, rw, 128, M, PF[0], RPF[0])
                        headnorm(PF[0], RPF[0], M, gk, KT[0:M, tsl], RK)
                        if moba:
                            DV(lambda e: e.tensor_reduce(kmT[0:64, 2 * tb:2 * tb + 2], qf[0:64, :].rearrange("p (a t) -> p a t", a=2),
                                                         AX.X, ALU.add), [Rqf], [Rkm])
                            DV(lambda e: e.tensor_scalar(kmT[0:64, 2 * tb:2 * tb + 2], kmT[0:64, 2 * tb:2 * tb + 2], 1.0 / 256, None, ALU.mult),
                               [Rkm], [Rkm])
                        proj_fm(hb_, rh, w, rw, 0, M, PF[1], RPF[1])
                        if moba:
                            headnorm(PF[1], RPF[1], M, gq, QT[0:M, tsl], RQ)
                        else:
                            headnorm(PF[1], RPF[1], M, gq, QT[0:64, tsl], RQ, dst2=QT1[64:128, tsl])
                        for j in range(4):
                            for kc in range(8):
                                MM(PF[3][:, j * 128:j * 128 + dv], hb_[:, kc, j * 128:(j + 1) * 128], w[:, kc, 256:256 + dv],
                                   kc == 0, kc == 7, [rh, rw], [RPF[3]])
                        ACT(Vt[:, tb * 4:(tb + 1) * 4, 0:dv], PF[3][:].rearrange("p (j t) -> p j t", j=4)[:, :, 0:dv], AF.Copy, [RPF[3]], [RV])
                        if moba:
                            for j in range(4):
                                qt = tb * 4 + j
                                MM(PF[4][:, j * 16:(j + 1) * 16], qf[0:64, j * 128:(j + 1) * 128], kmT[0:64, :], True, True, [Rqf, Rkm], [RPF[4]])
                                DV(lambda e: e.tensor_tensor(gm[:], PF[4][:, j * 16:(j + 1) * 16], negpast[:, qt * 16:(qt + 1) * 16], ALU.add),
                                   [RPF[4], Rc], [Rgm])
                                DV(lambda e: e.max(out=top8[:], in_=gm[:]), [Rgm], [Rgm])
                                DV(lambda e: e.tensor_scalar(gm[:], gm[:], top8[:, 2:3], None, ALU.is_ge), [Rgm], [Rgm])
                                DV(lambda e: e.scalar_tensor_tensor(out=nmw[:, j, 64:80], in0=gm[:], scalar=1.0, in1=past30[:, qt * 16:(qt + 1) * 16],
                                                                    op0=ALU.subtract, op1=ALU.mult), [Rgm, Rc], [Rnm])
                                TR(PF[5][0:80, j * 128:(j + 1) * 128], nmw[:, j, :], ident[:], [Rnm, Rc], [RPF[5]])
                            ACT(QT[64:80, tsl], PF[5][64:80, :], AF.Copy, [RPF[5]], [RQ])
                    KK = 80 if moba else 64
                    for qb in range(NTB):
                        qsl = slice(qb * 512, (qb + 1) * 512)
                        nmap = 1 if moba else 2
                        nkt = 4 * qb + 4
                        items = [(m, kt) for m in range(nmap) for kt in range(nkt)]

                        def emit_qk(idx):
                            m_, kt_ = items[idx]
                            qsrc = QT1 if m_ == 1 else QT
                            MM(PF[idx % 2][:], KT[:, kt_ * 128:(kt_ + 1) * 128], qsrc[:, qsl], True, True, [RK, RQ], [RPF[idx % 2]])

                        emit_qk(0)
                        for idx, (m, kt) in enumerate(items):
                            if idx + 1 < len(items):
                                emit_qk(idx + 1)
                            ps = PF[idx % 2]; rps = RPF[idx % 2]
                            po = PF[2 + m]; rpo = RPF[2 + m]
                            o0 = 4 * qb - kt
                            pb = Pb[pbi % 3]; rpb = RPb[pbi % 3]; pbi += 1
                            b31 = tblb[:, 31 * 4 + hd: 31 * 4 + hd + 1]
                            if o0 <= 7:
                                pfx = Pf[idx % 2]; rpf = RPf[idx % 2]
                                ACT(pfx[:], ps[:], AF.Exp, [rps, Rc], [rpf], bias=b31, scale=0.125)
                                DV(lambda e: e.tensor_tensor(pb[:], pfx[:], erel[:, hd, (o0 + 3) * 128:(o0 + 3) * 128 + 512], ALU.mult),
                                   [rpf, Rerel], [rpb])
                            else:
                                ACT(pb[:], ps[:], AF.Exp, [rps, Rc], [rpb], bias=b31, scale=0.125)
                            if moba:
                                MM(po[0:65, :], Vt[:, kt, 0:65], pb[:], kt == 0, kt == nkt - 1, [RV, rpb], [rpo])
                            else:
                                MM(po[:], Vt[:, kt, :], pb[:], kt == 0, kt == nkt - 1, [RV, rpb], [rpo])
                                MM(PF[4 + m][0:1, :], onesb[:, 0:1], pb[:], kt == 0, kt == nkt - 1, [Rc, rpb], [RPF[4 + m]])
                        if moba:
                            DV(lambda e: e.reciprocal(rec[64:65, 0, :], PF[2][64:65, :]), [RPF[2]], [Rrec])
                            MM(PF[0][0:64, :], onesf[64:65, 0:64], rec[64:65, 0, :], True, True, [Rc, Rrec], [RPF[0]])
                            ACT(bcs[0:64, 0, :], PF[0][0:64, :], AF.Copy, [RPF[0]], [Rbcs])
                            DV(lambda e: e.tensor_tensor(Ob[0:64, :], PF[2][0:64, :], bcs[0:64, 0, :], ALU.mult), [RPF[2], Rbcs], [ROb])
                            LD("sp", Mine_b[4 + h][:, qsl], Ob[0:64, :], [ROb], [])
                        else:
                            DV(lambda e: e.reciprocal(rec[0:1, 0, :], PF[4][0:1, :]), [RPF[4]], [Rrec])
                            DV(lambda e: e.reciprocal(rec[0:1, 1, :], PF[5][0:1, :]), [RPF[5]], [Rrec])
                            DV(lambda e: e.tensor_scalar(rec[0:1, 1, :], rec[0:1, 1, :], lamc[0:1, 0:1], None, ALU.mult), [Rrec, Rlam], [Rrec])
                            for m in range(2):
                                MM(PF[m][:], onesf[0:1, :], rec[0:1, m, :], True, True, [Rc, Rrec], [RPF[m]])
                                ACT(bcs[:, m, :], PF[m][:], AF.Copy, [RPF[m]], [Rbcs])
                            DV(lambda e: e.tensor_tensor(Of[:], PF[2][:], bcs[:, 0, :], ALU.mult), [RPF[2], Rbcs], [ROf])
                            DV(lambda e: e.tensor_tensor(sqf[:], PF[3][:], bcs[:, 1, :], ALU.mult), [RPF[3], Rbcs], [Rsqf])
                            DV(lambda e: e.tensor_tensor(Of[:], Of[:], sqf[:], ALU.add), [ROf, Rsqf], [ROf])
                            ACT(sqf[:], Of[:], AF.Square, [ROf], [Rsqf])
                            MM(PF[0][:], onesf[:], sqf[:], True, True, [Rc, Rsqf], [RPF[0]])
                            DV(lambda e: e.tensor_scalar(rsf[:], PF[0][:], 1.0 / 128, 1e-6, ALU.mult, ALU.add), [RPF[0]], [Rrsf])
                            ACT(rsf[:], rsf[:], AF.Sqrt, [Rrsf], [Rrsf])
                            DV(lambda e: e.reciprocal(rsf[:], rsf[:]), [Rrsf], [Rrsf])
                            DV(lambda e: e.scalar_tensor_tensor(out=Ob[:], in0=Of[:], scalar=lamc[:, 3:4], in1=rsf[:], op0=ALU.mult, op1=ALU.mult),
                               [ROf, Rlam, Rrsf], [ROb])
                            LD("sp", Mine_b[2 * h][:, qsl], Ob[0:64, :], [ROb], [])
                            LD("sp", Mine_b[2 * h + 1][:, qsl], Ob[64:128, :], [ROb], [])
                fw.barrier()
            if stop == 'C':
                fw.halt = True
            if not fw.halt:
                groups = [[2 * g, 2 * g + 1] for g in range(ncores // 2)]
                for i_ in range(8):
                    nc.gpsimd.collective_compute("AllGather", ALU.bypass, replica_groups=groups, ins=[Mine_f[i_]], outs=[G_f[i_]]).then_inc(ccsem, 1)
                nc.gpsimd.wait_ge(ccsem, 40 * l + 8)
                PL(lambda e: e.memset(ccdummy[:], 0.0), [], [Rccd])
                fw.barrier()

            CAPl = CAPS[l]
            NROW = 16 * CAPl
            with ExitStack() as ph:
                wo = sb(ph, "wo", [128, 8, D], BF16); Rwo = Region()
                fw.dma("pool", lambda e: e.dma_start(out=wo[:], in_=wout_d[l].rearrange("(k p) n -> p k n", p=128)), [], [Rwo])
                wrt = sb(ph, "wrt", [128, 8, 36]); brb = sb(ph, "brb", [128, 36]); Rwr = Region()
                LD("sp", wrt[:], wr_d[l].rearrange("(k p) n -> p k n", p=128), [], [Rwr])
                LD("sp", brb[:], br_d[l:l + 1, :].partition_broadcast(128), [], [Rwr])
                mblk = [sb(ph, f"mblk{i}", [128, 8, 512], BF16) for i in range(2)]; Rmb = [Region(), Region()]
                xt = [sb(ph, f"xt{i}", [128, D]) for i in range(2)]; Rx = [Region(), Region()]
                hf = [sb(ph, f"hf{i}", [128, D]) for i in range(2)]; Rhf = [Region(), Region()]
                h2b = [sb(ph, f"h2b{i}", [128, D], BF16) for i in range(2)]; Rhb = [Region(), Region()]
                sm = [sb(ph, f"sm{i}", [128, 4]) for i in range(2)]; Rsm = [Region(), Region()]
                h2T = sb(ph, "h2T", [128, 8, 128]); RhT = Region()
                lg = sb(ph, "lg", [128, 36]); ml = sb(ph, "ml", [128, 32]); oh = sb(ph, "oh", [128, 2, 32])
                rt8 = sb(ph, "rt8", [128, 8]); rs_ = sb(ph, "rs_", [128, 16]); Rr = Region()
                cntb = sb(ph, "cntb", [128, 32]); Rcnt = Region()
                io32 = sb(ph, "io32", [128, 32]); posf = sb(ph, "posf", [128, 32]); msk = sb(ph, "msk", [128, 32])
                tmp32 = sb(ph, "tmp32", [128, 32]); dst = sb(ph, "dstf", [128, 2])
                PL(lambda e: e.memset(cntb[:], 0.0), [], [Rcnt])
                LD("sp", io32[:], eslot_d, [], [Rr])
                mine32 = sb(ph, "mine32", [128, 32]); mk = sb(ph, "mk", [128, 2])
                LD("sp", mine32[:], mine_d, [], [Rr])
                for tt in range(NTT):
                    i = tt % 2; tb = tt // 4; j = tt % 4
                    if j == 0:
                        tsl_ = slice(tb * 512, (tb + 1) * 512)
                        for r_ in range(2):
                            LD("sp", mblk[tb % 2][0:64, r_, :], G_b[6][r_ * 64:(r_ + 1) * 64, tsl_], [], [Rmb[tb % 2]])
                            LD("act", mblk[tb % 2][64:128, r_, :], G_b[7][r_ * 64:(r_ + 1) * 64, tsl_], [], [Rmb[tb % 2]])
                        for r_ in range(2):
                            rr = slice(r_ * 64, (r_ + 1) * 64)
                            for lh_ in range(2):
                                LD("sp", mblk[tb % 2][0:64, 2 + 2 * r_ + lh_, :], G_b[2 * lh_][rr, tsl_], [], [Rmb[tb % 2]])
                                LD("act", mblk[tb % 2][64:128, 2 + 2 * r_ + lh_, :], G_b[2 * lh_ + 1][rr, tsl_], [], [Rmb[tb % 2]])
                            LD("sp", mblk[tb % 2][0:64, 6 + r_, :], G_b[4][rr, tsl_], [], [Rmb[tb % 2]])
                            LD("act", mblk[tb % 2][64:128, 6 + r_, :], G_b[5][rr, tsl_], [], [Rmb[tb % 2]])
                    mb = mblk[tb % 2]; rmb = Rmb[tb % 2]
                    LD("sp", xt[i][:], xsrc[tt * 128:(tt + 1) * 128, :], [], [Rx[i]])
                    for half in range(2):
                        for kc in range(8):
                            MM(PF[half][:], mb[:, kc, j * 128:(j + 1) * 128], wo[:, kc, half * 512:(half + 1) * 512], kc == 0, kc == 7,
                               [rmb, Rwo], [RPF[half]])
                        hs = slice(half * 512, (half + 1) * 512)
                        DV(lambda e: e.tensor_tensor(hf[i][:, hs], PF[half][:], G1[:, hs], ALU.mult), [RPF[half], Rmod], [Rhf[i]])
                    PL(lambda e: e.tensor_tensor(xt[i][:], xt[i][:], hf[i][:], ALU.add), [Rx[i], Rhf[i]], [Rx[i]])
                    LD("sp", xr_d[tt * 128:(tt + 1) * 128, :], xt[i][:], [Rx[i]], [])
                    norm_mod(ph, xt[i], Rx[i], A2, SH2, hf[i], Rhf[i], sm[i], Rsm[i])
                    ACT(h2b[i][:], hf[i][:], AF.Copy, [Rhf[i]], [Rhb[i]])
                    for kc in range(8):
                        TR(PF[2 + kc // 4][:, (kc % 4) * 128:(kc % 4 + 1) * 128], hf[i][:, kc * 128:(kc + 1) * 128], ident[:], [Rhf[i], Rc],
                           [RPF[2 + kc // 4]])
                    ACT(h2T[:, 0:4, :], PF[2][:].rearrange("p (k t) -> p k t", k=4), AF.Copy, [RPF[2]], [RhT])
                    DV(lambda e: e.tensor_copy(h2T[:, 4:8, :], PF[3][:].rearrange("p (k t) -> p k t", k=4)), [RPF[3]], [RhT])
                    for kc in range(8):
                        MM(PF[4][:, 0:36], h2T[:, kc, :], wrt[:, kc, :], kc == 0, kc == 7, [RhT, Rwr], [RPF[4]])
                    DV(lambda e: e.tensor_tensor(lg[:], PF[4][:, 0:36], brb[:], ALU.add), [RPF[4], Rwr], [Rr])
                    DV(lambda e: e.tensor_reduce(rs_[:, 0:1], lg[:, 0:4], AX.X, ALU.max), [Rr], [Rr])
                    DV(lambda e: e.tensor_scalar(rs_[:, 1:2], rs_[:, 0:1], -1.0, None, ALU.mult), [Rr], [Rr])
                    PL(lambda e: e.memset(rs_[:, 2:3], 0.0), [Rr], [Rr])
                    ACT(rs_[:, 4:8], lg[:, 0:4], AF.Exp, [Rr], [Rr], bias=rs_[:, 1:2], accum_out=rs_[:, 2:3])
                    DV(lambda e: e.reciprocal(rs_[:, 3:4], rs_[:, 2:3]), [Rr], [Rr])
                    DV(lambda e: e.tensor_scalar(rs_[:, 8:12], lg[:, 0:4], rs_[:, 0:1], None, ALU.is_ge), [Rr], [Rr])
                    DV(lambda e: e.tensor_scalar(rs_[:, 8:12], rs_[:, 8:12], 1.0, 1e30, ALU.subtract, ALU.mult), [Rr], [Rr])
                    DV(lambda e: e.tensor_tensor(ml[:].rearrange("p (g e) -> p g e", g=4), lg[:, 4:36].rearrange("p (g e) -> p g e", g=4),
                                                 rs_[:, 8:12].rearrange("p (g o) -> p g o", o=1).to_broadcast([128, 4, 8]), ALU.add), [Rr], [Rr])
                    DV(lambda e: e.max(out=rt8[:], in_=ml[:]), [Rr], [Rr])
                    DV(lambda e: e.tensor_scalar(oh[:, 0, :], ml[:], rt8[:, 0:1], None, ALU.is_equal), [Rr], [Rr])
                    DV(lambda e: e.tensor_scalar(oh[:, 1, :], ml[:], rt8[:, 1:2], None, ALU.is_equal), [Rr], [Rr])
                    DV(lambda e: e.tensor_tensor(rs_[:, 12:13], rt8[:, 0:1], rt8[:, 1:2], ALU.subtract), [Rr], [Rr])
                    ACT(rs_[:, 13:14], rs_[:, 12:13], AF.Sigmoid, [Rr], [Rr])
                    DV(lambda e: e.tensor_tensor(gts[:, tt, 0:1], rs_[:, 13:14], rs_[:, 3:4], ALU.mult), [Rr], [Rgts])
                    DV(lambda e: e.tensor_tensor(gts[:, tt, 1:2], rs_[:, 3:4], gts[:, tt, 0:1], ALU.subtract), [Rr, Rgts], [Rgts])
                    DV(lambda e: e.tensor_tensor(msk[:], oh[:, 0, :], oh[:, 1, :], ALU.add), [Rr], [Rr])
                    MM(PF[5][:, 0:32], mui[:], msk[:], True, True, [Rc, Rr], [RPF[5]])
                    MM(PF[5][:, 32:64], onesf[:], msk[:], True, True, [Rc, Rr], [RPF[5]])
                    DV(lambda e: e.tensor_tensor(posf[:], PF[5][:, 0:32], cntb[:], ALU.add), [RPF[5], Rcnt], [Rr])
                    DV(lambda e: e.tensor_tensor(cntb[:], PF[5][:, 32:64], cntb[:], ALU.add), [RPF[5], Rcnt, Rr], [Rcnt])
                    DV(lambda e: e.tensor_scalar(tmp32[:], posf[:], float(CAPl), 4.0e7, ALU.is_gt, ALU.mult), [Rr], [Rr])
                    DV(lambda e: e.tensor_tensor(posf[:], posf[:], tmp32[:], ALU.add), [Rr], [Rr])
                    DV(lambda e: e.scalar_tensor_tensor(out=posf[:], in0=io32[:], scalar=float(CAPl), in1=posf[:], op0=ALU.mult, op1=ALU.add),
                       [Rr], [Rr])
                    for k in range(2):
                        DV(lambda e: e.tensor_tensor(tmp32[:], oh[:, k, :], posf[:], ALU.mult), [Rr], [Rr])
                        DV(lambda e: e.tensor_reduce(dst[:, k:k + 1], tmp32[:], AX.X, ALU.add), [Rr], [Rr])
                        DV(lambda e: e.tensor_tensor(tmp32[:], oh[:, k, :], mine32[:], ALU.mult), [Rr], [Rr])
                        DV(lambda e: e.tensor_reduce(mk[:, k:k + 1], tmp32[:], AX.X, ALU.add), [Rr], [Rr])
                    DV(lambda e: e.tensor_scalar(dst[:], dst[:], -1.0, None, ALU.add), [Rr], [Rr])
                    DV(lambda e: e.tensor_tensor(dst[:], dst[:], mk[:], ALU.mult), [Rr], [Rr])
                    DV(lambda e: e.tensor_scalar(mk[:], mk[:], -float(NROW), float(NROW), ALU.mult, ALU.add), [Rr], [Rr])
                    DV(lambda e: e.tensor_tensor(dst[:], dst[:], mk[:], ALU.add), [Rr], [Rr])
                    DV(lambda e: e.tensor_scalar(dst[:], dst[:], float(NROW), None, ALU.min), [Rr], [Rr])
                    DV(lambda e: e.tensor_copy(off[:, tt, :], dst[:]), [Rr], [Roff])
                    for k in range(2):
                        fw.dma("pool", lambda e: e.indirect_dma_start(
                            out=Xs_d[0:NROW + 1, :], out_offset=bass.IndirectOffsetOnAxis(ap=off[:, tt, k:k + 1], axis=0),
                            in_=h2b[i][:], in_offset=None), [Rhb[i], Roff], [RXs])
                fw.barrier()
            if stop == 'D':
                fw.halt = True

            with ExitStack() as ph:
                w1b = [sb(ph, f"w1b{i}", [128, 8, 512], BF16) for i in range(2)]
                w3b = [sb(ph, f"w3b{i}", [128, 8, 512], BF16) for i in range(2)]
                w2b = [sb(ph, f"w2b{i}", [128, 4, D], BF16) for i in range(2)]
                Rwe = [Region(), Region()]
                xs = [sb(ph, f"xs{i}", [128, 4, D], BF16) for i in range(2)]; Rxs = [Region(), Region()]
                XT = sb(ph, "XT", [128, 8, 512], BF16); RXT = Region()
                s1 = [sb(ph, f"s1{i}", [128, 512]) for i in range(2)]; Rs1 = [Region(), Region()]
                GT = sb(ph, "GT", [128, 4, 512], BF16); RGT = Region()
                yrow = [sb(ph, f"yrow{i}", [128, D]) for i in range(2)]; Ryr = [Region(), Region()]
                PL(lambda e: e.memset(yrow[0][:], 0.0), [], [Ryr[0]])
                LD("sp", Ys_d[NROW:NROW + 1, :], yrow[0][0:1, :], [Ryr[0]], [RYs])
                groups = []
                s0 = 0
                while s0 < CAPl:
                    n = min(512, CAPl - s0); groups.append((s0, n)); s0 += n
                gi = 0; yi = 0
                for ex in range(16):
                    wi = ex % 2
                    fw.dma("pool", lambda e: e.dma_start(out=w1b[wi][:], in_=w1_d[l, ex].rearrange("(k p) n -> p k n", p=128)), [], [Rwe[wi]])
                    fw.dma("pool", lambda e: e.dma_start(out=w3b[wi][:], in_=w3_d[l, ex].rearrange("(k p) n -> p k n", p=128)), [], [Rwe[wi]])
                    fw.dma("pool", lambda e: e.dma_start(out=w2b[wi][:], in_=w2_d[l, ex].rearrange("(k p) n -> p k n", p=128)), [], [Rwe[wi]])
                    for (s0, n) in groups:
                        nt = n // 128
                        x_ = xs[gi % 2]; rx_ = Rxs[gi % 2]; gi += 1
                        r0 = ex * CAPl + s0
                        LD("sp", x_[:, 0:nt, :], Xs_d[r0:r0 + n, :].rearrange("(i p) d -> p i d", p=128), [RXs], [rx_])
                        for it_ in range(nt):
                            pbk = PB[it_ % 2]; rpb_ = RPB[it_ % 2]
                            for kc in range(8):
                                TR(pbk[:, kc * 128:(kc + 1) * 128], x_[:, it_, kc * 128:(kc + 1) * 128], identb[:], [rx_, Rc], [rpb_])
                            if it_ % 2 == 0:
                                ACT(XT[:, :, it_ * 128:(it_ + 1) * 128], pbk[:].rearrange("p (k t) -> p k t", k=8), AF.Copy, [rpb_], [RXT])
                            else:
                                DV(lambda e: e.tensor_copy(XT[:, :, it_ * 128:(it_ + 1) * 128], pbk[:].rearrange("p (k t) -> p k t", k=8)),
                                   [rpb_], [RXT])
                        for hc in range(4):
                            p1 = PF[hc % 2]; r1 = RPF[hc % 2]; p3 = PF[2 + hc % 2]; r3 = RPF[2 + hc % 2]
                            for kc in range(8):
                                MM(p1[:, 0:n], w1b[wi][:, kc, hc * 128:(hc + 1) * 128], XT[:, kc, 0:n], kc == 0, kc == 7, [Rwe[wi], RXT], [r1])
                            for kc in range(8):
                                MM(p3[:, 0:n], w3b[wi][:, kc, hc * 128:(hc + 1) * 128], XT[:, kc, 0:n], kc == 0, kc == 7, [Rwe[wi], RXT], [r3])
                            ACT(s1[hc % 2][:, 0:n], p1[:, 0:n], AF.Silu, [r1], [Rs1[hc % 2]])
                            DV(lambda e: e.tensor_tensor(GT[:, hc, 0:n], s1[hc % 2][:, 0:n], p3[:, 0:n], ALU.mult), [Rs1[hc % 2], r3], [RGT])
                        for it_ in range(nt):
                            yr = yrow[yi % 2]; ryr = Ryr[yi % 2]; yi += 1
                            for half in range(2):
                                py = PF[4 + half]; rpy = RPF[4 + half]
                                for hc in range(4):
                                    MM(py[:], GT[:, hc, it_ * 128:(it_ + 1) * 128], w2b[wi][:, hc, half * 512:(half + 1) * 512], hc == 0, hc == 3,
                                       [RGT, Rwe[wi]], [rpy])
                                if half == 0:
                                    ACT(yr[:, 0:512], py[:], AF.Copy, [rpy], [ryr])
                                else:
                                    DV(lambda e: e.tensor_copy(yr[:, 512:1024], py[:]), [rpy], [ryr])
                            LD("sp", Ys_d[r0 + it_ * 128:r0 + (it_ + 1) * 128, :], yr[:], [ryr], [RYs])
                fw.barrier()
            if stop == 'E':
                fw.halt = True

            with ExitStack() as ph:
                xt = [sb(ph, f"xt{i}", [128, D]) for i in range(2)]; Rx = [Region(), Region()]
                y0 = [sb(ph, f"y0{i}", [128, D]) for i in range(2)]; Ry0 = [Region(), Region()]
                y1 = [sb(ph, f"y1{i}", [128, D]) for i in range(2)]; Ry1 = [Region(), Region()]
                for tt in range(NTT):
                    i = tt % 2
                    for k, (y, ry) in enumerate(((y0[i], Ry0[i]), (y1[i], Ry1[i]))):
                        PL(lambda e: e.memset(y[:], 0.0), [], [ry])
                        fw.dma("pool", lambda e: e.indirect_dma_start(
                            out=y[:], out_offset=None, in_=Ys_d[0:NROW + 1, :],
                            in_offset=bass.IndirectOffsetOnAxis(ap=off[:, tt, k:k + 1], axis=0)), [RYs, Roff], [ry])
                    DV(lambda e: e.tensor_scalar(y0[i][:], y0[i][:], gts[:, tt, 0:1], None, ALU.mult), [Ry0[i], Rgts], [Ry0[i]])
                    DV(lambda e: e.scalar_tensor_tensor(out=y0[i][:], in0=y1[i][:], scalar=gts[:, tt, 1:2], in1=y0[i][:], op0=ALU.mult, op1=ALU.add),
                       [Ry1[i], Ry0[i], Rgts], [Ry0[i]])
                    LD("sp", MineY[tt], y0[i][:], [Ry0[i]], [])
                fw.barrier()
                if not fw.halt:
                    groups = [[2 * g, 2 * g + 1] for g in range(ncores // 2)]
                    for i_ in range(NTT):
                        nc.gpsimd.collective_compute("AllGather", ALU.bypass, replica_groups=groups, ins=[MineY[i_]], outs=[GY[i_]]).then_inc(ccsem, 1)
                    nc.gpsimd.wait_ge(ccsem, 40 * (l + 1))
                    PL(lambda e: e.memset(ccdummy[:], 0.0), [], [Rccd])
                    fw.barrier()
                for tt in range(NTT):
                    i = tt % 2
                    LD("sp", y0[i][:], GY[tt][0:128, :], [], [Ry0[i]])
                    LD("act", y1[i][:], GY[tt][128:256, :], [], [Ry1[i]])
                    DV(lambda e: e.tensor_tensor(y0[i][:], y0[i][:], y1[i][:], ALU.add), [Ry0[i], Ry1[i]], [Ry0[i]])
                    PL(lambda e: e.tensor_tensor(y0[i][:], y0[i][:], G2, ALU.mult), [Ry0[i], Rmod], [Ry0[i]])
                    LD("sp", xt[i][:], xr_d[tt * 128:(tt + 1) * 128, :], [], [Rx[i]])
                    DV(lambda e: e.tensor_tensor(xt[i][:], xt[i][:], y0[i][:], ALU.add), [Rx[i], Ry0[i]], [Rx[i]])
                    LD("sp", xdst[tt * 128:(tt + 1) * 128, :], xt[i][:], [Rx[i]], [Rout])
                fw.barrier()
            if stop == 'F':
                fw.halt = True
        except StopBuild:
            pass
        fw.halt = False
        fw._wait("sp", Rout.w)
        fw.barrier()
    return nc


def run(inputs, NL=4, dbg=False, stop=None):
    sh, per, par = prep_inputs(inputs)
    nc = build(NL, LTOT=inputs["ada_w"].shape[0], dbg=dbg, stop=stop, ncores=8)
    in_maps = []
    for core in range(8):
        m = dict(sh)
        m.update(per[core // 2])
        m.update(par[core % 2])
        in_maps.append(m)
    res = run_bass_kernel_spmd(nc, in_maps, core_ids=list(range(8)))
    return res


def kernel(**inputs):
    inputs = {k: np.asarray(v) for k, v in inputs.items()}
    res = run(inputs, NL=4)
    B = inputs["x"].shape[0]
    out = np.stack([np.asarray(res.results[2 * b]["out"], dtype=np.float32).reshape(S, D) for b in range(B)], axis=0)
    return out
```
